# Optimizing a Trainium2 kernel written in Bass

```python
import math
import numpy as np
import jax
import jax.numpy as jnp
from jax import lax

D_MODEL = 1024
BATCH = 16
SEQ = 2048
DEPTH = 2

N_MIXERS = 4
GROUP_W = D_MODEL // N_MIXERS
EPS = 1e-6
ROPE_THETA = 500000.0
Q_BLOCK = 128
NEG = -1e30
MAX_START = 4096

MLA_HEADS = 4
MLA_ROPE = 32
MLA_NOPE = 64
MLA_V = GROUP_W // MLA_HEADS
MLA_QK = MLA_ROPE + MLA_NOPE
MLA_Q_RANK = 3 * GROUP_W // 4
MLA_KV_RANK = GROUP_W // 2

LRU_W = GROUP_W
LRU_BLOCKS = 4
LRU_BW = LRU_W // LRU_BLOCKS
CONV_W = 4
LRU_C = 8.0

S5_W = GROUP_W
S5_CH = 16
S5_GROUPS = S5_W // S5_CH
S5_P = 64

NSA_HEADS = 4
NSA_DK = GROUP_W // NSA_HEADS
ROT_DIM = NSA_DK // 4
CMP_LEN = 32
CMP_STRIDE = 16
CMP_HID = 128
SEL_LEN = 64
SEL_TOPK = 5
WIN = 512

N_GROUPS = 4
EXP_PER_GROUP = 4
N_EXPERTS = N_GROUPS * EXP_PER_GROUP
D_EXPERT = 256
TOPK_IN_GROUP = 2

IN_COLS = (MLA_Q_RANK, MLA_KV_RANK, MLA_ROPE, LRU_W, LRU_W, S5_W, NSA_HEADS * NSA_DK, 6 * NSA_DK, 3 * NSA_HEADS)
IN_SPLITS = tuple(sum(IN_COLS[:i + 1]) for i in range(len(IN_COLS) - 1))
D_IN = sum(IN_COLS)

kernel_name = 'hybrid_mla_rglru_s5_nsa_hmoe'


def rms_norm(x, g):
    xf = x.astype(jnp.float32)
    y = xf * lax.rsqrt(jnp.mean(xf * xf, axis=-1, keepdims=True) + EPS) * g.astype(jnp.float32)
    return y.astype(x.dtype)


def rope(x, pos, rot_dim):
    half = rot_dim // 2
    inv = ROPE_THETA ** (-jnp.arange(half, dtype=jnp.float32) * 2.0 / rot_dim)
    ang = pos.astype(jnp.float32)[..., None] * inv
    c = jnp.cos(ang)[:, :, None, :]
    s = jnp.sin(ang)[:, :, None, :]
    xf = x.astype(jnp.float32)
    x1 = xf[..., :half]
    x2 = xf[..., half:rot_dim]
    out = jnp.concatenate([x1 * c - x2 * s, x1 * s + x2 * c, xf[..., rot_dim:]], axis=-1)
    return out.astype(x.dtype)


def masked_softmax(s, mask):
    s = jnp.where(mask, s.astype(jnp.float32), NEG)
    p = jax.nn.softmax(s, axis=-1)
    return jnp.where(mask, p, 0.0)


def linear_scan(a, b):
    def comb(l, r):
        return (r[0] * l[0], r[0] * l[1] + r[1])
    return lax.associative_scan(comb, (a, b), axis=1)[1]


def causal_attention(q, k, v, scale):
    B, S, H, Dq = q.shape
    nq = S // Q_BLOCK
    qb = q.reshape(B, nq, Q_BLOCK, H, Dq).transpose(1, 0, 2, 3, 4)
    kpos = jnp.arange(S)

    def block(args):
        qc, c = args
        s = jnp.einsum('bqhd,bkhd->bhqk', qc, k) * scale
        qpos = c * Q_BLOCK + jnp.arange(Q_BLOCK)
        p = masked_softmax(s, kpos[None, :] <= qpos[:, None])
        return jnp.einsum('bhqk,bkhd->bqhd', p.astype(v.dtype), v)

    o = lax.map(block, (qb, jnp.arange(nq)))
    return o.transpose(1, 0, 2, 3, 4).reshape(B, S, H, v.shape[-1])


def mla(c_q, c_kv, k_pe, pos, g_cq, g_ckv, w_uq, w_ukv, g_q, g_k):
    B, S, _ = c_q.shape
    H = MLA_HEADS
    q = (rms_norm(c_q, g_cq) @ w_uq).reshape(B, S, H, MLA_QK)
    kv = (rms_norm(c_kv, g_ckv) @ w_ukv).reshape(B, S, H, MLA_NOPE + MLA_V)
    k_nope, v = kv[..., :MLA_NOPE], kv[..., MLA_NOPE:]
    k_rope = jnp.broadcast_to(k_pe[:, :, None, :], (B, S, H, MLA_ROPE))
    k = jnp.concatenate([k_rope, k_nope], axis=-1)
    q = rope(rms_norm(q, g_q), pos, MLA_ROPE)
    k = rope(rms_norm(k, g_k), pos, MLA_ROPE)
    o = causal_attention(q, k, v, MLA_QK ** -0.5)
    return o.reshape(B, S, H * MLA_V)


def rglru(xb, gb, w_conv, b_conv, w_a, b_a, w_i, b_i, lam):
    B, S, C = xb.shape
    xp = jnp.pad(xb, ((0, 0), (CONV_W - 1, 0), (0, 0)))
    u = b_conv
    for j in range(CONV_W):
        u = u + xp[:, j:j + S] * w_conv[j]
    ub = u.reshape(B, S, LRU_BLOCKS, LRU_BW)
    r = jax.nn.sigmoid(jnp.einsum('bshi,hij->bshj', ub, w_a) + b_a).reshape(B, S, C).astype(jnp.float32)
    gi = jax.nn.sigmoid(jnp.einsum('bshi,hij->bshj', ub, w_i) + b_i).reshape(B, S, C).astype(jnp.float32)
    log_a = -LRU_C * r * jax.nn.softplus(-lam.astype(jnp.float32))
    a = jnp.exp(log_a)
    mult = jnp.sqrt(jnp.maximum(-jnp.expm1(2.0 * log_a), 0.0))
    mult = jnp.where(jnp.arange(S)[None, :, None] == 0, 1.0, mult)
    h = linear_scan(a, mult * gi * u.astype(jnp.float32))
    y = h * jax.nn.gelu(gb.astype(jnp.float32))
    return y.astype(xb.dtype)


def s5(u, a_re, a_im, log_dt, b_re, b_im, c_re, c_im, d, w_glu, b_glu):
    B, S, _ = u.shape
    f32 = jnp.float32
    uf = u.astype(f32).reshape(B, S, S5_GROUPS, S5_CH)
    a_re = a_re.astype(f32)
    a_im = a_im.astype(f32)
    b_re = b_re.astype(f32)
    b_im = b_im.astype(f32)
    dt = jnp.exp(log_dt.astype(f32))[:, None]
    mag = jnp.exp(dt * a_re)
    ab_re = mag * jnp.cos(dt * a_im)
    ab_im = mag * jnp.sin(dt * a_im)
    den = a_re * a_re + a_im * a_im
    n_re = ab_re - 1.0
    g_re = (n_re * a_re + ab_im * a_im) / den
    g_im = (ab_im * a_re - n_re * a_im) / den
    bb_re = g_re[..., None] * b_re - g_im[..., None] * b_im
    bb_im = g_re[..., None] * b_im + g_im[..., None] * b_re
    bu_re = jnp.einsum('bsgh,gph->bsgp', uf, bb_re)
    bu_im = jnp.einsum('bsgh,gph->bsgp', uf, bb_im)
    ar = jnp.broadcast_to(ab_re, bu_re.shape)
    ai = jnp.broadcast_to(ab_im, bu_re.shape)

    def comb(l, r):
        ar1, ai1, br1, bi1 = l
        ar2, ai2, br2, bi2 = r
        return (ar2 * ar1 - ai2 * ai1, ar2 * ai1 + ai2 * ar1,
                ar2 * br1 - ai2 * bi1 + br2, ar2 * bi1 + ai2 * br1 + bi2)

    _, _, h_re, h_im = lax.associative_scan(comb, (ar, ai, bu_re, bu_im), axis=1)
    y = (jnp.einsum('bsgp,ghp->bsgh', h_re, c_re.astype(f32))
         - jnp.einsum('bsgp,ghp->bsgh', h_im, c_im.astype(f32)))
    y = y.reshape(B, S, S5_W) + d.astype(f32) * u.astype(f32)
    y = jax.nn.gelu(y)
    y = y * jax.nn.sigmoid(y @ w_glu.astype(f32) + b_glu.astype(f32))
    return y.astype(u.dtype)


def nsa(q, kv, gate_logits, pos, g_q, g_k, pe_k, w1_k, w2_k, pe_v, w1_v, w2_v):
    B, S, _ = q.shape
    H, Dh = NSA_HEADS, NSA_DK
    scale = Dh ** -0.5
    k_cmp, v_cmp, k_slc, v_slc, k_win, v_win = jnp.split(kv, 6, axis=-1)
    qh = rope(rms_norm(q.reshape(B, S, H, Dh), g_q), pos, ROT_DIM)
    qidx = jnp.arange(S)

    def key_prep(k, g, p):
        return rope(rms_norm(k, g)[:, :, None, :], p, ROT_DIM)[:, :, 0, :]

    nc = (S - CMP_LEN) // CMP_STRIDE + 1
    blk = np.arange(nc)[:, None] * CMP_STRIDE + np.arange(CMP_LEN)[None, :]
    blk_end = blk[:, -1]

    def compress(t, pe, w1, w2):
        tb = (t[:, blk] + pe).reshape(B, nc, CMP_LEN * Dh)
        return jax.nn.gelu(tb @ w1) @ w2

    kc = key_prep(compress(k_cmp, pe_k, w1_k, w2_k), g_k[0], pos[:, blk_end])
    vc = compress(v_cmp, pe_v, w1_v, w2_v)
    s_c = jnp.einsum('bshd,bcd->bhsc', qh, kc) * scale
    p_c = masked_softmax(s_c, blk_end[None, :] <= qidx[:, None])
    o_cmp = jnp.einsum('bhsc,bcd->bshd', p_c.astype(vc.dtype), vc)

    nsb = S // SEL_LEN
    cs = np.arange(nc) * CMP_STRIDE
    ss = np.arange(nsb) * SEL_LEN
    ov = np.clip(np.minimum(cs[:, None] + CMP_LEN, ss[None, :] + SEL_LEN)
                 - np.maximum(cs[:, None], ss[None, :]), 0, None) / CMP_STRIDE
    imp = jnp.einsum('bhsc,cj->bsj', p_c, jnp.asarray(ov, jnp.float32))
    cur = qidx // SEL_LEN
    sb = jnp.arange(nsb)
    forced = (sb[None, :] == 0) | (sb[None, :] == cur[:, None]) | (sb[None, :] == cur[:, None] - 1)
    future = sb[None, :] > cur[:, None]
    score = jnp.where(future, -jnp.inf, jnp.where(forced, jnp.inf, imp))
    ksel = min(SEL_TOPK, nsb)
    _, sel = lax.top_k(score, ksel)
    k_s = key_prep(k_slc, g_k[1], pos)
    nq = S // Q_BLOCK
    gather = jax.vmap(lambda kb, ib: kb[ib])

    def sel_block(args):
        qc, sc, qp = args
        tok = (sc[..., None] * SEL_LEN + jnp.arange(SEL_LEN)).reshape(B, Q_BLOCK, ksel * SEL_LEN)
        kg = gather(k_s, tok)
        vg = gather(v_slc, tok)
        s = jnp.einsum('bqhd,bqnd->bhqn', qc, kg) * scale
        p = masked_softmax(s, (tok <= qp[None, :, None])[:, None])
        return jnp.einsum('bhqn,bqnd->bqhd', p.astype(vg.dtype), vg)

    q_ch = qh.reshape(B, nq, Q_BLOCK, H, Dh).transpose(1, 0, 2, 3, 4)
    sel_ch = sel.reshape(B, nq, Q_BLOCK, ksel).transpose(1, 0, 2, 3)
    o_slc = lax.map(sel_block, (q_ch, sel_ch, qidx.reshape(nq, Q_BLOCK)))
    o_slc = o_slc.transpose(1, 0, 2, 3, 4).reshape(B, S, H, Dh)

    k_w = key_prep(k_win, g_k[2], pos)
    span = WIN + Q_BLOCK
    kidx = np.arange(nq)[:, None] * Q_BLOCK + np.arange(span)[None, :]
    kpos = kidx - WIN
    kw = jnp.pad(k_w, ((0, 0), (WIN, 0), (0, 0)))[:, kidx]
    vw = jnp.pad(v_win, ((0, 0), (WIN, 0), (0, 0)))[:, kidx]
    qb = qh.reshape(B, nq, Q_BLOCK, H, Dh)
    s_w = jnp.einsum('bcqhd,bckd->bhcqk', qb, kw) * scale
    qpos = np.arange(nq)[:, None] * Q_BLOCK + np.arange(Q_BLOCK)[None, :]
    mask_w = ((kpos[:, None, :] <= qpos[:, :, None]) & (qpos[:, :, None] - kpos[:, None, :] < WIN)
              & (kpos[:, None, :] >= 0))
    p_w = masked_softmax(s_w, mask_w)
    o_win = jnp.einsum('bhcqk,bckd->bcqhd', p_w.astype(vw.dtype), vw).reshape(B, S, H, Dh)

    g = jax.nn.sigmoid(gate_logits.astype(jnp.float32)).reshape(B, S, H, 3)
    o = g[..., 0:1] * o_cmp + g[..., 1:2] * o_slc + g[..., 2:3] * o_win
    return o.reshape(B, S, H * Dh).astype(q.dtype)


def hier_moe(h, w_rg, b_rg, w_re, b_re, w_gate, w_up, w_down):
    B, S, D = h.shape
    f32 = jnp.float32
    t = h.reshape(B * S, D)
    lg = (t @ w_rg).astype(f32) + b_rg.astype(f32)
    pg = jax.nn.softmax(lg, axis=-1)
    gi = jnp.argmax(lg, axis=-1)
    pg_top = jnp.take_along_axis(pg, gi[:, None], axis=1)
    le = ((t @ w_re).astype(f32) + b_re.astype(f32)).reshape(-1, N_GROUPS, EXP_PER_GROUP)
    le_g = jnp.take_along_axis(le, gi[:, None, None], axis=1)[:, 0]
    top_v, top_i = lax.top_k(jax.nn.softmax(le_g, axis=-1), TOPK_IN_GROUP)
    w = pg_top * top_v / jnp.sum(top_v, axis=-1, keepdims=True)
    eid = gi[:, None] * EXP_PER_GROUP + top_i
    comb = jnp.sum(jax.nn.one_hot(eid, N_EXPERTS, dtype=f32) * w[..., None], axis=1)
    y = jnp.zeros((B * S, D), f32)
    for e in range(N_EXPERTS):
        he = jax.nn.silu(t @ w_gate[e]) * (t @ w_up[e])
        y = y + comb[:, e:e + 1] * (he @ w_down[e]).astype(f32)
    return y.reshape(B, S, D).astype(h.dtype)


def setup_inputs(seed: int = 0) -> dict:
    key = jax.random.key(seed)
    ks = iter(jax.random.split(key, 64))
    f32 = jnp.float32
    L = DEPTH

    def rn(shape, scale):
        return scale * jax.random.normal(next(ks), shape, f32)

    def gain(shape):
        return 1.0 + rn(shape, 0.02)

    x = rn((BATCH, SEQ, D_MODEL), 1.0)
    start = jax.random.randint(next(ks), (BATCH, 1), 0, MAX_START, dtype=jnp.int32)
    positions = start + jnp.arange(SEQ, dtype=jnp.int32)[None, :]
    u_lam = jax.random.uniform(next(ks), (L, LRU_W), f32, 0.9, 0.999)
    s_lam = u_lam ** (1.0 / LRU_C)
    lru_lambda = jnp.log(s_lam) - jnp.log1p(-s_lam)
    s5_log_dt = jax.random.uniform(next(ks), (L, S5_GROUPS), f32, math.log(1e-3), math.log(1e-1))
    return {
        'x': x,
        'positions': positions,
        'mix_norm': gain((L, D_MODEL)),
        'w_in': rn((L, D_MODEL, D_IN), D_MODEL ** -0.5),
        'mla_g_cq': gain((L, MLA_Q_RANK)),
        'mla_g_ckv': gain((L, MLA_KV_RANK)),
        'mla_w_uq': rn((L, MLA_Q_RANK, MLA_HEADS * MLA_QK), MLA_Q_RANK ** -0.5),
        'mla_w_ukv': rn((L, MLA_KV_RANK, MLA_HEADS * (MLA_NOPE + MLA_V)), MLA_KV_RANK ** -0.5),
        'mla_g_q': gain((L, MLA_QK)),
        'mla_g_k': gain((L, MLA_QK)),
        'lru_conv_w': rn((L, CONV_W, LRU_W), CONV_W ** -0.5),
        'lru_conv_b': rn((L, LRU_W), 0.02),
        'lru_w_a': rn((L, LRU_BLOCKS, LRU_BW, LRU_BW), LRU_BW ** -0.5),
        'lru_b_a': rn((L, LRU_BLOCKS, LRU_BW), 0.1),
        'lru_w_i': rn((L, LRU_BLOCKS, LRU_BW, LRU_BW), LRU_BW ** -0.5),
        'lru_b_i': rn((L, LRU_BLOCKS, LRU_BW), 0.1),
        'lru_lambda': lru_lambda,
        's5_a_re': -0.5 + rn((L, S5_GROUPS, S5_P), 0.01),
        's5_a_im': math.pi * jnp.arange(S5_P, dtype=f32) + rn((L, S5_GROUPS, S5_P), 0.01),
        's5_log_dt': s5_log_dt,
        's5_b_re': rn((L, S5_GROUPS, S5_P, S5_CH), (2.0 * S5_CH) ** -0.5),
        's5_b_im': rn((L, S5_GROUPS, S5_P, S5_CH), (2.0 * S5_CH) ** -0.5),
        's5_c_re': rn((L, S5_GROUPS, S5_CH, S5_P), (2.0 * S5_P) ** -0.5),
        's5_c_im': rn((L, S5_GROUPS, S5_CH, S5_P), (2.0 * S5_P) ** -0.5),
        's5_d': rn((L, S5_W), 1.0),
        's5_w_glu': rn((L, S5_W, S5_W), S5_W ** -0.5),
        's5_b_glu': rn((L, S5_W), 0.02),
        'nsa_g_q': gain((L, NSA_DK)),
        'nsa_g_k': gain((L, 3, NSA_DK)),
        'nsa_pe_k': rn((L, CMP_LEN, NSA_DK), 0.02),
        'nsa_w1_k': rn((L, CMP_LEN * NSA_DK, CMP_HID), (CMP_LEN * NSA_DK) ** -0.5),
        'nsa_w2_k': rn((L, CMP_HID, NSA_DK), CMP_HID ** -0.5),
        'nsa_pe_v': rn((L, CMP_LEN, NSA_DK), 0.02),
        'nsa_w1_v': rn((L, CMP_LEN * NSA_DK, CMP_HID), (CMP_LEN * NSA_DK) ** -0.5),
        'nsa_w2_v': rn((L, CMP_HID, NSA_DK), CMP_HID ** -0.5),
        'out_norm': gain((L, N_MIXERS, GROUP_W)),
        'w_out': rn((L, D_MODEL, D_MODEL), D_MODEL ** -0.5),
        'ffn_norm': gain((L, D_MODEL)),
        'moe_w_rg': rn((L, D_MODEL, N_GROUPS), D_MODEL ** -0.5),
        'moe_b_rg': rn((L, N_GROUPS), 0.01),
        'moe_w_re': rn((L, D_MODEL, N_EXPERTS), D_MODEL ** -0.5),
        'moe_b_re': rn((L, N_EXPERTS), 0.01),
        'moe_w_gate': rn((L, N_EXPERTS, D_MODEL, D_EXPERT), D_MODEL ** -0.5),
        'moe_w_up': rn((L, N_EXPERTS, D_MODEL, D_EXPERT), D_MODEL ** -0.5),
        'moe_w_down': rn((L, N_EXPERTS, D_EXPERT, D_MODEL), D_EXPERT ** -0.5),
    }


def reference(x, positions, mix_norm, w_in, mla_g_cq, mla_g_ckv, mla_w_uq, mla_w_ukv, mla_g_q, mla_g_k,
              lru_conv_w, lru_conv_b, lru_w_a, lru_b_a, lru_w_i, lru_b_i, lru_lambda,
              s5_a_re, s5_a_im, s5_log_dt, s5_b_re, s5_b_im, s5_c_re, s5_c_im, s5_d, s5_w_glu, s5_b_glu,
              nsa_g_q, nsa_g_k, nsa_pe_k, nsa_w1_k, nsa_w2_k, nsa_pe_v, nsa_w1_v, nsa_w2_v,
              out_norm, w_out, ffn_norm, moe_w_rg, moe_b_rg, moe_w_re, moe_b_re,
              moe_w_gate, moe_w_up, moe_w_down):
    for l in range(DEPTH):
        h = rms_norm(x, mix_norm[l])
        c_q, c_kv, k_pe, lru_x, lru_gate, s5_u, nsa_q, nsa_kv, nsa_gate = jnp.split(
            h @ w_in[l], IN_SPLITS, axis=-1)
        y_a = mla(c_q, c_kv, k_pe, positions, mla_g_cq[l], mla_g_ckv[l], mla_w_uq[l], mla_w_ukv[l],
                  mla_g_q[l], mla_g_k[l])
        y_b = rglru(lru_x, lru_gate, lru_conv_w[l], lru_conv_b[l], lru_w_a[l], lru_b_a[l],
                    lru_w_i[l], lru_b_i[l], lru_lambda[l])
        y_c = s5(s5_u, s5_a_re[l], s5_a_im[l], s5_log_dt[l], s5_b_re[l], s5_b_im[l], s5_c_re[l],
                 s5_c_im[l], s5_d[l], s5_w_glu[l], s5_b_glu[l])
        y_d = nsa(nsa_q, nsa_kv, nsa_gate, positions, nsa_g_q[l], nsa_g_k[l], nsa_pe_k[l], nsa_w1_k[l],
                  nsa_w2_k[l], nsa_pe_v[l], nsa_w1_v[l], nsa_w2_v[l])
        y = jnp.stack([y_a, y_b, y_c, y_d], axis=2)
        y = rms_norm(y, out_norm[l]).reshape(x.shape)
        x = x + y @ w_out[l]
        x = x + hier_moe(rms_norm(x, ffn_norm[l]), moe_w_rg[l], moe_b_rg[l], moe_w_re[l], moe_b_re[l],
                         moe_w_gate[l], moe_w_up[l], moe_w_down[l])
    return x
```

```python
import numpy as np
from contextlib import ExitStack
import concourse.bass as bass
import concourse.mybir as mybir
from concourse.bass_utils import run_bass_kernel_spmd

F32 = mybir.dt.float32
BF16 = mybir.dt.bfloat16
I32 = mybir.dt.int32
AF = mybir.ActivationFunctionType
ALU = mybir.AluOpType
AX = mybir.AxisListType

T = 2048
NT = 16
D = 1024
DC = 8
DEPTH = 2
EPS = 1e-6
TWO_PI = 6.283185307179586
BIGNEG = 30000.0
EPOCH = 30000


class Buf:
    __slots__ = ("name", "last_w", "readers", "excl")

    def __init__(self, name, excl=False):
        self.name = name
        self.last_w = None
        self.readers = []
        self.excl = excl


class Prod:
    def __init__(self, K, key, step):
        self.K = K
        self.key = key
        self.step = step
        self.count = 0
        self.sems = []

    def sem_for(self, idx):
        ep = idx // EPOCH
        while len(self.sems) <= ep:
            self.sems.append(self.K.new_sem(f"{self.key}_{len(self.sems)}"))
        return self.sems[ep], ((idx % EPOCH) + 1) * self.step, ep


class _PEProxy:
    def __init__(self, real):
        self.real = real
        self.last_stop = True

    def matmul(self, *a, **k):
        self.last_stop = bool(k.get("stop", True))
        return self.real.matmul(*a, **k)

    def transpose(self, *a, **k):
        self.last_stop = True
        return self.real.transpose(*a, **k)


class Kern:
    def __init__(self, nc, n_dma_lanes=16):
        self.nc = nc
        self._sem_ctx = []
        self.prods = {}
        self.engs = {"pe": nc.tensor, "act": nc.scalar, "dve": nc.vector, "pool": nc.gpsimd, "sp": nc.sync}
        for k in self.engs:
            self.prods[k] = Prod(self, k, 1)
        self.lanes = {}
        self.lane_rr = {}
        for q in ("sp", "pool", "act"):
            self.lanes[q] = []
            self.lane_rr[q] = 0
            for i in range(n_dma_lanes // 2):
                p = Prod(self, f"dma_{q}{i}", 16)
                self.prods[p.key] = p
                self.lanes[q].append(p)
        self._pe_proxy = _PEProxy(nc.tensor)
        self._switch = None
        self.waited = {}
        self.n_inst = 0
        self.n_wait = 0

    def new_sem(self, name):
        ctx = self.nc.semaphore(name)
        s = ctx.__enter__()
        self._sem_ctx.append(ctx)
        return s

    def close(self):
        for c in reversed(self._sem_ctx):
            c.__exit__(None, None, None)
        self._sem_ctx = []

    def _deps(self, me_key, reads, writes):
        deps = set()
        for b in reads:
            if b.last_w is not None:
                deps.add(b.last_w)
            if b.excl:
                for r in b.readers:
                    if r[0] != me_key:
                        deps.add(r)
        for b in writes:
            if b.last_w is not None:
                deps.add(b.last_w)
            deps.update(b.readers)
        return deps

    def _emit_waits(self, engname, deps, self_key=None):
        eng = self.engs[engname]
        need = {}
        for (pk, idx) in deps:
            if pk == self_key and pk == "pe":
                continue
            sem, val, ep = self.prods[pk].sem_for(idx)
            k = (pk, ep)
            if need.get(k, (None, 0))[1] < val:
                need[k] = (sem, val)
        for (pk, ep), (sem, val) in need.items():
            wk = (engname, pk, ep)
            if self.waited.get(wk, 0) >= val:
                continue
            eng.wait_ge(sem, val)
            self.n_wait += 1
            self.waited[wk] = val

    def _record(self, me, reads, writes):
        for b in reads:
            b.readers.append(me)
            if len(b.readers) > 48:
                b.readers = b.readers[-48:]
        for b in writes:
            b.last_w = me
            b.readers = []

    def op(self, engname, fn, reads=(), writes=()):
        prod = self.prods[engname]
        deps = self._deps(engname, reads, writes)
        self._emit_waits(engname, deps, self_key=engname)
        if engname == "pe":
            self._pe_proxy.last_stop = True
            ins = fn(self._pe_proxy)
            inc = self._pe_proxy.last_stop
        else:
            ins = fn(self.engs[engname])
            inc = True
        idx = prod.count
        self.n_inst += 1
        if inc:
            sem, val, ep = prod.sem_for(idx)
            ins.then_inc(sem, 1)
            prod.count += 1
        self._record((engname, idx), reads, writes)
        if self._switch is not None:
            self._switch()
        return ins

    def dma(self, qname, out, in_, reads=(), writes=(), **kw):
        lane = self.lanes[qname][self.lane_rr[qname]]
        self.lane_rr[qname] = (self.lane_rr[qname] + 1) % len(self.lanes[qname])
        deps = self._deps(lane.key, reads, writes)
        if lane.count > 0:
            deps.add((lane.key, lane.count - 1))
        self._emit_waits(qname, deps)
        idx = lane.count
        sem, val, ep = lane.sem_for(idx)
        ins = self.engs[qname].dma_start(out=out, in_=in_, **kw)
        ins.then_inc(sem, 16)
        lane.count += 1
        self.n_inst += 1
        self._record((lane.key, idx), reads, writes)
        if self._switch is not None:
            self._switch()
        return ins

    def barrier_all(self):
        deps = set()
        for pk, p in self.prods.items():
            if p.count > 0:
                deps.add((pk, p.count - 1))
        for e in self.engs:
            self._emit_waits(e, deps)


class NS:
    def __init__(self, **kw):
        self.__dict__.update(kw)


class Interleaver:
    def __init__(self, K):
        self.K = K

    def run(self, n, W, mk, body):
        import threading
        K = self.K
        W = max(1, min(W, n))
        ctxs = [mk(w) for w in range(W)]
        if W == 1:
            for i in range(n):
                body(i, ctxs[0])
            return
        sems = [threading.Semaphore(0) for _ in range(W)]
        alive = [True] * W
        done = threading.Event()
        err = []
        state = {"cur": 0}

        def next_live(w):
            for d in range(1, W + 1):
                v = (w + d) % W
                if alive[v]:
                    return v
            return None

        def switch():
            w = state["cur"]
            v = next_live(w)
            if v is None or v == w:
                return
            state["cur"] = v
            sems[v].release()
            sems[w].acquire()

        def worker(w):
            sems[w].acquire()
            try:
                if not err:
                    for i in range(w, n, W):
                        body(i, ctxs[w])
                        if err:
                            break
            except BaseException as e:
                err.append(e)
            alive[w] = False
            v = next_live(w)
            if v is None:
                done.set()
            else:
                state["cur"] = v
                sems[v].release()

        ths = [threading.Thread(target=worker, args=(w,)) for w in range(W)]
        for t in ths:
            t.start()
        K._switch = switch
        state["cur"] = 0
        sems[0].release()
        done.wait()
        K._switch = None
        for t in ths:
            t.join()
        if err:
            raise err[0]


class Pool:
    _uid = [0]

    def __init__(self, es, nc, name, shape, dtype, n, psum=False):
        self.items = []
        Pool._uid[0] += 1
        name = f"{name}_u{Pool._uid[0]}_"
        for i in range(n):
            if psum:
                t = es.enter_context(nc.psum_tensor(f"{name}{i}", shape, dtype))
            else:
                t = es.enter_context(nc.sbuf_tensor(f"{name}{i}", shape, dtype))
            self.items.append((t, Buf(f"{name}{i}", excl=psum)))
        self.i = 0

    def get(self):
        it = self.items[self.i]
        self.i = (self.i + 1) % len(self.items)
        return it

    def sub(self, idxs):
        p = Pool.__new__(Pool)
        p.items = [self.items[k] for k in idxs]
        p.i = 0
        return p


W_SPECS = {
    "mix_norm": [DEPTH, D], "ffn_norm": [DEPTH, D], "w_in": [DEPTH, D, 1772], "w_out": [DEPTH, D, D],
    "mla_g_cq": [DEPTH, 192], "mla_g_ckv": [DEPTH, 128], "mla_w_uq": [DEPTH, 192, 384],
    "mla_w_ukv": [DEPTH, 128, 512], "mla_g_q": [DEPTH, 96], "mla_g_k": [DEPTH, 96],
    "lru_cw": [DEPTH, 128, 2, 4], "lru_vec": [DEPTH, 128, 5, 2], "lru_wa": [DEPTH, 2, 128, 128],
    "lru_wi": [DEPTH, 2, 128, 128],
    "s5_par": [DEPTH, 128, 3, 8], "s5_b": [DEPTH, 128, 2, 8, 16], "s5_c": [DEPTH, 2, 8, 128, 128],
    "s5_vec": [DEPTH, 128, 3, 2], "s5_w_glu": [DEPTH, 256, 256],
    "nsa_g_q": [DEPTH, 64], "nsa_g_k": [DEPTH, 3, 64], "nsa_pe": [DEPTH, 2, 64, 32],
    "nsa_w1": [DEPTH, 2, 64, 32, 128], "nsa_w2": [DEPTH, 2, 128, 64],
    "out_norm": [DEPTH, 4, 256], "out_norm_t": [DEPTH, 128, 8],
    "moe_wr": [DEPTH, D, 20], "moe_br": [DEPTH, 20],
    "moe_w_gate": [DEPTH, 16, D, 256], "moe_w_up": [DEPTH, 16, D, 256], "moe_w_down": [DEPTH, 16, 256, D],
    "c_ident": [128, 128], "c_tri": [128, 128], "c_anti": [128, 128], "c_cmpmask": [128, T],
    "c_ov": [128, 32], "c_keep": [128, NT, 32], "c_base": [128, NT, 32], "c_E": [32, NT, 128],
    "c_inv_mla": [128, 16], "c_inv_nsa": [128, 8], "c_selE": [32, 16, 128],
}


def build_program(n_seq=2, depth=DEPTH, dbg=None, phases=("mla", "lru", "s5", "nsa", "moe")):
    nc = bass.Bass("TRN2", target_bir_lowering=False)
    dr = {}
    dr["x"] = nc.dram_tensor("x", [n_seq, T, D], F32, kind="ExternalInput").ap()
    dr["pos"] = nc.dram_tensor("pos", [n_seq, T], I32, kind="ExternalInput").ap()
    for k, shp in W_SPECS.items():
        dr[k] = nc.dram_tensor(k, shp, F32, kind="ExternalInput").ap()
    out_d = nc.dram_tensor("out", [n_seq, T, D], F32, kind="ExternalOutput").ap()
    dbg_d = {}
    if dbg:
        dbg_d["ymT"] = nc.dram_tensor("dbg_ymT", [4, 128, 2, T], F32, kind="ExternalOutput").ap()
        dbg_d["x"] = nc.dram_tensor("dbg_x", [T, D], F32, kind="ExternalOutput").ap()
        dbg_d["xnT"] = nc.dram_tensor("dbg_xnT", [128, DC, T], F32, kind="ExternalOutput").ap()

    K = Kern(nc)
    IL = Interleaver(K)
    Bout = Buf("out")
    with ExitStack() as top:
        def sb(es, name, shape, dt):
            Pool._uid[0] += 1
            return es.enter_context(nc.sbuf_tensor(f"{name}_u{Pool._uid[0]}", shape, dt))

        x_sb = sb(top, "x_sb", [128, NT, D], F32)
        Bx = [[Buf(f"x{i}_{h}") for h in range(2)] for i in range(NT)]
        xnT = sb(top, "xnT", [128, DC, T], BF16)
        BxnT = [[Buf(f"xnT{i}_{h}") for h in range(2)] for i in range(NT)]
        ymT_box = {}
        l_box = [0]

        def xnT_bufs(ch):
            return [BxnT[i][h] for i in range(ch * 4, ch * 4 + 4) for h in range(2)]

        ident = sb(top, "ident", [128, 128], F32); Bident = Buf("ident")
        identb = sb(top, "identb", [128, 128], BF16); Bidentb = Buf("identb")
        ones16 = sb(top, "ones16", [128, 128], BF16); Bones = Buf("ones16")
        tri4 = sb(top, "tri4", [128, 4, 128], BF16); Btri = Buf("tri4")
        anti4 = sb(top, "anti4", [128, 4, 128], BF16); Banti = Buf("anti4")
        posf = sb(top, "posf", [128, NT], F32); Bposf = Buf("posf")
        posi = sb(top, "posi", [128, NT], I32); Bposi = Buf("posi")
        gain = sb(top, "gain", [128, D], F32); Bgain = Buf("gain")
        ss = sb(top, "ss", [128, NT], F32); Bss = Buf("ss")
        rs = sb(top, "rs", [128, NT], F32); Brs = Buf("rs")
        epsb = sb(top, "epsb", [128, 1], F32); Beps = Buf("epsb")

        psg = Pool(top, nc, "psg", [128, 512], F32, 6, psum=True)
        psa = Pool(top, nc, "psa", [128, 512], F32, 2, psum=True)
        allps = psg.sub(range(6))
        allps.items = psg.items + psa.items

        K.dma("sp", ident[:], dr["c_ident"][:], writes=[Bident])
        K.op("act", lambda e: e.copy(identb[:], ident[:]), reads=[Bident], writes=[Bidentb])
        K.op("dve", lambda e: e.memset(ones16[:], 1.0), writes=[Bones])
        K.op("dve", lambda e: e.memset(epsb[:], EPS), writes=[Beps])
        for h in range(4):
            K.dma("pool", tri4[:, h, :], dr["c_tri"][:], writes=[Btri])
            K.dma("pool", anti4[:, h, :], dr["c_anti"][:], writes=[Banti])

        def _zeroed(pool):
            for (t_, b_) in pool.items:
                K.op("pool", lambda e: e.memset(t_[:], 0.0), writes=[b_])
            return pool

        def phase_end(es):
            K.barrier_all()
            es.close()

        def rstd_from(out_ap, in_ap, n_feat, reads, writes, eng_tmp=None):
            K.op("act", lambda e: e.activation(out_ap, in_ap, AF.Sqrt, bias=epsb[0:out_ap.shape[0], 0:1], scale=1.0 / n_feat),
                 reads=list(reads) + [Beps], writes=writes)
            K.op("dve", lambda e: e.reciprocal(out_ap, out_ap), reads=writes, writes=writes)

        def norm_phase(s, l, which, router):
            es = ExitStack()
            tmpA = Pool(es, nc, "nrmA", [128, D], BF16, 2)
            K.dma("sp", gain[:], dr[which][l:l + 1, :].to_broadcast([128, D]), writes=[Bgain])
            if router:
                wr = sb(es, "wr", [128, DC, 20], F32); Bwr = Buf("wr")
                br = sb(es, "br", [128, 20], F32); Bbr = Buf("br")
                K.dma("sp", wr[:], dr["moe_wr"][l].rearrange("(c p) n -> p c n", p=128), writes=[Bwr])
                K.dma("sp", br[:], dr["moe_br"][l:l + 1, :].to_broadcast([128, 20]), writes=[Bbr])
            for i in range(NT):
                junk, Bj = tmpA.get()
                K.op("act", lambda e: e.activation(junk[:], x_sb[:, i, :], AF.Square, accum_out=ss[:, i:i + 1]),
                     reads=Bx[i], writes=[Bj, Bss])
            rstd_from(rs[:, :], ss[:, :], D, [Bss], [Brs])
            def _mk_nr(w):
                per = 8 // 2
                d_ = dict(psg=allps.sub(range(w * per, w * per + per)), tmpA=Pool(es, nc, "nrmX", [128, D], F32, 1))
                if router:
                    d_["xT32"] = Pool(es, nc, "xT32", [128, DC, 128], F32, 1)
                    d_["rt"] = Pool(es, nc, "rt", [128, 96], F32, 2)
                else:
                    d_["xb"] = Pool(es, nc, "nrmB", [128, D], BF16, 1)
                return NS(**d_)

            def _body_nr(i, P):
                if not router:
                    xb, Bxb = P.xb.get()
                    K.op("dve", lambda e: e.scalar_tensor_tensor(xb[:], x_sb[:, i, :], rs[:, i:i + 1], gain[:], ALU.mult, ALU.mult),
                         reads=Bx[i] + [Brs, Bgain], writes=[Bxb])
                    pb, Bpb = P.psg.get()
                    pbb = pb[:, :].bitcast(BF16)
                    for c in range(DC):
                        K.op("pe", lambda e: e.transpose(pbb[:, c * 128:(c + 1) * 128], xb[:, c * 128:(c + 1) * 128], identb[:]),
                             reads=[Bxb, Bidentb], writes=[Bpb])
                    K.op("act", lambda e: e.copy(xnT[:, :, i * 128:(i + 1) * 128], pbb[:, :].rearrange("p (c t) -> p c t", c=DC)),
                         reads=[Bpb], writes=[BxnT[i][0], BxnT[i][1]])
                    return
                xn, Bxn = P.tmpA.get()
                K.op("dve", lambda e: e.scalar_tensor_tensor(xn[:], x_sb[:, i, :], rs[:, i:i + 1], gain[:], ALU.mult, ALU.mult),
                     reads=Bx[i] + [Brs, Bgain], writes=[Bxn])
                if router:
                    xt, Bxt = P.xT32.get()
                for half in range(2):
                    pb, Bpb = P.psg.get()
                    for cc in range(4):
                        c = half * 4 + cc
                        K.op("pe", lambda e: e.transpose(pb[:, cc * 128:(cc + 1) * 128], xn[:, c * 128:(c + 1) * 128], ident[:]),
                             reads=[Bxn, Bident], writes=[Bpb])
                    src = pb[:, :].rearrange("p (c t) -> p c t", c=4)
                    K.op("act", lambda e: e.copy(xnT[:, half * 4:half * 4 + 4, i * 128:(i + 1) * 128], src),
                         reads=[Bpb], writes=[BxnT[i][half]])
                    if router:
                        K.op("dve", lambda e: e.tensor_copy(xt[:, half * 4:half * 4 + 4, :], src), reads=[Bpb], writes=[Bxt])
                if router:
                    lg, Blg = P.psg.get()
                    for c in range(DC):
                        K.op("pe", lambda e: e.matmul(lg[:, 0:20], xt[:, c, :], wr[:, c, :], start=(c == 0), stop=(c == DC - 1)),
                             reads=[Bxt, Bwr], writes=[Blg])
                    r, Br_ = P.rt.get()
                    R = [Br_]
                    Lg = r[:, 0:20]; m = r[:, 20:21]; nm = r[:, 21:22]; e4 = r[:, 22:26]; se = r[:, 26:27]
                    oh = r[:, 27:31]; pen = r[:, 31:35]; lem = r[:, 35:51]; top8 = r[:, 51:59]; sel = r[:, 59:75]
                    nv1 = r[:, 75:76]; den = r[:, 76:77]; fac = r[:, 77:78]
                    r2, Br2 = P.rt.get()
                    ew = r2[:, 0:16]; sw = r2[:, 16:32]; comb = r2[:, 32:48]
                    R2 = [Br2]
                    K.op("dve", lambda e: e.tensor_tensor(Lg, lg[:, 0:20], br[:], ALU.add), reads=[Blg, Bbr], writes=R)
                    K.op("dve", lambda e: e.tensor_reduce(m, r[:, 0:4], AX.X, ALU.max), reads=R, writes=R)
                    K.op("dve", lambda e: e.tensor_scalar(nm, m, -1.0, None, ALU.mult), reads=R, writes=R)
                    K.op("act", lambda e: e.activation(e4, r[:, 0:4], AF.Exp, bias=nm, accum_out=se), reads=R, writes=R)
                    K.op("dve", lambda e: e.tensor_scalar(oh, r[:, 0:4], m, None, ALU.is_ge), reads=R, writes=R)
                    K.op("dve", lambda e: e.tensor_scalar(pen, oh, 1.0, 1e30, ALU.subtract, ALU.mult), reads=R, writes=R)
                    K.op("dve", lambda e: e.tensor_tensor(lem.rearrange("p (g i) -> p g i", g=4),
                                                          r[:, 4:20].rearrange("p (g i) -> p g i", g=4),
                                                          pen.unsqueeze(2).to_broadcast([128, 4, 4]), ALU.add), reads=R, writes=R)
                    K.op("dve", lambda e: e.max(top8, lem), reads=R, writes=R)
                    K.op("dve", lambda e: e.tensor_scalar(sel, lem, r[:, 52:53], None, ALU.is_ge), reads=R, writes=R)
                    K.op("dve", lambda e: e.tensor_scalar(nv1, r[:, 51:52], -1.0, None, ALU.mult), reads=R, writes=R)
                    K.op("act", lambda e: e.activation(ew, lem, AF.Exp, bias=nv1), reads=R, writes=R2)
                    K.op("dve", lambda e: e.scalar_tensor_tensor(sw, sel, 1.0, ew, ALU.mult, ALU.mult, accum_out=den), reads=R + R2, writes=R + R2)
                    K.op("dve", lambda e: e.tensor_tensor(fac, den, se, ALU.mult), reads=R, writes=R)
                    K.op("dve", lambda e: e.reciprocal(fac, fac), reads=R, writes=R)
                    K.op("dve", lambda e: e.tensor_scalar(comb, sw, fac, None, ALU.mult), reads=R + R2, writes=R2)
                    chl = r2[:, 48:64].bitcast(BF16)
                    K.op("dve", lambda e: e.tensor_copy(chl[:, 0:16], comb), reads=R2, writes=R2)
                    K.op("dve", lambda e: e.tensor_copy(r2[:, 64:80], chl[:, 0:16]), reads=R2, writes=R2)
                    K.op("dve", lambda e: e.tensor_tensor(chl[:, 16:32], comb, r2[:, 64:80], ALU.subtract), reads=R2, writes=R2)
                    pt, Bpt = P.psg.get()
                    ptb = pt[:, :].bitcast(BF16)
                    K.op("pe", lambda e: e.transpose(ptb[0:32, 0:128], chl, identb[:]), reads=R2 + [Bidentb], writes=[Bpt])
                    K.op("act", lambda e: e.copy(combT[0:32, i * 128:(i + 1) * 128], ptb[0:32, 0:128]), reads=[Bpt], writes=[BcombT[i // 4]])

            IL.run(NT, 2, _mk_nr, _body_nr)
            phase_end(es)

        def moe_phase(s, l):
            es = ExitStack()
            selE = sb(es, "selE", [128, 16, 128], BF16); BselE = Buf("selE")
            K.op("pool", lambda e: e.memset(selE[:], 0.0), writes=[BselE])
            K.dma("pool", selE[0:32], dr["c_selE"][:], writes=[BselE])
            wgu = Pool(es, nc, "wgu", [128, DC, 512], BF16, 2)
            wdp = Pool(es, nc, "wdp", [128, 2, D], BF16, 2)
            cbp = Pool(es, nc, "cbp", [128, 512], F32, 2)
            sgp = Pool(es, nc, "sgp", [128, 512], F32, 3)
            hep = Pool(es, nc, "hep", [128, 2, 512], BF16, 2)
            def load_expert(ex):
                wg, Bwg = wgu.get()
                wd, Bwd = wdp.get()
                K.dma("pool", wg[:, :, 0:256], dr["moe_w_gate"][l, ex].rearrange("(c p) f -> p c f", p=128), writes=[Bwg])
                K.dma("pool", wg[:, :, 256:512], dr["moe_w_up"][l, ex].rearrange("(c p) f -> p c f", p=128), writes=[Bwg])
                K.dma("pool", wd[:], dr["moe_w_down"][l, ex].rearrange("(c p) f -> p c f", p=128), writes=[Bwd])
                return wg, Bwg, wd, Bwd
            W = {}
            W[0] = load_expert(0)
            norm_phase(s, l, "ffn_norm", True)
            steps = [(ex, ch) for ex in range(16) for ch in range(4)]
            hes = {}

            def stage_a(ex, ch):
                wg, Bwg, wd, Bwd = W[ex]
                cs = slice(ch * 512, (ch + 1) * 512)
                cbps, Bcbps = psg.get()
                K.op("pe", lambda e: e.matmul(cbps[:, :], selE[:, ex, :], combT[:, cs], start=True, stop=True),
                     reads=[BselE, BcombT[ch]], writes=[Bcbps])
                cb, Bcb = cbp.get()
                K.op("act", lambda e: e.copy(cb[:], cbps[:, :]), reads=[Bcbps], writes=[Bcb])
                he, Bhe = hep.get()
                for fc in range(2):
                    gps, Bgps = psg.get()
                    ups, Bups = psg.get()
                    for c in range(DC):
                        K.op("pe", lambda e: e.matmul(gps[:, :], wg[:, c, fc * 128:(fc + 1) * 128], xnT[:, c, cs],
                                                      start=(c == 0), stop=(c == DC - 1)),
                             reads=[Bwg] + xnT_bufs(ch), writes=[Bgps])
                    for c in range(DC):
                        K.op("pe", lambda e: e.matmul(ups[:, :], wg[:, c, 256 + fc * 128:256 + (fc + 1) * 128], xnT[:, c, cs],
                                                      start=(c == 0), stop=(c == DC - 1)),
                             reads=[Bwg] + xnT_bufs(ch), writes=[Bups])
                    sg, Bsg = sgp.get()
                    K.op("act", lambda e: e.activation(sg[:], gps[:, :], AF.Silu), reads=[Bgps], writes=[Bsg])
                    K.op("pool", lambda e: e.tensor_tensor(sg[:], sg[:], cb[:], ALU.mult), reads=[Bsg, Bcb], writes=[Bsg])
                    K.op("dve", lambda e: e.tensor_tensor(he[:, fc, :], sg[:], ups[:, :], ALU.mult), reads=[Bsg, Bups], writes=[Bhe])
                hes[(ex, ch)] = (he, Bhe)

            def stage_b(ex, ch):
                wg, Bwg, wd, Bwd = W[ex]
                he, Bhe = hes.pop((ex, ch))
                for ts in range(4):
                    i = ch * 4 + ts
                    for half in range(2):
                        ops_, Bops = psg.get()
                        for fc in range(2):
                            K.op("pe", lambda e: e.matmul(ops_[:, :], he[:, fc, ts * 128:(ts + 1) * 128],
                                                          wd[:, fc, half * 512:(half + 1) * 512], start=(fc == 0), stop=(fc == 1)),
                                 reads=[Bhe, Bwd], writes=[Bops])
                        xs = x_sb[:, i, half * 512:(half + 1) * 512]
                        K.op("dve", lambda e: e.tensor_tensor(xs, xs, ops_[:, :], ALU.add), reads=[Bops, Bx[i][half]], writes=[Bx[i][half]])
                    if ex == 15 and io_box["final"] and not dbg:
                        K.dma("sp", out_d[s, i * 128:(i + 1) * 128, :], x_sb[:, i, :], reads=Bx[i], writes=[Bout])
                        io_box["stored"].add((s, i))
                        if s + 1 < n_seq:
                            K.dma("sp", x_sb[:, i, :], dr["x"][s + 1, i * 128:(i + 1) * 128, :], writes=Bx[i])
                            io_box["loaded"].add((s + 1, i))

            for k, (ex, ch) in enumerate(steps):
                stage_a(ex, ch)
                if k > 0:
                    pex, pch = steps[k - 1]
                    stage_b(pex, pch)
                    if pch == 3 and ex + 1 < 16:
                        W.pop(pex)
                        W[ex + 1] = load_expert(ex + 1)
                elif ex + 1 < 16:
                    W[1] = load_expert(1)
            stage_b(*steps[-1])
            phase_end(es)

        def alloc_ymT(es, m):
            ymT = sb(es, f"ymT{m}", [128, 2, T], BF16)
            BymT = [Buf(f"ymT{m}_{ch}") for ch in range(4)]
            ymT_box["t"] = ymT; ymT_box["b"] = BymT
            wo = sb(es, f"wo{m}", [128, 2, D], BF16); Bwo = Buf("wo")
            for c in range(2):
                K.dma("pool", wo[:, c, :], dr["w_out"][l_box[0], 256 * m + c * 128:256 * m + (c + 1) * 128, :], writes=[Bwo])
            ymT_box["wo"] = wo; ymT_box["Bwo"] = Bwo
            return ymT, BymT

        def wout_partial(es, l, m):
            ymT = ymT_box["t"]; BymT = ymT_box["b"]
            wo = ymT_box["wo"]; Bwo = ymT_box["Bwo"]
            for i in range(NT):
                for half in range(2):
                    ops_, Bops = psg.get()
                    for c in range(2):
                        K.op("pe", lambda e: e.matmul(ops_[:, :], ymT[:, c, i * 128:(i + 1) * 128], wo[:, c, half * 512:(half + 1) * 512],
                                                      start=(c == 0), stop=(c == 1)),
                             reads=[Bwo, BymT[i // 4]], writes=[Bops])
                    xs = x_sb[:, i, half * 512:(half + 1) * 512]
                    K.op("dve", lambda e: e.tensor_tensor(xs, xs, ops_[:, :], ALU.add), reads=[Bops, Bx[i][half]], writes=[Bx[i][half]])

        def fm_groupnorm(es_name, yv, Byv, m, l, gcol):
            es = ExitStack()
            sqp = Pool(es, nc, es_name + "sq", [128, 2, 512], BF16, 2)
            rsp = Pool(es, nc, es_name + "rs", [128, 512], F32, 2)
            for ch in range(4):
                cs = slice(ch * 512, (ch + 1) * 512)
                sq, Bsq = sqp.get()
                K.op("act", lambda e: e.activation(sq[:, :, :], yv[:, :, cs], AF.Square), reads=[Byv], writes=[Bsq])
                sps, Bsps = psg.get()
                for c in range(2):
                    K.op("pe", lambda e: e.matmul(sps[:, :], ones16[:], sq[:, c, :], start=(c == 0), stop=(c == 1)),
                         reads=[Bones, Bsq], writes=[Bsps])
                rr, Brr = rsp.get()
                rstd_from(rr[:], sps[:, :], 256, [Bsps], [Brr])
                for c in range(2):
                    K.op("dve", lambda e: e.scalar_tensor_tensor(ymT_box["t"][:, c, cs], yv[:, c, cs], gcol[:, 2 * m + c:2 * m + c + 1],
                                                                 rr[:], ALU.mult, ALU.mult),
                         reads=[Byv, Brr, Bgcol], writes=[ymT_box["b"][ch]])
            K.barrier_all()
            es.close()

        def dbg_dump(m):
            if dbg:
                K.dma("pool", dbg_d["ymT"][m], ymT_box["t"][:], reads=ymT_box["b"], writes=[Bout])

        def lru_phase(s, l):
            es = ExitStack()
            ymT, BymT = alloc_ymT(es, 1)
            cw = sb(es, "lcw", [128, 2, 4], F32); Bcw = Buf("lcw")
            vec = sb(es, "lvec", [128, 5, 2], F32); Bvec = Buf("lvec")
            K.dma("sp", cw[:], dr["lru_cw"][l], writes=[Bcw])
            K.dma("sp", vec[:], dr["lru_vec"][l], writes=[Bvec])
            wa = sb(es, "lwa", [128, 2, 128], BF16); Bwa = Buf("lwa")
            wi = sb(es, "lwi", [128, 2, 128], BF16); Bwi = Buf("lwi")
            for c in range(2):
                K.dma("pool", wa[:, c, :], dr["lru_wa"][l, c], writes=[Bwa])
                K.dma("pool", wi[:, c, :], dr["lru_wi"][l, c], writes=[Bwi])
            K.op("act", lambda e: e.activation(vec[:, 4, :], vec[:, 3, :], AF.Exp, scale=-1.0), reads=[Bvec], writes=[Bvec])
            K.op("act", lambda e: e.activation(vec[:, 4, :], vec[:, 4, :], AF.Ln, bias=1.0), reads=[Bvec], writes=[Bvec])
            K.op("dve", lambda e: e.tensor_scalar(vec[:, 4, :], vec[:, 4, :], -8.0, None, ALU.mult), reads=[Bvec], writes=[Bvec])
            yv = sb(es, "lyv", [128, 2, T], F32); Byv = Buf("lyv")
            e2 = ExitStack()

            def _mk_l(w):
                per = 8 // 2
                return NS(psg=allps.sub(range(w * per, w * per + per)),
                          xp=Pool(e2, nc, "lxp", [128, T + 3], F32, 1), u=Pool(e2, nc, "lu", [128, T], F32, 1),
                          wl=Pool(e2, nc, "lwl", [128, DC, 256], BF16, 1), tp=Pool(e2, nc, "ltp", [128, 512], F32, 5),
                          u16=Pool(e2, nc, "lu16", [128, 512], BF16, 2))

            def _body_l(c, P):
                xp, Bxp = P.xp.get()
                u, Bu = P.u.get()
                wl, Bwl = P.wl.get()
                K.dma("pool", wl[:, :, 0:128], dr["w_in"][l, :, 352 + c * 128:352 + (c + 1) * 128].rearrange("(k p) n -> p k n", p=128), writes=[Bwl])
                K.dma("pool", wl[:, :, 128:256], dr["w_in"][l, :, 608 + c * 128:608 + (c + 1) * 128].rearrange("(k p) n -> p k n", p=128), writes=[Bwl])
                K.op("dve", lambda e: e.memset(xp[:, 0:3], 0.0), writes=[Bxp])
                for ch in range(4):
                    cs = slice(ch * 512, (ch + 1) * 512)
                    p1, Bp1 = P.psg.get()
                    for k in range(DC):
                        K.op("pe", lambda e: e.matmul(p1[:, :], wl[:, k, 0:128], xnT[:, k, cs], start=(k == 0), stop=(k == DC - 1)),
                             reads=[Bwl] + xnT_bufs(ch), writes=[Bp1])
                    K.op("act", lambda e: e.copy(xp[:, 3 + ch * 512:3 + (ch + 1) * 512], p1[:, :]), reads=[Bp1], writes=[Bxp])
                    p2, Bp2 = P.psg.get()
                    for k in range(DC):
                        K.op("pe", lambda e: e.matmul(p2[:, :], wl[:, k, 128:256], xnT[:, k, cs], start=(k == 0), stop=(k == DC - 1)),
                             reads=[Bwl] + xnT_bufs(ch), writes=[Bp2])
                    K.op("act", lambda e: e.activation(yv[:, c, cs], p2[:, :], AF.Gelu_apprx_tanh), reads=[Bp2], writes=[Byv])
                K.op("dve", lambda e: e.tensor_scalar(u[:], xp[:, 0:T], cw[:, c, 0:1], vec[:, 0, c:c + 1], ALU.mult, ALU.add),
                     reads=[Bxp, Bcw, Bvec], writes=[Bu])
                for j in range(1, 4):
                    K.op("dve", lambda e: e.scalar_tensor_tensor(u[:], xp[:, j:j + T], cw[:, c, j:j + 1], u[:], ALU.mult, ALU.add),
                         reads=[Bxp, Bcw, Bu], writes=[Bu])
                for ch in range(4):
                    cs = slice(ch * 512, (ch + 1) * 512)
                    u16, Bu16 = P.u16.get()
                    K.op("act", lambda e: e.copy(u16[:], u[:, cs]), reads=[Bu], writes=[Bu16])
                    pa, Bpa = P.psg.get()
                    K.op("pe", lambda e: e.matmul(pa[:, :], wa[:, c, :], u16[:], start=True, stop=True), reads=[Bwa, Bu16], writes=[Bpa])
                    pi, Bpi = P.psg.get()
                    K.op("pe", lambda e: e.matmul(pi[:, :], wi[:, c, :], u16[:], start=True, stop=True), reads=[Bwi, Bu16], writes=[Bpi])
                    r_, Br_ = P.tp.get()
                    gi, Bgi = P.tp.get()
                    mu, Bmu = P.tp.get()
                    aa, Baa = P.tp.get()
                    K.op("act", lambda e: e.activation(r_[:], pa[:, :], AF.Sigmoid, bias=vec[:, 1, c:c + 1]), reads=[Bpa, Bvec], writes=[Br_])
                    K.op("act", lambda e: e.activation(gi[:], pi[:, :], AF.Sigmoid, bias=vec[:, 2, c:c + 1]), reads=[Bpi, Bvec], writes=[Bgi])
                    K.op("act", lambda e: e.activation(aa[:], r_[:], AF.Exp, scale=vec[:, 4, c:c + 1]), reads=[Br_, Bvec], writes=[Baa])
                    K.op("act", lambda e: e.activation(mu[:], aa[:], AF.Square), reads=[Baa], writes=[Bmu])
                    K.op("act", lambda e: e.activation(mu[:], mu[:], AF.Sqrt, scale=-1.0, bias=1.0), reads=[Bmu], writes=[Bmu])
                    if ch == 0:
                        K.op("dve", lambda e: e.memset(mu[:, 0:1], 1.0), reads=[Bmu], writes=[Bmu])
                    K.op("pool", lambda e: e.tensor_tensor(mu[:], mu[:], gi[:], ALU.mult), reads=[Bmu, Bgi], writes=[Bmu])
                    K.op("dve", lambda e: e.tensor_tensor(xp[:, cs], mu[:], u[:, cs], ALU.mult), reads=[Bmu, Bu, Bxp], writes=[Bxp])
                    init = 0.0 if ch == 0 else u[:, ch * 512 - 1:ch * 512]
                    K.op("dve", lambda e: e.tensor_tensor_scan(u[:, cs], aa[:], xp[:, cs], init, ALU.mult, ALU.add),
                         reads=[Baa, Bxp, Bu], writes=[Bu])
                    K.op("dve", lambda e: e.tensor_tensor(yv[:, c, cs], yv[:, c, cs], u[:, cs], ALU.mult), reads=[Byv, Bu], writes=[Byv])
            IL.run(2, 2, _mk_l, _body_l)
            K.barrier_all()
            e2.close()
            fm_groupnorm("lg", yv, Byv, 1, l, gcolt)
            dbg_dump(1)
            wout_partial(es, l, 1)
            phase_end(es)

        def s5_phase(s, l):
            es = ExitStack()
            ymT, BymT = alloc_ymT(es, 2)
            par = sb(es, "s5par", [128, 3, 8], F32); Bpar = Buf("s5par")
            bb = sb(es, "s5b", [128, 2, 8, 16], F32); Bbb = Buf("s5b")
            vec = sb(es, "s5vec", [128, 3, 2], F32); Bvec = Buf("s5vec")
            K.dma("sp", par[:], dr["s5_par"][l], writes=[Bpar])
            K.dma("sp", bb[:], dr["s5_b"][l], writes=[Bbb])
            K.dma("sp", vec[:], dr["s5_vec"][l], writes=[Bvec])
            wglu = sb(es, "s5glu", [128, 2, 256], BF16); Bwglu = Buf("s5glu")
            K.dma("pool", wglu[:], dr["s5_w_glu"][l].rearrange("(c p) n -> p c n", p=128), writes=[Bwglu])
            ypre = sb(es, "s5ypre", [128, 2, T], F32); Bypre = Buf("s5ypre")
            u16 = sb(es, "s5u16", [128, 2, T], BF16); Bu16 = Buf("s5u16")
            es2 = ExitStack()
            ws = sb(es2, "ws5", [128, DC, 256], BF16); Bws = Buf("ws5")
            K.dma("pool", ws[:], dr["w_in"][l, :, 864:1120].rearrange("(c p) n -> p c n", p=128), writes=[Bws])
            for c in range(2):
                for ch in range(4):
                    cs = slice(ch * 512, (ch + 1) * 512)
                    p1, Bp1 = psg.get()
                    for k in range(DC):
                        K.op("pe", lambda e: e.matmul(p1[:, :], ws[:, k, c * 128:(c + 1) * 128], xnT[:, k, cs], start=(k == 0), stop=(k == DC - 1)),
                             reads=[Bws] + xnT_bufs(ch), writes=[Bp1])
                    K.op("act", lambda e: e.activation(ypre[:, c, cs], p1[:, :], AF.Identity, scale=vec[:, 0, c:c + 1]), reads=[Bp1, Bvec], writes=[Bypre])
                    K.op("dve", lambda e: e.tensor_copy(u16[:, c, cs], p1[:, :]), reads=[Bp1], writes=[Bu16])
            K.barrier_all()
            es2.close()
            sc = sb(es, "s5sc", [128, 16, 8], F32); Bsc = Buf("s5sc")
            SC = [Bsc]
            are = par[:, 0, :]; aim = par[:, 1, :]; ldt = par[:, 2, :]
            dt = sc[:, 0, :]; mag = sc[:, 1, :]; th = sc[:, 2, :]; cth = sc[:, 3, :]; sth = sc[:, 4, :]
            abr = sc[:, 5, :]; abi = sc[:, 6, :]; den = sc[:, 7, :]; gre = sc[:, 8, :]; gim = sc[:, 9, :]
            t0 = sc[:, 10, :]; t1 = sc[:, 11, :]; nre = sc[:, 12, :]
            ki = sb(es, "s5ki", [128, 8], I32); Bki = Buf("s5ki")
            RP = [Bpar, Bsc]
            K.op("act", lambda e: e.activation(dt, ldt, AF.Exp), reads=RP, writes=SC)
            K.op("dve", lambda e: e.tensor_tensor(t0, dt, are, ALU.mult), reads=RP, writes=SC)
            K.op("act", lambda e: e.activation(mag, t0, AF.Exp), reads=RP, writes=SC)
            K.op("dve", lambda e: e.tensor_tensor(th, dt, aim, ALU.mult), reads=RP, writes=SC)
            for (o, shift) in ((sth, 0.0), (cth, 0.5 * np.pi)):
                K.op("dve", lambda e: e.tensor_scalar(ki[:, :], th, shift, 1.0 / TWO_PI, ALU.add, ALU.mult), reads=RP, writes=[Bki])
                K.op("dve", lambda e: e.tensor_copy(t1, ki[:, :]), reads=[Bki], writes=SC)
                K.op("dve", lambda e: e.scalar_tensor_tensor(t1, t1, -TWO_PI, th, ALU.mult, ALU.add), reads=RP, writes=SC)
                K.op("dve", lambda e: e.tensor_scalar(t1, t1, shift, None, ALU.add), reads=RP, writes=SC)
                K.op("dve", lambda e: e.tensor_scalar(t1, t1, 3.14159, -3.14159, ALU.min, ALU.max), reads=RP, writes=SC)
                K.op("act", lambda e: e.activation(o, t1, AF.Sin), reads=RP, writes=SC)
            K.op("dve", lambda e: e.tensor_tensor(abr, mag, cth, ALU.mult), reads=RP, writes=SC)
            K.op("dve", lambda e: e.tensor_tensor(abi, mag, sth, ALU.mult), reads=RP, writes=SC)
            K.op("dve", lambda e: e.tensor_tensor(den, are, are, ALU.mult), reads=RP, writes=SC)
            K.op("dve", lambda e: e.tensor_tensor(t0, aim, aim, ALU.mult), reads=RP, writes=SC)
            K.op("dve", lambda e: e.tensor_tensor(den, den, t0, ALU.add), reads=RP, writes=SC)
            K.op("dve", lambda e: e.reciprocal(den, den), reads=RP, writes=SC)
            K.op("dve", lambda e: e.tensor_scalar(nre, abr, -1.0, None, ALU.add), reads=RP, writes=SC)
            K.op("dve", lambda e: e.tensor_tensor(t0, nre, are, ALU.mult), reads=RP, writes=SC)
            K.op("dve", lambda e: e.tensor_tensor(t1, abi, aim, ALU.mult), reads=RP, writes=SC)
            K.op("dve", lambda e: e.tensor_tensor(gre, t0, t1, ALU.add), reads=RP, writes=SC)
            K.op("dve", lambda e: e.tensor_tensor(gre, gre, den, ALU.mult), reads=RP, writes=SC)
            K.op("dve", lambda e: e.tensor_tensor(t0, abi, are, ALU.mult), reads=RP, writes=SC)
            K.op("dve", lambda e: e.tensor_tensor(t1, nre, aim, ALU.mult), reads=RP, writes=SC)
            K.op("dve", lambda e: e.tensor_tensor(gim, t0, t1, ALU.subtract), reads=RP, writes=SC)
            K.op("dve", lambda e: e.tensor_tensor(gim, gim, den, ALU.mult), reads=RP, writes=SC)
            bbar = sb(es, "s5bbar", [128, 2, 8, 16], F32); Bbbar = Buf("s5bbar")
            btmp = sb(es, "s5btmp", [128, 8, 16], F32); Bbtmp = Buf("s5btmp")
            gre_b = gre.unsqueeze(2).to_broadcast([128, 8, 16]); gim_b = gim.unsqueeze(2).to_broadcast([128, 8, 16])
            K.op("dve", lambda e: e.tensor_tensor(bbar[:, 0], bb[:, 0], gre_b, ALU.mult), reads=[Bbb, Bsc], writes=[Bbbar])
            K.op("dve", lambda e: e.tensor_tensor(btmp[:], bb[:, 1], gim_b, ALU.mult), reads=[Bbb, Bsc], writes=[Bbtmp])
            K.op("dve", lambda e: e.tensor_tensor(bbar[:, 0], bbar[:, 0], btmp[:], ALU.subtract), reads=[Bbbar, Bbtmp], writes=[Bbbar])
            K.op("dve", lambda e: e.tensor_tensor(bbar[:, 1], bb[:, 1], gre_b, ALU.mult), reads=[Bbb, Bsc], writes=[Bbbar])
            K.op("dve", lambda e: e.tensor_tensor(btmp[:], bb[:, 0], gim_b, ALU.mult), reads=[Bbb, Bsc, Bbbar], writes=[Bbtmp])
            K.op("dve", lambda e: e.tensor_tensor(bbar[:, 1], bbar[:, 1], btmp[:], ALU.add), reads=[Bbbar, Bbtmp], writes=[Bbbar])
            mmA = sb(es, "s5mmA", [128, 10, 2, 8], F32); BmmA = Buf("s5mmA")
            mtA = sb(es, "s5mtA", [128, 3, 8], F32); BmtA = Buf("s5mtA")
            K.op("act", lambda e: e.copy(mmA[:, 0, 0, :], sc[:, 3, :]), reads=[Bsc], writes=[BmmA])
            K.op("act", lambda e: e.copy(mmA[:, 0, 1, :], sc[:, 4, :]), reads=[Bsc], writes=[BmmA])
            for k in range(1, 10):
                pr_ = mmA[:, k - 1, 0, :]; pi_ = mmA[:, k - 1, 1, :]
                K.op("dve", lambda e: e.tensor_tensor(mtA[:, 0, :], pr_, pr_, ALU.mult), reads=[BmmA], writes=[BmtA])
                K.op("dve", lambda e: e.tensor_tensor(mtA[:, 1, :], pi_, pi_, ALU.mult), reads=[BmmA], writes=[BmtA])
                K.op("dve", lambda e: e.tensor_tensor(mmA[:, k, 0, :], mtA[:, 0, :], mtA[:, 1, :], ALU.subtract), reads=[BmtA, BmmA], writes=[BmmA])
                K.op("dve", lambda e: e.tensor_tensor(mtA[:, 2, :], pr_, pi_, ALU.mult), reads=[BmmA], writes=[BmtA])
                K.op("dve", lambda e: e.tensor_scalar(mmA[:, k, 1, :], mtA[:, 2, :], 2.0, None, ALU.mult), reads=[BmtA, BmmA], writes=[BmmA])
            e3 = ExitStack()
            def _mk_s5(w):
                per = 8 // 2
                nb = per - 1
                return NS(psg=allps.sub(range(w * per, w * per + nb)), psa=allps.sub(range(w * per + nb, (w + 1) * per)), bexpP=Pool(e3, nc, "s5bexp", [128, 2, 128], F32, 1), blp=Pool(e3, nc, "s5bl", [128, 2, 128], BF16, 1), cwp=Pool(e3, nc, "s5cw", [128, 4, 128], BF16, 1), ctmp=Pool(e3, nc, "s5ct", [128, 2, 128], F32, 1), cosP=Pool(e3, nc, "s5cos", [128, 512], F32, 1), sinP=Pool(e3, nc, "s5sin", [128, 512], F32, 1), mmP=Pool(e3, nc, "s5mm", [128, 12, 2], F32, 1), mtP=Pool(e3, nc, "s5mt", [128, 8], F32, 1), carP=Pool(e3, nc, "s5car", [128, 4], F32, 1), big=Pool(e3, nc, "s5big", [128, 512], F32, 6), pr16=Pool(e3, nc, "s5pr", [128, 4, 512], BF16, 1))
            def _body_s5(j, P):
                bexp, Bbexp = P.bexpP.get()
                cosT, Bcos = P.cosP.get()
                sinT, Bsin = P.sinP.get()
                mm_, Bmm_unused = P.mmP.get()
                mt, Bmt = P.mtP.get()
                car, Bcar = P.carP.get()
                ct_ = (32 * j) // 128
                off = (32 * j) % 128
                K.op("pool", lambda e: e.memset(bexp[:], 0.0), reads=[], writes=[Bbexp])
                for ri in range(2):
                    K.op("pool", lambda e: e.tensor_copy(bexp[0:64, ri, off:off + 16], bbar[0:64, ri, j, :]), reads=[Bbbar], writes=[Bbexp])
                    K.op("pool", lambda e: e.tensor_copy(bexp[64:128, ri, off + 16:off + 32], bbar[64:128, ri, j, :]), reads=[Bbbar], writes=[Bbexp])
                pt, Bpt = P.psg.get()
                for ri in range(2):
                    K.op("pe", lambda e: e.transpose(pt[:, ri * 128:(ri + 1) * 128], bexp[:, ri, :], ident[:]), reads=[Bbexp, Bident], writes=[Bpt])
                bl, Bbl = P.blp.get()
                K.op("act", lambda e: e.copy(bl[:, :, :], pt[:, 0:256].rearrange("p (r n) -> p r n", r=2)), reads=[Bpt], writes=[Bbl])
                ct, Bct = P.ctmp.get()
                for ri in range(2):
                    K.dma("sp", ct[:, ri, :], dr["s5_c"][l, ri, j], writes=[Bct])
                cw4, Bcw4 = P.cwp.get()
                K.op("act", lambda e: e.copy(cw4[:, 0, :], ct[:, 0, :]), reads=[Bct], writes=[Bcw4])
                K.op("act", lambda e: e.mul(cw4[:, 1, :], ct[:, 0, :], -1.0), reads=[Bct], writes=[Bcw4])
                K.op("act", lambda e: e.mul(cw4[:, 2, :], ct[:, 1, :], -1.0), reads=[Bct], writes=[Bcw4])
                K.op("act", lambda e: e.mul(cw4[:, 3, :], ct[:, 1, :], -1.0), reads=[Bct], writes=[Bcw4])
                K.op("dve", lambda e: e.memset(cosT[:, 0:1], 1.0), writes=[Bcos])
                K.op("dve", lambda e: e.memset(sinT[:, 0:1], 0.0), writes=[Bsin])
                tb, Btb = P.big.get()
                for k in range(9):
                    n = 1 << k
                    mr = mmA[:, k, 0, j:j + 1]; mi = mmA[:, k, 1, j:j + 1]
                    K.op("dve", lambda e: e.tensor_scalar(tb[:, 0:n], sinT[:, 0:n], mi, None, ALU.mult), reads=[Bsin, BmmA], writes=[Btb])
                    K.op("dve", lambda e: e.scalar_tensor_tensor(cosT[:, n:2 * n], cosT[:, 0:n], mr, tb[:, 0:n], ALU.mult, ALU.subtract),
                         reads=[Bcos, BmmA, Btb], writes=[Bcos])
                    K.op("dve", lambda e: e.tensor_scalar(tb[:, 0:n], sinT[:, 0:n], mr, None, ALU.mult), reads=[Bsin, BmmA, Bcos], writes=[Btb])
                    K.op("dve", lambda e: e.scalar_tensor_tensor(sinT[:, n:2 * n], cosT[:, 0:n], mi, tb[:, 0:n], ALU.mult, ALU.add),
                         reads=[Bcos, BmmA, Btb], writes=[Bsin])
                magb = sc[:, 1, j:j + 1].to_broadcast([128, 512])
                m9r = mmA[:, 9, 0, j:j + 1]; m9i = mmA[:, 9, 1, j:j + 1]
                for ch in range(4):
                    cs = slice(ch * 512, (ch + 1) * 512)
                    pre, Bpre = P.psg.get()
                    K.op("pe", lambda e: e.matmul(pre[:, :], bl[:, 0, :], u16[:, ct_, cs], start=True, stop=True), reads=[Bbl, Bu16], writes=[Bpre])
                    pim, Bpim = P.psg.get()
                    K.op("pe", lambda e: e.matmul(pim[:, :], bl[:, 1, :], u16[:, ct_, cs], start=True, stop=True), reads=[Bbl, Bu16], writes=[Bpim])
                    brr, Bbrr = P.big.get()
                    bri, Bbri = P.big.get()
                    ta, Bta = P.big.get()
                    tb2, Btb2 = P.big.get()
                    K.op("dve", lambda e: e.tensor_tensor(brr[:], cosT[:], pre[:, :], ALU.mult), reads=[Bcos, Bpre], writes=[Bbrr])
                    K.op("dve", lambda e: e.tensor_tensor(ta[:], sinT[:], pim[:, :], ALU.mult), reads=[Bsin, Bpim], writes=[Bta])
                    K.op("pool", lambda e: e.tensor_tensor(brr[:], brr[:], ta[:], ALU.add), reads=[Bbrr, Bta], writes=[Bbrr])
                    K.op("dve", lambda e: e.tensor_tensor(bri[:], cosT[:], pim[:, :], ALU.mult), reads=[Bcos, Bpim], writes=[Bbri])
                    K.op("dve", lambda e: e.tensor_tensor(tb2[:], sinT[:], pre[:, :], ALU.mult), reads=[Bsin, Bpre], writes=[Btb2])
                    K.op("pool", lambda e: e.tensor_tensor(bri[:], bri[:], tb2[:], ALU.subtract), reads=[Bbri, Btb2], writes=[Bbri])
                    if ch == 0:
                        ir, ii = 0.0, 0.0
                    else:
                        K.op("dve", lambda e: e.tensor_tensor(mt[:, 4:5], car[:, 0:1], m9r, ALU.mult), reads=[Bcar, BmmA], writes=[Bmt])
                        K.op("dve", lambda e: e.tensor_tensor(mt[:, 5:6], car[:, 1:2], m9i, ALU.mult), reads=[Bcar, BmmA], writes=[Bmt])
                        K.op("dve", lambda e: e.tensor_tensor(mt[:, 6:7], car[:, 1:2], m9r, ALU.mult), reads=[Bcar, BmmA], writes=[Bmt])
                        K.op("dve", lambda e: e.tensor_tensor(mt[:, 7:8], car[:, 0:1], m9i, ALU.mult), reads=[Bcar, BmmA], writes=[Bmt])
                        K.op("dve", lambda e: e.tensor_tensor(car[:, 2:3], mt[:, 4:5], mt[:, 5:6], ALU.subtract), reads=[Bmt, Bcar], writes=[Bcar])
                        K.op("dve", lambda e: e.tensor_tensor(car[:, 3:4], mt[:, 6:7], mt[:, 7:8], ALU.add), reads=[Bmt, Bcar], writes=[Bcar])
                        ir, ii = car[:, 2:3], car[:, 3:4]
                    K.op("dve", lambda e: e.tensor_tensor_scan(ta[:], magb, brr[:], ir, ALU.mult, ALU.add), reads=[Bsc, Bbrr, Bta, Bcar], writes=[Bta])
                    K.op("dve", lambda e: e.tensor_tensor_scan(tb2[:], magb, bri[:], ii, ALU.mult, ALU.add), reads=[Bsc, Bbri, Btb2, Bcar], writes=[Btb2])
                    K.op("act", lambda e: e.copy(car[:, 0:1], ta[:, 511:512]), reads=[Bta, Bcar], writes=[Bcar])
                    K.op("act", lambda e: e.copy(car[:, 1:2], tb2[:, 511:512]), reads=[Btb2, Bcar], writes=[Bcar])
                    pp, Bpp = P.pr16.get()
                    K.op("dve", lambda e: e.tensor_tensor(pp[:, 0, :], cosT[:], ta[:], ALU.mult), reads=[Bcos, Bta], writes=[Bpp])
                    K.op("pool", lambda e: e.tensor_tensor(pp[:, 1, :], sinT[:], tb2[:], ALU.mult), reads=[Bsin, Btb2], writes=[Bpp])
                    K.op("dve", lambda e: e.tensor_tensor(pp[:, 2, :], sinT[:], ta[:], ALU.mult), reads=[Bsin, Bta], writes=[Bpp])
                    K.op("pool", lambda e: e.tensor_tensor(pp[:, 3, :], cosT[:], tb2[:], ALU.mult), reads=[Bcos, Btb2], writes=[Bpp])
                    yp, Byp = P.psg.get()
                    for v in range(4):
                        K.op("pe", lambda e: e.matmul(yp[:, :], cw4[:, v, :], pp[:, v, :], start=(v == 0), stop=(v == 3)), reads=[Bcw4, Bpp], writes=[Byp])
                    K.op("dve", lambda e: e.tensor_tensor(ypre[:, ct_, cs], ypre[:, ct_, cs], yp[:, :], ALU.add), reads=[Byp, Bypre], writes=[Bypre])
            IL.run(8, 2, _mk_s5, _body_s5)
            K.barrier_all()
            e3.close()
            yg16 = sb(es, "s5yg16", [128, 2, T], BF16); Byg16 = Buf("s5yg16")
            for c in range(2):
                K.op("act", lambda e: e.activation(ypre[:, c, :], ypre[:, c, :], AF.Gelu_apprx_tanh), reads=[Bypre], writes=[Bypre])
                K.op("act", lambda e: e.copy(yg16[:, c, :], ypre[:, c, :]), reads=[Bypre], writes=[Byg16])
            sgp = Pool(es, nc, "s5sg", [128, 512], F32, 2)
            for c in range(2):
                for ch in range(4):
                    cs = slice(ch * 512, (ch + 1) * 512)
                    zp, Bzp = psg.get()
                    for k in range(2):
                        K.op("pe", lambda e: e.matmul(zp[:, :], wglu[:, k, c * 128:(c + 1) * 128], yg16[:, k, cs], start=(k == 0), stop=(k == 1)),
                             reads=[Bwglu, Byg16], writes=[Bzp])
                    sg, Bsg = sgp.get()
                    K.op("act", lambda e: e.activation(sg[:], zp[:, :], AF.Sigmoid, bias=vec[:, 1, c:c + 1]), reads=[Bzp, Bvec], writes=[Bsg])
                    K.op("dve", lambda e: e.tensor_tensor(ypre[:, c, cs], ypre[:, c, cs], sg[:], ALU.mult), reads=[Bypre, Bsg], writes=[Bypre])
            fm_groupnorm("sg", ypre, Bypre, 2, l, gcolt)
            dbg_dump(2)
            wout_partial(es, l, 2)
            phase_end(es)

        def rope_tables(es, name, inv_name, half, pos_cols, Bpos_in, npart=128):
            ncol = pos_cols.shape[1]
            inv = sb(es, name + "inv", [128, half], F32); Binv = Buf(name + "inv")
            K.dma("sp", inv[:], dr[inv_name][:], writes=[Binv])
            ang = sb(es, name + "ang", [128, ncol, half], F32); Bang = Buf(name + "ang")
            tmp = sb(es, name + "tmp", [128, ncol, half], F32); Btmp = Buf(name + "tmp")
            kk = sb(es, name + "kk", [128, ncol, half], I32); Bkk = Buf(name + "kk")
            cs_ = sb(es, name + "cs", [128, 2, ncol, half], F32); Bcs = Buf(name + "cs")
            P = npart
            K.op("dve", lambda e: e.tensor_tensor(ang[0:P], inv[0:P].unsqueeze(1).to_broadcast([P, ncol, half]),
                                                  pos_cols.unsqueeze(2).to_broadcast([P, ncol, half]), ALU.mult),
                 reads=[Binv, Bpos_in], writes=[Bang])
            for idx, shift in ((0, 0.5 * np.pi), (1, 0.0)):
                K.op("dve", lambda e: e.tensor_scalar(kk[0:P], ang[0:P], shift, 1.0 / TWO_PI, ALU.add, ALU.mult), reads=[Bang], writes=[Bkk])
                K.op("dve", lambda e: e.tensor_copy(tmp[0:P], kk[0:P]), reads=[Bkk], writes=[Btmp])
                K.op("dve", lambda e: e.scalar_tensor_tensor(tmp[0:P], tmp[0:P], -TWO_PI, ang[0:P], ALU.mult, ALU.add), reads=[Btmp, Bang], writes=[Btmp])
                K.op("dve", lambda e: e.tensor_scalar(tmp[0:P], tmp[0:P], shift, None, ALU.add), reads=[Btmp], writes=[Btmp])
                K.op("dve", lambda e: e.tensor_scalar(tmp[0:P], tmp[0:P], 3.14159, -3.14159, ALU.min, ALU.max), reads=[Btmp], writes=[Btmp])
                K.op("act", lambda e: e.activation(cs_[0:P, idx], tmp[0:P], AF.Sin), reads=[Btmp], writes=[Bcs])
            return cs_, Bcs

        def apply_rope(dst, src, cos_t, sin_t, nh, half, tmp, reads, writes, Btmp):
            x1 = src[:, :, 0:half]; x2 = src[:, :, half:2 * half]
            cb = cos_t.unsqueeze(1).to_broadcast([128, nh, half]); sb_ = sin_t.unsqueeze(1).to_broadcast([128, nh, half])
            ta = tmp[:, 0:nh, 0:half]; tb_ = tmp[:, 0:nh, half:2 * half]
            K.op("dve", lambda e: e.tensor_tensor(ta, x1, cb, ALU.mult), reads=reads, writes=[Btmp])
            K.op("dve", lambda e: e.tensor_tensor(tb_, x2, sb_, ALU.mult), reads=reads, writes=[Btmp])
            K.op("dve", lambda e: e.tensor_tensor(dst[:, :, 0:half], ta, tb_, ALU.subtract), reads=[Btmp], writes=writes)
            K.op("dve", lambda e: e.tensor_tensor(ta, x1, sb_, ALU.mult), reads=reads + [Btmp], writes=[Btmp])
            K.op("dve", lambda e: e.tensor_tensor(tb_, x2, cb, ALU.mult), reads=reads + [Btmp], writes=[Btmp])
            K.op("dve", lambda e: e.tensor_tensor(dst[:, :, half:2 * half], ta, tb_, ALU.add), reads=[Btmp], writes=writes)

        def attn_block_group(s_items, acc, Bacc, first_group, last_group):
            pass

        def mla_phase(s, l):
            es = ExitStack()
            ymT, BymT = alloc_ymT(es, 0)
            qT = sb(es, "mqT", [128, 4, T], BF16); BqT = [Buf(f"mqT{i}") for i in range(NT)]
            kT = sb(es, "mkT", [128, 4, T], BF16); BkT = [Buf(f"mkT{i}") for i in range(NT)]
            K.op("pool", lambda e: e.memset(qT[64:128], 0.0), writes=BqT)
            K.op("pool", lambda e: e.memset(kT[64:128], 0.0), writes=BkT)
            va = sb(es, "mva", [128, NT, 4, 65], BF16); Bva = [Buf(f"mva{i}") for i in range(NT)]
            K.op("pool", lambda e: e.memset(va[:, :, :, 64:65], 1.0), writes=Bva)
            e1 = ExitStack()
            wm = sb(e1, "wm", [128, DC, 352], BF16); Bwm = Buf("wm")
            K.dma("pool", wm[:], dr["w_in"][l, :, 0:352].rearrange("(c p) n -> p c n", p=128), writes=[Bwm])
            wuq = sb(e1, "wuq", [128, 2, 384], BF16); Bwuq = Buf("wuq")
            K.dma("pool", wuq[:, 0, :], dr["mla_w_uq"][l, 0:128, :], writes=[Bwuq])
            K.dma("pool", wuq[0:64, 1, :], dr["mla_w_uq"][l, 128:192, :], writes=[Bwuq])
            wukv = sb(e1, "wukv", [128, 512], BF16); Bwukv = Buf("wukv")
            K.dma("pool", wukv[:], dr["mla_w_ukv"][l], writes=[Bwukv])
            gv = sb(e1, "mgv", [128, 192 + 128 + 96 + 96], F32); Bgv = Buf("mgv")
            K.dma("sp", gv[:, 0:192], dr["mla_g_cq"][l:l + 1, :].to_broadcast([128, 192]), writes=[Bgv])
            K.dma("sp", gv[:, 192:320], dr["mla_g_ckv"][l:l + 1, :].to_broadcast([128, 128]), writes=[Bgv])
            K.dma("sp", gv[:, 320:416], dr["mla_g_q"][l:l + 1, :].to_broadcast([128, 96]), writes=[Bgv])
            K.dma("sp", gv[:, 416:512], dr["mla_g_k"][l:l + 1, :].to_broadcast([128, 96]), writes=[Bgv])
            g_cq = gv[:, 0:192]; g_ckv = gv[:, 192:320]; g_q = gv[:, 320:416]; g_k = gv[:, 416:512]
            cs_t, Bcs_t = rope_tables(e1, "mr", "c_inv_mla", 16, posf[:, :], Bposf)
            def _mk_mp(w):
                per = 8 // 3
                nb = per - 0
                return NS(psg=allps.sub(range(w * per, w * per + nb)), psa=allps.sub(range(w * per + nb, (w + 1) * per)), st=Pool(e1, nc, "mst", [128, 16], F32, 2), cn=Pool(e1, nc, "mcn", [128, 320], BF16, 1), cT=Pool(e1, nc, "mcT", [128, 3, 128], BF16, 1), qn=Pool(e1, nc, "mqn", [128, 4, 96], F32, 1), kn=Pool(e1, nc, "mkn", [128, 4, 96], F32, 1), qr=Pool(e1, nc, "mqr", [128, 8, 96], BF16, 1), jk=Pool(e1, nc, "mjk", [128, 192], F32, 1), rtmp=Pool(e1, nc, "mrt", [128, 4, 32], F32, 1), kpe=Pool(e1, nc, "mkpe", [128, 32], F32, 1))
            def _body_mp(i, P):
                ts_ = slice(i * 128, (i + 1) * 128)
                pp, Bpp = P.psg.get()
                for c in range(DC):
                    K.op("pe", lambda e: e.matmul(pp[:, 0:352], xnT[:, c, ts_], wm[:, c, :], start=(c == 0), stop=(c == DC - 1)),
                         reads=[Bwm, BxnT[i][0], BxnT[i][1]], writes=[Bpp])
                sq, Bsq = P.st.get()
                j_, Bj_ = P.jk.get()
                K.op("act", lambda e: e.activation(j_[:, 0:192], pp[:, 0:192], AF.Square, accum_out=sq[:, 0:1]), reads=[Bpp], writes=[Bj_, Bsq])
                K.op("act", lambda e: e.activation(j_[:, 0:128], pp[:, 192:320], AF.Square, accum_out=sq[:, 1:2]), reads=[Bpp], writes=[Bj_, Bsq])
                K.op("act", lambda e: e.activation(j_[:, 0:32], pp[:, 320:352], AF.Square, accum_out=sq[:, 2:3]), reads=[Bpp], writes=[Bj_, Bsq])
                K.op("dve", lambda e: e.tensor_scalar(sq[:, 0:1], sq[:, 0:1], 128.0 / 192.0, None, ALU.mult), reads=[Bsq], writes=[Bsq])
                rstd_from(sq[:, 4:6], sq[:, 0:2], 128, [Bsq], [Bsq])
                c_n, Bc_n = P.cn.get()
                K.op("dve", lambda e: e.scalar_tensor_tensor(c_n[:, 0:192], pp[:, 0:192], sq[:, 4:5], g_cq, ALU.mult, ALU.mult),
                     reads=[Bpp, Bsq, Bgv], writes=[Bc_n])
                K.op("dve", lambda e: e.scalar_tensor_tensor(c_n[:, 192:320], pp[:, 192:320], sq[:, 5:6], g_ckv, ALU.mult, ALU.mult),
                     reads=[Bpp, Bsq, Bgv], writes=[Bc_n])
                kp, Bkp = P.kpe.get()
                K.op("act", lambda e: e.copy(kp[:], pp[:, 320:352]), reads=[Bpp], writes=[Bkp])
                pt, Bpt = P.psg.get()
                ptb = pt[:, :].bitcast(BF16)
                K.op("pe", lambda e: e.transpose(ptb[:, 0:128], c_n[:, 0:128], identb[:]), reads=[Bc_n, Bidentb], writes=[Bpt])
                K.op("pe", lambda e: e.transpose(ptb[0:64, 128:256], c_n[:, 128:192], identb[:]), reads=[Bc_n, Bidentb], writes=[Bpt])
                K.op("pe", lambda e: e.transpose(ptb[:, 256:384], c_n[:, 192:320], identb[:]), reads=[Bc_n, Bidentb], writes=[Bpt])
                ct, Bct = P.cT.get()
                K.op("act", lambda e: e.copy(ct[:, 0, :], ptb[:, 0:128]), reads=[Bpt], writes=[Bct])
                K.op("act", lambda e: e.copy(ct[0:64, 1, :], ptb[0:64, 128:256]), reads=[Bpt], writes=[Bct])
                K.op("act", lambda e: e.copy(ct[:, 2, :], ptb[:, 256:384]), reads=[Bpt], writes=[Bct])
                pq, Bpq = P.psg.get()
                K.op("pe", lambda e: e.matmul(pq[:, 0:384], ct[:, 0, :], wuq[:, 0, :], start=True, stop=False), reads=[Bct, Bwuq], writes=[Bpq])
                K.op("pe", lambda e: e.matmul(pq[:, 0:384], ct[0:64, 1, :], wuq[0:64, 1, :], start=False, stop=True), reads=[Bct, Bwuq], writes=[Bpq])
                pkv, Bpkv = P.psg.get()
                K.op("pe", lambda e: e.matmul(pkv[:, :], ct[:, 2, :], wukv[:], start=True, stop=True), reads=[Bct, Bwukv], writes=[Bpkv])
                pq3 = pq[:, 0:384].rearrange("p (h d) -> p h d", h=4)
                pkv3 = pkv[:, :].rearrange("p (h d) -> p h d", h=4)
                sq2, Bsq2 = P.st.get()
                for h in range(4):
                    K.op("act", lambda e: e.activation(j_[:, 0:96], pq3[:, h, :], AF.Square, accum_out=sq2[:, h:h + 1]), reads=[Bpq], writes=[Bj_, Bsq2])
                    K.op("act", lambda e: e.activation(j_[:, 0:64], pkv3[:, h, 0:64], AF.Square, accum_out=sq2[:, 4 + h:5 + h]), reads=[Bpkv], writes=[Bj_, Bsq2])
                K.op("dve", lambda e: e.tensor_scalar(sq2[:, 4:8], sq2[:, 4:8], sq[:, 2:3], None, ALU.add), reads=[Bsq2, Bsq], writes=[Bsq2])
                rstd_from(sq2[:, 8:16], sq2[:, 0:8], 96, [Bsq2], [Bsq2])
                q_n, Bq_n = P.qn.get()
                k_n, Bk_n = P.kn.get()
                for h in range(4):
                    K.op("dve", lambda e: e.scalar_tensor_tensor(q_n[:, h, :], pq3[:, h, :], sq2[:, 8 + h:9 + h], g_q, ALU.mult, ALU.mult),
                         reads=[Bpq, Bsq2, Bgv], writes=[Bq_n])
                    K.op("dve", lambda e: e.scalar_tensor_tensor(k_n[:, h, 32:96], pkv3[:, h, 0:64], sq2[:, 12 + h:13 + h], g_k[:, 32:96], ALU.mult, ALU.mult),
                         reads=[Bpkv, Bsq2, Bgv], writes=[Bk_n])
                    K.op("dve", lambda e: e.scalar_tensor_tensor(k_n[:, h, 0:32], kp[:], sq2[:, 12 + h:13 + h], g_k[:, 0:32], ALU.mult, ALU.mult),
                         reads=[Bkp, Bsq2, Bgv], writes=[Bk_n])
                q_r, Bq_r = P.qr.get()
                rt_, Brt_ = P.rtmp.get()
                apply_rope(q_r[:, 0:4], q_n[:, :, :], cs_t[:, 0, i, :], cs_t[:, 1, i, :], 4, 16, rt_, [Bq_n, Bcs_t], [Bq_r], Brt_)
                K.op("act", lambda e: e.copy(q_r[:, 0:4, 32:96], q_n[:, :, 32:96]), reads=[Bq_n], writes=[Bq_r])
                apply_rope(q_r[:, 4:8], k_n[:, :, :], cs_t[:, 0, i, :], cs_t[:, 1, i, :], 4, 16, rt_, [Bk_n, Bcs_t], [Bq_r], Brt_)
                K.op("act", lambda e: e.copy(q_r[:, 4:8, 32:96], k_n[:, :, 32:96]), reads=[Bk_n], writes=[Bq_r])
                K.op("act", lambda e: e.copy(va[:, i, :, 0:64], pkv3[:, :, 64:128]), reads=[Bpkv], writes=[Bva[i]])
                for grp in range(2):
                    pt2, Bpt2 = P.psg.get()
                    pt2b = pt2[:, :].bitcast(BF16)
                    for h in range(4):
                        K.op("pe", lambda e: e.transpose(pt2b[0:96, h * 128:(h + 1) * 128], q_r[:, grp * 4 + h, :], identb[:]),
                             reads=[Bq_r, Bidentb], writes=[Bpt2])
                    dstT = qT if grp == 0 else kT
                    BdT = BqT if grp == 0 else BkT
                    K.op("act" if grp == 0 else "dve",
                         (lambda e: e.copy(dstT[0:96, :, ts_], pt2b[0:96, 0:512].rearrange("p (h t) -> p h t", h=4))) if grp == 0 else
                         (lambda e: e.tensor_copy(dstT[0:96, :, ts_], pt2b[0:96, 0:512].rearrange("p (h t) -> p h t", h=4))),
                         reads=[Bpt2], writes=[BdT[i]])
            IL.run(NT, 3, _mk_mp, _body_mp)
            K.barrier_all()
            e1.close()
            scale = 96 ** -0.5
            def _mk_ma(w):
                per = 8 // 4
                nb = per - 1
                return NS(psg=allps.sub(range(w * per, w * per + nb)), psa=allps.sub(range(w * per + nb, (w + 1) * per)), pexp=Pool(es, nc, "mpe", [128, 512], BF16, 3), yo=Pool(es, nc, "myo", [128, 256], F32, 2), yst=Pool(es, nc, "myst", [128, 8], F32, 2), yb=Pool(es, nc, "myb", [128, 256], BF16, 2))
            def _body_ma(qt, P):
                qs = slice(qt * 128, (qt + 1) * 128)
                y_o, By_o = P.yo.get()
                yst_, Byst = P.yst.get()
                for h in range(4):
                    acc, Bacc = P.psa.get()
                    nk = qt + 1
                    for g0 in range(0, nk, 4):
                        kts = list(range(g0, min(g0 + 4, nk)))
                        sp_, Bsp = P.psg.get()
                        for a, kt in enumerate(kts):
                            K.op("pe", lambda e: e.matmul(sp_[:, a * 128:(a + 1) * 128], kT[:, h, kt * 128:(kt + 1) * 128], qT[:, h, qs], start=True, stop=True),
                                 reads=[BkT[kt], BqT[qt]], writes=[Bsp])
                        pe_, Bpe = P.pexp.get()
                        w = len(kts) * 128
                        K.op("act", lambda e: e.activation(pe_[:, 0:w], sp_[:, 0:w], AF.Exp, scale=scale), reads=[Bsp], writes=[Bpe])
                        if kts[-1] == qt:
                            a = len(kts) - 1
                            K.op("pool", lambda e: e.tensor_tensor(pe_[:, a * 128:(a + 1) * 128], pe_[:, a * 128:(a + 1) * 128], tri4[:, 0, :], ALU.mult),
                                 reads=[Bpe, Btri], writes=[Bpe])
                        for a, kt in enumerate(kts):
                            K.op("pe", lambda e: e.matmul(acc[:, 0:65], pe_[:, a * 128:(a + 1) * 128], va[:, kt, h, :], start=(kt == 0), stop=(kt == qt)),
                                 reads=[Bpe, Bva[kt]], writes=[Bacc])
                    K.op("dve", lambda e: e.reciprocal(yst_[:, h:h + 1], acc[:, 64:65]), reads=[Bacc], writes=[Byst])
                    K.op("dve", lambda e: e.tensor_scalar(y_o[:, h * 64:(h + 1) * 64], acc[:, 0:64], yst_[:, h:h + 1], None, ALU.mult),
                         reads=[Bacc, Byst], writes=[By_o])
                tm_groupnorm(P.psg, y_o, By_o, yst_, Byst, P.yb, 0, l, qt)
            IL.run(NT, 4, _mk_ma, _body_ma)
            dbg_dump(0)
            wout_partial(es, l, 0)
            phase_end(es)

        def tm_groupnorm(psgp, y_o, By_o, yst_, Byst, ybpool, m, l, qt):
            y_b, By_b = ybpool.get()
            K.op("act", lambda e: e.activation(y_b[:], y_o[:], AF.Square, accum_out=yst_[:, 4:5]), reads=[By_o], writes=[By_b, Byst])
            K.op("act", lambda e: e.activation(yst_[:, 5:6], yst_[:, 4:5], AF.Ln, bias=epsb[:, 0:1], scale=1.0 / 256), reads=[Byst, Beps], writes=[Byst])
            K.op("act", lambda e: e.activation(yst_[:, 5:6], yst_[:, 5:6], AF.Exp, scale=-0.5), reads=[Byst], writes=[Byst])
            K.op("dve", lambda e: e.scalar_tensor_tensor(y_b[:], y_o[:], yst_[:, 5:6], gon[:, m, :], ALU.mult, ALU.mult),
                 reads=[By_o, Byst, Bgon, By_b], writes=[By_b])
            pt, Bpt = psgp.get()
            ptb = pt[:, :].bitcast(BF16)
            for c in range(2):
                K.op("pe", lambda e: e.transpose(ptb[:, c * 128:(c + 1) * 128], y_b[:, c * 128:(c + 1) * 128], identb[:]), reads=[By_b, Bidentb], writes=[Bpt])
            K.op("act", lambda e: e.copy(ymT_box["t"][:, :, qt * 128:(qt + 1) * 128], ptb[:, 0:256].rearrange("p (c t) -> p c t", c=2)),
                 reads=[Bpt], writes=[ymT_box["b"][qt // 4]])

        def nsa_phase(s, l):
            es = ExitStack()
            ymT, BymT = alloc_ymT(es, 3)
            gv = sb(es, "ngv", [128, 4, 64], F32); Bgv = Buf("ngv")
            K.dma("sp", gv[:, 0, :], dr["nsa_g_q"][l:l + 1, :].to_broadcast([128, 64]), writes=[Bgv])
            for b3 in range(3):
                K.dma("sp", gv[:, 1 + b3, :], dr["nsa_g_k"][l, b3:b3 + 1, :].to_broadcast([128, 64]), writes=[Bgv])
            qT = sb(es, "nqT", [128, NT, 4, 128], BF16); BqT = [Buf(f"nqT{i}") for i in range(NT)]
            kTs = sb(es, "nkTs", [128, T], BF16); BkTs = [Buf(f"nkTs{i}") for i in range(NT)]
            kTw = sb(es, "nkTw", [128, T], BF16); BkTw = [Buf(f"nkTw{i}") for i in range(NT)]
            K.op("pool", lambda e: e.memset(qT[64:128], 0.0), writes=BqT)
            K.op("pool", lambda e: e.memset(kTs[64:128], 0.0), writes=BkTs)
            K.op("pool", lambda e: e.memset(kTw[64:128], 0.0), writes=BkTw)
            vs = sb(es, "nvs", [128, NT, 65], BF16); Bvs = [Buf(f"nvs{i}") for i in range(NT)]
            vw = sb(es, "nvw", [128, NT, 65], BF16); Bvw = [Buf(f"nvw{i}") for i in range(NT)]
            gts = sb(es, "ngts", [128, NT, 12], F32); Bgts = [Buf(f"ngts{i}") for i in range(NT)]
            kcT = sb(es, "nkcT", [128, 128], BF16); BkcT = Buf("nkcT")
            K.op("pool", lambda e: e.memset(kcT[64:128], 0.0), writes=[BkcT])
            vc = sb(es, "nvc", [128, 97], BF16); Bvc = Buf("nvc")
            K.op("pool", lambda e: e.memset(vs[:, :, 64:65], 1.0), writes=Bvs)
            K.op("pool", lambda e: e.memset(vw[:, :, 64:65], 1.0), writes=Bvw)
            K.op("pool", lambda e: e.memset(vc[:, 64:65], 1.0), writes=[Bvc])
            K.dma("pool", vc[:, 65:97], dr["c_ov"][:], writes=[Bvc])
            e1 = ExitStack()
            wn = sb(e1, "wn", [128, DC, 652], BF16); Bwn = Buf("wn")
            K.dma("pool", wn[:], dr["w_in"][l, :, 1120:1772].rearrange("(c p) n -> p c n", p=128), writes=[Bwn])
            cs_t, Bcs_t = rope_tables(e1, "nr", "c_inv_nsa", 8, posf[:, :], Bposf)
            pci = sb(e1, "npci", [128, 1], I32); Bpci = Buf("npci")
            pcf = sb(e1, "npcf", [128, 1], F32); Bpcf = Buf("npcf")
            K.op("dve", lambda e: e.memset(pcf[:], 0.0), writes=[Bpcf])
            pos_src = dr["pos"][s:s + 1, 31::16].rearrange("o n -> n o")
            K.dma("sp", pci[0:127, :], pos_src, writes=[Bpci], allow_slow_non_contiguous=True)
            K.op("dve", lambda e: e.tensor_copy(pcf[0:127, :], pci[0:127, :]), reads=[Bpci, Bpcf], writes=[Bpcf])
            csc, Bcsc = rope_tables(e1, "nrc", "c_inv_nsa", 8, pcf[:, :], Bpcf)
            cmpT = sb(e1, "ncmpT", [128, T], BF16); BcmpT = Buf("ncmpT")
            for ch in range(4):
                cs = slice(ch * 512, (ch + 1) * 512)
                p1, Bp1 = psg.get()
                for k in range(DC):
                    K.op("pe", lambda e: e.matmul(p1[:, :], wn[:, k, 256:384], xnT[:, k, cs], start=(k == 0), stop=(k == DC - 1)),
                         reads=[Bwn] + xnT_bufs(ch), writes=[Bp1])
                K.op("act", lambda e: e.copy(cmpT[:, cs], p1[:, :]), reads=[Bp1], writes=[BcmpT])
            def _mk_np(w):
                per = 8 // 4
                nb = per - 0
                return NS(psg=allps.sub(range(w * per, w * per + nb)), psa=allps.sub(range(w * per + nb, (w + 1) * per)), st=Pool(e1, nc, "nst", [128, 16], F32, 2), jk=Pool(e1, nc, "njk", [128, 64], F32, 1), qn=Pool(e1, nc, "nqn", [128, 6, 64], F32, 1), qr=Pool(e1, nc, "nqr", [128, 6, 64], BF16, 1), rtmp=Pool(e1, nc, "nrt", [128, 6, 16], F32, 1))
            def _body_np(i, P):
                ts_ = slice(i * 128, (i + 1) * 128)
                pa, Bpa = P.psg.get()
                pb, Bpb = P.psg.get()
                for c in range(DC):
                    K.op("pe", lambda e: e.matmul(pa[:, 0:256], xnT[:, c, ts_], wn[:, c, 0:256], start=(c == 0), stop=(c == DC - 1)),
                         reads=[Bwn, BxnT[i][0], BxnT[i][1]], writes=[Bpa])
                for c in range(DC):
                    K.op("pe", lambda e: e.matmul(pb[:, 0:268], xnT[:, c, ts_], wn[:, c, 384:652], start=(c == 0), stop=(c == DC - 1)),
                         reads=[Bwn, BxnT[i][0], BxnT[i][1]], writes=[Bpb])
                pa3 = pa[:, 0:256].rearrange("p (h d) -> p h d", h=4)
                sq, Bsq = P.st.get()
                j_, Bj_ = P.jk.get()
                for h in range(4):
                    K.op("act", lambda e: e.activation(j_[:], pa3[:, h, :], AF.Square, accum_out=sq[:, h:h + 1]), reads=[Bpa], writes=[Bj_, Bsq])
                K.op("act", lambda e: e.activation(j_[:], pb[:, 0:64], AF.Square, accum_out=sq[:, 4:5]), reads=[Bpb], writes=[Bj_, Bsq])
                K.op("act", lambda e: e.activation(j_[:], pb[:, 128:192], AF.Square, accum_out=sq[:, 5:6]), reads=[Bpb], writes=[Bj_, Bsq])
                rstd_from(sq[:, 8:14], sq[:, 0:6], 64, [Bsq], [Bsq])
                q_n, Bq_n = P.qn.get()
                for h in range(4):
                    K.op("dve", lambda e: e.scalar_tensor_tensor(q_n[:, h, :], pa3[:, h, :], sq[:, 8 + h:9 + h], gv[:, 0, :], ALU.mult, ALU.mult),
                         reads=[Bpa, Bsq, Bgv], writes=[Bq_n])
                K.op("dve", lambda e: e.scalar_tensor_tensor(q_n[:, 4, :], pb[:, 0:64], sq[:, 12:13], gv[:, 2, :], ALU.mult, ALU.mult),
                     reads=[Bpb, Bsq, Bgv], writes=[Bq_n])
                K.op("dve", lambda e: e.scalar_tensor_tensor(q_n[:, 5, :], pb[:, 128:192], sq[:, 13:14], gv[:, 3, :], ALU.mult, ALU.mult),
                     reads=[Bpb, Bsq, Bgv], writes=[Bq_n])
                q_r, Bq_r = P.qr.get()
                rt_, Brt_ = P.rtmp.get()
                apply_rope(q_r[:, :, :], q_n[:, :, :], cs_t[:, 0, i, :], cs_t[:, 1, i, :], 6, 8, rt_, [Bq_n, Bcs_t], [Bq_r], Brt_)
                K.op("act", lambda e: e.copy(q_r[:, :, 16:64], q_n[:, :, 16:64]), reads=[Bq_n], writes=[Bq_r])
                K.op("act", lambda e: e.copy(vs[:, i, 0:64], pb[:, 64:128]), reads=[Bpb], writes=[Bvs[i]])
                K.op("act", lambda e: e.copy(vw[:, i, 0:64], pb[:, 192:256]), reads=[Bpb], writes=[Bvw[i]])
                K.op("act", lambda e: e.copy(gts[:, i, :], pb[:, 256:268]), reads=[Bpb], writes=[Bgts[i]])
                pt, Bpt = P.psg.get()
                ptb = pt[:, :].bitcast(BF16)
                for h in range(6):
                    K.op("pe", lambda e: e.transpose(ptb[0:64, h * 128:(h + 1) * 128], q_r[:, h, :], identb[:]), reads=[Bq_r, Bidentb], writes=[Bpt])
                K.op("act", lambda e: e.copy(qT[0:64, i, :, :], ptb[0:64, 0:512].rearrange("p (h t) -> p h t", h=4)), reads=[Bpt], writes=[BqT[i]])
                K.op("dve", lambda e: e.tensor_copy(kTs[0:64, ts_], ptb[0:64, 512:640]), reads=[Bpt], writes=[BkTs[i]])
                K.op("dve", lambda e: e.tensor_copy(kTw[0:64, ts_], ptb[0:64, 640:768]), reads=[Bpt], writes=[BkTw[i]])
            IL.run(NT, 4, _mk_np, _body_np)
            st = Pool(e1, nc, "nst2", [128, 16], F32, 1)
            jk = Pool(e1, nc, "njk2", [128, 64], F32, 1)
            qn = Pool(e1, nc, "nqn2", [128, 6, 64], F32, 1)
            qr = Pool(e1, nc, "nqr2", [128, 6, 64], BF16, 1)
            rtmp = Pool(e1, nc, "nrt2", [128, 6, 16], F32, 1)
            w1 = sb(e1, "nw1", [128, 32, 128], BF16); Bw1 = Buf("nw1")
            pe16 = sb(e1, "npe16", [128, 32], BF16); Bpe16 = Buf("npe16")
            w2 = sb(e1, "nw2", [128, 2, 64], BF16); Bw2 = Buf("nw2")
            for kv in range(2):
                K.dma("pool", w1[kv * 64:kv * 64 + 64], dr["nsa_w1"][l, kv], writes=[Bw1])
                K.dma("pool", pe16[kv * 64:kv * 64 + 64, :], dr["nsa_pe"][l, kv], writes=[Bpe16])
                K.dma("pool", w2[:, kv, :], dr["nsa_w2"][l, kv], writes=[Bw2])
            hb = sb(e1, "nhb", [128, 2], F32); Bhb = Buf("nhb")
            hid = sb(e1, "nhid", [128, 2, 128], BF16); Bhid = Buf("nhid")
            for kv in range(2):
                rows = slice(kv * 64, kv * 64 + 64)
                ph, Bph = psg.get()
                pbias, Bpbias = psg.get()
                for j in range(32):
                    lw = w1[rows, j, :]
                    rhs = cmpT[rows, j:j + 16 * 126 + 1:16]
                    K.op("pe", lambda e: e.matmul(ph[:, 0:127], lw, rhs, start=(j == 0), stop=(j == 31)), reads=[Bw1, BcmpT], writes=[Bph])
                for j in range(32):
                    lw = w1[rows, j, :]
                    K.op("pe", lambda e: e.matmul(pbias[:, 0:1], lw, pe16[rows, j:j + 1], start=(j == 0), stop=(j == 31)), reads=[Bw1, Bpe16], writes=[Bpbias])
                K.op("act", lambda e: e.copy(hb[:, kv:kv + 1], pbias[:, 0:1]), reads=[Bpbias], writes=[Bhb])
                K.op("act", lambda e: e.activation(hid[:, kv, 0:127], ph[:, 0:127], AF.Gelu_apprx_tanh, bias=hb[:, kv:kv + 1]), reads=[Bph, Bhb], writes=[Bhid])
                po, Bpo = psg.get()
                K.op("pe", lambda e: e.matmul(po[0:127, 0:64], hid[:, kv, 0:127], w2[:, kv, :], start=True, stop=True), reads=[Bhid, Bw2], writes=[Bpo])
                if kv == 1:
                    K.op("act", lambda e: e.copy(vc[0:127, 0:64], po[0:127, 0:64]), reads=[Bpo], writes=[Bvc])
                else:
                    sq, Bsq = st.get()
                    j_, Bj_ = jk.get()
                    q_n, Bq_n = qn.get()
                    q_r, Bq_r = qr.get()
                    rt_, Brt_ = rtmp.get()
                    K.op("dve", lambda e: e.memset(q_n[:, 0, :], 0.0), writes=[Bq_n])
                    K.op("act", lambda e: e.activation(j_[0:127, :], po[0:127, 0:64], AF.Square, accum_out=sq[0:127, 0:1]), reads=[Bpo], writes=[Bj_, Bsq])
                    rstd_from(sq[0:127, 1:2], sq[0:127, 0:1], 64, [Bsq], [Bsq])
                    K.op("dve", lambda e: e.scalar_tensor_tensor(q_n[0:127, 0, :], po[0:127, 0:64], sq[0:127, 1:2], gv[0:127, 1, :], ALU.mult, ALU.mult),
                         reads=[Bpo, Bsq, Bgv, Bq_n], writes=[Bq_n])
                    apply_rope(q_r[:, 0:1, :], q_n[:, 0:1, :], csc[:, 0, 0, :], csc[:, 1, 0, :], 1, 8, rt_, [Bq_n, Bcsc], [Bq_r], Brt_)
                    K.op("act", lambda e: e.copy(q_r[:, 0:1, 16:64], q_n[:, 0:1, 16:64]), reads=[Bq_n], writes=[Bq_r])
                    pt, Bpt = psg.get()
                    ptb = pt[:, :].bitcast(BF16)
                    K.op("pe", lambda e: e.transpose(ptb[0:64, 0:128], q_r[:, 0, :], identb[:]), reads=[Bq_r, Bidentb], writes=[Bpt])
                    K.op("act", lambda e: e.copy(kcT[0:64, :], ptb[0:64, 0:128]), reads=[Bpt], writes=[BkcT])
            K.barrier_all()
            e1.close()
            cmask = sb(es, "ncmask", [128, T], BF16); Bcmask = Buf("ncmask")
            K.dma("pool", cmask[:], dr["c_cmpmask"][:], writes=[Bcmask])
            keep = sb(es, "nkeep", [128, NT, 32], F32); Bkeep = Buf("nkeep")
            base = sb(es, "nbase", [128, NT, 32], F32); Bbase = Buf("nbase")
            K.dma("sp", keep[:], dr["c_keep"][:], writes=[Bkeep])
            K.dma("sp", base[:], dr["c_base"][:], writes=[Bbase])
            Em = sb(es, "nEm", [128, NT, 128], BF16); BEm = Buf("nEm")
            K.op("pool", lambda e: e.memset(Em[:], 0.0), writes=[BEm])
            K.dma("pool", Em[0:32], dr["c_E"][:], writes=[BEm])
            scale = 64 ** -0.5
            def _mk_na(w):
                per = 8 // 4
                nb = per - 1
                return NS(psg=allps.sub(range(w * per, w * per + nb)), psa=allps.sub(range(w * per + nb, (w + 1) * per)), nsp=_zeroed(Pool(es, nc, "nselT", [128, 4, 128], BF16, 1)), pexp=Pool(es, nc, "npx", [128, 512], BF16, 3), sst=Pool(es, nc, "nsst", [128, 80], F32, 1), impp=Pool(es, nc, "nimp", [128, 32], F32, 1), yo=Pool(es, nc, "nyo", [128, 256], F32, 1), yst=Pool(es, nc, "nyst", [128, 8], F32, 1), yb=Pool(es, nc, "nyb", [128, 256], BF16, 1))
            def _body_na(qt, P):
                qs = slice(qt * 128, (qt + 1) * 128)
                qrhs = qT[:, qt].rearrange("p h t -> p (h t)")
                y_o, By_o = P.yo.get()
                yst_, Byst = P.yst.get()
                st_, Bst_ = P.sst.get()
                imp, Bimp = P.impp.get()
                K.op("act", lambda e: e.activation(gts[:, qt, :], gts[:, qt, :], AF.Exp, scale=-1.0), reads=[Bgts[qt]], writes=[Bgts[qt]])
                K.op("dve", lambda e: e.tensor_scalar(gts[:, qt, :], gts[:, qt, :], 1.0, None, ALU.add), reads=[Bgts[qt]], writes=[Bgts[qt]])
                K.op("dve", lambda e: e.reciprocal(gts[:, qt, :], gts[:, qt, :]), reads=[Bgts[qt]], writes=[Bgts[qt]])
                sp_, Bsp = P.psg.get()
                K.op("pe", lambda e: e.matmul(sp_[0:127, :], kcT[:, 0:127], qrhs, start=True, stop=True), reads=[BkcT, BqT[qt]], writes=[Bsp])
                pe_, Bpe = P.pexp.get()
                K.op("act", lambda e: e.activation(pe_[0:127, :], sp_[0:127, :], AF.Exp, scale=scale), reads=[Bsp], writes=[Bpe])
                K.op("pool", lambda e: e.tensor_tensor(pe_[0:127, :].rearrange("p (h t) -> p h t", h=4), pe_[0:127, :].rearrange("p (h t) -> p h t", h=4),
                                                       cmask[0:127, qs].unsqueeze(1).to_broadcast([127, 4, 128]), ALU.mult),
                     reads=[Bpe, Bcmask], writes=[Bpe])
                for h in range(4):
                    acc, Bacc = P.psa.get()
                    K.op("pe", lambda e: e.matmul(acc[:, 0:97], pe_[0:127, h * 128:(h + 1) * 128], vc[0:127, :], start=True, stop=True),
                         reads=[Bpe, Bvc], writes=[Bacc])
                    K.op("dve", lambda e: e.tensor_scalar(st_[:, h:h + 1], acc[:, 64:65], 1e-30, None, ALU.add), reads=[Bacc], writes=[Bst_])
                    K.op("dve", lambda e: e.reciprocal(st_[:, h:h + 1], st_[:, h:h + 1]), reads=[Bst_], writes=[Bst_])
                    if h == 0:
                        K.op("dve", lambda e: e.tensor_scalar(imp[:], acc[:, 65:97], st_[:, h:h + 1], None, ALU.mult), reads=[Bacc, Bst_], writes=[Bimp])
                    else:
                        K.op("dve", lambda e: e.scalar_tensor_tensor(imp[:], acc[:, 65:97], st_[:, h:h + 1], imp[:], ALU.mult, ALU.add),
                             reads=[Bacc, Bst_, Bimp], writes=[Bimp])
                    K.op("dve", lambda e: e.tensor_tensor(st_[:, 4 + h:5 + h], st_[:, h:h + 1], gts[:, qt, 3 * h:3 * h + 1], ALU.mult), reads=[Bst_, Bgts[qt]], writes=[Bst_])
                    K.op("dve", lambda e: e.tensor_scalar(y_o[:, h * 64:(h + 1) * 64], acc[:, 0:64], st_[:, 4 + h:5 + h], None, ALU.mult),
                         reads=[Bacc, Bst_], writes=[By_o])
                K.op("dve", lambda e: e.tensor_tensor(imp[:], imp[:], keep[:, qt, :], ALU.mult), reads=[Bimp, Bkeep], writes=[Bimp])
                K.op("dve", lambda e: e.tensor_tensor(imp[:], imp[:], base[:, qt, :], ALU.add), reads=[Bimp, Bbase], writes=[Bimp])
                K.op("dve", lambda e: e.max(st_[:, 8:16], imp[:]), reads=[Bimp], writes=[Bst_])
                K.op("dve", lambda e: e.tensor_scalar(st_[:, 16:48], imp[:], st_[:, 12:13], -1.0, ALU.is_ge, ALU.add), reads=[Bimp, Bst_], writes=[Bst_])
                pt, Bpt = P.psg.get()
                K.op("pe", lambda e: e.transpose(pt[0:32, 0:128], st_[:, 16:48], ident[:]), reads=[Bst_, Bident], writes=[Bpt])
                nselT, BnselT = P.nsp.get()
                K.op("act", lambda e: e.copy(nselT[0:32, :, :], pt[0:32, 0:128].unsqueeze(1).to_broadcast([32, 4, 128])), reads=[Bpt], writes=[BnselT])
                for br_ in range(2):
                    kTb = kTs if br_ == 0 else kTw
                    BkTb = BkTs if br_ == 0 else BkTw
                    vb = vs if br_ == 0 else vw
                    Bvb = Bvs if br_ == 0 else Bvw
                    kts = list(range(0, qt + 1)) if br_ == 0 else list(range(max(0, qt - 4), qt + 1))
                    acc, Bacc = P.psa.get()
                    K.op("dve", lambda e: e.memset(acc[:, 0:260], 0.0), writes=[Bacc])
                    for kt in kts:
                        sp_, Bsp = P.psg.get()
                        K.op("pe", lambda e: e.matmul(sp_[:, :], kTb[:, kt * 128:(kt + 1) * 128], qrhs, start=True, stop=(br_ == 1)),
                             reads=[BkTb[kt], BqT[qt]], writes=[Bsp])
                        if br_ == 0:
                            K.op("pe", lambda e: e.matmul(sp_[:, :], Em[:, kt, :], nselT[:, :, :].rearrange("p h t -> p (h t)"), start=False, stop=True),
                                 reads=[BEm, BnselT], writes=[Bsp])
                        pe_, Bpe = P.pexp.get()
                        K.op("act", lambda e: e.activation(pe_[:, :], sp_[:, :], AF.Exp, scale=scale), reads=[Bsp], writes=[Bpe])
                        if kt == qt:
                            K.op("pool", lambda e: e.tensor_tensor(pe_[:, :], pe_[:, :], tri4[:].rearrange("p h t -> p (h t)"), ALU.mult),
                                 reads=[Bpe, Btri], writes=[Bpe])
                        elif br_ == 1 and kt == qt - 4:
                            K.op("pool", lambda e: e.tensor_tensor(pe_[:, :], pe_[:, :], anti4[:].rearrange("p h t -> p (h t)"), ALU.mult),
                                 reads=[Bpe, Banti], writes=[Bpe])
                        for h in range(4):
                            co = h * 65
                            K.op("pe", lambda e: e.matmul(acc[:, co:co + 65], pe_[:, h * 128:(h + 1) * 128], vb[:, kt, :], start=False, stop=(kt == kts[-1]),
                                                          skip_group_check=True),
                                 reads=[Bpe, Bvb[kt]], writes=[Bacc])
                    for h in range(4):
                        co = h * 65
                        cc_ = 50 + 4 * br_ + h
                        K.op("dve", lambda e: e.reciprocal(st_[:, cc_:cc_ + 1], acc[:, co + 64:co + 65]), reads=[Bacc], writes=[Bst_])
                        K.op("dve", lambda e: e.tensor_tensor(st_[:, cc_:cc_ + 1], st_[:, cc_:cc_ + 1], gts[:, qt, 3 * h + 1 + br_:3 * h + 2 + br_], ALU.mult),
                             reads=[Bst_, Bgts[qt]], writes=[Bst_])
                        K.op("dve", lambda e: e.scalar_tensor_tensor(y_o[:, h * 64:(h + 1) * 64], acc[:, co:co + 64], st_[:, cc_:cc_ + 1], y_o[:, h * 64:(h + 1) * 64],
                                                                     ALU.mult, ALU.add),
                             reads=[Bacc, Bst_, By_o], writes=[By_o])
                tm_groupnorm(P.psg, y_o, By_o, yst_, Byst, P.yb, 3, l, qt)
            IL.run(NT, 4, _mk_na, _body_na)
            dbg_dump(3)
            wout_partial(es, l, 3)
            phase_end(es)

        combT = sb(top, "combT", [128, T], BF16)
        BcombT = [Buf(f"combT{c}") for c in range(4)]
        K.op("pool", lambda e: e.memset(combT[:], 0.0), writes=BcombT)
        gon = sb(top, "gon", [128, 4, 256], F32); Bgon = Buf("gon")
        gcolt = sb(top, "gcolt", [128, 8], F32); Bgcol = Buf("gcolt")

        io_box = {"final": False, "stored": set(), "loaded": set()}
        for s in range(n_seq):
            for i in range(NT):
                if (s, i) not in io_box["loaded"]:
                    K.dma("sp", x_sb[:, i, :], dr["x"][s, i * 128:(i + 1) * 128, :], writes=Bx[i])
            K.dma("sp", posi[:], dr["pos"][s].rearrange("(n p) -> p n", p=128), writes=[Bposi], allow_slow_non_contiguous=True)
            K.op("dve", lambda e: e.tensor_copy(posf[:], posi[:]), reads=[Bposi], writes=[Bposf])
            for l in range(depth):
                for m in range(4):
                    K.dma("sp", gon[:, m, :], dr["out_norm"][l, m:m + 1, :].to_broadcast([128, 256]), writes=[Bgon])
                K.dma("sp", gcolt[:], dr["out_norm_t"][l], writes=[Bgcol])
                l_box[0] = l
                io_box["final"] = (l == depth - 1) and ("moe" in phases)
                norm_phase(s, l, "mix_norm", False)
                if "mla" in phases:
                    mla_phase(s, l)
                if "lru" in phases:
                    lru_phase(s, l)
                if "s5" in phases:
                    s5_phase(s, l)
                if "nsa" in phases:
                    nsa_phase(s, l)
                if "moe" in phases:
                    moe_phase(s, l)
            if dbg:
                K.dma("pool", dbg_d["xnT"][:], xnT[:], reads=[b for t_ in BxnT for b in t_], writes=[Bout])
                for i in range(NT):
                    K.dma("sp", dbg_d["x"][i * 128:(i + 1) * 128, :], x_sb[:, i, :], reads=Bx[i], writes=[Bout])
            for i in range(NT):
                if (s, i) not in io_box["stored"]:
                    K.dma("sp", out_d[s, i * 128:(i + 1) * 128, :], x_sb[:, i, :], reads=Bx[i], writes=[Bout])
            K.barrier_all()
        K.barrier_all()
        K.close()
    return nc, K


def prep_weights(inp):
    f = np.float32
    L = DEPTH
    w = {}
    for k in ("mix_norm", "ffn_norm", "w_in", "w_out", "mla_g_cq", "mla_g_ckv", "mla_w_uq", "mla_w_ukv", "mla_g_q", "mla_g_k",
              "s5_w_glu", "nsa_g_q", "nsa_g_k", "out_norm", "moe_w_gate", "moe_w_up", "moe_w_down"):
        w[k] = np.ascontiguousarray(inp[k], dtype=f)
    def pc(v):
        return np.ascontiguousarray(np.asarray(v, f).reshape(L, 2, 128).transpose(0, 2, 1))
    w["lru_cw"] = np.ascontiguousarray(np.asarray(inp["lru_conv_w"], f).reshape(L, 4, 2, 128).transpose(0, 3, 2, 1))
    w["lru_vec"] = np.ascontiguousarray(np.stack([pc(inp["lru_conv_b"]), pc(np.asarray(inp["lru_b_a"]).reshape(L, 256)),
                                                  pc(np.asarray(inp["lru_b_i"]).reshape(L, 256)), pc(inp["lru_lambda"]),
                                                  np.zeros((L, 128, 2), f)], axis=2))
    for nm, src in (("lru_wa", "lru_w_a"), ("lru_wi", "lru_w_i")):
        a = np.zeros((L, 2, 128, 128), f)
        W = np.asarray(inp[src], f)
        for c in range(2):
            for hh in range(2):
                a[:, c, hh * 64:(hh + 1) * 64, hh * 64:(hh + 1) * 64] = W[:, 2 * c + hh]
        w[nm] = a
    def st(v):
        return np.asarray(v, f).reshape(L, 8, 128).transpose(0, 2, 1)
    ldt = np.repeat(np.asarray(inp["s5_log_dt"], f)[:, :, None], 64, axis=2)
    w["s5_par"] = np.ascontiguousarray(np.stack([st(inp["s5_a_re"]), st(inp["s5_a_im"]), st(ldt)], axis=2))
    def stb(v):
        return np.asarray(v, f).reshape(L, 8, 128, 16).transpose(0, 2, 1, 3)
    w["s5_b"] = np.ascontiguousarray(np.stack([stb(inp["s5_b_re"]), stb(inp["s5_b_im"])], axis=2))
    cpad = np.zeros((L, 2, 8, 128, 128), f)
    for ri, nm in enumerate(("s5_c_re", "s5_c_im")):
        Cm = np.asarray(inp[nm], f)
        for g in range(16):
            j = g // 2
            rows = slice((g % 2) * 64, (g % 2) * 64 + 64)
            cols = slice((16 * g) % 128, (16 * g) % 128 + 16)
            cpad[:, ri, j, rows, cols] = Cm[:, g].transpose(0, 2, 1)
    w["s5_c"] = cpad
    w["s5_vec"] = np.ascontiguousarray(np.stack([pc(inp["s5_d"]), pc(inp["s5_b_glu"]), np.zeros((L, 128, 2), f)], axis=2))
    w["nsa_pe"] = np.ascontiguousarray(np.stack([np.asarray(inp["nsa_pe_k"], f).transpose(0, 2, 1),
                                                 np.asarray(inp["nsa_pe_v"], f).transpose(0, 2, 1)], axis=1))
    w["nsa_w1"] = np.ascontiguousarray(np.stack([np.asarray(inp["nsa_w1_k"], f).reshape(L, 32, 64, 128).transpose(0, 2, 1, 3),
                                                 np.asarray(inp["nsa_w1_v"], f).reshape(L, 32, 64, 128).transpose(0, 2, 1, 3)], axis=1))
    w["nsa_w2"] = np.ascontiguousarray(np.stack([np.asarray(inp["nsa_w2_k"], f), np.asarray(inp["nsa_w2_v"], f)], axis=1))
    w["out_norm_t"] = np.ascontiguousarray(np.asarray(inp["out_norm"], f).reshape(L, 8, 128).transpose(0, 2, 1))
    w["moe_wr"] = np.ascontiguousarray(np.concatenate([np.asarray(inp["moe_w_rg"], f), np.asarray(inp["moe_w_re"], f)], axis=2))
    w["moe_br"] = np.ascontiguousarray(np.concatenate([np.asarray(inp["moe_b_rg"], f), np.asarray(inp["moe_b_re"], f)], axis=1))
    w["c_ident"] = np.eye(128, dtype=f)
    kk = np.arange(128)[:, None]; qq = np.arange(128)[None, :]
    w["c_tri"] = (qq >= kk).astype(f)
    w["c_anti"] = (kk > qq).astype(f)
    cc = np.arange(128)[:, None]; tq = np.arange(T)[None, :]
    w["c_cmpmask"] = ((16 * cc + 31 <= tq) & (cc < 127)).astype(f)
    csn = np.arange(127) * 16; ssn = np.arange(32) * 64
    ov = np.clip(np.minimum(csn[:, None] + 32, ssn[None, :] + 64) - np.maximum(csn[:, None], ssn[None, :]), 0, None) / 16.0
    ovp = np.zeros((128, 32), f); ovp[:127] = ov
    w["c_ov"] = ovp
    tpos = np.arange(T); cur = tpos // 64; sbk = np.arange(32)
    forced = (sbk[None, :] == 0) | (sbk[None, :] == cur[:, None]) | (sbk[None, :] == cur[:, None] - 1)
    future = sbk[None, :] > cur[:, None]
    keep = (~forced & ~future).astype(f)
    base = np.where(future, -1e30, np.where(forced, 1e30, 0.0)).astype(f)
    w["c_keep"] = np.ascontiguousarray(keep.reshape(NT, 128, 32).transpose(1, 0, 2))
    w["c_base"] = np.ascontiguousarray(base.reshape(NT, 128, 32).transpose(1, 0, 2))
    E = np.zeros((32, NT, 128), f)
    for kt in range(NT):
        for m_ in range(128):
            E[2 * kt + m_ // 64, kt, m_] = BIGNEG
    w["c_E"] = E
    w["c_inv_mla"] = np.tile((500000.0 ** (-np.arange(16, dtype=np.float64) * 2.0 / 32)).astype(f)[None, :], (128, 1))
    w["c_inv_nsa"] = np.tile((500000.0 ** (-np.arange(8, dtype=np.float64) * 2.0 / 16)).astype(f)[None, :], (128, 1))
    sE = np.zeros((32, 16, 128), f)
    for e_ in range(16):
        sE[e_, e_, :] = 1.0
        sE[16 + e_, e_, :] = 1.0
    w["c_selE"] = sE
    for k, shp in W_SPECS.items():
        assert list(w[k].shape) == shp, (k, w[k].shape, shp)
    return w


_CACHE = {}


def kernel(**inputs):
    n_cores = 8
    x = np.ascontiguousarray(inputs["x"], dtype=np.float32)
    pos = np.ascontiguousarray(inputs["positions"], dtype=np.int32)
    w = prep_weights(inputs)
    if "nc" not in _CACHE:
        _CACHE["nc"] = build_program(n_seq=2)[0]
    nc = _CACHE["nc"]
    in_maps = []
    for c in range(n_cores):
        m = dict(w)
        m["x"] = x[2 * c:2 * c + 2]
        m["pos"] = pos[2 * c:2 * c + 2]
        in_maps.append(m)
    res = run_bass_kernel_spmd(nc, in_maps, core_ids=list(range(n_cores)))
    return np.concatenate([r["out"] for r in res.results], axis=0)
```

```python
import numpy as np
from contextlib import ExitStack
import concourse.bass as bass
import concourse.mybir as mybir
from concourse.bass_utils import run_bass_kernel_spmd

F32 = mybir.dt.float32
BF16 = mybir.dt.bfloat16
I32 = mybir.dt.int32
AF = mybir.ActivationFunctionType
ALU = mybir.AluOpType
AX = mybir.AxisListType

T = 2048
NT = 16
D = 1024
DC = 8
DEPTH = 2
EPS = 1e-6
TWO_PI = 6.283185307179586
BIGNEG = 30000.0
EPOCH = 30000


class Buf:
    __slots__ = ("name", "last_w", "readers", "excl")

    def __init__(self, name, excl=False):
        self.name = name
        self.last_w = None
        self.readers = []
        self.excl = excl


class Prod:
    def __init__(self, K, key, step):
        self.K = K
        self.key = key
        self.step = step
        self.count = 0
        self.sems = []

    def sem_for(self, idx):
        ep = idx // EPOCH
        while len(self.sems) <= ep:
            self.sems.append(self.K.new_sem(f"{self.key}_{len(self.sems)}"))
        return self.sems[ep], ((idx % EPOCH) + 1) * self.step, ep


class _PEProxy:
    def __init__(self, real):
        self.real = real
        self.last_stop = True

    def matmul(self, *a, **k):
        self.last_stop = bool(k.get("stop", True))
        return self.real.matmul(*a, **k)

    def transpose(self, *a, **k):
        self.last_stop = True
        return self.real.transpose(*a, **k)


class Kern:
    def __init__(self, nc, n_dma_lanes=16):
        self.nc = nc
        self._sem_ctx = []
        self.prods = {}
        self.engs = {"pe": nc.tensor, "act": nc.scalar, "dve": nc.vector, "pool": nc.gpsimd, "sp": nc.sync}
        for k in self.engs:
            self.prods[k] = Prod(self, k, 1)
        self.lanes = {}
        self.lane_rr = {}
        for q in ("sp", "pool", "act"):
            self.lanes[q] = []
            self.lane_rr[q] = 0
            for i in range(n_dma_lanes // 2):
                p = Prod(self, f"dma_{q}{i}", 16)
                self.prods[p.key] = p
                self.lanes[q].append(p)
        self._pe_proxy = _PEProxy(nc.tensor)
        self._switch = None
        self.waited = {}
        self.n_inst = 0
        self.n_wait = 0

    def new_sem(self, name):
        ctx = self.nc.semaphore(name)
        s = ctx.__enter__()
        self._sem_ctx.append(ctx)
        return s

    def close(self):
        for c in reversed(self._sem_ctx):
            c.__exit__(None, None, None)
        self._sem_ctx = []

    def _deps(self, me_key, reads, writes):
        deps = set()
        for b in reads:
            if b.last_w is not None:
                deps.add(b.last_w)
            if b.excl:
                for r in b.readers:
                    if r[0] != me_key:
                        deps.add(r)
        for b in writes:
            if b.last_w is not None:
                deps.add(b.last_w)
            deps.update(b.readers)
        return deps

    def _emit_waits(self, engname, deps, self_key=None, attach=False):
        eng = self.engs[engname]
        need = {}
        for (pk, idx) in deps:
            if pk == self_key and pk == "pe":
                continue
            sem, val, ep = self.prods[pk].sem_for(idx)
            k = (pk, ep)
            if need.get(k, (None, 0))[1] < val:
                need[k] = (sem, val)
        pend = []
        for (pk, ep), (sem, val) in need.items():
            wk = (engname, pk, ep)
            if self.waited.get(wk, 0) >= val:
                continue
            pend.append((sem, val))
            self.waited[wk] = val
        last = pend.pop() if (attach and pend) else None
        for (sem, val) in pend:
            eng.wait_ge(sem, val)
            self.n_wait += 1
        return last

    def _record(self, me, reads, writes):
        for b in reads:
            b.readers.append(me)
            if len(b.readers) > 48:
                b.readers = b.readers[-48:]
        for b in writes:
            b.last_w = me
            b.readers = []

    def op(self, engname, fn, reads=(), writes=()):
        prod = self.prods[engname]
        deps = self._deps(engname, reads, writes)
        last = self._emit_waits(engname, deps, self_key=engname, attach=True)
        if engname == "pe":
            self._pe_proxy.last_stop = True
            ins = fn(self._pe_proxy)
            inc = self._pe_proxy.last_stop
        else:
            ins = fn(self.engs[engname])
            inc = True
        if last is not None:
            ins._wait_ge(last[0], last[1])
        idx = prod.count
        self.n_inst += 1
        if inc:
            sem, val, ep = prod.sem_for(idx)
            ins.then_inc(sem, 1)
            prod.count += 1
        self._record((engname, idx), reads, writes)
        if self._switch is not None:
            self._switch()
        return ins

    def dma(self, qname, out, in_, reads=(), writes=(), **kw):
        lane = self.lanes[qname][self.lane_rr[qname]]
        self.lane_rr[qname] = (self.lane_rr[qname] + 1) % len(self.lanes[qname])
        deps = self._deps(lane.key, reads, writes)
        if lane.count > 0:
            deps.add((lane.key, lane.count - 1))
        last = self._emit_waits(qname, deps, attach=True)
        idx = lane.count
        sem, val, ep = lane.sem_for(idx)
        ins = self.engs[qname].dma_start(out=out, in_=in_, **kw)
        if last is not None:
            ins._wait_ge(last[0], last[1])
        ins.then_inc(sem, 16)
        lane.count += 1
        self.n_inst += 1
        self._record((lane.key, idx), reads, writes)
        if self._switch is not None:
            self._switch()
        return ins

    def barrier_all(self):
        deps = set()
        for pk, p in self.prods.items():
            if p.count > 0:
                deps.add((pk, p.count - 1))
        for e in self.engs:
            self._emit_waits(e, deps)


class NS:
    def __init__(self, **kw):
        self.__dict__.update(kw)


class Interleaver:
    def __init__(self, K):
        self.K = K

    def run(self, n, W, mk, body):
        import threading
        K = self.K
        W = max(1, min(W, n))
        ctxs = [mk(w) for w in range(W)]
        if W == 1:
            for i in range(n):
                body(i, ctxs[0])
            return
        sems = [threading.Semaphore(0) for _ in range(W)]
        alive = [True] * W
        done = threading.Event()
        err = []
        state = {"cur": 0}

        def next_live(w):
            for d in range(1, W + 1):
                v = (w + d) % W
                if alive[v]:
                    return v
            return None

        def switch():
            w = state["cur"]
            v = next_live(w)
            if v is None or v == w:
                return
            state["cur"] = v
            sems[v].release()
            sems[w].acquire()

        def worker(w):
            sems[w].acquire()
            try:
                if not err:
                    for i in range(w, n, W):
                        body(i, ctxs[w])
                        if err:
                            break
            except BaseException as e:
                err.append(e)
            alive[w] = False
            v = next_live(w)
            if v is None:
                done.set()
            else:
                state["cur"] = v
                sems[v].release()

        ths = [threading.Thread(target=worker, args=(w,)) for w in range(W)]
        for t in ths:
            t.start()
        K._switch = switch
        state["cur"] = 0
        sems[0].release()
        done.wait()
        K._switch = None
        for t in ths:
            t.join()
        if err:
            raise err[0]


class Pool:
    _uid = [0]

    def __init__(self, es, nc, name, shape, dtype, n, psum=False):
        self.items = []
        Pool._uid[0] += 1
        name = f"{name}_u{Pool._uid[0]}_"
        for i in range(n):
            if psum:
                t = es.enter_context(nc.psum_tensor(f"{name}{i}", shape, dtype))
            else:
                t = es.enter_context(nc.sbuf_tensor(f"{name}{i}", shape, dtype))
            self.items.append((t, Buf(f"{name}{i}", excl=psum)))
        self.i = 0

    def get(self):
        it = self.items[self.i]
        self.i = (self.i + 1) % len(self.items)
        return it

    def sub(self, idxs):
        p = Pool.__new__(Pool)
        p.items = [self.items[k] for k in idxs]
        p.i = 0
        return p


W_SPECS = {
    "mix_norm": [DEPTH, D], "ffn_norm": [DEPTH, D], "w_in": [DEPTH, D, 1772], "w_out": [DEPTH, D, D],
    "mla_g_cq": [DEPTH, 192], "mla_g_ckv": [DEPTH, 128], "mla_w_uq": [DEPTH, 192, 384],
    "mla_w_ukv": [DEPTH, 128, 512], "mla_g_q": [DEPTH, 96], "mla_g_k": [DEPTH, 96],
    "lru_cw": [DEPTH, 128, 2, 4], "lru_vec": [DEPTH, 128, 5, 2], "lru_wa": [DEPTH, 2, 128, 128],
    "lru_wi": [DEPTH, 2, 128, 128],
    "s5_par": [DEPTH, 128, 3, 8], "s5_b": [DEPTH, 128, 2, 8, 16], "s5_c": [DEPTH, 2, 8, 128, 128],
    "s5_vec": [DEPTH, 128, 3, 2], "s5_w_glu": [DEPTH, 256, 256],
    "nsa_g_q": [DEPTH, 64], "nsa_g_k": [DEPTH, 3, 64], "nsa_pe": [DEPTH, 2, 64, 32],
    "nsa_w1": [DEPTH, 2, 64, 32, 128], "nsa_w2": [DEPTH, 2, 128, 64],
    "out_norm": [DEPTH, 4, 256], "out_norm_t": [DEPTH, 128, 8],
    "moe_wr": [DEPTH, D, 20], "moe_br": [DEPTH, 20],
    "moe_w_gate": [DEPTH, 16, D, 256], "moe_w_up": [DEPTH, 16, D, 256], "moe_w_down": [DEPTH, 16, 256, D],
    "c_ident": [128, 128], "c_tri": [128, 128], "c_anti": [128, 128], "c_cmpmask": [128, T],
    "c_ov": [128, 32], "c_keep": [128, NT, 32], "c_base": [128, NT, 32], "c_E": [32, NT, 128],
    "c_inv_mla": [128, 16], "c_inv_nsa": [128, 8], "c_selE": [32, 16, 128],
}


def build_program(n_seq=2, depth=DEPTH, dbg=None, phases=("mla", "lru", "s5", "nsa", "moe")):
    nc = bass.Bass("TRN2", target_bir_lowering=False)
    dr = {}
    dr["x"] = nc.dram_tensor("x", [n_seq, T, D], F32, kind="ExternalInput").ap()
    dr["pos"] = nc.dram_tensor("pos", [n_seq, T], I32, kind="ExternalInput").ap()
    for k, shp in W_SPECS.items():
        dr[k] = nc.dram_tensor(k, shp, F32, kind="ExternalInput").ap()
    out_d = nc.dram_tensor("out", [n_seq, T, D], F32, kind="ExternalOutput").ap()
    dbg_d = {}
    if dbg:
        dbg_d["ymT"] = nc.dram_tensor("dbg_ymT", [4, 128, 2, T], F32, kind="ExternalOutput").ap()
        dbg_d["x"] = nc.dram_tensor("dbg_x", [T, D], F32, kind="ExternalOutput").ap()
        dbg_d["xnT"] = nc.dram_tensor("dbg_xnT", [128, DC, T], F32, kind="ExternalOutput").ap()

    K = Kern(nc)
    IL = Interleaver(K)
    Bout = Buf("out")
    with ExitStack() as top:
        def sb(es, name, shape, dt):
            Pool._uid[0] += 1
            return es.enter_context(nc.sbuf_tensor(f"{name}_u{Pool._uid[0]}", shape, dt))

        x_sb = sb(top, "x_sb", [128, NT, D], F32)
        Bx = [[Buf(f"x{i}_{h}") for h in range(2)] for i in range(NT)]
        xnT = sb(top, "xnT", [128, DC, T], BF16)
        BxnT = [[Buf(f"xnT{i}_{h}") for h in range(2)] for i in range(NT)]
        ymT_box = {}
        l_box = [0]

        def xnT_bufs(ch):
            return [BxnT[i][h] for i in range(ch * 4, ch * 4 + 4) for h in range(2)]

        ident = sb(top, "ident", [128, 128], F32); Bident = Buf("ident")
        identb = sb(top, "identb", [128, 128], BF16); Bidentb = Buf("identb")
        ones16 = sb(top, "ones16", [128, 128], BF16); Bones = Buf("ones16")
        tri4 = sb(top, "tri4", [128, 4, 128], BF16); Btri = Buf("tri4")
        anti4 = sb(top, "anti4", [128, 4, 128], BF16); Banti = Buf("anti4")
        posf = sb(top, "posf", [128, NT], F32); Bposf = Buf("posf")
        posi = sb(top, "posi", [128, NT], I32); Bposi = Buf("posi")
        gain = sb(top, "gain", [128, D], F32); Bgain = Buf("gain")
        ss = sb(top, "ss", [128, NT], F32); Bss = Buf("ss")
        rs = sb(top, "rs", [128, NT], F32); Brs = Buf("rs")
        epsb = sb(top, "epsb", [128, 1], F32); Beps = Buf("epsb")

        psg = Pool(top, nc, "psg", [128, 512], F32, 6, psum=True)
        psa = Pool(top, nc, "psa", [128, 512], F32, 2, psum=True)
        allps = psg.sub(range(6))
        allps.items = psg.items + psa.items

        K.dma("sp", ident[:], dr["c_ident"][:], writes=[Bident])
        K.op("act", lambda e: e.copy(identb[:], ident[:]), reads=[Bident], writes=[Bidentb])
        K.op("dve", lambda e: e.memset(ones16[:], 1.0), writes=[Bones])
        K.op("dve", lambda e: e.memset(epsb[:], EPS), writes=[Beps])
        for h in range(4):
            K.dma("pool", tri4[:, h, :], dr["c_tri"][:], writes=[Btri])
            K.dma("pool", anti4[:, h, :], dr["c_anti"][:], writes=[Banti])

        def _zeroed(pool):
            for (t_, b_) in pool.items:
                K.op("pool", lambda e: e.memset(t_[:], 0.0), writes=[b_])
            return pool

        def phase_end(es):
            K.barrier_all()
            es.close()

        def rstd_from(out_ap, in_ap, n_feat, reads, writes, eng_tmp=None):
            K.op("act", lambda e: e.activation(out_ap, in_ap, AF.Sqrt, bias=epsb[0:out_ap.shape[0], 0:1], scale=1.0 / n_feat),
                 reads=list(reads) + [Beps], writes=writes)
            K.op("dve", lambda e: e.reciprocal(out_ap, out_ap), reads=writes, writes=writes)

        def norm_phase(s, l, which, router):
            es = ExitStack()
            tmpA = Pool(es, nc, "nrmA", [128, D], BF16, 2)
            K.dma("sp", gain[:], dr[which][l:l + 1, :].to_broadcast([128, D]), writes=[Bgain])
            if router:
                wr = sb(es, "wr", [128, DC, 20], F32); Bwr = Buf("wr")
                br = sb(es, "br", [128, 20], F32); Bbr = Buf("br")
                K.dma("sp", wr[:], dr["moe_wr"][l].rearrange("(c p) n -> p c n", p=128), writes=[Bwr])
                K.dma("sp", br[:], dr["moe_br"][l:l + 1, :].to_broadcast([128, 20]), writes=[Bbr])
            for i in range(NT):
                junk, Bj = tmpA.get()
                K.op("act", lambda e: e.activation(junk[:], x_sb[:, i, :], AF.Square, accum_out=ss[:, i:i + 1]),
                     reads=Bx[i], writes=[Bj, Bss])
            rstd_from(rs[:, :], ss[:, :], D, [Bss], [Brs])
            def _mk_nr(w):
                per = 8 // 2
                d_ = dict(psg=allps.sub(range(w * per, w * per + per)), tmpA=Pool(es, nc, "nrmX", [128, D], F32, 1))
                if router:
                    d_["xT32"] = Pool(es, nc, "xT32", [128, DC, 128], F32, 1)
                    d_["rt"] = Pool(es, nc, "rt", [128, 96], F32, 2)
                else:
                    d_["xb"] = Pool(es, nc, "nrmB", [128, D], BF16, 1)
                return NS(**d_)

            def _body_nr(i, P):
                if not router:
                    xb, Bxb = P.xb.get()
                    K.op("dve", lambda e: e.scalar_tensor_tensor(xb[:], x_sb[:, i, :], rs[:, i:i + 1], gain[:], ALU.mult, ALU.mult),
                         reads=Bx[i] + [Brs, Bgain], writes=[Bxb])
                    pb, Bpb = P.psg.get()
                    pbb = pb[:, :].bitcast(BF16)
                    for c in range(DC):
                        K.op("pe", lambda e: e.transpose(pbb[:, c * 128:(c + 1) * 128], xb[:, c * 128:(c + 1) * 128], identb[:]),
                             reads=[Bxb, Bidentb], writes=[Bpb])
                    K.op("act", lambda e: e.copy(xnT[:, :, i * 128:(i + 1) * 128], pbb[:, :].rearrange("p (c t) -> p c t", c=DC)),
                         reads=[Bpb], writes=[BxnT[i][0], BxnT[i][1]])
                    return
                xn, Bxn = P.tmpA.get()
                K.op("dve", lambda e: e.scalar_tensor_tensor(xn[:], x_sb[:, i, :], rs[:, i:i + 1], gain[:], ALU.mult, ALU.mult),
                     reads=Bx[i] + [Brs, Bgain], writes=[Bxn])
                if router:
                    xt, Bxt = P.xT32.get()
                for half in range(2):
                    pb, Bpb = P.psg.get()
                    for cc in range(4):
                        c = half * 4 + cc
                        K.op("pe", lambda e: e.transpose(pb[:, cc * 128:(cc + 1) * 128], xn[:, c * 128:(c + 1) * 128], ident[:]),
                             reads=[Bxn, Bident], writes=[Bpb])
                    src = pb[:, :].rearrange("p (c t) -> p c t", c=4)
                    K.op("act", lambda e: e.copy(xnT[:, half * 4:half * 4 + 4, i * 128:(i + 1) * 128], src),
                         reads=[Bpb], writes=[BxnT[i][half]])
                    if router:
                        K.op("dve", lambda e: e.tensor_copy(xt[:, half * 4:half * 4 + 4, :], src), reads=[Bpb], writes=[Bxt])
                if router:
                    lg, Blg = P.psg.get()
                    for c in range(DC):
                        K.op("pe", lambda e: e.matmul(lg[:, 0:20], xt[:, c, :], wr[:, c, :], start=(c == 0), stop=(c == DC - 1)),
                             reads=[Bxt, Bwr], writes=[Blg])
                    r, Br_ = P.rt.get()
                    R = [Br_]
                    Lg = r[:, 0:20]; m = r[:, 20:21]; nm = r[:, 21:22]; e4 = r[:, 22:26]; se = r[:, 26:27]
                    oh = r[:, 27:31]; pen = r[:, 31:35]; lem = r[:, 35:51]; top8 = r[:, 51:59]; sel = r[:, 59:75]
                    nv1 = r[:, 75:76]; den = r[:, 76:77]; fac = r[:, 77:78]
                    r2, Br2 = P.rt.get()
                    ew = r2[:, 0:16]; sw = r2[:, 16:32]; comb = r2[:, 32:48]
                    R2 = [Br2]
                    K.op("dve", lambda e: e.tensor_tensor(Lg, lg[:, 0:20], br[:], ALU.add), reads=[Blg, Bbr], writes=R)
                    K.op("dve", lambda e: e.tensor_reduce(m, r[:, 0:4], AX.X, ALU.max), reads=R, writes=R)
                    K.op("dve", lambda e: e.tensor_scalar(nm, m, -1.0, None, ALU.mult), reads=R, writes=R)
                    K.op("act", lambda e: e.activation(e4, r[:, 0:4], AF.Exp, bias=nm, accum_out=se), reads=R, writes=R)
                    K.op("dve", lambda e: e.tensor_scalar(oh, r[:, 0:4], m, None, ALU.is_ge), reads=R, writes=R)
                    K.op("dve", lambda e: e.tensor_scalar(pen, oh, 1.0, 1e30, ALU.subtract, ALU.mult), reads=R, writes=R)
                    K.op("dve", lambda e: e.tensor_tensor(lem.rearrange("p (g i) -> p g i", g=4),
                                                          r[:, 4:20].rearrange("p (g i) -> p g i", g=4),
                                                          pen.unsqueeze(2).to_broadcast([128, 4, 4]), ALU.add), reads=R, writes=R)
                    K.op("dve", lambda e: e.max(top8, lem), reads=R, writes=R)
                    K.op("dve", lambda e: e.tensor_scalar(sel, lem, r[:, 52:53], None, ALU.is_ge), reads=R, writes=R)
                    K.op("dve", lambda e: e.tensor_scalar(nv1, r[:, 51:52], -1.0, None, ALU.mult), reads=R, writes=R)
                    K.op("act", lambda e: e.activation(ew, lem, AF.Exp, bias=nv1), reads=R, writes=R2)
                    K.op("dve", lambda e: e.scalar_tensor_tensor(sw, sel, 1.0, ew, ALU.mult, ALU.mult, accum_out=den), reads=R + R2, writes=R + R2)
                    K.op("dve", lambda e: e.tensor_tensor(fac, den, se, ALU.mult), reads=R, writes=R)
                    K.op("dve", lambda e: e.reciprocal(fac, fac), reads=R, writes=R)
                    K.op("dve", lambda e: e.tensor_scalar(comb, sw, fac, None, ALU.mult), reads=R + R2, writes=R2)
                    chl = r2[:, 48:64].bitcast(BF16)
                    K.op("dve", lambda e: e.tensor_copy(chl[:, 0:16], comb), reads=R2, writes=R2)
                    K.op("dve", lambda e: e.tensor_copy(r2[:, 64:80], chl[:, 0:16]), reads=R2, writes=R2)
                    K.op("dve", lambda e: e.tensor_tensor(chl[:, 16:32], comb, r2[:, 64:80], ALU.subtract), reads=R2, writes=R2)
                    pt, Bpt = P.psg.get()
                    ptb = pt[:, :].bitcast(BF16)
                    K.op("pe", lambda e: e.transpose(ptb[0:32, 0:128], chl, identb[:]), reads=R2 + [Bidentb], writes=[Bpt])
                    K.op("act", lambda e: e.copy(combT[0:32, i * 128:(i + 1) * 128], ptb[0:32, 0:128]), reads=[Bpt], writes=[BcombT[i // 4]])

            IL.run(NT, 2, _mk_nr, _body_nr)
            phase_end(es)

        def moe_phase(s, l):
            es = ExitStack()
            selE = sb(es, "selE", [128, 16, 128], BF16); BselE = Buf("selE")
            K.op("pool", lambda e: e.memset(selE[:], 0.0), writes=[BselE])
            K.dma("pool", selE[0:32], dr["c_selE"][:], writes=[BselE])
            wgu = Pool(es, nc, "wgu", [128, DC, 512], BF16, 2)
            wdp = Pool(es, nc, "wdp", [128, 2, D], BF16, 2)
            cbp = Pool(es, nc, "cbp", [128, 512], F32, 2)
            sgp = Pool(es, nc, "sgp", [128, 512], F32, 3)
            hep = Pool(es, nc, "hep", [128, 2, 512], BF16, 2)
            def load_expert(ex):
                wg, Bwg = wgu.get()
                wd, Bwd = wdp.get()
                K.dma("pool", wg[:, :, 0:256], dr["moe_w_gate"][l, ex].rearrange("(c p) f -> p c f", p=128), writes=[Bwg])
                K.dma("pool", wg[:, :, 256:512], dr["moe_w_up"][l, ex].rearrange("(c p) f -> p c f", p=128), writes=[Bwg])
                K.dma("pool", wd[:], dr["moe_w_down"][l, ex].rearrange("(c p) f -> p c f", p=128), writes=[Bwd])
                return wg, Bwg, wd, Bwd
            W = {}
            W[0] = load_expert(0)
            norm_phase(s, l, "ffn_norm", True)
            steps = [(ex, ch) for ex in range(16) for ch in range(4)]
            hes = {}

            def stage_a(ex, ch):
                wg, Bwg, wd, Bwd = W[ex]
                cs = slice(ch * 512, (ch + 1) * 512)
                cbps, Bcbps = psg.get()
                K.op("pe", lambda e: e.matmul(cbps[:, :], selE[:, ex, :], combT[:, cs], start=True, stop=True),
                     reads=[BselE, BcombT[ch]], writes=[Bcbps])
                cb, Bcb = cbp.get()
                K.op("act", lambda e: e.copy(cb[:], cbps[:, :]), reads=[Bcbps], writes=[Bcb])
                he, Bhe = hep.get()
                for fc in range(2):
                    gps, Bgps = psg.get()
                    ups, Bups = psg.get()
                    for c in range(DC):
                        K.op("pe", lambda e: e.matmul(gps[:, :], wg[:, c, fc * 128:(fc + 1) * 128], xnT[:, c, cs],
                                                      start=(c == 0), stop=(c == DC - 1)),
                             reads=[Bwg] + xnT_bufs(ch), writes=[Bgps])
                    for c in range(DC):
                        K.op("pe", lambda e: e.matmul(ups[:, :], wg[:, c, 256 + fc * 128:256 + (fc + 1) * 128], xnT[:, c, cs],
                                                      start=(c == 0), stop=(c == DC - 1)),
                             reads=[Bwg] + xnT_bufs(ch), writes=[Bups])
                    sg, Bsg = sgp.get()
                    K.op("act", lambda e: e.activation(sg[:], gps[:, :], AF.Silu), reads=[Bgps], writes=[Bsg])
                    K.op("pool", lambda e: e.tensor_tensor(sg[:], sg[:], cb[:], ALU.mult), reads=[Bsg, Bcb], writes=[Bsg])
                    K.op("dve", lambda e: e.tensor_tensor(he[:, fc, :], sg[:], ups[:, :], ALU.mult), reads=[Bsg, Bups], writes=[Bhe])
                hes[(ex, ch)] = (he, Bhe)

            def stage_b(ex, ch):
                wg, Bwg, wd, Bwd = W[ex]
                he, Bhe = hes.pop((ex, ch))
                for ts in range(4):
                    i = ch * 4 + ts
                    for half in range(2):
                        ops_, Bops = psg.get()
                        for fc in range(2):
                            K.op("pe", lambda e: e.matmul(ops_[:, :], he[:, fc, ts * 128:(ts + 1) * 128],
                                                          wd[:, fc, half * 512:(half + 1) * 512], start=(fc == 0), stop=(fc == 1)),
                                 reads=[Bhe, Bwd], writes=[Bops])
                        xs = x_sb[:, i, half * 512:(half + 1) * 512]
                        K.op("dve", lambda e: e.tensor_tensor(xs, xs, ops_[:, :], ALU.add), reads=[Bops, Bx[i][half]], writes=[Bx[i][half]])
                    if ex == 15 and io_box["final"] and not dbg:
                        K.dma("sp", out_d[s, i * 128:(i + 1) * 128, :], x_sb[:, i, :], reads=Bx[i], writes=[Bout])
                        io_box["stored"].add((s, i))
                        if s + 1 < n_seq:
                            K.dma("sp", x_sb[:, i, :], dr["x"][s + 1, i * 128:(i + 1) * 128, :], writes=Bx[i])
                            io_box["loaded"].add((s + 1, i))

            for k, (ex, ch) in enumerate(steps):
                stage_a(ex, ch)
                if k > 0:
                    pex, pch = steps[k - 1]
                    stage_b(pex, pch)
                    if pch == 3 and ex + 1 < 16:
                        W.pop(pex)
                        W[ex + 1] = load_expert(ex + 1)
                elif ex + 1 < 16:
                    W[1] = load_expert(1)
            stage_b(*steps[-1])
            phase_end(es)

        def alloc_ymT(es, m):
            ymT = sb(es, f"ymT{m}", [128, 2, T], BF16)
            BymT = [Buf(f"ymT{m}_{ch}") for ch in range(4)]
            ymT_box["t"] = ymT; ymT_box["b"] = BymT
            wo = sb(es, f"wo{m}", [128, 2, D], BF16); Bwo = Buf("wo")
            for c in range(2):
                K.dma("pool", wo[:, c, :], dr["w_out"][l_box[0], 256 * m + c * 128:256 * m + (c + 1) * 128, :], writes=[Bwo])
            ymT_box["wo"] = wo; ymT_box["Bwo"] = Bwo
            return ymT, BymT

        def wout_partial(es, l, m):
            ymT = ymT_box["t"]; BymT = ymT_box["b"]
            wo = ymT_box["wo"]; Bwo = ymT_box["Bwo"]
            for i in range(NT):
                for half in range(2):
                    ops_, Bops = psg.get()
                    for c in range(2):
                        K.op("pe", lambda e: e.matmul(ops_[:, :], ymT[:, c, i * 128:(i + 1) * 128], wo[:, c, half * 512:(half + 1) * 512],
                                                      start=(c == 0), stop=(c == 1)),
                             reads=[Bwo, BymT[i // 4]], writes=[Bops])
                    xs = x_sb[:, i, half * 512:(half + 1) * 512]
                    K.op("dve", lambda e: e.tensor_tensor(xs, xs, ops_[:, :], ALU.add), reads=[Bops, Bx[i][half]], writes=[Bx[i][half]])

        def fm_groupnorm(es_name, yv, Byv, m, l, gcol):
            es = ExitStack()
            sqp = Pool(es, nc, es_name + "sq", [128, 2, 512], BF16, 2)
            rsp = Pool(es, nc, es_name + "rs", [128, 512], F32, 2)
            for ch in range(4):
                cs = slice(ch * 512, (ch + 1) * 512)
                sq, Bsq = sqp.get()
                K.op("act", lambda e: e.activation(sq[:, :, :], yv[:, :, cs], AF.Square), reads=[Byv], writes=[Bsq])
                sps, Bsps = psg.get()
                for c in range(2):
                    K.op("pe", lambda e: e.matmul(sps[:, :], ones16[:], sq[:, c, :], start=(c == 0), stop=(c == 1)),
                         reads=[Bones, Bsq], writes=[Bsps])
                rr, Brr = rsp.get()
                rstd_from(rr[:], sps[:, :], 256, [Bsps], [Brr])
                for c in range(2):
                    K.op("dve", lambda e: e.scalar_tensor_tensor(ymT_box["t"][:, c, cs], yv[:, c, cs], gcol[:, 2 * m + c:2 * m + c + 1],
                                                                 rr[:], ALU.mult, ALU.mult),
                         reads=[Byv, Brr, Bgcol], writes=[ymT_box["b"][ch]])
            K.barrier_all()
            es.close()

        def dbg_dump(m):
            if dbg:
                K.dma("pool", dbg_d["ymT"][m], ymT_box["t"][:], reads=ymT_box["b"], writes=[Bout])

        def lru_phase(s, l):
            es = ExitStack()
            ymT, BymT = alloc_ymT(es, 1)
            cw = sb(es, "lcw", [128, 2, 4], F32); Bcw = Buf("lcw")
            vec = sb(es, "lvec", [128, 5, 2], F32); Bvec = Buf("lvec")
            K.dma("sp", cw[:], dr["lru_cw"][l], writes=[Bcw])
            K.dma("sp", vec[:], dr["lru_vec"][l], writes=[Bvec])
            wa = sb(es, "lwa", [128, 2, 128], BF16); Bwa = Buf("lwa")
            wi = sb(es, "lwi", [128, 2, 128], BF16); Bwi = Buf("lwi")
            for c in range(2):
                K.dma("pool", wa[:, c, :], dr["lru_wa"][l, c], writes=[Bwa])
                K.dma("pool", wi[:, c, :], dr["lru_wi"][l, c], writes=[Bwi])
            K.op("act", lambda e: e.activation(vec[:, 4, :], vec[:, 3, :], AF.Exp, scale=-1.0), reads=[Bvec], writes=[Bvec])
            K.op("act", lambda e: e.activation(vec[:, 4, :], vec[:, 4, :], AF.Ln, bias=1.0), reads=[Bvec], writes=[Bvec])
            K.op("dve", lambda e: e.tensor_scalar(vec[:, 4, :], vec[:, 4, :], -8.0, None, ALU.mult), reads=[Bvec], writes=[Bvec])
            yv = sb(es, "lyv", [128, 2, T], F32); Byv = Buf("lyv")
            e2 = ExitStack()

            def _mk_l(w):
                per = 8 // 2
                return NS(psg=allps.sub(range(w * per, w * per + per)),
                          xp=Pool(e2, nc, "lxp", [128, T + 3], F32, 1), u=Pool(e2, nc, "lu", [128, T], F32, 1),
                          wl=Pool(e2, nc, "lwl", [128, DC, 256], BF16, 1), tp=Pool(e2, nc, "ltp", [128, 512], F32, 5),
                          u16=Pool(e2, nc, "lu16", [128, 512], BF16, 2))

            def _body_l(c, P):
                xp, Bxp = P.xp.get()
                u, Bu = P.u.get()
                wl, Bwl = P.wl.get()
                K.dma("pool", wl[:, :, 0:128], dr["w_in"][l, :, 352 + c * 128:352 + (c + 1) * 128].rearrange("(k p) n -> p k n", p=128), writes=[Bwl])
                K.dma("pool", wl[:, :, 128:256], dr["w_in"][l, :, 608 + c * 128:608 + (c + 1) * 128].rearrange("(k p) n -> p k n", p=128), writes=[Bwl])
                K.op("dve", lambda e: e.memset(xp[:, 0:3], 0.0), writes=[Bxp])
                for ch in range(4):
                    cs = slice(ch * 512, (ch + 1) * 512)
                    p1, Bp1 = P.psg.get()
                    for k in range(DC):
                        K.op("pe", lambda e: e.matmul(p1[:, :], wl[:, k, 0:128], xnT[:, k, cs], start=(k == 0), stop=(k == DC - 1)),
                             reads=[Bwl] + xnT_bufs(ch), writes=[Bp1])
                    K.op("act", lambda e: e.copy(xp[:, 3 + ch * 512:3 + (ch + 1) * 512], p1[:, :]), reads=[Bp1], writes=[Bxp])
                    p2, Bp2 = P.psg.get()
                    for k in range(DC):
                        K.op("pe", lambda e: e.matmul(p2[:, :], wl[:, k, 128:256], xnT[:, k, cs], start=(k == 0), stop=(k == DC - 1)),
                             reads=[Bwl] + xnT_bufs(ch), writes=[Bp2])
                    K.op("act", lambda e: e.activation(yv[:, c, cs], p2[:, :], AF.Gelu_apprx_tanh), reads=[Bp2], writes=[Byv])
                K.op("dve", lambda e: e.tensor_scalar(u[:], xp[:, 0:T], cw[:, c, 0:1], vec[:, 0, c:c + 1], ALU.mult, ALU.add),
                     reads=[Bxp, Bcw, Bvec], writes=[Bu])
                for j in range(1, 4):
                    K.op("dve", lambda e: e.scalar_tensor_tensor(u[:], xp[:, j:j + T], cw[:, c, j:j + 1], u[:], ALU.mult, ALU.add),
                         reads=[Bxp, Bcw, Bu], writes=[Bu])
                for ch in range(4):
                    cs = slice(ch * 512, (ch + 1) * 512)
                    u16, Bu16 = P.u16.get()
                    K.op("act", lambda e: e.copy(u16[:], u[:, cs]), reads=[Bu], writes=[Bu16])
                    pa, Bpa = P.psg.get()
                    K.op("pe", lambda e: e.matmul(pa[:, :], wa[:, c, :], u16[:], start=True, stop=True), reads=[Bwa, Bu16], writes=[Bpa])
                    pi, Bpi = P.psg.get()
                    K.op("pe", lambda e: e.matmul(pi[:, :], wi[:, c, :], u16[:], start=True, stop=True), reads=[Bwi, Bu16], writes=[Bpi])
                    r_, Br_ = P.tp.get()
                    gi, Bgi = P.tp.get()
                    mu, Bmu = P.tp.get()
                    aa, Baa = P.tp.get()
                    K.op("act", lambda e: e.activation(r_[:], pa[:, :], AF.Sigmoid, bias=vec[:, 1, c:c + 1]), reads=[Bpa, Bvec], writes=[Br_])
                    K.op("act", lambda e: e.activation(gi[:], pi[:, :], AF.Sigmoid, bias=vec[:, 2, c:c + 1]), reads=[Bpi, Bvec], writes=[Bgi])
                    K.op("act", lambda e: e.activation(aa[:], r_[:], AF.Exp, scale=vec[:, 4, c:c + 1]), reads=[Br_, Bvec], writes=[Baa])
                    K.op("act", lambda e: e.activation(mu[:], aa[:], AF.Square), reads=[Baa], writes=[Bmu])
                    K.op("act", lambda e: e.activation(mu[:], mu[:], AF.Sqrt, scale=-1.0, bias=1.0), reads=[Bmu], writes=[Bmu])
                    if ch == 0:
                        K.op("dve", lambda e: e.memset(mu[:, 0:1], 1.0), reads=[Bmu], writes=[Bmu])
                    K.op("pool", lambda e: e.tensor_tensor(mu[:], mu[:], gi[:], ALU.mult), reads=[Bmu, Bgi], writes=[Bmu])
                    K.op("dve", lambda e: e.tensor_tensor(xp[:, cs], mu[:], u[:, cs], ALU.mult), reads=[Bmu, Bu, Bxp], writes=[Bxp])
                    init = 0.0 if ch == 0 else u[:, ch * 512 - 1:ch * 512]
                    K.op("dve", lambda e: e.tensor_tensor_scan(u[:, cs], aa[:], xp[:, cs], init, ALU.mult, ALU.add),
                         reads=[Baa, Bxp, Bu], writes=[Bu])
                    K.op("dve", lambda e: e.tensor_tensor(yv[:, c, cs], yv[:, c, cs], u[:, cs], ALU.mult), reads=[Byv, Bu], writes=[Byv])
            IL.run(2, 2, _mk_l, _body_l)
            K.barrier_all()
            e2.close()
            fm_groupnorm("lg", yv, Byv, 1, l, gcolt)
            dbg_dump(1)
            wout_partial(es, l, 1)
            phase_end(es)

        def s5_phase(s, l):
            es = ExitStack()
            ymT, BymT = alloc_ymT(es, 2)
            par = sb(es, "s5par", [128, 3, 8], F32); Bpar = Buf("s5par")
            bb = sb(es, "s5b", [128, 2, 8, 16], F32); Bbb = Buf("s5b")
            vec = sb(es, "s5vec", [128, 3, 2], F32); Bvec = Buf("s5vec")
            K.dma("sp", par[:], dr["s5_par"][l], writes=[Bpar])
            K.dma("sp", bb[:], dr["s5_b"][l], writes=[Bbb])
            K.dma("sp", vec[:], dr["s5_vec"][l], writes=[Bvec])
            wglu = sb(es, "s5glu", [128, 2, 256], BF16); Bwglu = Buf("s5glu")
            K.dma("pool", wglu[:], dr["s5_w_glu"][l].rearrange("(c p) n -> p c n", p=128), writes=[Bwglu])
            ypre = sb(es, "s5ypre", [128, 2, T], F32); Bypre = Buf("s5ypre")
            u16 = sb(es, "s5u16", [128, 2, T], BF16); Bu16 = Buf("s5u16")
            es2 = ExitStack()
            ws = sb(es2, "ws5", [128, DC, 256], BF16); Bws = Buf("ws5")
            K.dma("pool", ws[:], dr["w_in"][l, :, 864:1120].rearrange("(c p) n -> p c n", p=128), writes=[Bws])
            for c in range(2):
                for ch in range(4):
                    cs = slice(ch * 512, (ch + 1) * 512)
                    p1, Bp1 = psg.get()
                    for k in range(DC):
                        K.op("pe", lambda e: e.matmul(p1[:, :], ws[:, k, c * 128:(c + 1) * 128], xnT[:, k, cs], start=(k == 0), stop=(k == DC - 1)),
                             reads=[Bws] + xnT_bufs(ch), writes=[Bp1])
                    K.op("act", lambda e: e.activation(ypre[:, c, cs], p1[:, :], AF.Identity, scale=vec[:, 0, c:c + 1]), reads=[Bp1, Bvec], writes=[Bypre])
                    K.op("dve", lambda e: e.tensor_copy(u16[:, c, cs], p1[:, :]), reads=[Bp1], writes=[Bu16])
            K.barrier_all()
            es2.close()
            sc = sb(es, "s5sc", [128, 16, 8], F32); Bsc = Buf("s5sc")
            SC = [Bsc]
            are = par[:, 0, :]; aim = par[:, 1, :]; ldt = par[:, 2, :]
            dt = sc[:, 0, :]; mag = sc[:, 1, :]; th = sc[:, 2, :]; cth = sc[:, 3, :]; sth = sc[:, 4, :]
            abr = sc[:, 5, :]; abi = sc[:, 6, :]; den = sc[:, 7, :]; gre = sc[:, 8, :]; gim = sc[:, 9, :]
            t0 = sc[:, 10, :]; t1 = sc[:, 11, :]; nre = sc[:, 12, :]
            ki = sb(es, "s5ki", [128, 8], I32); Bki = Buf("s5ki")
            RP = [Bpar, Bsc]
            K.op("act", lambda e: e.activation(dt, ldt, AF.Exp), reads=RP, writes=SC)
            K.op("dve", lambda e: e.tensor_tensor(t0, dt, are, ALU.mult), reads=RP, writes=SC)
            K.op("act", lambda e: e.activation(mag, t0, AF.Exp), reads=RP, writes=SC)
            K.op("dve", lambda e: e.tensor_tensor(th, dt, aim, ALU.mult), reads=RP, writes=SC)
            for (o, shift) in ((sth, 0.0), (cth, 0.5 * np.pi)):
                K.op("dve", lambda e: e.tensor_scalar(ki[:, :], th, shift, 1.0 / TWO_PI, ALU.add, ALU.mult), reads=RP, writes=[Bki])
                K.op("dve", lambda e: e.tensor_copy(t1, ki[:, :]), reads=[Bki], writes=SC)
                K.op("dve", lambda e: e.scalar_tensor_tensor(t1, t1, -TWO_PI, th, ALU.mult, ALU.add), reads=RP, writes=SC)
                K.op("dve", lambda e: e.tensor_scalar(t1, t1, shift, None, ALU.add), reads=RP, writes=SC)
                K.op("dve", lambda e: e.tensor_scalar(t1, t1, 3.14159, -3.14159, ALU.min, ALU.max), reads=RP, writes=SC)
                K.op("act", lambda e: e.activation(o, t1, AF.Sin), reads=RP, writes=SC)
            K.op("dve", lambda e: e.tensor_tensor(abr, mag, cth, ALU.mult), reads=RP, writes=SC)
            K.op("dve", lambda e: e.tensor_tensor(abi, mag, sth, ALU.mult), reads=RP, writes=SC)
            K.op("dve", lambda e: e.tensor_tensor(den, are, are, ALU.mult), reads=RP, writes=SC)
            K.op("dve", lambda e: e.tensor_tensor(t0, aim, aim, ALU.mult), reads=RP, writes=SC)
            K.op("dve", lambda e: e.tensor_tensor(den, den, t0, ALU.add), reads=RP, writes=SC)
            K.op("dve", lambda e: e.reciprocal(den, den), reads=RP, writes=SC)
            K.op("dve", lambda e: e.tensor_scalar(nre, abr, -1.0, None, ALU.add), reads=RP, writes=SC)
            K.op("dve", lambda e: e.tensor_tensor(t0, nre, are, ALU.mult), reads=RP, writes=SC)
            K.op("dve", lambda e: e.tensor_tensor(t1, abi, aim, ALU.mult), reads=RP, writes=SC)
            K.op("dve", lambda e: e.tensor_tensor(gre, t0, t1, ALU.add), reads=RP, writes=SC)
            K.op("dve", lambda e: e.tensor_tensor(gre, gre, den, ALU.mult), reads=RP, writes=SC)
            K.op("dve", lambda e: e.tensor_tensor(t0, abi, are, ALU.mult), reads=RP, writes=SC)
            K.op("dve", lambda e: e.tensor_tensor(t1, nre, aim, ALU.mult), reads=RP, writes=SC)
            K.op("dve", lambda e: e.tensor_tensor(gim, t0, t1, ALU.subtract), reads=RP, writes=SC)
            K.op("dve", lambda e: e.tensor_tensor(gim, gim, den, ALU.mult), reads=RP, writes=SC)
            bbar = sb(es, "s5bbar", [128, 2, 8, 16], F32); Bbbar = Buf("s5bbar")
            btmp = sb(es, "s5btmp", [128, 8, 16], F32); Bbtmp = Buf("s5btmp")
            gre_b = gre.unsqueeze(2).to_broadcast([128, 8, 16]); gim_b = gim.unsqueeze(2).to_broadcast([128, 8, 16])
            K.op("dve", lambda e: e.tensor_tensor(bbar[:, 0], bb[:, 0], gre_b, ALU.mult), reads=[Bbb, Bsc], writes=[Bbbar])
            K.op("dve", lambda e: e.tensor_tensor(btmp[:], bb[:, 1], gim_b, ALU.mult), reads=[Bbb, Bsc], writes=[Bbtmp])
            K.op("dve", lambda e: e.tensor_tensor(bbar[:, 0], bbar[:, 0], btmp[:], ALU.subtract), reads=[Bbbar, Bbtmp], writes=[Bbbar])
            K.op("dve", lambda e: e.tensor_tensor(bbar[:, 1], bb[:, 1], gre_b, ALU.mult), reads=[Bbb, Bsc], writes=[Bbbar])
            K.op("dve", lambda e: e.tensor_tensor(btmp[:], bb[:, 0], gim_b, ALU.mult), reads=[Bbb, Bsc, Bbbar], writes=[Bbtmp])
            K.op("dve", lambda e: e.tensor_tensor(bbar[:, 1], bbar[:, 1], btmp[:], ALU.add), reads=[Bbbar, Bbtmp], writes=[Bbbar])
            mmA = sb(es, "s5mmA", [128, 10, 2, 8], F32); BmmA = Buf("s5mmA")
            mtA = sb(es, "s5mtA", [128, 3, 8], F32); BmtA = Buf("s5mtA")
            K.op("act", lambda e: e.copy(mmA[:, 0, 0, :], sc[:, 3, :]), reads=[Bsc], writes=[BmmA])
            K.op("act", lambda e: e.copy(mmA[:, 0, 1, :], sc[:, 4, :]), reads=[Bsc], writes=[BmmA])
            for k in range(1, 10):
                pr_ = mmA[:, k - 1, 0, :]; pi_ = mmA[:, k - 1, 1, :]
                K.op("dve", lambda e: e.tensor_tensor(mtA[:, 0, :], pr_, pr_, ALU.mult), reads=[BmmA], writes=[BmtA])
                K.op("dve", lambda e: e.tensor_tensor(mtA[:, 1, :], pi_, pi_, ALU.mult), reads=[BmmA], writes=[BmtA])
                K.op("dve", lambda e: e.tensor_tensor(mmA[:, k, 0, :], mtA[:, 0, :], mtA[:, 1, :], ALU.subtract), reads=[BmtA, BmmA], writes=[BmmA])
                K.op("dve", lambda e: e.tensor_tensor(mtA[:, 2, :], pr_, pi_, ALU.mult), reads=[BmmA], writes=[BmtA])
                K.op("dve", lambda e: e.tensor_scalar(mmA[:, k, 1, :], mtA[:, 2, :], 2.0, None, ALU.mult), reads=[BmtA, BmmA], writes=[BmmA])
            e3 = ExitStack()
            def _mk_s5(w):
                per = 8 // 2
                nb = per - 1
                return NS(psg=allps.sub(range(w * per, w * per + nb)), psa=allps.sub(range(w * per + nb, (w + 1) * per)), bexpP=Pool(e3, nc, "s5bexp", [128, 2, 128], F32, 1), blp=Pool(e3, nc, "s5bl", [128, 2, 128], BF16, 1), cwp=Pool(e3, nc, "s5cw", [128, 4, 128], BF16, 1), ctmp=Pool(e3, nc, "s5ct", [128, 2, 128], F32, 1), cosP=Pool(e3, nc, "s5cos", [128, 512], F32, 1), sinP=Pool(e3, nc, "s5sin", [128, 512], F32, 1), mmP=Pool(e3, nc, "s5mm", [128, 12, 2], F32, 1), mtP=Pool(e3, nc, "s5mt", [128, 8], F32, 1), carP=Pool(e3, nc, "s5car", [128, 4], F32, 1), big=Pool(e3, nc, "s5big", [128, 512], F32, 6), pr16=Pool(e3, nc, "s5pr", [128, 4, 512], BF16, 1))
            def _body_s5(j, P):
                bexp, Bbexp = P.bexpP.get()
                cosT, Bcos = P.cosP.get()
                sinT, Bsin = P.sinP.get()
                mm_, Bmm_unused = P.mmP.get()
                mt, Bmt = P.mtP.get()
                car, Bcar = P.carP.get()
                ct_ = (32 * j) // 128
                off = (32 * j) % 128
                K.op("pool", lambda e: e.memset(bexp[:], 0.0), reads=[], writes=[Bbexp])
                for ri in range(2):
                    K.op("pool", lambda e: e.tensor_copy(bexp[0:64, ri, off:off + 16], bbar[0:64, ri, j, :]), reads=[Bbbar], writes=[Bbexp])
                    K.op("pool", lambda e: e.tensor_copy(bexp[64:128, ri, off + 16:off + 32], bbar[64:128, ri, j, :]), reads=[Bbbar], writes=[Bbexp])
                pt, Bpt = P.psg.get()
                for ri in range(2):
                    K.op("pe", lambda e: e.transpose(pt[:, ri * 128:(ri + 1) * 128], bexp[:, ri, :], ident[:]), reads=[Bbexp, Bident], writes=[Bpt])
                bl, Bbl = P.blp.get()
                K.op("act", lambda e: e.copy(bl[:, :, :], pt[:, 0:256].rearrange("p (r n) -> p r n", r=2)), reads=[Bpt], writes=[Bbl])
                ct, Bct = P.ctmp.get()
                for ri in range(2):
                    K.dma("sp", ct[:, ri, :], dr["s5_c"][l, ri, j], writes=[Bct])
                cw4, Bcw4 = P.cwp.get()
                K.op("act", lambda e: e.copy(cw4[:, 0, :], ct[:, 0, :]), reads=[Bct], writes=[Bcw4])
                K.op("act", lambda e: e.mul(cw4[:, 1, :], ct[:, 0, :], -1.0), reads=[Bct], writes=[Bcw4])
                K.op("act", lambda e: e.mul(cw4[:, 2, :], ct[:, 1, :], -1.0), reads=[Bct], writes=[Bcw4])
                K.op("act", lambda e: e.mul(cw4[:, 3, :], ct[:, 1, :], -1.0), reads=[Bct], writes=[Bcw4])
                K.op("dve", lambda e: e.memset(cosT[:, 0:1], 1.0), writes=[Bcos])
                K.op("dve", lambda e: e.memset(sinT[:, 0:1], 0.0), writes=[Bsin])
                tb, Btb = P.big.get()
                for k in range(9):
                    n = 1 << k
                    mr = mmA[:, k, 0, j:j + 1]; mi = mmA[:, k, 1, j:j + 1]
                    K.op("dve", lambda e: e.tensor_scalar(tb[:, 0:n], sinT[:, 0:n], mi, None, ALU.mult), reads=[Bsin, BmmA], writes=[Btb])
                    K.op("dve", lambda e: e.scalar_tensor_tensor(cosT[:, n:2 * n], cosT[:, 0:n], mr, tb[:, 0:n], ALU.mult, ALU.subtract),
                         reads=[Bcos, BmmA, Btb], writes=[Bcos])
                    K.op("dve", lambda e: e.tensor_scalar(tb[:, 0:n], sinT[:, 0:n], mr, None, ALU.mult), reads=[Bsin, BmmA, Bcos], writes=[Btb])
                    K.op("dve", lambda e: e.scalar_tensor_tensor(sinT[:, n:2 * n], cosT[:, 0:n], mi, tb[:, 0:n], ALU.mult, ALU.add),
                         reads=[Bcos, BmmA, Btb], writes=[Bsin])
                magb = sc[:, 1, j:j + 1].to_broadcast([128, 512])
                m9r = mmA[:, 9, 0, j:j + 1]; m9i = mmA[:, 9, 1, j:j + 1]
                for ch in range(4):
                    cs = slice(ch * 512, (ch + 1) * 512)
                    pre, Bpre = P.psg.get()
                    K.op("pe", lambda e: e.matmul(pre[:, :], bl[:, 0, :], u16[:, ct_, cs], start=True, stop=True), reads=[Bbl, Bu16], writes=[Bpre])
                    pim, Bpim = P.psg.get()
                    K.op("pe", lambda e: e.matmul(pim[:, :], bl[:, 1, :], u16[:, ct_, cs], start=True, stop=True), reads=[Bbl, Bu16], writes=[Bpim])
                    brr, Bbrr = P.big.get()
                    bri, Bbri = P.big.get()
                    ta, Bta = P.big.get()
                    tb2, Btb2 = P.big.get()
                    K.op("dve", lambda e: e.tensor_tensor(brr[:], cosT[:], pre[:, :], ALU.mult), reads=[Bcos, Bpre], writes=[Bbrr])
                    K.op("dve", lambda e: e.tensor_tensor(ta[:], sinT[:], pim[:, :], ALU.mult), reads=[Bsin, Bpim], writes=[Bta])
                    K.op("pool", lambda e: e.tensor_tensor(brr[:], brr[:], ta[:], ALU.add), reads=[Bbrr, Bta], writes=[Bbrr])
                    K.op("dve", lambda e: e.tensor_tensor(bri[:], cosT[:], pim[:, :], ALU.mult), reads=[Bcos, Bpim], writes=[Bbri])
                    K.op("dve", lambda e: e.tensor_tensor(tb2[:], sinT[:], pre[:, :], ALU.mult), reads=[Bsin, Bpre], writes=[Btb2])
                    K.op("pool", lambda e: e.tensor_tensor(bri[:], bri[:], tb2[:], ALU.subtract), reads=[Bbri, Btb2], writes=[Bbri])
                    if ch == 0:
                        ir, ii = 0.0, 0.0
                    else:
                        K.op("dve", lambda e: e.tensor_tensor(mt[:, 4:5], car[:, 0:1], m9r, ALU.mult), reads=[Bcar, BmmA], writes=[Bmt])
                        K.op("dve", lambda e: e.tensor_tensor(mt[:, 5:6], car[:, 1:2], m9i, ALU.mult), reads=[Bcar, BmmA], writes=[Bmt])
                        K.op("dve", lambda e: e.tensor_tensor(mt[:, 6:7], car[:, 1:2], m9r, ALU.mult), reads=[Bcar, BmmA], writes=[Bmt])
                        K.op("dve", lambda e: e.tensor_tensor(mt[:, 7:8], car[:, 0:1], m9i, ALU.mult), reads=[Bcar, BmmA], writes=[Bmt])
                        K.op("dve", lambda e: e.tensor_tensor(car[:, 2:3], mt[:, 4:5], mt[:, 5:6], ALU.subtract), reads=[Bmt, Bcar], writes=[Bcar])
                        K.op("dve", lambda e: e.tensor_tensor(car[:, 3:4], mt[:, 6:7], mt[:, 7:8], ALU.add), reads=[Bmt, Bcar], writes=[Bcar])
                        ir, ii = car[:, 2:3], car[:, 3:4]
                    K.op("dve", lambda e: e.tensor_tensor_scan(ta[:], magb, brr[:], ir, ALU.mult, ALU.add), reads=[Bsc, Bbrr, Bta, Bcar], writes=[Bta])
                    K.op("dve", lambda e: e.tensor_tensor_scan(tb2[:], magb, bri[:], ii, ALU.mult, ALU.add), reads=[Bsc, Bbri, Btb2, Bcar], writes=[Btb2])
                    K.op("act", lambda e: e.copy(car[:, 0:1], ta[:, 511:512]), reads=[Bta, Bcar], writes=[Bcar])
                    K.op("act", lambda e: e.copy(car[:, 1:2], tb2[:, 511:512]), reads=[Btb2, Bcar], writes=[Bcar])
                    pp, Bpp = P.pr16.get()
                    K.op("dve", lambda e: e.tensor_tensor(pp[:, 0, :], cosT[:], ta[:], ALU.mult), reads=[Bcos, Bta], writes=[Bpp])
                    K.op("pool", lambda e: e.tensor_tensor(pp[:, 1, :], sinT[:], tb2[:], ALU.mult), reads=[Bsin, Btb2], writes=[Bpp])
                    K.op("dve", lambda e: e.tensor_tensor(pp[:, 2, :], sinT[:], ta[:], ALU.mult), reads=[Bsin, Bta], writes=[Bpp])
                    K.op("pool", lambda e: e.tensor_tensor(pp[:, 3, :], cosT[:], tb2[:], ALU.mult), reads=[Bcos, Btb2], writes=[Bpp])
                    yp, Byp = P.psg.get()
                    for v in range(4):
                        K.op("pe", lambda e: e.matmul(yp[:, :], cw4[:, v, :], pp[:, v, :], start=(v == 0), stop=(v == 3)), reads=[Bcw4, Bpp], writes=[Byp])
                    K.op("dve", lambda e: e.tensor_tensor(ypre[:, ct_, cs], ypre[:, ct_, cs], yp[:, :], ALU.add), reads=[Byp, Bypre], writes=[Bypre])
            IL.run(8, 2, _mk_s5, _body_s5)
            K.barrier_all()
            e3.close()
            yg16 = sb(es, "s5yg16", [128, 2, T], BF16); Byg16 = Buf("s5yg16")
            for c in range(2):
                K.op("act", lambda e: e.activation(ypre[:, c, :], ypre[:, c, :], AF.Gelu_apprx_tanh), reads=[Bypre], writes=[Bypre])
                K.op("act", lambda e: e.copy(yg16[:, c, :], ypre[:, c, :]), reads=[Bypre], writes=[Byg16])
            sgp = Pool(es, nc, "s5sg", [128, 512], F32, 2)
            for c in range(2):
                for ch in range(4):
                    cs = slice(ch * 512, (ch + 1) * 512)
                    zp, Bzp = psg.get()
                    for k in range(2):
                        K.op("pe", lambda e: e.matmul(zp[:, :], wglu[:, k, c * 128:(c + 1) * 128], yg16[:, k, cs], start=(k == 0), stop=(k == 1)),
                             reads=[Bwglu, Byg16], writes=[Bzp])
                    sg, Bsg = sgp.get()
                    K.op("act", lambda e: e.activation(sg[:], zp[:, :], AF.Sigmoid, bias=vec[:, 1, c:c + 1]), reads=[Bzp, Bvec], writes=[Bsg])
                    K.op("dve", lambda e: e.tensor_tensor(ypre[:, c, cs], ypre[:, c, cs], sg[:], ALU.mult), reads=[Bypre, Bsg], writes=[Bypre])
            fm_groupnorm("sg", ypre, Bypre, 2, l, gcolt)
            dbg_dump(2)
            wout_partial(es, l, 2)
            phase_end(es)

        def rope_tables(es, name, inv_name, half, pos_cols, Bpos_in, npart=128):
            ncol = pos_cols.shape[1]
            inv = sb(es, name + "inv", [128, half], F32); Binv = Buf(name + "inv")
            K.dma("sp", inv[:], dr[inv_name][:], writes=[Binv])
            ang = sb(es, name + "ang", [128, ncol, half], F32); Bang = Buf(name + "ang")
            tmp = sb(es, name + "tmp", [128, ncol, half], F32); Btmp = Buf(name + "tmp")
            kk = sb(es, name + "kk", [128, ncol, half], I32); Bkk = Buf(name + "kk")
            cs_ = sb(es, name + "cs", [128, 2, ncol, half], F32); Bcs = Buf(name + "cs")
            P = npart
            K.op("dve", lambda e: e.tensor_tensor(ang[0:P], inv[0:P].unsqueeze(1).to_broadcast([P, ncol, half]),
                                                  pos_cols.unsqueeze(2).to_broadcast([P, ncol, half]), ALU.mult),
                 reads=[Binv, Bpos_in], writes=[Bang])
            for idx, shift in ((0, 0.5 * np.pi), (1, 0.0)):
                K.op("dve", lambda e: e.tensor_scalar(kk[0:P], ang[0:P], shift, 1.0 / TWO_PI, ALU.add, ALU.mult), reads=[Bang], writes=[Bkk])
                K.op("dve", lambda e: e.tensor_copy(tmp[0:P], kk[0:P]), reads=[Bkk], writes=[Btmp])
                K.op("dve", lambda e: e.scalar_tensor_tensor(tmp[0:P], tmp[0:P], -TWO_PI, ang[0:P], ALU.mult, ALU.add), reads=[Btmp, Bang], writes=[Btmp])
                K.op("dve", lambda e: e.tensor_scalar(tmp[0:P], tmp[0:P], shift, None, ALU.add), reads=[Btmp], writes=[Btmp])
                K.op("dve", lambda e: e.tensor_scalar(tmp[0:P], tmp[0:P], 3.14159, -3.14159, ALU.min, ALU.max), reads=[Btmp], writes=[Btmp])
                K.op("act", lambda e: e.activation(cs_[0:P, idx], tmp[0:P], AF.Sin), reads=[Btmp], writes=[Bcs])
            return cs_, Bcs

        def apply_rope(dst, src, cos_t, sin_t, nh, half, tmp, reads, writes, Btmp):
            x1 = src[:, :, 0:half]; x2 = src[:, :, half:2 * half]
            cb = cos_t.unsqueeze(1).to_broadcast([128, nh, half]); sb_ = sin_t.unsqueeze(1).to_broadcast([128, nh, half])
            ta = tmp[:, 0:nh, 0:half]; tb_ = tmp[:, 0:nh, half:2 * half]
            K.op("dve", lambda e: e.tensor_tensor(ta, x1, cb, ALU.mult), reads=reads, writes=[Btmp])
            K.op("dve", lambda e: e.tensor_tensor(tb_, x2, sb_, ALU.mult), reads=reads, writes=[Btmp])
            K.op("dve", lambda e: e.tensor_tensor(dst[:, :, 0:half], ta, tb_, ALU.subtract), reads=[Btmp], writes=writes)
            K.op("dve", lambda e: e.tensor_tensor(ta, x1, sb_, ALU.mult), reads=reads + [Btmp], writes=[Btmp])
            K.op("dve", lambda e: e.tensor_tensor(tb_, x2, cb, ALU.mult), reads=reads + [Btmp], writes=[Btmp])
            K.op("dve", lambda e: e.tensor_tensor(dst[:, :, half:2 * half], ta, tb_, ALU.add), reads=[Btmp], writes=writes)

        def attn_block_group(s_items, acc, Bacc, first_group, last_group):
            pass

        def mla_phase(s, l):
            es = ExitStack()
            ymT, BymT = alloc_ymT(es, 0)
            qT = sb(es, "mqT", [128, 4, T], BF16); BqT = [Buf(f"mqT{i}") for i in range(NT)]
            kT = sb(es, "mkT", [128, 4, T], BF16); BkT = [Buf(f"mkT{i}") for i in range(NT)]
            K.op("pool", lambda e: e.memset(qT[64:128], 0.0), writes=BqT)
            K.op("pool", lambda e: e.memset(kT[64:128], 0.0), writes=BkT)
            va = sb(es, "mva", [128, NT, 4, 65], BF16); Bva = [Buf(f"mva{i}") for i in range(NT)]
            K.op("pool", lambda e: e.memset(va[:, :, :, 64:65], 1.0), writes=Bva)
            e1 = ExitStack()
            wm = sb(e1, "wm", [128, DC, 352], BF16); Bwm = Buf("wm")
            K.dma("pool", wm[:], dr["w_in"][l, :, 0:352].rearrange("(c p) n -> p c n", p=128), writes=[Bwm])
            wuq = sb(e1, "wuq", [128, 2, 384], BF16); Bwuq = Buf("wuq")
            K.dma("pool", wuq[:, 0, :], dr["mla_w_uq"][l, 0:128, :], writes=[Bwuq])
            K.dma("pool", wuq[0:64, 1, :], dr["mla_w_uq"][l, 128:192, :], writes=[Bwuq])
            wukv = sb(e1, "wukv", [128, 512], BF16); Bwukv = Buf("wukv")
            K.dma("pool", wukv[:], dr["mla_w_ukv"][l], writes=[Bwukv])
            gv = sb(e1, "mgv", [128, 192 + 128 + 96 + 96], F32); Bgv = Buf("mgv")
            K.dma("sp", gv[:, 0:192], dr["mla_g_cq"][l:l + 1, :].to_broadcast([128, 192]), writes=[Bgv])
            K.dma("sp", gv[:, 192:320], dr["mla_g_ckv"][l:l + 1, :].to_broadcast([128, 128]), writes=[Bgv])
            K.dma("sp", gv[:, 320:416], dr["mla_g_q"][l:l + 1, :].to_broadcast([128, 96]), writes=[Bgv])
            K.dma("sp", gv[:, 416:512], dr["mla_g_k"][l:l + 1, :].to_broadcast([128, 96]), writes=[Bgv])
            g_cq = gv[:, 0:192]; g_ckv = gv[:, 192:320]; g_q = gv[:, 320:416]; g_k = gv[:, 416:512]
            cs_t, Bcs_t = rope_tables(e1, "mr", "c_inv_mla", 16, posf[:, :], Bposf)
            def _mk_mp(w):
                per = 8 // 3
                nb = per - 0
                return NS(psg=allps.sub(range(w * per, w * per + nb)), psa=allps.sub(range(w * per + nb, (w + 1) * per)), st=Pool(e1, nc, "mst", [128, 16], F32, 2), cn=Pool(e1, nc, "mcn", [128, 320], BF16, 1), cT=Pool(e1, nc, "mcT", [128, 3, 128], BF16, 1), qn=Pool(e1, nc, "mqn", [128, 4, 96], F32, 1), kn=Pool(e1, nc, "mkn", [128, 4, 96], F32, 1), qr=Pool(e1, nc, "mqr", [128, 8, 96], BF16, 1), jk=Pool(e1, nc, "mjk", [128, 192], F32, 1), rtmp=Pool(e1, nc, "mrt", [128, 4, 32], F32, 1), kpe=Pool(e1, nc, "mkpe", [128, 32], F32, 1))
            def _body_mp(i, P):
                ts_ = slice(i * 128, (i + 1) * 128)
                pp, Bpp = P.psg.get()
                for c in range(DC):
                    K.op("pe", lambda e: e.matmul(pp[:, 0:352], xnT[:, c, ts_], wm[:, c, :], start=(c == 0), stop=(c == DC - 1)),
                         reads=[Bwm, BxnT[i][0], BxnT[i][1]], writes=[Bpp])
                sq, Bsq = P.st.get()
                j_, Bj_ = P.jk.get()
                K.op("act", lambda e: e.activation(j_[:, 0:192], pp[:, 0:192], AF.Square, accum_out=sq[:, 0:1]), reads=[Bpp], writes=[Bj_, Bsq])
                K.op("act", lambda e: e.activation(j_[:, 0:128], pp[:, 192:320], AF.Square, accum_out=sq[:, 1:2]), reads=[Bpp], writes=[Bj_, Bsq])
                K.op("act", lambda e: e.activation(j_[:, 0:32], pp[:, 320:352], AF.Square, accum_out=sq[:, 2:3]), reads=[Bpp], writes=[Bj_, Bsq])
                K.op("dve", lambda e: e.tensor_scalar(sq[:, 0:1], sq[:, 0:1], 128.0 / 192.0, None, ALU.mult), reads=[Bsq], writes=[Bsq])
                rstd_from(sq[:, 4:6], sq[:, 0:2], 128, [Bsq], [Bsq])
                c_n, Bc_n = P.cn.get()
                K.op("dve", lambda e: e.scalar_tensor_tensor(c_n[:, 0:192], pp[:, 0:192], sq[:, 4:5], g_cq, ALU.mult, ALU.mult),
                     reads=[Bpp, Bsq, Bgv], writes=[Bc_n])
                K.op("dve", lambda e: e.scalar_tensor_tensor(c_n[:, 192:320], pp[:, 192:320], sq[:, 5:6], g_ckv, ALU.mult, ALU.mult),
                     reads=[Bpp, Bsq, Bgv], writes=[Bc_n])
                kp, Bkp = P.kpe.get()
                K.op("act", lambda e: e.copy(kp[:], pp[:, 320:352]), reads=[Bpp], writes=[Bkp])
                pt, Bpt = P.psg.get()
                ptb = pt[:, :].bitcast(BF16)
                K.op("pe", lambda e: e.transpose(ptb[:, 0:128], c_n[:, 0:128], identb[:]), reads=[Bc_n, Bidentb], writes=[Bpt])
                K.op("pe", lambda e: e.transpose(ptb[0:64, 128:256], c_n[:, 128:192], identb[:]), reads=[Bc_n, Bidentb], writes=[Bpt])
                K.op("pe", lambda e: e.transpose(ptb[:, 256:384], c_n[:, 192:320], identb[:]), reads=[Bc_n, Bidentb], writes=[Bpt])
                ct, Bct = P.cT.get()
                K.op("act", lambda e: e.copy(ct[:, 0, :], ptb[:, 0:128]), reads=[Bpt], writes=[Bct])
                K.op("act", lambda e: e.copy(ct[0:64, 1, :], ptb[0:64, 128:256]), reads=[Bpt], writes=[Bct])
                K.op("act", lambda e: e.copy(ct[:, 2, :], ptb[:, 256:384]), reads=[Bpt], writes=[Bct])
                pq, Bpq = P.psg.get()
                K.op("pe", lambda e: e.matmul(pq[:, 0:384], ct[:, 0, :], wuq[:, 0, :], start=True, stop=False), reads=[Bct, Bwuq], writes=[Bpq])
                K.op("pe", lambda e: e.matmul(pq[:, 0:384], ct[0:64, 1, :], wuq[0:64, 1, :], start=False, stop=True), reads=[Bct, Bwuq], writes=[Bpq])
                pkv, Bpkv = P.psg.get()
                K.op("pe", lambda e: e.matmul(pkv[:, :], ct[:, 2, :], wukv[:], start=True, stop=True), reads=[Bct, Bwukv], writes=[Bpkv])
                pq3 = pq[:, 0:384].rearrange("p (h d) -> p h d", h=4)
                pkv3 = pkv[:, :].rearrange("p (h d) -> p h d", h=4)
                sq2, Bsq2 = P.st.get()
                for h in range(4):
                    K.op("act", lambda e: e.activation(j_[:, 0:96], pq3[:, h, :], AF.Square, accum_out=sq2[:, h:h + 1]), reads=[Bpq], writes=[Bj_, Bsq2])
                    K.op("act", lambda e: e.activation(j_[:, 0:64], pkv3[:, h, 0:64], AF.Square, accum_out=sq2[:, 4 + h:5 + h]), reads=[Bpkv], writes=[Bj_, Bsq2])
                K.op("dve", lambda e: e.tensor_scalar(sq2[:, 4:8], sq2[:, 4:8], sq[:, 2:3], None, ALU.add), reads=[Bsq2, Bsq], writes=[Bsq2])
                rstd_from(sq2[:, 8:16], sq2[:, 0:8], 96, [Bsq2], [Bsq2])
                q_n, Bq_n = P.qn.get()
                k_n, Bk_n = P.kn.get()
                for h in range(4):
                    K.op("dve", lambda e: e.scalar_tensor_tensor(q_n[:, h, :], pq3[:, h, :], sq2[:, 8 + h:9 + h], g_q, ALU.mult, ALU.mult),
                         reads=[Bpq, Bsq2, Bgv], writes=[Bq_n])
                    K.op("dve", lambda e: e.scalar_tensor_tensor(k_n[:, h, 32:96], pkv3[:, h, 0:64], sq2[:, 12 + h:13 + h], g_k[:, 32:96], ALU.mult, ALU.mult),
                         reads=[Bpkv, Bsq2, Bgv], writes=[Bk_n])
                    K.op("dve", lambda e: e.scalar_tensor_tensor(k_n[:, h, 0:32], kp[:], sq2[:, 12 + h:13 + h], g_k[:, 0:32], ALU.mult, ALU.mult),
                         reads=[Bkp, Bsq2, Bgv], writes=[Bk_n])
                q_r, Bq_r = P.qr.get()
                rt_, Brt_ = P.rtmp.get()
                apply_rope(q_r[:, 0:4], q_n[:, :, :], cs_t[:, 0, i, :], cs_t[:, 1, i, :], 4, 16, rt_, [Bq_n, Bcs_t], [Bq_r], Brt_)
                K.op("act", lambda e: e.copy(q_r[:, 0:4, 32:96], q_n[:, :, 32:96]), reads=[Bq_n], writes=[Bq_r])
                apply_rope(q_r[:, 4:8], k_n[:, :, :], cs_t[:, 0, i, :], cs_t[:, 1, i, :], 4, 16, rt_, [Bk_n, Bcs_t], [Bq_r], Brt_)
                K.op("act", lambda e: e.copy(q_r[:, 4:8, 32:96], k_n[:, :, 32:96]), reads=[Bk_n], writes=[Bq_r])
                K.op("act", lambda e: e.copy(va[:, i, :, 0:64], pkv3[:, :, 64:128]), reads=[Bpkv], writes=[Bva[i]])
                for grp in range(2):
                    pt2, Bpt2 = P.psg.get()
                    pt2b = pt2[:, :].bitcast(BF16)
                    for h in range(4):
                        K.op("pe", lambda e: e.transpose(pt2b[0:96, h * 128:(h + 1) * 128], q_r[:, grp * 4 + h, :], identb[:]),
                             reads=[Bq_r, Bidentb], writes=[Bpt2])
                    dstT = qT if grp == 0 else kT
                    BdT = BqT if grp == 0 else BkT
                    K.op("act" if grp == 0 else "dve",
                         (lambda e: e.copy(dstT[0:96, :, ts_], pt2b[0:96, 0:512].rearrange("p (h t) -> p h t", h=4))) if grp == 0 else
                         (lambda e: e.tensor_copy(dstT[0:96, :, ts_], pt2b[0:96, 0:512].rearrange("p (h t) -> p h t", h=4))),
                         reads=[Bpt2], writes=[BdT[i]])
            IL.run(NT, 3, _mk_mp, _body_mp)
            K.barrier_all()
            e1.close()
            scale = 96 ** -0.5
            def _mk_ma(w):
                per = 8 // 4
                nb = per - 1
                return NS(psg=allps.sub(range(w * per, w * per + nb)), psa=allps.sub(range(w * per + nb, (w + 1) * per)), pexp=Pool(es, nc, "mpe", [128, 512], BF16, 3), yo=Pool(es, nc, "myo", [128, 256], F32, 2), yst=Pool(es, nc, "myst", [128, 8], F32, 2), yb=Pool(es, nc, "myb", [128, 256], BF16, 2))
            def _body_ma(qt, P):
                qs = slice(qt * 128, (qt + 1) * 128)
                y_o, By_o = P.yo.get()
                yst_, Byst = P.yst.get()
                for h in range(4):
                    acc, Bacc = P.psa.get()
                    nk = qt + 1
                    for g0 in range(0, nk, 4):
                        kts = list(range(g0, min(g0 + 4, nk)))
                        sp_, Bsp = P.psg.get()
                        for a, kt in enumerate(kts):
                            K.op("pe", lambda e: e.matmul(sp_[:, a * 128:(a + 1) * 128], kT[:, h, kt * 128:(kt + 1) * 128], qT[:, h, qs], start=True, stop=True),
                                 reads=[BkT[kt], BqT[qt]], writes=[Bsp])
                        pe_, Bpe = P.pexp.get()
                        w = len(kts) * 128
                        K.op("act", lambda e: e.activation(pe_[:, 0:w], sp_[:, 0:w], AF.Exp, scale=scale), reads=[Bsp], writes=[Bpe])
                        if kts[-1] == qt:
                            a = len(kts) - 1
                            K.op("pool", lambda e: e.tensor_tensor(pe_[:, a * 128:(a + 1) * 128], pe_[:, a * 128:(a + 1) * 128], tri4[:, 0, :], ALU.mult),
                                 reads=[Bpe, Btri], writes=[Bpe])
                        for a, kt in enumerate(kts):
                            K.op("pe", lambda e: e.matmul(acc[:, 0:65], pe_[:, a * 128:(a + 1) * 128], va[:, kt, h, :], start=(kt == 0), stop=(kt == qt)),
                                 reads=[Bpe, Bva[kt]], writes=[Bacc])
                    K.op("dve", lambda e: e.reciprocal(yst_[:, h:h + 1], acc[:, 64:65]), reads=[Bacc], writes=[Byst])
                    K.op("dve", lambda e: e.tensor_scalar(y_o[:, h * 64:(h + 1) * 64], acc[:, 0:64], yst_[:, h:h + 1], None, ALU.mult),
                         reads=[Bacc, Byst], writes=[By_o])
                tm_groupnorm(P.psg, y_o, By_o, yst_, Byst, P.yb, 0, l, qt)
            IL.run(NT, 4, _mk_ma, _body_ma)
            dbg_dump(0)
            wout_partial(es, l, 0)
            phase_end(es)

        def tm_groupnorm(psgp, y_o, By_o, yst_, Byst, ybpool, m, l, qt):
            y_b, By_b = ybpool.get()
            K.op("act", lambda e: e.activation(y_b[:], y_o[:], AF.Square, accum_out=yst_[:, 4:5]), reads=[By_o], writes=[By_b, Byst])
            K.op("act", lambda e: e.activation(yst_[:, 5:6], yst_[:, 4:5], AF.Ln, bias=epsb[:, 0:1], scale=1.0 / 256), reads=[Byst, Beps], writes=[Byst])
            K.op("act", lambda e: e.activation(yst_[:, 5:6], yst_[:, 5:6], AF.Exp, scale=-0.5), reads=[Byst], writes=[Byst])
            K.op("dve", lambda e: e.scalar_tensor_tensor(y_b[:], y_o[:], yst_[:, 5:6], gon[:, m, :], ALU.mult, ALU.mult),
                 reads=[By_o, Byst, Bgon, By_b], writes=[By_b])
            pt, Bpt = psgp.get()
            ptb = pt[:, :].bitcast(BF16)
            for c in range(2):
                K.op("pe", lambda e: e.transpose(ptb[:, c * 128:(c + 1) * 128], y_b[:, c * 128:(c + 1) * 128], identb[:]), reads=[By_b, Bidentb], writes=[Bpt])
            K.op("act", lambda e: e.copy(ymT_box["t"][:, :, qt * 128:(qt + 1) * 128], ptb[:, 0:256].rearrange("p (c t) -> p c t", c=2)),
                 reads=[Bpt], writes=[ymT_box["b"][qt // 4]])

        def nsa_phase(s, l):
            es = ExitStack()
            ymT, BymT = alloc_ymT(es, 3)
            gv = sb(es, "ngv", [128, 4, 64], F32); Bgv = Buf("ngv")
            K.dma("sp", gv[:, 0, :], dr["nsa_g_q"][l:l + 1, :].to_broadcast([128, 64]), writes=[Bgv])
            for b3 in range(3):
                K.dma("sp", gv[:, 1 + b3, :], dr["nsa_g_k"][l, b3:b3 + 1, :].to_broadcast([128, 64]), writes=[Bgv])
            qT = sb(es, "nqT", [128, NT, 4, 128], BF16); BqT = [Buf(f"nqT{i}") for i in range(NT)]
            kTs = sb(es, "nkTs", [128, T], BF16); BkTs = [Buf(f"nkTs{i}") for i in range(NT)]
            kTw = sb(es, "nkTw", [128, T], BF16); BkTw = [Buf(f"nkTw{i}") for i in range(NT)]
            K.op("pool", lambda e: e.memset(qT[64:128], 0.0), writes=BqT)
            K.op("pool", lambda e: e.memset(kTs[64:128], 0.0), writes=BkTs)
            K.op("pool", lambda e: e.memset(kTw[64:128], 0.0), writes=BkTw)
            vs = sb(es, "nvs", [128, NT, 65], BF16); Bvs = [Buf(f"nvs{i}") for i in range(NT)]
            vw = sb(es, "nvw", [128, NT, 65], BF16); Bvw = [Buf(f"nvw{i}") for i in range(NT)]
            gts = sb(es, "ngts", [128, NT, 12], F32); Bgts = [Buf(f"ngts{i}") for i in range(NT)]
            kcT = sb(es, "nkcT", [128, 128], BF16); BkcT = Buf("nkcT")
            K.op("pool", lambda e: e.memset(kcT[64:128], 0.0), writes=[BkcT])
            vc = sb(es, "nvc", [128, 97], BF16); Bvc = Buf("nvc")
            K.op("pool", lambda e: e.memset(vs[:, :, 64:65], 1.0), writes=Bvs)
            K.op("pool", lambda e: e.memset(vw[:, :, 64:65], 1.0), writes=Bvw)
            K.op("pool", lambda e: e.memset(vc[:, 64:65], 1.0), writes=[Bvc])
            K.dma("pool", vc[:, 65:97], dr["c_ov"][:], writes=[Bvc])
            e1 = ExitStack()
            wn = sb(e1, "wn", [128, DC, 652], BF16); Bwn = Buf("wn")
            K.dma("pool", wn[:], dr["w_in"][l, :, 1120:1772].rearrange("(c p) n -> p c n", p=128), writes=[Bwn])
            cs_t, Bcs_t = rope_tables(e1, "nr", "c_inv_nsa", 8, posf[:, :], Bposf)
            pci = sb(e1, "npci", [128, 1], I32); Bpci = Buf("npci")
            pcf = sb(e1, "npcf", [128, 1], F32); Bpcf = Buf("npcf")
            K.op("dve", lambda e: e.memset(pcf[:], 0.0), writes=[Bpcf])
            pos_src = dr["pos"][s:s + 1, 31::16].rearrange("o n -> n o")
            K.dma("sp", pci[0:127, :], pos_src, writes=[Bpci], allow_slow_non_contiguous=True)
            K.op("dve", lambda e: e.tensor_copy(pcf[0:127, :], pci[0:127, :]), reads=[Bpci, Bpcf], writes=[Bpcf])
            csc, Bcsc = rope_tables(e1, "nrc", "c_inv_nsa", 8, pcf[:, :], Bpcf)
            cmpT = sb(e1, "ncmpT", [128, T], BF16); BcmpT = Buf("ncmpT")
            for ch in range(4):
                cs = slice(ch * 512, (ch + 1) * 512)
                p1, Bp1 = psg.get()
                for k in range(DC):
                    K.op("pe", lambda e: e.matmul(p1[:, :], wn[:, k, 256:384], xnT[:, k, cs], start=(k == 0), stop=(k == DC - 1)),
                         reads=[Bwn] + xnT_bufs(ch), writes=[Bp1])
                K.op("act", lambda e: e.copy(cmpT[:, cs], p1[:, :]), reads=[Bp1], writes=[BcmpT])
            def _mk_np(w):
                per = 8 // 4
                nb = per - 0
                return NS(psg=allps.sub(range(w * per, w * per + nb)), psa=allps.sub(range(w * per + nb, (w + 1) * per)), st=Pool(e1, nc, "nst", [128, 16], F32, 2), jk=Pool(e1, nc, "njk", [128, 64], F32, 1), qn=Pool(e1, nc, "nqn", [128, 6, 64], F32, 1), qr=Pool(e1, nc, "nqr", [128, 6, 64], BF16, 1), rtmp=Pool(e1, nc, "nrt", [128, 6, 16], F32, 1))
            def _body_np(i, P):
                ts_ = slice(i * 128, (i + 1) * 128)
                pa, Bpa = P.psg.get()
                pb, Bpb = P.psg.get()
                for c in range(DC):
                    K.op("pe", lambda e: e.matmul(pa[:, 0:256], xnT[:, c, ts_], wn[:, c, 0:256], start=(c == 0), stop=(c == DC - 1)),
                         reads=[Bwn, BxnT[i][0], BxnT[i][1]], writes=[Bpa])
                for c in range(DC):
                    K.op("pe", lambda e: e.matmul(pb[:, 0:268], xnT[:, c, ts_], wn[:, c, 384:652], start=(c == 0), stop=(c == DC - 1)),
                         reads=[Bwn, BxnT[i][0], BxnT[i][1]], writes=[Bpb])
                pa3 = pa[:, 0:256].rearrange("p (h d) -> p h d", h=4)
                sq, Bsq = P.st.get()
                j_, Bj_ = P.jk.get()
                for h in range(4):
                    K.op("act", lambda e: e.activation(j_[:], pa3[:, h, :], AF.Square, accum_out=sq[:, h:h + 1]), reads=[Bpa], writes=[Bj_, Bsq])
                K.op("act", lambda e: e.activation(j_[:], pb[:, 0:64], AF.Square, accum_out=sq[:, 4:5]), reads=[Bpb], writes=[Bj_, Bsq])
                K.op("act", lambda e: e.activation(j_[:], pb[:, 128:192], AF.Square, accum_out=sq[:, 5:6]), reads=[Bpb], writes=[Bj_, Bsq])
                rstd_from(sq[:, 8:14], sq[:, 0:6], 64, [Bsq], [Bsq])
                q_n, Bq_n = P.qn.get()
                for h in range(4):
                    K.op("dve", lambda e: e.scalar_tensor_tensor(q_n[:, h, :], pa3[:, h, :], sq[:, 8 + h:9 + h], gv[:, 0, :], ALU.mult, ALU.mult),
                         reads=[Bpa, Bsq, Bgv], writes=[Bq_n])
                K.op("dve", lambda e: e.scalar_tensor_tensor(q_n[:, 4, :], pb[:, 0:64], sq[:, 12:13], gv[:, 2, :], ALU.mult, ALU.mult),
                     reads=[Bpb, Bsq, Bgv], writes=[Bq_n])
                K.op("dve", lambda e: e.scalar_tensor_tensor(q_n[:, 5, :], pb[:, 128:192], sq[:, 13:14], gv[:, 3, :], ALU.mult, ALU.mult),
                     reads=[Bpb, Bsq, Bgv], writes=[Bq_n])
                q_r, Bq_r = P.qr.get()
                rt_, Brt_ = P.rtmp.get()
                apply_rope(q_r[:, :, :], q_n[:, :, :], cs_t[:, 0, i, :], cs_t[:, 1, i, :], 6, 8, rt_, [Bq_n, Bcs_t], [Bq_r], Brt_)
                K.op("act", lambda e: e.copy(q_r[:, :, 16:64], q_n[:, :, 16:64]), reads=[Bq_n], writes=[Bq_r])
                K.op("act", lambda e: e.copy(vs[:, i, 0:64], pb[:, 64:128]), reads=[Bpb], writes=[Bvs[i]])
                K.op("act", lambda e: e.copy(vw[:, i, 0:64], pb[:, 192:256]), reads=[Bpb], writes=[Bvw[i]])
                K.op("act", lambda e: e.copy(gts[:, i, :], pb[:, 256:268]), reads=[Bpb], writes=[Bgts[i]])
                pt, Bpt = P.psg.get()
                ptb = pt[:, :].bitcast(BF16)
                for h in range(6):
                    K.op("pe", lambda e: e.transpose(ptb[0:64, h * 128:(h + 1) * 128], q_r[:, h, :], identb[:]), reads=[Bq_r, Bidentb], writes=[Bpt])
                K.op("act", lambda e: e.copy(qT[0:64, i, :, :], ptb[0:64, 0:512].rearrange("p (h t) -> p h t", h=4)), reads=[Bpt], writes=[BqT[i]])
                K.op("dve", lambda e: e.tensor_copy(kTs[0:64, ts_], ptb[0:64, 512:640]), reads=[Bpt], writes=[BkTs[i]])
                K.op("dve", lambda e: e.tensor_copy(kTw[0:64, ts_], ptb[0:64, 640:768]), reads=[Bpt], writes=[BkTw[i]])
            IL.run(NT, 4, _mk_np, _body_np)
            st = Pool(e1, nc, "nst2", [128, 16], F32, 1)
            jk = Pool(e1, nc, "njk2", [128, 64], F32, 1)
            qn = Pool(e1, nc, "nqn2", [128, 6, 64], F32, 1)
            qr = Pool(e1, nc, "nqr2", [128, 6, 64], BF16, 1)
            rtmp = Pool(e1, nc, "nrt2", [128, 6, 16], F32, 1)
            w1 = sb(e1, "nw1", [128, 32, 128], BF16); Bw1 = Buf("nw1")
            pe16 = sb(e1, "npe16", [128, 32], BF16); Bpe16 = Buf("npe16")
            w2 = sb(e1, "nw2", [128, 2, 64], BF16); Bw2 = Buf("nw2")
            for kv in range(2):
                K.dma("pool", w1[kv * 64:kv * 64 + 64], dr["nsa_w1"][l, kv], writes=[Bw1])
                K.dma("pool", pe16[kv * 64:kv * 64 + 64, :], dr["nsa_pe"][l, kv], writes=[Bpe16])
                K.dma("pool", w2[:, kv, :], dr["nsa_w2"][l, kv], writes=[Bw2])
            hb = sb(e1, "nhb", [128, 2], F32); Bhb = Buf("nhb")
            hid = sb(e1, "nhid", [128, 2, 128], BF16); Bhid = Buf("nhid")
            for kv in range(2):
                rows = slice(kv * 64, kv * 64 + 64)
                ph, Bph = psg.get()
                pbias, Bpbias = psg.get()
                for j in range(32):
                    lw = w1[rows, j, :]
                    rhs = cmpT[rows, j:j + 16 * 126 + 1:16]
                    K.op("pe", lambda e: e.matmul(ph[:, 0:127], lw, rhs, start=(j == 0), stop=(j == 31)), reads=[Bw1, BcmpT], writes=[Bph])
                for j in range(32):
                    lw = w1[rows, j, :]
                    K.op("pe", lambda e: e.matmul(pbias[:, 0:1], lw, pe16[rows, j:j + 1], start=(j == 0), stop=(j == 31)), reads=[Bw1, Bpe16], writes=[Bpbias])
                K.op("act", lambda e: e.copy(hb[:, kv:kv + 1], pbias[:, 0:1]), reads=[Bpbias], writes=[Bhb])
                K.op("act", lambda e: e.activation(hid[:, kv, 0:127], ph[:, 0:127], AF.Gelu_apprx_tanh, bias=hb[:, kv:kv + 1]), reads=[Bph, Bhb], writes=[Bhid])
                po, Bpo = psg.get()
                K.op("pe", lambda e: e.matmul(po[0:127, 0:64], hid[:, kv, 0:127], w2[:, kv, :], start=True, stop=True), reads=[Bhid, Bw2], writes=[Bpo])
                if kv == 1:
                    K.op("act", lambda e: e.copy(vc[0:127, 0:64], po[0:127, 0:64]), reads=[Bpo], writes=[Bvc])
                else:
                    sq, Bsq = st.get()
                    j_, Bj_ = jk.get()
                    q_n, Bq_n = qn.get()
                    q_r, Bq_r = qr.get()
                    rt_, Brt_ = rtmp.get()
                    K.op("dve", lambda e: e.memset(q_n[:, 0, :], 0.0), writes=[Bq_n])
                    K.op("act", lambda e: e.activation(j_[0:127, :], po[0:127, 0:64], AF.Square, accum_out=sq[0:127, 0:1]), reads=[Bpo], writes=[Bj_, Bsq])
                    rstd_from(sq[0:127, 1:2], sq[0:127, 0:1], 64, [Bsq], [Bsq])
                    K.op("dve", lambda e: e.scalar_tensor_tensor(q_n[0:127, 0, :], po[0:127, 0:64], sq[0:127, 1:2], gv[0:127, 1, :], ALU.mult, ALU.mult),
                         reads=[Bpo, Bsq, Bgv, Bq_n], writes=[Bq_n])
                    apply_rope(q_r[:, 0:1, :], q_n[:, 0:1, :], csc[:, 0, 0, :], csc[:, 1, 0, :], 1, 8, rt_, [Bq_n, Bcsc], [Bq_r], Brt_)
                    K.op("act", lambda e: e.copy(q_r[:, 0:1, 16:64], q_n[:, 0:1, 16:64]), reads=[Bq_n], writes=[Bq_r])
                    pt, Bpt = psg.get()
                    ptb = pt[:, :].bitcast(BF16)
                    K.op("pe", lambda e: e.transpose(ptb[0:64, 0:128], q_r[:, 0, :], identb[:]), reads=[Bq_r, Bidentb], writes=[Bpt])
                    K.op("act", lambda e: e.copy(kcT[0:64, :], ptb[0:64, 0:128]), reads=[Bpt], writes=[BkcT])
            K.barrier_all()
            e1.close()
            cmask = sb(es, "ncmask", [128, T], BF16); Bcmask = Buf("ncmask")
            K.dma("pool", cmask[:], dr["c_cmpmask"][:], writes=[Bcmask])
            keep = sb(es, "nkeep", [128, NT, 32], F32); Bkeep = Buf("nkeep")
            base = sb(es, "nbase", [128, NT, 32], F32); Bbase = Buf("nbase")
            K.dma("sp", keep[:], dr["c_keep"][:], writes=[Bkeep])
            K.dma("sp", base[:], dr["c_base"][:], writes=[Bbase])
            Em = sb(es, "nEm", [128, NT, 128], BF16); BEm = Buf("nEm")
            K.op("pool", lambda e: e.memset(Em[:], 0.0), writes=[BEm])
            K.dma("pool", Em[0:32], dr["c_E"][:], writes=[BEm])
            scale = 64 ** -0.5
            def _mk_na(w):
                per = 8 // 4
                nb = per - 1
                return NS(psg=allps.sub(range(w * per, w * per + nb)), psa=allps.sub(range(w * per + nb, (w + 1) * per)), nsp=_zeroed(Pool(es, nc, "nselT", [128, 4, 128], BF16, 1)), pexp=Pool(es, nc, "npx", [128, 512], BF16, 3), sst=Pool(es, nc, "nsst", [128, 80], F32, 1), impp=Pool(es, nc, "nimp", [128, 32], F32, 1), yo=Pool(es, nc, "nyo", [128, 256], F32, 1), yst=Pool(es, nc, "nyst", [128, 8], F32, 1), yb=Pool(es, nc, "nyb", [128, 256], BF16, 1))
            def _body_na(qt, P):
                qs = slice(qt * 128, (qt + 1) * 128)
                qrhs = qT[:, qt].rearrange("p h t -> p (h t)")
                y_o, By_o = P.yo.get()
                yst_, Byst = P.yst.get()
                st_, Bst_ = P.sst.get()
                imp, Bimp = P.impp.get()
                K.op("act", lambda e: e.activation(gts[:, qt, :], gts[:, qt, :], AF.Exp, scale=-1.0), reads=[Bgts[qt]], writes=[Bgts[qt]])
                K.op("dve", lambda e: e.tensor_scalar(gts[:, qt, :], gts[:, qt, :], 1.0, None, ALU.add), reads=[Bgts[qt]], writes=[Bgts[qt]])
                K.op("dve", lambda e: e.reciprocal(gts[:, qt, :], gts[:, qt, :]), reads=[Bgts[qt]], writes=[Bgts[qt]])
                sp_, Bsp = P.psg.get()
                K.op("pe", lambda e: e.matmul(sp_[0:127, :], kcT[:, 0:127], qrhs, start=True, stop=True), reads=[BkcT, BqT[qt]], writes=[Bsp])
                pe_, Bpe = P.pexp.get()
                K.op("act", lambda e: e.activation(pe_[0:127, :], sp_[0:127, :], AF.Exp, scale=scale), reads=[Bsp], writes=[Bpe])
                K.op("pool", lambda e: e.tensor_tensor(pe_[0:127, :].rearrange("p (h t) -> p h t", h=4), pe_[0:127, :].rearrange("p (h t) -> p h t", h=4),
                                                       cmask[0:127, qs].unsqueeze(1).to_broadcast([127, 4, 128]), ALU.mult),
                     reads=[Bpe, Bcmask], writes=[Bpe])
                for h in range(4):
                    acc, Bacc = P.psa.get()
                    K.op("pe", lambda e: e.matmul(acc[:, 0:97], pe_[0:127, h * 128:(h + 1) * 128], vc[0:127, :], start=True, stop=True),
                         reads=[Bpe, Bvc], writes=[Bacc])
                    K.op("dve", lambda e: e.tensor_scalar(st_[:, h:h + 1], acc[:, 64:65], 1e-30, None, ALU.add), reads=[Bacc], writes=[Bst_])
                    K.op("dve", lambda e: e.reciprocal(st_[:, h:h + 1], st_[:, h:h + 1]), reads=[Bst_], writes=[Bst_])
                    if h == 0:
                        K.op("dve", lambda e: e.tensor_scalar(imp[:], acc[:, 65:97], st_[:, h:h + 1], None, ALU.mult), reads=[Bacc, Bst_], writes=[Bimp])
                    else:
                        K.op("dve", lambda e: e.scalar_tensor_tensor(imp[:], acc[:, 65:97], st_[:, h:h + 1], imp[:], ALU.mult, ALU.add),
                             reads=[Bacc, Bst_, Bimp], writes=[Bimp])
                    K.op("dve", lambda e: e.tensor_tensor(st_[:, 4 + h:5 + h], st_[:, h:h + 1], gts[:, qt, 3 * h:3 * h + 1], ALU.mult), reads=[Bst_, Bgts[qt]], writes=[Bst_])
                    K.op("dve", lambda e: e.tensor_scalar(y_o[:, h * 64:(h + 1) * 64], acc[:, 0:64], st_[:, 4 + h:5 + h], None, ALU.mult),
                         reads=[Bacc, Bst_], writes=[By_o])
                K.op("dve", lambda e: e.tensor_tensor(imp[:], imp[:], keep[:, qt, :], ALU.mult), reads=[Bimp, Bkeep], writes=[Bimp])
                K.op("dve", lambda e: e.tensor_tensor(imp[:], imp[:], base[:, qt, :], ALU.add), reads=[Bimp, Bbase], writes=[Bimp])
                K.op("dve", lambda e: e.max(st_[:, 8:16], imp[:]), reads=[Bimp], writes=[Bst_])
                K.op("dve", lambda e: e.tensor_scalar(st_[:, 16:48], imp[:], st_[:, 12:13], -1.0, ALU.is_ge, ALU.add), reads=[Bimp, Bst_], writes=[Bst_])
                pt, Bpt = P.psg.get()
                K.op("pe", lambda e: e.transpose(pt[0:32, 0:128], st_[:, 16:48], ident[:]), reads=[Bst_, Bident], writes=[Bpt])
                nselT, BnselT = P.nsp.get()
                K.op("act", lambda e: e.copy(nselT[0:32, :, :], pt[0:32, 0:128].unsqueeze(1).to_broadcast([32, 4, 128])), reads=[Bpt], writes=[BnselT])
                for br_ in range(2):
                    kTb = kTs if br_ == 0 else kTw
                    BkTb = BkTs if br_ == 0 else BkTw
                    vb = vs if br_ == 0 else vw
                    Bvb = Bvs if br_ == 0 else Bvw
                    kts = list(range(0, qt + 1)) if br_ == 0 else list(range(max(0, qt - 4), qt + 1))
                    acc, Bacc = P.psa.get()
                    K.op("dve", lambda e: e.memset(acc[:, 0:260], 0.0), writes=[Bacc])
                    for kt in kts:
                        sp_, Bsp = P.psg.get()
                        K.op("pe", lambda e: e.matmul(sp_[:, :], kTb[:, kt * 128:(kt + 1) * 128], qrhs, start=True, stop=(br_ == 1)),
                             reads=[BkTb[kt], BqT[qt]], writes=[Bsp])
                        if br_ == 0:
                            K.op("pe", lambda e: e.matmul(sp_[:, :], Em[:, kt, :], nselT[:, :, :].rearrange("p h t -> p (h t)"), start=False, stop=True),
                                 reads=[BEm, BnselT], writes=[Bsp])
                        pe_, Bpe = P.pexp.get()
                        K.op("act", lambda e: e.activation(pe_[:, :], sp_[:, :], AF.Exp, scale=scale), reads=[Bsp], writes=[Bpe])
                        if kt == qt:
                            K.op("pool", lambda e: e.tensor_tensor(pe_[:, :], pe_[:, :], tri4[:].rearrange("p h t -> p (h t)"), ALU.mult),
                                 reads=[Bpe, Btri], writes=[Bpe])
                        elif br_ == 1 and kt == qt - 4:
                            K.op("pool", lambda e: e.tensor_tensor(pe_[:, :], pe_[:, :], anti4[:].rearrange("p h t -> p (h t)"), ALU.mult),
                                 reads=[Bpe, Banti], writes=[Bpe])
                        for h in range(4):
                            co = h * 65
                            K.op("pe", lambda e: e.matmul(acc[:, co:co + 65], pe_[:, h * 128:(h + 1) * 128], vb[:, kt, :], start=False, stop=(kt == kts[-1]),
                                                          skip_group_check=True),
                                 reads=[Bpe, Bvb[kt]], writes=[Bacc])
                    for h in range(4):
                        co = h * 65
                        cc_ = 50 + 4 * br_ + h
                        K.op("dve", lambda e: e.reciprocal(st_[:, cc_:cc_ + 1], acc[:, co + 64:co + 65]), reads=[Bacc], writes=[Bst_])
                        K.op("dve", lambda e: e.tensor_tensor(st_[:, cc_:cc_ + 1], st_[:, cc_:cc_ + 1], gts[:, qt, 3 * h + 1 + br_:3 * h + 2 + br_], ALU.mult),
                             reads=[Bst_, Bgts[qt]], writes=[Bst_])
                        K.op("dve", lambda e: e.scalar_tensor_tensor(y_o[:, h * 64:(h + 1) * 64], acc[:, co:co + 64], st_[:, cc_:cc_ + 1], y_o[:, h * 64:(h + 1) * 64],
                                                                     ALU.mult, ALU.add),
                             reads=[Bacc, Bst_, By_o], writes=[By_o])
                tm_groupnorm(P.psg, y_o, By_o, yst_, Byst, P.yb, 3, l, qt)
            IL.run(NT, 4, _mk_na, _body_na)
            dbg_dump(3)
            wout_partial(es, l, 3)
            phase_end(es)

        combT = sb(top, "combT", [128, T], BF16)
        BcombT = [Buf(f"combT{c}") for c in range(4)]
        K.op("pool", lambda e: e.memset(combT[:], 0.0), writes=BcombT)
        gon = sb(top, "gon", [128, 4, 256], F32); Bgon = Buf("gon")
        gcolt = sb(top, "gcolt", [128, 8], F32); Bgcol = Buf("gcolt")

        io_box = {"final": False, "stored": set(), "loaded": set()}
        for s in range(n_seq):
            for i in range(NT):
                if (s, i) not in io_box["loaded"]:
                    K.dma("sp", x_sb[:, i, :], dr["x"][s, i * 128:(i + 1) * 128, :], writes=Bx[i])
            K.dma("sp", posi[:], dr["pos"][s].rearrange("(n p) -> p n", p=128), writes=[Bposi], allow_slow_non_contiguous=True)
            K.op("dve", lambda e: e.tensor_copy(posf[:], posi[:]), reads=[Bposi], writes=[Bposf])
            for l in range(depth):
                for m in range(4):
                    K.dma("sp", gon[:, m, :], dr["out_norm"][l, m:m + 1, :].to_broadcast([128, 256]), writes=[Bgon])
                K.dma("sp", gcolt[:], dr["out_norm_t"][l], writes=[Bgcol])
                l_box[0] = l
                io_box["final"] = (l == depth - 1) and ("moe" in phases)
                norm_phase(s, l, "mix_norm", False)
                if "mla" in phases:
                    mla_phase(s, l)
                if "lru" in phases:
                    lru_phase(s, l)
                if "s5" in phases:
                    s5_phase(s, l)
                if "nsa" in phases:
                    nsa_phase(s, l)
                if "moe" in phases:
                    moe_phase(s, l)
            if dbg:
                K.dma("pool", dbg_d["xnT"][:], xnT[:], reads=[b for t_ in BxnT for b in t_], writes=[Bout])
                for i in range(NT):
                    K.dma("sp", dbg_d["x"][i * 128:(i + 1) * 128, :], x_sb[:, i, :], reads=Bx[i], writes=[Bout])
            for i in range(NT):
                if (s, i) not in io_box["stored"]:
                    K.dma("sp", out_d[s, i * 128:(i + 1) * 128, :], x_sb[:, i, :], reads=Bx[i], writes=[Bout])
            K.barrier_all()
        K.barrier_all()
        K.close()
    return nc, K


def prep_weights(inp):
    f = np.float32
    L = DEPTH
    w = {}
    for k in ("mix_norm", "ffn_norm", "w_in", "w_out", "mla_g_cq", "mla_g_ckv", "mla_w_uq", "mla_w_ukv", "mla_g_q", "mla_g_k",
              "s5_w_glu", "nsa_g_q", "nsa_g_k", "out_norm", "moe_w_gate", "moe_w_up", "moe_w_down"):
        w[k] = np.ascontiguousarray(inp[k], dtype=f)
    def pc(v):
        return np.ascontiguousarray(np.asarray(v, f).reshape(L, 2, 128).transpose(0, 2, 1))
    w["lru_cw"] = np.ascontiguousarray(np.asarray(inp["lru_conv_w"], f).reshape(L, 4, 2, 128).transpose(0, 3, 2, 1))
    w["lru_vec"] = np.ascontiguousarray(np.stack([pc(inp["lru_conv_b"]), pc(np.asarray(inp["lru_b_a"]).reshape(L, 256)),
                                                  pc(np.asarray(inp["lru_b_i"]).reshape(L, 256)), pc(inp["lru_lambda"]),
                                                  np.zeros((L, 128, 2), f)], axis=2))
    for nm, src in (("lru_wa", "lru_w_a"), ("lru_wi", "lru_w_i")):
        a = np.zeros((L, 2, 128, 128), f)
        W = np.asarray(inp[src], f)
        for c in range(2):
            for hh in range(2):
                a[:, c, hh * 64:(hh + 1) * 64, hh * 64:(hh + 1) * 64] = W[:, 2 * c + hh]
        w[nm] = a
    def st(v):
        return np.asarray(v, f).reshape(L, 8, 128).transpose(0, 2, 1)
    ldt = np.repeat(np.asarray(inp["s5_log_dt"], f)[:, :, None], 64, axis=2)
    w["s5_par"] = np.ascontiguousarray(np.stack([st(inp["s5_a_re"]), st(inp["s5_a_im"]), st(ldt)], axis=2))
    def stb(v):
        return np.asarray(v, f).reshape(L, 8, 128, 16).transpose(0, 2, 1, 3)
    w["s5_b"] = np.ascontiguousarray(np.stack([stb(inp["s5_b_re"]), stb(inp["s5_b_im"])], axis=2))
    cpad = np.zeros((L, 2, 8, 128, 128), f)
    for ri, nm in enumerate(("s5_c_re", "s5_c_im")):
        Cm = np.asarray(inp[nm], f)
        for g in range(16):
            j = g // 2
            rows = slice((g % 2) * 64, (g % 2) * 64 + 64)
            cols = slice((16 * g) % 128, (16 * g) % 128 + 16)
            cpad[:, ri, j, rows, cols] = Cm[:, g].transpose(0, 2, 1)
    w["s5_c"] = cpad
    w["s5_vec"] = np.ascontiguousarray(np.stack([pc(inp["s5_d"]), pc(inp["s5_b_glu"]), np.zeros((L, 128, 2), f)], axis=2))
    w["nsa_pe"] = np.ascontiguousarray(np.stack([np.asarray(inp["nsa_pe_k"], f).transpose(0, 2, 1),
                                                 np.asarray(inp["nsa_pe_v"], f).transpose(0, 2, 1)], axis=1))
    w["nsa_w1"] = np.ascontiguousarray(np.stack([np.asarray(inp["nsa_w1_k"], f).reshape(L, 32, 64, 128).transpose(0, 2, 1, 3),
                                                 np.asarray(inp["nsa_w1_v"], f).reshape(L, 32, 64, 128).transpose(0, 2, 1, 3)], axis=1))
    w["nsa_w2"] = np.ascontiguousarray(np.stack([np.asarray(inp["nsa_w2_k"], f), np.asarray(inp["nsa_w2_v"], f)], axis=1))
    w["out_norm_t"] = np.ascontiguousarray(np.asarray(inp["out_norm"], f).reshape(L, 8, 128).transpose(0, 2, 1))
    w["moe_wr"] = np.ascontiguousarray(np.concatenate([np.asarray(inp["moe_w_rg"], f), np.asarray(inp["moe_w_re"], f)], axis=2))
    w["moe_br"] = np.ascontiguousarray(np.concatenate([np.asarray(inp["moe_b_rg"], f), np.asarray(inp["moe_b_re"], f)], axis=1))
    w["c_ident"] = np.eye(128, dtype=f)
    kk = np.arange(128)[:, None]; qq = np.arange(128)[None, :]
    w["c_tri"] = (qq >= kk).astype(f)
    w["c_anti"] = (kk > qq).astype(f)
    cc = np.arange(128)[:, None]; tq = np.arange(T)[None, :]
    w["c_cmpmask"] = ((16 * cc + 31 <= tq) & (cc < 127)).astype(f)
    csn = np.arange(127) * 16; ssn = np.arange(32) * 64
    ov = np.clip(np.minimum(csn[:, None] + 32, ssn[None, :] + 64) - np.maximum(csn[:, None], ssn[None, :]), 0, None) / 16.0
    ovp = np.zeros((128, 32), f); ovp[:127] = ov
    w["c_ov"] = ovp
    tpos = np.arange(T); cur = tpos // 64; sbk = np.arange(32)
    forced = (sbk[None, :] == 0) | (sbk[None, :] == cur[:, None]) | (sbk[None, :] == cur[:, None] - 1)
    future = sbk[None, :] > cur[:, None]
    keep = (~forced & ~future).astype(f)
    base = np.where(future, -1e30, np.where(forced, 1e30, 0.0)).astype(f)
    w["c_keep"] = np.ascontiguousarray(keep.reshape(NT, 128, 32).transpose(1, 0, 2))
    w["c_base"] = np.ascontiguousarray(base.reshape(NT, 128, 32).transpose(1, 0, 2))
    E = np.zeros((32, NT, 128), f)
    for kt in range(NT):
        for m_ in range(128):
            E[2 * kt + m_ // 64, kt, m_] = BIGNEG
    w["c_E"] = E
    w["c_inv_mla"] = np.tile((500000.0 ** (-np.arange(16, dtype=np.float64) * 2.0 / 32)).astype(f)[None, :], (128, 1))
    w["c_inv_nsa"] = np.tile((500000.0 ** (-np.arange(8, dtype=np.float64) * 2.0 / 16)).astype(f)[None, :], (128, 1))
    sE = np.zeros((32, 16, 128), f)
    for e_ in range(16):
        sE[e_, e_, :] = 1.0
        sE[16 + e_, e_, :] = 1.0
    w["c_selE"] = sE
    for k, shp in W_SPECS.items():
        assert list(w[k].shape) == shp, (k, w[k].shape, shp)
    return w


_CACHE = {}


def kernel(**inputs):
    n_cores = 8
    x = np.ascontiguousarray(inputs["x"], dtype=np.float32)
    pos = np.ascontiguousarray(inputs["positions"], dtype=np.int32)
    w = prep_weights(inputs)
    if "nc" not in _CACHE:
        _CACHE["nc"] = build_program(n_seq=2)[0]
    nc = _CACHE["nc"]
    in_maps = []
    for c in range(n_cores):
        m = dict(w)
        m["x"] = x[2 * c:2 * c + 2]
        m["pos"] = pos[2 * c:2 * c + 2]
        in_maps.append(m)
    res = run_bass_kernel_spmd(nc, in_maps, core_ids=list(range(n_cores)))
    return np.concatenate([r["out"] for r in res.results], axis=0)
```

```python
import numpy as np
from contextlib import ExitStack
import concourse.bass as bass
import concourse.mybir as mybir
from concourse.bass_utils import run_bass_kernel_spmd

F32 = mybir.dt.float32
BF16 = mybir.dt.bfloat16
I32 = mybir.dt.int32
AF = mybir.ActivationFunctionType
ALU = mybir.AluOpType
AX = mybir.AxisListType

T = 2048
NT = 16
D = 1024
DC = 8
DEPTH = 2
EPS = 1e-6
TWO_PI = 6.283185307179586
BIGNEG = 30000.0
EPOCH = 30000


class Buf:
    __slots__ = ("name", "last_w", "readers", "excl")

    def __init__(self, name, excl=False):
        self.name = name
        self.last_w = None
        self.readers = []
        self.excl = excl


class Prod:
    def __init__(self, K, key, step):
        self.K = K
        self.key = key
        self.step = step
        self.count = 0
        self.sems = []

    def sem_for(self, idx):
        ep = idx // EPOCH
        while len(self.sems) <= ep:
            self.sems.append(self.K.new_sem(f"{self.key}_{len(self.sems)}"))
        return self.sems[ep], ((idx % EPOCH) + 1) * self.step, ep


class _PEProxy:
    def __init__(self, real):
        self.real = real
        self.last_stop = True

    def matmul(self, *a, **k):
        self.last_stop = bool(k.get("stop", True))
        return self.real.matmul(*a, **k)

    def transpose(self, *a, **k):
        self.last_stop = True
        return self.real.transpose(*a, **k)


class Kern:
    def __init__(self, nc, n_dma_lanes=16):
        self.nc = nc
        self._sem_ctx = []
        self.prods = {}
        self.engs = {"pe": nc.tensor, "act": nc.scalar, "dve": nc.vector, "pool": nc.gpsimd, "sp": nc.sync}
        for k in self.engs:
            self.prods[k] = Prod(self, k, 1)
        self.lanes = {}
        self.lane_rr = {}
        for q in ("sp", "pool", "act"):
            self.lanes[q] = []
            self.lane_rr[q] = 0
            for i in range(n_dma_lanes // 2):
                p = Prod(self, f"dma_{q}{i}", 16)
                self.prods[p.key] = p
                self.lanes[q].append(p)
        self._pe_proxy = _PEProxy(nc.tensor)
        self.bar_scratch = None
        self._switch = None
        self.waited = {}
        self.n_inst = 0
        self.n_wait = 0

    def new_sem(self, name):
        ctx = self.nc.semaphore(name)
        s = ctx.__enter__()
        self._sem_ctx.append(ctx)
        return s

    def close(self):
        for c in reversed(self._sem_ctx):
            c.__exit__(None, None, None)
        self._sem_ctx = []

    def _deps(self, me_key, reads, writes):
        deps = set()
        for b in reads:
            if b.last_w is not None:
                deps.add(b.last_w)
            if b.excl:
                for r in b.readers:
                    if r[0] != me_key:
                        deps.add(r)
        for b in writes:
            if b.last_w is not None:
                deps.add(b.last_w)
            deps.update(b.readers)
        return deps

    def _emit_waits(self, engname, deps, self_key=None, attach=False):
        eng = self.engs[engname]
        need = {}
        for (pk, idx) in deps:
            if pk == self_key and pk == "pe":
                continue
            sem, val, ep = self.prods[pk].sem_for(idx)
            k = (pk, ep)
            if need.get(k, (None, 0))[1] < val:
                need[k] = (sem, val)
        pend = []
        for (pk, ep), (sem, val) in need.items():
            wk = (engname, pk, ep)
            if self.waited.get(wk, 0) >= val:
                continue
            pend.append((sem, val))
            self.waited[wk] = val
        last = pend.pop() if (attach and pend) else None
        for (sem, val) in pend:
            eng.wait_ge(sem, val)
            self.n_wait += 1
        return last

    def _record(self, me, reads, writes):
        for b in reads:
            b.readers.append(me)
            if len(b.readers) > 48:
                b.readers = b.readers[-48:]
        for b in writes:
            b.last_w = me
            b.readers = []

    def op(self, engname, fn, reads=(), writes=()):
        prod = self.prods[engname]
        deps = self._deps(engname, reads, writes)
        last = self._emit_waits(engname, deps, self_key=engname, attach=True)
        if engname == "pe":
            self._pe_proxy.last_stop = True
            ins = fn(self._pe_proxy)
            inc = self._pe_proxy.last_stop
        else:
            ins = fn(self.engs[engname])
            inc = True
        if last is not None:
            ins._wait_ge(last[0], last[1])
        idx = prod.count
        self.n_inst += 1
        if inc:
            sem, val, ep = prod.sem_for(idx)
            ins.then_inc(sem, 1)
            prod.count += 1
        self._record((engname, idx), reads, writes)
        if self._switch is not None:
            self._switch()
        return ins

    def dma(self, qname, out, in_, reads=(), writes=(), **kw):
        lane = self.lanes[qname][self.lane_rr[qname]]
        self.lane_rr[qname] = (self.lane_rr[qname] + 1) % len(self.lanes[qname])
        deps = self._deps(lane.key, reads, writes)
        if lane.count > 0:
            deps.add((lane.key, lane.count - 1))
        last = self._emit_waits(qname, deps, attach=True)
        idx = lane.count
        sem, val, ep = lane.sem_for(idx)
        ins = self.engs[qname].dma_start(out=out, in_=in_, **kw)
        if last is not None:
            ins._wait_ge(last[0], last[1])
        ins.then_inc(sem, 16)
        lane.count += 1
        self.n_inst += 1
        self._record((lane.key, idx), reads, writes)
        if self._switch is not None:
            self._switch()
        return ins

    def barrier_all(self):
        deps = set()
        for pk, p in self.prods.items():
            if p.count > 0:
                deps.add((pk, p.count - 1))
        if self.bar_scratch is None:
            for e in self.engs:
                self._emit_waits(e, deps)
            return
        self._emit_waits("pool", deps)
        snap = {k: v for k, v in self.waited.items() if k[0] == "pool"}
        sw = self._switch
        self._switch = None
        scr = self.bar_scratch
        self.op("pool", lambda e: e.memset(scr, 0.0))
        self._switch = sw
        idx = self.prods["pool"].count - 1
        for e in self.engs:
            if e == "pool":
                continue
            self._emit_waits(e, {("pool", idx)})
            for (_, pk, ep), val in snap.items():
                wk = (e, pk, ep)
                if self.waited.get(wk, 0) < val:
                    self.waited[wk] = val


class NS:
    def __init__(self, **kw):
        self.__dict__.update(kw)


class Interleaver:
    def __init__(self, K):
        self.K = K

    def run(self, n, W, mk, body):
        import threading
        K = self.K
        W = max(1, min(W, n))
        ctxs = [mk(w) for w in range(W)]
        if W == 1:
            for i in range(n):
                body(i, ctxs[0])
            return
        sems = [threading.Semaphore(0) for _ in range(W)]
        alive = [True] * W
        done = threading.Event()
        err = []
        state = {"cur": 0}

        def next_live(w):
            for d in range(1, W + 1):
                v = (w + d) % W
                if alive[v]:
                    return v
            return None

        def switch():
            w = state["cur"]
            v = next_live(w)
            if v is None or v == w:
                return
            state["cur"] = v
            sems[v].release()
            sems[w].acquire()

        def worker(w):
            sems[w].acquire()
            try:
                if not err:
                    for i in range(w, n, W):
                        body(i, ctxs[w])
                        if err:
                            break
            except BaseException as e:
                err.append(e)
            alive[w] = False
            v = next_live(w)
            if v is None:
                done.set()
            else:
                state["cur"] = v
                sems[v].release()

        ths = [threading.Thread(target=worker, args=(w,)) for w in range(W)]
        for t in ths:
            t.start()
        K._switch = switch
        state["cur"] = 0
        sems[0].release()
        done.wait()
        K._switch = None
        for t in ths:
            t.join()
        if err:
            raise err[0]


class Pool:
    _uid = [0]

    def __init__(self, es, nc, name, shape, dtype, n, psum=False):
        self.items = []
        Pool._uid[0] += 1
        name = f"{name}_u{Pool._uid[0]}_"
        for i in range(n):
            if psum:
                t = es.enter_context(nc.psum_tensor(f"{name}{i}", shape, dtype))
            else:
                t = es.enter_context(nc.sbuf_tensor(f"{name}{i}", shape, dtype))
            self.items.append((t, Buf(f"{name}{i}", excl=psum)))
        self.i = 0

    def get(self):
        it = self.items[self.i]
        self.i = (self.i + 1) % len(self.items)
        return it

    def sub(self, idxs):
        p = Pool.__new__(Pool)
        p.items = [self.items[k] for k in idxs]
        p.i = 0
        return p


W_SPECS = {
    "mix_norm": [DEPTH, D], "ffn_norm": [DEPTH, D], "w_in": [DEPTH, D, 1772], "w_out": [DEPTH, D, D],
    "mla_g_cq": [DEPTH, 192], "mla_g_ckv": [DEPTH, 128], "mla_w_uq": [DEPTH, 192, 384],
    "mla_w_ukv": [DEPTH, 128, 512], "mla_g_q": [DEPTH, 96], "mla_g_k": [DEPTH, 96],
    "lru_cw": [DEPTH, 128, 2, 4], "lru_vec": [DEPTH, 128, 5, 2], "lru_wa": [DEPTH, 2, 128, 128],
    "lru_wi": [DEPTH, 2, 128, 128],
    "s5_par": [DEPTH, 128, 3, 8], "s5_b": [DEPTH, 128, 2, 8, 16], "s5_c": [DEPTH, 2, 8, 128, 128],
    "s5_vec": [DEPTH, 128, 3, 2], "s5_w_glu": [DEPTH, 256, 256],
    "nsa_g_q": [DEPTH, 64], "nsa_g_k": [DEPTH, 3, 64], "nsa_pe": [DEPTH, 2, 64, 32],
    "nsa_w1": [DEPTH, 2, 64, 32, 128], "nsa_w2": [DEPTH, 2, 128, 64],
    "out_norm": [DEPTH, 4, 256], "out_norm_t": [DEPTH, 128, 8],
    "moe_wr": [DEPTH, D, 20], "moe_br": [DEPTH, 20],
    "moe_w_gate": [DEPTH, 16, D, 256], "moe_w_up": [DEPTH, 16, D, 256], "moe_w_down": [DEPTH, 16, 256, D],
    "c_ident": [128, 128], "c_tri": [128, 128], "c_anti": [128, 128], "c_cmpmask": [128, T],
    "c_ov": [128, 32], "c_keep": [128, NT, 32], "c_base": [128, NT, 32], "c_E": [32, NT, 128],
    "c_inv_mla": [128, 16], "c_inv_nsa": [128, 8], "c_selE": [32, 16, 128],
}


def build_program(n_seq=2, depth=DEPTH, dbg=None, phases=("mla", "lru", "s5", "nsa", "moe")):
    nc = bass.Bass("TRN2", target_bir_lowering=False)
    dr = {}
    dr["x"] = nc.dram_tensor("x", [n_seq, T, D], F32, kind="ExternalInput").ap()
    dr["pos"] = nc.dram_tensor("pos", [n_seq, T], I32, kind="ExternalInput").ap()
    for k, shp in W_SPECS.items():
        dr[k] = nc.dram_tensor(k, shp, F32, kind="ExternalInput").ap()
    out_d = nc.dram_tensor("out", [n_seq, T, D], F32, kind="ExternalOutput").ap()
    dbg_d = {}
    if dbg:
        dbg_d["ymT"] = nc.dram_tensor("dbg_ymT", [4, 128, 2, T], F32, kind="ExternalOutput").ap()
        dbg_d["x"] = nc.dram_tensor("dbg_x", [T, D], F32, kind="ExternalOutput").ap()
        dbg_d["xnT"] = nc.dram_tensor("dbg_xnT", [128, DC, T], F32, kind="ExternalOutput").ap()

    K = Kern(nc)
    IL = Interleaver(K)
    Bout = Buf("out")
    with ExitStack() as top:
        def sb(es, name, shape, dt):
            Pool._uid[0] += 1
            return es.enter_context(nc.sbuf_tensor(f"{name}_u{Pool._uid[0]}", shape, dt))

        x_sb = sb(top, "x_sb", [128, NT, D], F32)
        Bx = [[Buf(f"x{i}_{h}") for h in range(2)] for i in range(NT)]
        xnT = sb(top, "xnT", [128, DC, T], BF16)
        BxnT = [[Buf(f"xnT{i}_{h}") for h in range(2)] for i in range(NT)]
        ymT_box = {}
        l_box = [0]

        def xnT_bufs(ch):
            return [BxnT[i][h] for i in range(ch * 4, ch * 4 + 4) for h in range(2)]

        ident = sb(top, "ident", [128, 128], F32); Bident = Buf("ident")
        identb = sb(top, "identb", [128, 128], BF16); Bidentb = Buf("identb")
        ones16 = sb(top, "ones16", [128, 128], BF16); Bones = Buf("ones16")
        tri4 = sb(top, "tri4", [128, 4, 128], BF16); Btri = Buf("tri4")
        anti4 = sb(top, "anti4", [128, 4, 128], BF16); Banti = Buf("anti4")
        posf = sb(top, "posf", [128, NT], F32); Bposf = Buf("posf")
        posi = sb(top, "posi", [128, NT], I32); Bposi = Buf("posi")
        gain = sb(top, "gain", [128, D], F32); Bgain = Buf("gain")
        ss = sb(top, "ss", [128, NT], F32); Bss = Buf("ss")
        rs = sb(top, "rs", [128, NT], F32); Brs = Buf("rs")
        epsb = sb(top, "epsb", [128, 1], F32); Beps = Buf("epsb")
        barscr = sb(top, "barscr", [128, 4], F32)
        K.bar_scratch = barscr[:, 0:1]

        psg = Pool(top, nc, "psg", [128, 512], F32, 6, psum=True)
        psa = Pool(top, nc, "psa", [128, 512], F32, 2, psum=True)
        allps = psg.sub(range(6))
        allps.items = psg.items + psa.items

        K.dma("sp", ident[:], dr["c_ident"][:], writes=[Bident])
        K.op("act", lambda e: e.copy(identb[:], ident[:]), reads=[Bident], writes=[Bidentb])
        K.op("dve", lambda e: e.memset(ones16[:], 1.0), writes=[Bones])
        K.op("dve", lambda e: e.memset(epsb[:], EPS), writes=[Beps])
        for h in range(4):
            K.dma("pool", tri4[:, h, :], dr["c_tri"][:], writes=[Btri])
            K.dma("pool", anti4[:, h, :], dr["c_anti"][:], writes=[Banti])

        def _zeroed(pool):
            for (t_, b_) in pool.items:
                K.op("pool", lambda e: e.memset(t_[:], 0.0), writes=[b_])
            return pool

        def phase_end(es):
            K.barrier_all()
            es.close()

        def rstd_from(out_ap, in_ap, n_feat, reads, writes, eng_tmp=None):
            K.op("act", lambda e: e.activation(out_ap, in_ap, AF.Sqrt, bias=epsb[0:out_ap.shape[0], 0:1], scale=1.0 / n_feat),
                 reads=list(reads) + [Beps], writes=writes)
            K.op("dve", lambda e: e.reciprocal(out_ap, out_ap), reads=writes, writes=writes)

        def norm_phase(s, l, which, router):
            es = ExitStack()
            tmpA = Pool(es, nc, "nrmA", [128, D], BF16, 2)
            K.dma("sp", gain[:], dr[which][l:l + 1, :].to_broadcast([128, D]), writes=[Bgain])
            if router:
                wr = sb(es, "wr", [128, DC, 20], F32); Bwr = Buf("wr")
                br = sb(es, "br", [128, 20], F32); Bbr = Buf("br")
                K.dma("sp", wr[:], dr["moe_wr"][l].rearrange("(c p) n -> p c n", p=128), writes=[Bwr])
                K.dma("sp", br[:], dr["moe_br"][l:l + 1, :].to_broadcast([128, 20]), writes=[Bbr])
            for i in range(NT):
                junk, Bj = tmpA.get()
                K.op("act", lambda e: e.activation(junk[:], x_sb[:, i, :], AF.Square, accum_out=ss[:, i:i + 1]),
                     reads=Bx[i], writes=[Bj, Bss])
            rstd_from(rs[:, :], ss[:, :], D, [Bss], [Brs])
            def _mk_nr(w):
                per = 8 // 2
                d_ = dict(psg=allps.sub(range(w * per, w * per + per)), tmpA=Pool(es, nc, "nrmX", [128, D], F32, 1))
                if router:
                    d_["xT32"] = Pool(es, nc, "xT32", [128, DC, 128], F32, 1)
                    d_["rt"] = Pool(es, nc, "rt", [128, 96], F32, 2)
                else:
                    d_["xb"] = Pool(es, nc, "nrmB", [128, D], BF16, 1)
                return NS(**d_)

            def _body_nr(i, P):
                if not router:
                    xb, Bxb = P.xb.get()
                    K.op("dve", lambda e: e.scalar_tensor_tensor(xb[:], x_sb[:, i, :], rs[:, i:i + 1], gain[:], ALU.mult, ALU.mult),
                         reads=Bx[i] + [Brs, Bgain], writes=[Bxb])
                    pb, Bpb = P.psg.get()
                    pbb = pb[:, :].bitcast(BF16)
                    for c in range(DC):
                        K.op("pe", lambda e: e.transpose(pbb[:, c * 128:(c + 1) * 128], xb[:, c * 128:(c + 1) * 128], identb[:]),
                             reads=[Bxb, Bidentb], writes=[Bpb])
                    K.op("act", lambda e: e.copy(xnT[:, :, i * 128:(i + 1) * 128], pbb[:, :].rearrange("p (c t) -> p c t", c=DC)),
                         reads=[Bpb], writes=[BxnT[i][0], BxnT[i][1]])
                    return
                xn, Bxn = P.tmpA.get()
                K.op("dve", lambda e: e.scalar_tensor_tensor(xn[:], x_sb[:, i, :], rs[:, i:i + 1], gain[:], ALU.mult, ALU.mult),
                     reads=Bx[i] + [Brs, Bgain], writes=[Bxn])
                if router:
                    xt, Bxt = P.xT32.get()
                for half in range(2):
                    pb, Bpb = P.psg.get()
                    for cc in range(4):
                        c = half * 4 + cc
                        K.op("pe", lambda e: e.transpose(pb[:, cc * 128:(cc + 1) * 128], xn[:, c * 128:(c + 1) * 128], ident[:]),
                             reads=[Bxn, Bident], writes=[Bpb])
                    src = pb[:, :].rearrange("p (c t) -> p c t", c=4)
                    K.op("act", lambda e: e.copy(xnT[:, half * 4:half * 4 + 4, i * 128:(i + 1) * 128], src),
                         reads=[Bpb], writes=[BxnT[i][half]])
                    if router:
                        K.op("dve", lambda e: e.tensor_copy(xt[:, half * 4:half * 4 + 4, :], src), reads=[Bpb], writes=[Bxt])
                if router:
                    lg, Blg = P.psg.get()
                    for c in range(DC):
                        K.op("pe", lambda e: e.matmul(lg[:, 0:20], xt[:, c, :], wr[:, c, :], start=(c == 0), stop=(c == DC - 1)),
                             reads=[Bxt, Bwr], writes=[Blg])
                    r, Br_ = P.rt.get()
                    R = [Br_]
                    Lg = r[:, 0:20]; m = r[:, 20:21]; nm = r[:, 21:22]; e4 = r[:, 22:26]; se = r[:, 26:27]
                    oh = r[:, 27:31]; pen = r[:, 31:35]; lem = r[:, 35:51]; top8 = r[:, 51:59]; sel = r[:, 59:75]
                    nv1 = r[:, 75:76]; den = r[:, 76:77]; fac = r[:, 77:78]
                    r2, Br2 = P.rt.get()
                    ew = r2[:, 0:16]; sw = r2[:, 16:32]; comb = r2[:, 32:48]
                    R2 = [Br2]
                    K.op("dve", lambda e: e.tensor_tensor(Lg, lg[:, 0:20], br[:], ALU.add), reads=[Blg, Bbr], writes=R)
                    K.op("dve", lambda e: e.tensor_reduce(m, r[:, 0:4], AX.X, ALU.max), reads=R, writes=R)
                    K.op("dve", lambda e: e.tensor_scalar(nm, m, -1.0, None, ALU.mult), reads=R, writes=R)
                    K.op("act", lambda e: e.activation(e4, r[:, 0:4], AF.Exp, bias=nm, accum_out=se), reads=R, writes=R)
                    K.op("dve", lambda e: e.tensor_scalar(oh, r[:, 0:4], m, None, ALU.is_ge), reads=R, writes=R)
                    K.op("dve", lambda e: e.tensor_scalar(pen, oh, 1.0, 1e30, ALU.subtract, ALU.mult), reads=R, writes=R)
                    K.op("dve", lambda e: e.tensor_tensor(lem.rearrange("p (g i) -> p g i", g=4),
                                                          r[:, 4:20].rearrange("p (g i) -> p g i", g=4),
                                                          pen.unsqueeze(2).to_broadcast([128, 4, 4]), ALU.add), reads=R, writes=R)
                    K.op("dve", lambda e: e.max(top8, lem), reads=R, writes=R)
                    K.op("dve", lambda e: e.tensor_scalar(sel, lem, r[:, 52:53], None, ALU.is_ge), reads=R, writes=R)
                    K.op("dve", lambda e: e.tensor_scalar(nv1, r[:, 51:52], -1.0, None, ALU.mult), reads=R, writes=R)
                    K.op("act", lambda e: e.activation(ew, lem, AF.Exp, bias=nv1), reads=R, writes=R2)
                    K.op("dve", lambda e: e.scalar_tensor_tensor(sw, sel, 1.0, ew, ALU.mult, ALU.mult, accum_out=den), reads=R + R2, writes=R + R2)
                    K.op("dve", lambda e: e.tensor_tensor(fac, den, se, ALU.mult), reads=R, writes=R)
                    K.op("dve", lambda e: e.reciprocal(fac, fac), reads=R, writes=R)
                    K.op("dve", lambda e: e.tensor_scalar(comb, sw, fac, None, ALU.mult), reads=R + R2, writes=R2)
                    chl = r2[:, 48:64].bitcast(BF16)
                    K.op("dve", lambda e: e.tensor_copy(chl[:, 0:16], comb), reads=R2, writes=R2)
                    K.op("dve", lambda e: e.tensor_copy(r2[:, 64:80], chl[:, 0:16]), reads=R2, writes=R2)
                    K.op("dve", lambda e: e.tensor_tensor(chl[:, 16:32], comb, r2[:, 64:80], ALU.subtract), reads=R2, writes=R2)
                    pt, Bpt = P.psg.get()
                    ptb = pt[:, :].bitcast(BF16)
                    K.op("pe", lambda e: e.transpose(ptb[0:32, 0:128], chl, identb[:]), reads=R2 + [Bidentb], writes=[Bpt])
                    K.op("act", lambda e: e.copy(combT[0:32, i * 128:(i + 1) * 128], ptb[0:32, 0:128]), reads=[Bpt], writes=[BcombT[i // 4]])

            IL.run(NT, 2, _mk_nr, _body_nr)
            phase_end(es)

        def moe_phase(s, l):
            es = ExitStack()
            selE = sb(es, "selE", [128, 16, 128], BF16); BselE = Buf("selE")
            K.op("pool", lambda e: e.memset(selE[:], 0.0), writes=[BselE])
            K.dma("pool", selE[0:32], dr["c_selE"][:], writes=[BselE])
            wgu = Pool(es, nc, "wgu", [128, DC, 512], BF16, 2)
            wdp = Pool(es, nc, "wdp", [128, 2, D], BF16, 2)
            cbp = Pool(es, nc, "cbp", [128, 512], F32, 2)
            sgp = Pool(es, nc, "sgp", [128, 512], F32, 3)
            hep = Pool(es, nc, "hep", [128, 2, 512], BF16, 2)
            def load_expert(ex):
                wg, Bwg = wgu.get()
                wd, Bwd = wdp.get()
                K.dma("pool", wg[:, :, 0:256], dr["moe_w_gate"][l, ex].rearrange("(c p) f -> p c f", p=128), writes=[Bwg])
                K.dma("pool", wg[:, :, 256:512], dr["moe_w_up"][l, ex].rearrange("(c p) f -> p c f", p=128), writes=[Bwg])
                K.dma("pool", wd[:], dr["moe_w_down"][l, ex].rearrange("(c p) f -> p c f", p=128), writes=[Bwd])
                return wg, Bwg, wd, Bwd
            W = {}
            W[0] = load_expert(0)
            norm_phase(s, l, "ffn_norm", True)
            steps = [(ex, ch) for ex in range(16) for ch in range(4)]
            hes = {}

            def stage_a(ex, ch):
                wg, Bwg, wd, Bwd = W[ex]
                cs = slice(ch * 512, (ch + 1) * 512)
                cbps, Bcbps = psg.get()
                K.op("pe", lambda e: e.matmul(cbps[:, :], selE[:, ex, :], combT[:, cs], start=True, stop=True),
                     reads=[BselE, BcombT[ch]], writes=[Bcbps])
                cb, Bcb = cbp.get()
                K.op("act", lambda e: e.copy(cb[:], cbps[:, :]), reads=[Bcbps], writes=[Bcb])
                he, Bhe = hep.get()
                for fc in range(2):
                    gps, Bgps = psg.get()
                    ups, Bups = psg.get()
                    for c in range(DC):
                        K.op("pe", lambda e: e.matmul(gps[:, :], wg[:, c, fc * 128:(fc + 1) * 128], xnT[:, c, cs],
                                                      start=(c == 0), stop=(c == DC - 1)),
                             reads=[Bwg] + xnT_bufs(ch), writes=[Bgps])
                    for c in range(DC):
                        K.op("pe", lambda e: e.matmul(ups[:, :], wg[:, c, 256 + fc * 128:256 + (fc + 1) * 128], xnT[:, c, cs],
                                                      start=(c == 0), stop=(c == DC - 1)),
                             reads=[Bwg] + xnT_bufs(ch), writes=[Bups])
                    sg, Bsg = sgp.get()
                    K.op("act", lambda e: e.activation(sg[:], gps[:, :], AF.Silu), reads=[Bgps], writes=[Bsg])
                    K.op("pool", lambda e: e.tensor_tensor(sg[:], sg[:], cb[:], ALU.mult), reads=[Bsg, Bcb], writes=[Bsg])
                    K.op("dve", lambda e: e.tensor_tensor(he[:, fc, :], sg[:], ups[:, :], ALU.mult), reads=[Bsg, Bups], writes=[Bhe])
                hes[(ex, ch)] = (he, Bhe)

            def stage_b(ex, ch):
                wg, Bwg, wd, Bwd = W[ex]
                he, Bhe = hes.pop((ex, ch))
                for ts in range(4):
                    i = ch * 4 + ts
                    for half in range(2):
                        ops_, Bops = psg.get()
                        for fc in range(2):
                            K.op("pe", lambda e: e.matmul(ops_[:, :], he[:, fc, ts * 128:(ts + 1) * 128],
                                                          wd[:, fc, half * 512:(half + 1) * 512], start=(fc == 0), stop=(fc == 1)),
                                 reads=[Bhe, Bwd], writes=[Bops])
                        xs = x_sb[:, i, half * 512:(half + 1) * 512]
                        K.op("dve", lambda e: e.tensor_tensor(xs, xs, ops_[:, :], ALU.add), reads=[Bops, Bx[i][half]], writes=[Bx[i][half]])
                    if ex == 15 and io_box["final"] and not dbg:
                        K.dma("sp", out_d[s, i * 128:(i + 1) * 128, :], x_sb[:, i, :], reads=Bx[i], writes=[Bout])
                        io_box["stored"].add((s, i))
                        if s + 1 < n_seq:
                            K.dma("sp", x_sb[:, i, :], dr["x"][s + 1, i * 128:(i + 1) * 128, :], writes=Bx[i])
                            io_box["loaded"].add((s + 1, i))

            for k, (ex, ch) in enumerate(steps):
                stage_a(ex, ch)
                if k > 0:
                    pex, pch = steps[k - 1]
                    stage_b(pex, pch)
                    if pch == 3 and ex + 1 < 16:
                        W.pop(pex)
                        W[ex + 1] = load_expert(ex + 1)
                elif ex + 1 < 16:
                    W[1] = load_expert(1)
            stage_b(*steps[-1])
            phase_end(es)

        def alloc_ymT(es, m):
            ymT = sb(es, f"ymT{m}", [128, 2, T], BF16)
            BymT = [Buf(f"ymT{m}_{ch}") for ch in range(4)]
            ymT_box["t"] = ymT; ymT_box["b"] = BymT
            wo = sb(es, f"wo{m}", [128, 2, D], BF16); Bwo = Buf("wo")
            for c in range(2):
                K.dma("pool", wo[:, c, :], dr["w_out"][l_box[0], 256 * m + c * 128:256 * m + (c + 1) * 128, :], writes=[Bwo])
            ymT_box["wo"] = wo; ymT_box["Bwo"] = Bwo
            return ymT, BymT

        def wout_partial(es, l, m):
            ymT = ymT_box["t"]; BymT = ymT_box["b"]
            wo = ymT_box["wo"]; Bwo = ymT_box["Bwo"]
            for i in range(NT):
                for half in range(2):
                    ops_, Bops = psg.get()
                    for c in range(2):
                        K.op("pe", lambda e: e.matmul(ops_[:, :], ymT[:, c, i * 128:(i + 1) * 128], wo[:, c, half * 512:(half + 1) * 512],
                                                      start=(c == 0), stop=(c == 1)),
                             reads=[Bwo, BymT[i // 4]], writes=[Bops])
                    xs = x_sb[:, i, half * 512:(half + 1) * 512]
                    K.op("dve", lambda e: e.tensor_tensor(xs, xs, ops_[:, :], ALU.add), reads=[Bops, Bx[i][half]], writes=[Bx[i][half]])

        def fm_groupnorm(es_phase, es_name, yv, Byv, m, l, gcol):
            es = es_phase
            sqp = Pool(es, nc, es_name + "sq", [128, 2, 512], BF16, 2)
            rsp = Pool(es, nc, es_name + "rs", [128, 512], F32, 2)
            for ch in range(4):
                cs = slice(ch * 512, (ch + 1) * 512)
                sq, Bsq = sqp.get()
                K.op("act", lambda e: e.activation(sq[:, :, :], yv[:, :, cs], AF.Square), reads=[Byv], writes=[Bsq])
                sps, Bsps = psg.get()
                for c in range(2):
                    K.op("pe", lambda e: e.matmul(sps[:, :], ones16[:], sq[:, c, :], start=(c == 0), stop=(c == 1)),
                         reads=[Bones, Bsq], writes=[Bsps])
                rr, Brr = rsp.get()
                rstd_from(rr[:], sps[:, :], 256, [Bsps], [Brr])
                for c in range(2):
                    K.op("dve", lambda e: e.scalar_tensor_tensor(ymT_box["t"][:, c, cs], yv[:, c, cs], gcol[:, 2 * m + c:2 * m + c + 1],
                                                                 rr[:], ALU.mult, ALU.mult),
                         reads=[Byv, Brr, Bgcol], writes=[ymT_box["b"][ch]])

        def dbg_dump(m):
            if dbg:
                K.dma("pool", dbg_d["ymT"][m], ymT_box["t"][:], reads=ymT_box["b"], writes=[Bout])

        def lru_phase(s, l):
            es = ExitStack()
            ymT, BymT = alloc_ymT(es, 1)
            cw = sb(es, "lcw", [128, 2, 4], F32); Bcw = Buf("lcw")
            vec = sb(es, "lvec", [128, 5, 2], F32); Bvec = Buf("lvec")
            K.dma("sp", cw[:], dr["lru_cw"][l], writes=[Bcw])
            K.dma("sp", vec[:], dr["lru_vec"][l], writes=[Bvec])
            wa = sb(es, "lwa", [128, 2, 128], BF16); Bwa = Buf("lwa")
            wi = sb(es, "lwi", [128, 2, 128], BF16); Bwi = Buf("lwi")
            for c in range(2):
                K.dma("pool", wa[:, c, :], dr["lru_wa"][l, c], writes=[Bwa])
                K.dma("pool", wi[:, c, :], dr["lru_wi"][l, c], writes=[Bwi])
            K.op("act", lambda e: e.activation(vec[:, 4, :], vec[:, 3, :], AF.Exp, scale=-1.0), reads=[Bvec], writes=[Bvec])
            K.op("act", lambda e: e.activation(vec[:, 4, :], vec[:, 4, :], AF.Ln, bias=1.0), reads=[Bvec], writes=[Bvec])
            K.op("dve", lambda e: e.tensor_scalar(vec[:, 4, :], vec[:, 4, :], -8.0, None, ALU.mult), reads=[Bvec], writes=[Bvec])
            yv = sb(es, "lyv", [128, 2, T], F32); Byv = Buf("lyv")
            e2 = ExitStack()

            def _mk_l(w):
                per = 8 // 2
                return NS(psg=allps.sub(range(w * per, w * per + per)),
                          xp=Pool(e2, nc, "lxp", [128, T + 3], F32, 1), u=Pool(e2, nc, "lu", [128, T], F32, 1),
                          wl=Pool(e2, nc, "lwl", [128, DC, 256], BF16, 1), tp=Pool(e2, nc, "ltp", [128, 512], F32, 5),
                          u16=Pool(e2, nc, "lu16", [128, 512], BF16, 2))

            def _body_l(c, P):
                xp, Bxp = P.xp.get()
                u, Bu = P.u.get()
                wl, Bwl = P.wl.get()
                K.dma("pool", wl[:, :, 0:128], dr["w_in"][l, :, 352 + c * 128:352 + (c + 1) * 128].rearrange("(k p) n -> p k n", p=128), writes=[Bwl])
                K.dma("pool", wl[:, :, 128:256], dr["w_in"][l, :, 608 + c * 128:608 + (c + 1) * 128].rearrange("(k p) n -> p k n", p=128), writes=[Bwl])
                K.op("dve", lambda e: e.memset(xp[:, 0:3], 0.0), writes=[Bxp])
                for ch in range(4):
                    cs = slice(ch * 512, (ch + 1) * 512)
                    p1, Bp1 = P.psg.get()
                    for k in range(DC):
                        K.op("pe", lambda e: e.matmul(p1[:, :], wl[:, k, 0:128], xnT[:, k, cs], start=(k == 0), stop=(k == DC - 1)),
                             reads=[Bwl] + xnT_bufs(ch), writes=[Bp1])
                    K.op("act", lambda e: e.copy(xp[:, 3 + ch * 512:3 + (ch + 1) * 512], p1[:, :]), reads=[Bp1], writes=[Bxp])
                    p2, Bp2 = P.psg.get()
                    for k in range(DC):
                        K.op("pe", lambda e: e.matmul(p2[:, :], wl[:, k, 128:256], xnT[:, k, cs], start=(k == 0), stop=(k == DC - 1)),
                             reads=[Bwl] + xnT_bufs(ch), writes=[Bp2])
                    K.op("act", lambda e: e.activation(yv[:, c, cs], p2[:, :], AF.Gelu_apprx_tanh), reads=[Bp2], writes=[Byv])
                K.op("dve", lambda e: e.tensor_scalar(u[:], xp[:, 0:T], cw[:, c, 0:1], vec[:, 0, c:c + 1], ALU.mult, ALU.add),
                     reads=[Bxp, Bcw, Bvec], writes=[Bu])
                for j in range(1, 4):
                    K.op("dve", lambda e: e.scalar_tensor_tensor(u[:], xp[:, j:j + T], cw[:, c, j:j + 1], u[:], ALU.mult, ALU.add),
                         reads=[Bxp, Bcw, Bu], writes=[Bu])
                for ch in range(4):
                    cs = slice(ch * 512, (ch + 1) * 512)
                    u16, Bu16 = P.u16.get()
                    K.op("act", lambda e: e.copy(u16[:], u[:, cs]), reads=[Bu], writes=[Bu16])
                    pa, Bpa = P.psg.get()
                    K.op("pe", lambda e: e.matmul(pa[:, :], wa[:, c, :], u16[:], start=True, stop=True), reads=[Bwa, Bu16], writes=[Bpa])
                    pi, Bpi = P.psg.get()
                    K.op("pe", lambda e: e.matmul(pi[:, :], wi[:, c, :], u16[:], start=True, stop=True), reads=[Bwi, Bu16], writes=[Bpi])
                    r_, Br_ = P.tp.get()
                    gi, Bgi = P.tp.get()
                    mu, Bmu = P.tp.get()
                    aa, Baa = P.tp.get()
                    K.op("act", lambda e: e.activation(r_[:], pa[:, :], AF.Sigmoid, bias=vec[:, 1, c:c + 1]), reads=[Bpa, Bvec], writes=[Br_])
                    K.op("act", lambda e: e.activation(gi[:], pi[:, :], AF.Sigmoid, bias=vec[:, 2, c:c + 1]), reads=[Bpi, Bvec], writes=[Bgi])
                    K.op("act", lambda e: e.activation(aa[:], r_[:], AF.Exp, scale=vec[:, 4, c:c + 1]), reads=[Br_, Bvec], writes=[Baa])
                    K.op("act", lambda e: e.activation(mu[:], aa[:], AF.Square), reads=[Baa], writes=[Bmu])
                    K.op("act", lambda e: e.activation(mu[:], mu[:], AF.Sqrt, scale=-1.0, bias=1.0), reads=[Bmu], writes=[Bmu])
                    if ch == 0:
                        K.op("dve", lambda e: e.memset(mu[:, 0:1], 1.0), reads=[Bmu], writes=[Bmu])
                    K.op("dve", lambda e: e.tensor_tensor(mu[:], mu[:], gi[:], ALU.mult), reads=[Bmu, Bgi], writes=[Bmu])
                    K.op("dve", lambda e: e.tensor_tensor(xp[:, cs], mu[:], u[:, cs], ALU.mult), reads=[Bmu, Bu, Bxp], writes=[Bxp])
                    init = 0.0 if ch == 0 else u[:, ch * 512 - 1:ch * 512]
                    K.op("dve", lambda e: e.tensor_tensor_scan(u[:, cs], aa[:], xp[:, cs], init, ALU.mult, ALU.add),
                         reads=[Baa, Bxp, Bu], writes=[Bu])
                    K.op("dve", lambda e: e.tensor_tensor(yv[:, c, cs], yv[:, c, cs], u[:, cs], ALU.mult), reads=[Byv, Bu], writes=[Byv])
            IL.run(2, 2, _mk_l, _body_l)
            K.barrier_all()
            e2.close()
            fm_groupnorm(es, "lg", yv, Byv, 1, l, gcolt)
            dbg_dump(1)
            wout_partial(es, l, 1)
            phase_end(es)

        def s5_phase(s, l):
            es = ExitStack()
            ymT, BymT = alloc_ymT(es, 2)
            par = sb(es, "s5par", [128, 3, 8], F32); Bpar = Buf("s5par")
            bb = sb(es, "s5b", [128, 2, 8, 16], F32); Bbb = Buf("s5b")
            vec = sb(es, "s5vec", [128, 3, 2], F32); Bvec = Buf("s5vec")
            K.dma("sp", par[:], dr["s5_par"][l], writes=[Bpar])
            K.dma("sp", bb[:], dr["s5_b"][l], writes=[Bbb])
            K.dma("sp", vec[:], dr["s5_vec"][l], writes=[Bvec])
            wglu = sb(es, "s5glu", [128, 2, 256], BF16); Bwglu = Buf("s5glu")
            K.dma("pool", wglu[:], dr["s5_w_glu"][l].rearrange("(c p) n -> p c n", p=128), writes=[Bwglu])
            ypre = sb(es, "s5ypre", [128, 2, T], F32); Bypre = Buf("s5ypre")
            u16 = sb(es, "s5u16", [128, 2, T], BF16); Bu16 = Buf("s5u16")
            es2 = ExitStack()
            ws = sb(es2, "ws5", [128, DC, 256], BF16); Bws = Buf("ws5")
            K.dma("pool", ws[:], dr["w_in"][l, :, 864:1120].rearrange("(c p) n -> p c n", p=128), writes=[Bws])
            for c in range(2):
                for ch in range(4):
                    cs = slice(ch * 512, (ch + 1) * 512)
                    p1, Bp1 = psg.get()
                    for k in range(DC):
                        K.op("pe", lambda e: e.matmul(p1[:, :], ws[:, k, c * 128:(c + 1) * 128], xnT[:, k, cs], start=(k == 0), stop=(k == DC - 1)),
                             reads=[Bws] + xnT_bufs(ch), writes=[Bp1])
                    K.op("act", lambda e: e.activation(ypre[:, c, cs], p1[:, :], AF.Identity, scale=vec[:, 0, c:c + 1]), reads=[Bp1, Bvec], writes=[Bypre])
                    K.op("dve", lambda e: e.tensor_copy(u16[:, c, cs], p1[:, :]), reads=[Bp1], writes=[Bu16])
            es.enter_context(es2)
            sc = sb(es, "s5sc", [128, 16, 8], F32); Bsc = Buf("s5sc")
            SC = [Bsc]
            are = par[:, 0, :]; aim = par[:, 1, :]; ldt = par[:, 2, :]
            dt = sc[:, 0, :]; mag = sc[:, 1, :]; th = sc[:, 2, :]; cth = sc[:, 3, :]; sth = sc[:, 4, :]
            abr = sc[:, 5, :]; abi = sc[:, 6, :]; den = sc[:, 7, :]; gre = sc[:, 8, :]; gim = sc[:, 9, :]
            t0 = sc[:, 10, :]; t1 = sc[:, 11, :]; nre = sc[:, 12, :]
            ki = sb(es, "s5ki", [128, 8], I32); Bki = Buf("s5ki")
            RP = [Bpar, Bsc]
            K.op("act", lambda e: e.activation(dt, ldt, AF.Exp), reads=RP, writes=SC)
            K.op("dve", lambda e: e.tensor_tensor(t0, dt, are, ALU.mult), reads=RP, writes=SC)
            K.op("act", lambda e: e.activation(mag, t0, AF.Exp), reads=RP, writes=SC)
            K.op("dve", lambda e: e.tensor_tensor(th, dt, aim, ALU.mult), reads=RP, writes=SC)
            for (o, shift) in ((sth, 0.0), (cth, 0.5 * np.pi)):
                K.op("dve", lambda e: e.tensor_scalar(ki[:, :], th, shift, 1.0 / TWO_PI, ALU.add, ALU.mult), reads=RP, writes=[Bki])
                K.op("dve", lambda e: e.tensor_copy(t1, ki[:, :]), reads=[Bki], writes=SC)
                K.op("dve", lambda e: e.scalar_tensor_tensor(t1, t1, -TWO_PI, th, ALU.mult, ALU.add), reads=RP, writes=SC)
                K.op("dve", lambda e: e.tensor_scalar(t1, t1, shift, None, ALU.add), reads=RP, writes=SC)
                K.op("dve", lambda e: e.tensor_scalar(t1, t1, 3.14159, -3.14159, ALU.min, ALU.max), reads=RP, writes=SC)
                K.op("act", lambda e: e.activation(o, t1, AF.Sin), reads=RP, writes=SC)
            K.op("dve", lambda e: e.tensor_tensor(abr, mag, cth, ALU.mult), reads=RP, writes=SC)
            K.op("dve", lambda e: e.tensor_tensor(abi, mag, sth, ALU.mult), reads=RP, writes=SC)
            K.op("dve", lambda e: e.tensor_tensor(den, are, are, ALU.mult), reads=RP, writes=SC)
            K.op("dve", lambda e: e.tensor_tensor(t0, aim, aim, ALU.mult), reads=RP, writes=SC)
            K.op("dve", lambda e: e.tensor_tensor(den, den, t0, ALU.add), reads=RP, writes=SC)
            K.op("dve", lambda e: e.reciprocal(den, den), reads=RP, writes=SC)
            K.op("dve", lambda e: e.tensor_scalar(nre, abr, -1.0, None, ALU.add), reads=RP, writes=SC)
            K.op("dve", lambda e: e.tensor_tensor(t0, nre, are, ALU.mult), reads=RP, writes=SC)
            K.op("dve", lambda e: e.tensor_tensor(t1, abi, aim, ALU.mult), reads=RP, writes=SC)
            K.op("dve", lambda e: e.tensor_tensor(gre, t0, t1, ALU.add), reads=RP, writes=SC)
            K.op("dve", lambda e: e.tensor_tensor(gre, gre, den, ALU.mult), reads=RP, writes=SC)
            K.op("dve", lambda e: e.tensor_tensor(t0, abi, are, ALU.mult), reads=RP, writes=SC)
            K.op("dve", lambda e: e.tensor_tensor(t1, nre, aim, ALU.mult), reads=RP, writes=SC)
            K.op("dve", lambda e: e.tensor_tensor(gim, t0, t1, ALU.subtract), reads=RP, writes=SC)
            K.op("dve", lambda e: e.tensor_tensor(gim, gim, den, ALU.mult), reads=RP, writes=SC)
            bbar = sb(es, "s5bbar", [128, 2, 8, 16], F32); Bbbar = Buf("s5bbar")
            btmp = sb(es, "s5btmp", [128, 8, 16], F32); Bbtmp = Buf("s5btmp")
            gre_b = gre.unsqueeze(2).to_broadcast([128, 8, 16]); gim_b = gim.unsqueeze(2).to_broadcast([128, 8, 16])
            K.op("dve", lambda e: e.tensor_tensor(bbar[:, 0], bb[:, 0], gre_b, ALU.mult), reads=[Bbb, Bsc], writes=[Bbbar])
            K.op("dve", lambda e: e.tensor_tensor(btmp[:], bb[:, 1], gim_b, ALU.mult), reads=[Bbb, Bsc], writes=[Bbtmp])
            K.op("dve", lambda e: e.tensor_tensor(bbar[:, 0], bbar[:, 0], btmp[:], ALU.subtract), reads=[Bbbar, Bbtmp], writes=[Bbbar])
            K.op("dve", lambda e: e.tensor_tensor(bbar[:, 1], bb[:, 1], gre_b, ALU.mult), reads=[Bbb, Bsc], writes=[Bbbar])
            K.op("dve", lambda e: e.tensor_tensor(btmp[:], bb[:, 0], gim_b, ALU.mult), reads=[Bbb, Bsc, Bbbar], writes=[Bbtmp])
            K.op("dve", lambda e: e.tensor_tensor(bbar[:, 1], bbar[:, 1], btmp[:], ALU.add), reads=[Bbbar, Bbtmp], writes=[Bbbar])
            mmA = sb(es, "s5mmA", [128, 10, 2, 8], F32); BmmA = Buf("s5mmA")
            mtA = sb(es, "s5mtA", [128, 3, 8], F32); BmtA = Buf("s5mtA")
            K.op("act", lambda e: e.copy(mmA[:, 0, 0, :], sc[:, 3, :]), reads=[Bsc], writes=[BmmA])
            K.op("act", lambda e: e.copy(mmA[:, 0, 1, :], sc[:, 4, :]), reads=[Bsc], writes=[BmmA])
            for k in range(1, 10):
                pr_ = mmA[:, k - 1, 0, :]; pi_ = mmA[:, k - 1, 1, :]
                K.op("dve", lambda e: e.tensor_tensor(mtA[:, 0, :], pr_, pr_, ALU.mult), reads=[BmmA], writes=[BmtA])
                K.op("dve", lambda e: e.tensor_tensor(mtA[:, 1, :], pi_, pi_, ALU.mult), reads=[BmmA], writes=[BmtA])
                K.op("dve", lambda e: e.tensor_tensor(mmA[:, k, 0, :], mtA[:, 0, :], mtA[:, 1, :], ALU.subtract), reads=[BmtA, BmmA], writes=[BmmA])
                K.op("dve", lambda e: e.tensor_tensor(mtA[:, 2, :], pr_, pi_, ALU.mult), reads=[BmmA], writes=[BmtA])
                K.op("dve", lambda e: e.tensor_scalar(mmA[:, k, 1, :], mtA[:, 2, :], 2.0, None, ALU.mult), reads=[BmtA, BmmA], writes=[BmmA])
            e3 = ExitStack()
            def _mk_s5(w):
                per = 8 // 2
                nb = per - 1
                return NS(psg=allps.sub(range(w * per, w * per + nb)), psa=allps.sub(range(w * per + nb, (w + 1) * per)), bexpP=Pool(e3, nc, "s5bexp", [128, 2, 128], F32, 1), blp=Pool(e3, nc, "s5bl", [128, 2, 128], BF16, 1), cwp=Pool(e3, nc, "s5cw", [128, 4, 128], BF16, 1), ctmp=Pool(e3, nc, "s5ct", [128, 2, 128], F32, 1), cosP=Pool(e3, nc, "s5cos", [128, 512], F32, 1), sinP=Pool(e3, nc, "s5sin", [128, 512], F32, 1), mmP=Pool(e3, nc, "s5mm", [128, 12, 2], F32, 1), mtP=Pool(e3, nc, "s5mt", [128, 8], F32, 1), carP=Pool(e3, nc, "s5car", [128, 4], F32, 1), big=Pool(e3, nc, "s5big", [128, 512], F32, 6), pr16=Pool(e3, nc, "s5pr", [128, 4, 512], BF16, 1))
            def _body_s5(j, P):
                bexp, Bbexp = P.bexpP.get()
                cosT, Bcos = P.cosP.get()
                sinT, Bsin = P.sinP.get()
                mm_, Bmm_unused = P.mmP.get()
                mt, Bmt = P.mtP.get()
                car, Bcar = P.carP.get()
                ct_ = (32 * j) // 128
                off = (32 * j) % 128
                K.op("pool", lambda e: e.memset(bexp[:], 0.0), reads=[], writes=[Bbexp])
                for ri in range(2):
                    K.op("pool", lambda e: e.tensor_copy(bexp[0:64, ri, off:off + 16], bbar[0:64, ri, j, :]), reads=[Bbbar], writes=[Bbexp])
                    K.op("pool", lambda e: e.tensor_copy(bexp[64:128, ri, off + 16:off + 32], bbar[64:128, ri, j, :]), reads=[Bbbar], writes=[Bbexp])
                pt, Bpt = P.psg.get()
                for ri in range(2):
                    K.op("pe", lambda e: e.transpose(pt[:, ri * 128:(ri + 1) * 128], bexp[:, ri, :], ident[:]), reads=[Bbexp, Bident], writes=[Bpt])
                bl, Bbl = P.blp.get()
                K.op("act", lambda e: e.copy(bl[:, :, :], pt[:, 0:256].rearrange("p (r n) -> p r n", r=2)), reads=[Bpt], writes=[Bbl])
                ct, Bct = P.ctmp.get()
                for ri in range(2):
                    K.dma("sp", ct[:, ri, :], dr["s5_c"][l, ri, j], writes=[Bct])
                cw4, Bcw4 = P.cwp.get()
                K.op("act", lambda e: e.copy(cw4[:, 0, :], ct[:, 0, :]), reads=[Bct], writes=[Bcw4])
                K.op("act", lambda e: e.mul(cw4[:, 1, :], ct[:, 0, :], -1.0), reads=[Bct], writes=[Bcw4])
                K.op("act", lambda e: e.mul(cw4[:, 2, :], ct[:, 1, :], -1.0), reads=[Bct], writes=[Bcw4])
                K.op("act", lambda e: e.mul(cw4[:, 3, :], ct[:, 1, :], -1.0), reads=[Bct], writes=[Bcw4])
                K.op("dve", lambda e: e.memset(cosT[:, 0:1], 1.0), writes=[Bcos])
                K.op("dve", lambda e: e.memset(sinT[:, 0:1], 0.0), writes=[Bsin])
                tb, Btb = P.big.get()
                for k in range(9):
                    n = 1 << k
                    mr = mmA[:, k, 0, j:j + 1]; mi = mmA[:, k, 1, j:j + 1]
                    K.op("dve", lambda e: e.tensor_scalar(tb[:, 0:n], sinT[:, 0:n], mi, None, ALU.mult), reads=[Bsin, BmmA], writes=[Btb])
                    K.op("dve", lambda e: e.scalar_tensor_tensor(cosT[:, n:2 * n], cosT[:, 0:n], mr, tb[:, 0:n], ALU.mult, ALU.subtract),
                         reads=[Bcos, BmmA, Btb], writes=[Bcos])
                    K.op("dve", lambda e: e.tensor_scalar(tb[:, 0:n], sinT[:, 0:n], mr, None, ALU.mult), reads=[Bsin, BmmA, Bcos], writes=[Btb])
                    K.op("dve", lambda e: e.scalar_tensor_tensor(sinT[:, n:2 * n], cosT[:, 0:n], mi, tb[:, 0:n], ALU.mult, ALU.add),
                         reads=[Bcos, BmmA, Btb], writes=[Bsin])
                magb = sc[:, 1, j:j + 1].to_broadcast([128, 512])
                m9r = mmA[:, 9, 0, j:j + 1]; m9i = mmA[:, 9, 1, j:j + 1]
                for ch in range(4):
                    cs = slice(ch * 512, (ch + 1) * 512)
                    pre, Bpre = P.psg.get()
                    K.op("pe", lambda e: e.matmul(pre[:, :], bl[:, 0, :], u16[:, ct_, cs], start=True, stop=True), reads=[Bbl, Bu16], writes=[Bpre])
                    pim, Bpim = P.psg.get()
                    K.op("pe", lambda e: e.matmul(pim[:, :], bl[:, 1, :], u16[:, ct_, cs], start=True, stop=True), reads=[Bbl, Bu16], writes=[Bpim])
                    brr, Bbrr = P.big.get()
                    bri, Bbri = P.big.get()
                    ta, Bta = P.big.get()
                    tb2, Btb2 = P.big.get()
                    K.op("dve", lambda e: e.tensor_tensor(brr[:], cosT[:], pre[:, :], ALU.mult), reads=[Bcos, Bpre], writes=[Bbrr])
                    K.op("dve", lambda e: e.tensor_tensor(ta[:], sinT[:], pim[:, :], ALU.mult), reads=[Bsin, Bpim], writes=[Bta])
                    K.op("pool", lambda e: e.tensor_tensor(brr[:], brr[:], ta[:], ALU.add), reads=[Bbrr, Bta], writes=[Bbrr])
                    K.op("dve", lambda e: e.tensor_tensor(bri[:], cosT[:], pim[:, :], ALU.mult), reads=[Bcos, Bpim], writes=[Bbri])
                    K.op("dve", lambda e: e.tensor_tensor(tb2[:], sinT[:], pre[:, :], ALU.mult), reads=[Bsin, Bpre], writes=[Btb2])
                    K.op("pool", lambda e: e.tensor_tensor(bri[:], bri[:], tb2[:], ALU.subtract), reads=[Bbri, Btb2], writes=[Bbri])
                    if ch == 0:
                        ir, ii = 0.0, 0.0
                    else:
                        K.op("dve", lambda e: e.tensor_tensor(mt[:, 4:5], car[:, 0:1], m9r, ALU.mult), reads=[Bcar, BmmA], writes=[Bmt])
                        K.op("dve", lambda e: e.tensor_tensor(mt[:, 5:6], car[:, 1:2], m9i, ALU.mult), reads=[Bcar, BmmA], writes=[Bmt])
                        K.op("dve", lambda e: e.tensor_tensor(mt[:, 6:7], car[:, 1:2], m9r, ALU.mult), reads=[Bcar, BmmA], writes=[Bmt])
                        K.op("dve", lambda e: e.tensor_tensor(mt[:, 7:8], car[:, 0:1], m9i, ALU.mult), reads=[Bcar, BmmA], writes=[Bmt])
                        K.op("dve", lambda e: e.tensor_tensor(car[:, 2:3], mt[:, 4:5], mt[:, 5:6], ALU.subtract), reads=[Bmt, Bcar], writes=[Bcar])
                        K.op("dve", lambda e: e.tensor_tensor(car[:, 3:4], mt[:, 6:7], mt[:, 7:8], ALU.add), reads=[Bmt, Bcar], writes=[Bcar])
                        ir, ii = car[:, 2:3], car[:, 3:4]
                    K.op("dve", lambda e: e.tensor_tensor_scan(ta[:], magb, brr[:], ir, ALU.mult, ALU.add), reads=[Bsc, Bbrr, Bta, Bcar], writes=[Bta])
                    K.op("dve", lambda e: e.tensor_tensor_scan(tb2[:], magb, bri[:], ii, ALU.mult, ALU.add), reads=[Bsc, Bbri, Btb2, Bcar], writes=[Btb2])
                    K.op("act", lambda e: e.copy(car[:, 0:1], ta[:, 511:512]), reads=[Bta, Bcar], writes=[Bcar])
                    K.op("act", lambda e: e.copy(car[:, 1:2], tb2[:, 511:512]), reads=[Btb2, Bcar], writes=[Bcar])
                    pp, Bpp = P.pr16.get()
                    K.op("dve", lambda e: e.tensor_tensor(pp[:, 0, :], cosT[:], ta[:], ALU.mult), reads=[Bcos, Bta], writes=[Bpp])
                    K.op("pool", lambda e: e.tensor_tensor(pp[:, 1, :], sinT[:], tb2[:], ALU.mult), reads=[Bsin, Btb2], writes=[Bpp])
                    K.op("dve", lambda e: e.tensor_tensor(pp[:, 2, :], sinT[:], ta[:], ALU.mult), reads=[Bsin, Bta], writes=[Bpp])
                    K.op("pool", lambda e: e.tensor_tensor(pp[:, 3, :], cosT[:], tb2[:], ALU.mult), reads=[Bcos, Btb2], writes=[Bpp])
                    yp, Byp = P.psg.get()
                    for v in range(4):
                        K.op("pe", lambda e: e.matmul(yp[:, :], cw4[:, v, :], pp[:, v, :], start=(v == 0), stop=(v == 3)), reads=[Bcw4, Bpp], writes=[Byp])
                    K.op("dve", lambda e: e.tensor_tensor(ypre[:, ct_, cs], ypre[:, ct_, cs], yp[:, :], ALU.add), reads=[Byp, Bypre], writes=[Bypre])
            IL.run(8, 2, _mk_s5, _body_s5)
            K.barrier_all()
            e3.close()
            yg16 = sb(es, "s5yg16", [128, 2, T], BF16); Byg16 = Buf("s5yg16")
            for c in range(2):
                K.op("act", lambda e: e.activation(ypre[:, c, :], ypre[:, c, :], AF.Gelu_apprx_tanh), reads=[Bypre], writes=[Bypre])
                K.op("act", lambda e: e.copy(yg16[:, c, :], ypre[:, c, :]), reads=[Bypre], writes=[Byg16])
            sgp = Pool(es, nc, "s5sg", [128, 512], F32, 2)
            for c in range(2):
                for ch in range(4):
                    cs = slice(ch * 512, (ch + 1) * 512)
                    zp, Bzp = psg.get()
                    for k in range(2):
                        K.op("pe", lambda e: e.matmul(zp[:, :], wglu[:, k, c * 128:(c + 1) * 128], yg16[:, k, cs], start=(k == 0), stop=(k == 1)),
                             reads=[Bwglu, Byg16], writes=[Bzp])
                    sg, Bsg = sgp.get()
                    K.op("act", lambda e: e.activation(sg[:], zp[:, :], AF.Sigmoid, bias=vec[:, 1, c:c + 1]), reads=[Bzp, Bvec], writes=[Bsg])
                    K.op("dve", lambda e: e.tensor_tensor(ypre[:, c, cs], ypre[:, c, cs], sg[:], ALU.mult), reads=[Bypre, Bsg], writes=[Bypre])
            fm_groupnorm(es, "sg", ypre, Bypre, 2, l, gcolt)
            dbg_dump(2)
            wout_partial(es, l, 2)
            phase_end(es)

        def rope_tables(es, name, inv_name, half, pos_cols, Bpos_in, npart=128):
            ncol = pos_cols.shape[1]
            inv = sb(es, name + "inv", [128, half], F32); Binv = Buf(name + "inv")
            K.dma("sp", inv[:], dr[inv_name][:], writes=[Binv])
            ang = sb(es, name + "ang", [128, ncol, half], F32); Bang = Buf(name + "ang")
            tmp = sb(es, name + "tmp", [128, ncol, half], F32); Btmp = Buf(name + "tmp")
            kk = sb(es, name + "kk", [128, ncol, half], I32); Bkk = Buf(name + "kk")
            cs_ = sb(es, name + "cs", [128, 2, ncol, half], F32); Bcs = Buf(name + "cs")
            P = npart
            K.op("dve", lambda e: e.tensor_tensor(ang[0:P], inv[0:P].unsqueeze(1).to_broadcast([P, ncol, half]),
                                                  pos_cols.unsqueeze(2).to_broadcast([P, ncol, half]), ALU.mult),
                 reads=[Binv, Bpos_in], writes=[Bang])
            for idx, shift in ((0, 0.5 * np.pi), (1, 0.0)):
                K.op("dve", lambda e: e.tensor_scalar(kk[0:P], ang[0:P], shift, 1.0 / TWO_PI, ALU.add, ALU.mult), reads=[Bang], writes=[Bkk])
                K.op("dve", lambda e: e.tensor_copy(tmp[0:P], kk[0:P]), reads=[Bkk], writes=[Btmp])
                K.op("dve", lambda e: e.scalar_tensor_tensor(tmp[0:P], tmp[0:P], -TWO_PI, ang[0:P], ALU.mult, ALU.add), reads=[Btmp, Bang], writes=[Btmp])
                K.op("dve", lambda e: e.tensor_scalar(tmp[0:P], tmp[0:P], shift, None, ALU.add), reads=[Btmp], writes=[Btmp])
                K.op("dve", lambda e: e.tensor_scalar(tmp[0:P], tmp[0:P], 3.14159, -3.14159, ALU.min, ALU.max), reads=[Btmp], writes=[Btmp])
                K.op("act", lambda e: e.activation(cs_[0:P, idx], tmp[0:P], AF.Sin), reads=[Btmp], writes=[Bcs])
            return cs_, Bcs

        def apply_rope(dst, src, cos_t, sin_t, nh, half, tmp, reads, writes, Btmp):
            x1 = src[:, :, 0:half]; x2 = src[:, :, half:2 * half]
            cb = cos_t.unsqueeze(1).to_broadcast([128, nh, half]); sb_ = sin_t.unsqueeze(1).to_broadcast([128, nh, half])
            ta = tmp[:, 0:nh, 0:half]; tb_ = tmp[:, 0:nh, half:2 * half]
            K.op("dve", lambda e: e.tensor_tensor(ta, x1, cb, ALU.mult), reads=reads, writes=[Btmp])
            K.op("dve", lambda e: e.tensor_tensor(tb_, x2, sb_, ALU.mult), reads=reads, writes=[Btmp])
            K.op("dve", lambda e: e.tensor_tensor(dst[:, :, 0:half], ta, tb_, ALU.subtract), reads=[Btmp], writes=writes)
            K.op("dve", lambda e: e.tensor_tensor(ta, x1, sb_, ALU.mult), reads=reads + [Btmp], writes=[Btmp])
            K.op("dve", lambda e: e.tensor_tensor(tb_, x2, cb, ALU.mult), reads=reads + [Btmp], writes=[Btmp])
            K.op("dve", lambda e: e.tensor_tensor(dst[:, :, half:2 * half], ta, tb_, ALU.add), reads=[Btmp], writes=writes)

        def attn_block_group(s_items, acc, Bacc, first_group, last_group):
            pass

        def mla_phase(s, l):
            es = ExitStack()
            ymT, BymT = alloc_ymT(es, 0)
            qT = sb(es, "mqT", [128, 4, T], BF16); BqT = [Buf(f"mqT{i}") for i in range(NT)]
            kT = sb(es, "mkT", [128, 4, T], BF16); BkT = [Buf(f"mkT{i}") for i in range(NT)]
            K.op("pool", lambda e: e.memset(qT[64:128], 0.0), writes=BqT)
            K.op("pool", lambda e: e.memset(kT[64:128], 0.0), writes=BkT)
            va = sb(es, "mva", [128, NT, 4, 65], BF16); Bva = [Buf(f"mva{i}") for i in range(NT)]
            K.op("pool", lambda e: e.memset(va[:, :, :, 64:65], 1.0), writes=Bva)
            e1 = ExitStack()
            wm = sb(e1, "wm", [128, DC, 352], BF16); Bwm = Buf("wm")
            K.dma("pool", wm[:], dr["w_in"][l, :, 0:352].rearrange("(c p) n -> p c n", p=128), writes=[Bwm])
            wuq = sb(e1, "wuq", [128, 2, 384], BF16); Bwuq = Buf("wuq")
            K.dma("pool", wuq[:, 0, :], dr["mla_w_uq"][l, 0:128, :], writes=[Bwuq])
            K.dma("pool", wuq[0:64, 1, :], dr["mla_w_uq"][l, 128:192, :], writes=[Bwuq])
            wukv = sb(e1, "wukv", [128, 512], BF16); Bwukv = Buf("wukv")
            K.dma("pool", wukv[:], dr["mla_w_ukv"][l], writes=[Bwukv])
            gv = sb(e1, "mgv", [128, 192 + 128 + 96 + 96], F32); Bgv = Buf("mgv")
            K.dma("sp", gv[:, 0:192], dr["mla_g_cq"][l:l + 1, :].to_broadcast([128, 192]), writes=[Bgv])
            K.dma("sp", gv[:, 192:320], dr["mla_g_ckv"][l:l + 1, :].to_broadcast([128, 128]), writes=[Bgv])
            K.dma("sp", gv[:, 320:416], dr["mla_g_q"][l:l + 1, :].to_broadcast([128, 96]), writes=[Bgv])
            K.dma("sp", gv[:, 416:512], dr["mla_g_k"][l:l + 1, :].to_broadcast([128, 96]), writes=[Bgv])
            g_cq = gv[:, 0:192]; g_ckv = gv[:, 192:320]; g_q = gv[:, 320:416]; g_k = gv[:, 416:512]
            cs_t, Bcs_t = rope_tables(e1, "mr", "c_inv_mla", 16, posf[:, :], Bposf)
            def _mk_mp(w):
                per = 8 // 3
                nb = per - 0
                return NS(psg=allps.sub(range(w * per, w * per + nb)), psa=allps.sub(range(w * per + nb, (w + 1) * per)), st=Pool(e1, nc, "mst", [128, 16], F32, 2), cn=Pool(e1, nc, "mcn", [128, 320], BF16, 1), cT=Pool(e1, nc, "mcT", [128, 3, 128], BF16, 1), qn=Pool(e1, nc, "mqn", [128, 4, 96], F32, 1), kn=Pool(e1, nc, "mkn", [128, 4, 96], F32, 1), qr=Pool(e1, nc, "mqr", [128, 8, 96], BF16, 1), jk=Pool(e1, nc, "mjk", [128, 192], F32, 1), rtmp=Pool(e1, nc, "mrt", [128, 4, 32], F32, 1), kpe=Pool(e1, nc, "mkpe", [128, 32], F32, 1))
            def _body_mp(i, P):
                ts_ = slice(i * 128, (i + 1) * 128)
                pp, Bpp = P.psg.get()
                for c in range(DC):
                    K.op("pe", lambda e: e.matmul(pp[:, 0:352], xnT[:, c, ts_], wm[:, c, :], start=(c == 0), stop=(c == DC - 1)),
                         reads=[Bwm, BxnT[i][0], BxnT[i][1]], writes=[Bpp])
                sq, Bsq = P.st.get()
                j_, Bj_ = P.jk.get()
                K.op("act", lambda e: e.activation(j_[:, 0:192], pp[:, 0:192], AF.Square, accum_out=sq[:, 0:1]), reads=[Bpp], writes=[Bj_, Bsq])
                K.op("act", lambda e: e.activation(j_[:, 0:128], pp[:, 192:320], AF.Square, accum_out=sq[:, 1:2]), reads=[Bpp], writes=[Bj_, Bsq])
                K.op("act", lambda e: e.activation(j_[:, 0:32], pp[:, 320:352], AF.Square, accum_out=sq[:, 2:3]), reads=[Bpp], writes=[Bj_, Bsq])
                K.op("dve", lambda e: e.tensor_scalar(sq[:, 0:1], sq[:, 0:1], 128.0 / 192.0, None, ALU.mult), reads=[Bsq], writes=[Bsq])
                rstd_from(sq[:, 4:6], sq[:, 0:2], 128, [Bsq], [Bsq])
                c_n, Bc_n = P.cn.get()
                K.op("dve", lambda e: e.scalar_tensor_tensor(c_n[:, 0:192], pp[:, 0:192], sq[:, 4:5], g_cq, ALU.mult, ALU.mult),
                     reads=[Bpp, Bsq, Bgv], writes=[Bc_n])
                K.op("dve", lambda e: e.scalar_tensor_tensor(c_n[:, 192:320], pp[:, 192:320], sq[:, 5:6], g_ckv, ALU.mult, ALU.mult),
                     reads=[Bpp, Bsq, Bgv], writes=[Bc_n])
                kp, Bkp = P.kpe.get()
                K.op("act", lambda e: e.copy(kp[:], pp[:, 320:352]), reads=[Bpp], writes=[Bkp])
                pt, Bpt = P.psg.get()
                ptb = pt[:, :].bitcast(BF16)
                K.op("pe", lambda e: e.transpose(ptb[:, 0:128], c_n[:, 0:128], identb[:]), reads=[Bc_n, Bidentb], writes=[Bpt])
                K.op("pe", lambda e: e.transpose(ptb[0:64, 128:256], c_n[:, 128:192], identb[:]), reads=[Bc_n, Bidentb], writes=[Bpt])
                K.op("pe", lambda e: e.transpose(ptb[:, 256:384], c_n[:, 192:320], identb[:]), reads=[Bc_n, Bidentb], writes=[Bpt])
                ct, Bct = P.cT.get()
                K.op("act", lambda e: e.copy(ct[:, 0, :], ptb[:, 0:128]), reads=[Bpt], writes=[Bct])
                K.op("act", lambda e: e.copy(ct[0:64, 1, :], ptb[0:64, 128:256]), reads=[Bpt], writes=[Bct])
                K.op("act", lambda e: e.copy(ct[:, 2, :], ptb[:, 256:384]), reads=[Bpt], writes=[Bct])
                pq, Bpq = P.psg.get()
                K.op("pe", lambda e: e.matmul(pq[:, 0:384], ct[:, 0, :], wuq[:, 0, :], start=True, stop=False), reads=[Bct, Bwuq], writes=[Bpq])
                K.op("pe", lambda e: e.matmul(pq[:, 0:384], ct[0:64, 1, :], wuq[0:64, 1, :], start=False, stop=True), reads=[Bct, Bwuq], writes=[Bpq])
                pkv, Bpkv = P.psg.get()
                K.op("pe", lambda e: e.matmul(pkv[:, :], ct[:, 2, :], wukv[:], start=True, stop=True), reads=[Bct, Bwukv], writes=[Bpkv])
                pq3 = pq[:, 0:384].rearrange("p (h d) -> p h d", h=4)
                pkv3 = pkv[:, :].rearrange("p (h d) -> p h d", h=4)
                sq2, Bsq2 = P.st.get()
                for h in range(4):
                    K.op("act", lambda e: e.activation(j_[:, 0:96], pq3[:, h, :], AF.Square, accum_out=sq2[:, h:h + 1]), reads=[Bpq], writes=[Bj_, Bsq2])
                    K.op("act", lambda e: e.activation(j_[:, 0:64], pkv3[:, h, 0:64], AF.Square, accum_out=sq2[:, 4 + h:5 + h]), reads=[Bpkv], writes=[Bj_, Bsq2])
                K.op("dve", lambda e: e.tensor_scalar(sq2[:, 4:8], sq2[:, 4:8], sq[:, 2:3], None, ALU.add), reads=[Bsq2, Bsq], writes=[Bsq2])
                rstd_from(sq2[:, 8:16], sq2[:, 0:8], 96, [Bsq2], [Bsq2])
                q_n, Bq_n = P.qn.get()
                k_n, Bk_n = P.kn.get()
                for h in range(4):
                    K.op("dve", lambda e: e.scalar_tensor_tensor(q_n[:, h, :], pq3[:, h, :], sq2[:, 8 + h:9 + h], g_q, ALU.mult, ALU.mult),
                         reads=[Bpq, Bsq2, Bgv], writes=[Bq_n])
                    K.op("dve", lambda e: e.scalar_tensor_tensor(k_n[:, h, 32:96], pkv3[:, h, 0:64], sq2[:, 12 + h:13 + h], g_k[:, 32:96], ALU.mult, ALU.mult),
                         reads=[Bpkv, Bsq2, Bgv], writes=[Bk_n])
                    K.op("dve", lambda e: e.scalar_tensor_tensor(k_n[:, h, 0:32], kp[:], sq2[:, 12 + h:13 + h], g_k[:, 0:32], ALU.mult, ALU.mult),
                         reads=[Bkp, Bsq2, Bgv], writes=[Bk_n])
                q_r, Bq_r = P.qr.get()
                rt_, Brt_ = P.rtmp.get()
                apply_rope(q_r[:, 0:4], q_n[:, :, :], cs_t[:, 0, i, :], cs_t[:, 1, i, :], 4, 16, rt_, [Bq_n, Bcs_t], [Bq_r], Brt_)
                K.op("act", lambda e: e.copy(q_r[:, 0:4, 32:96], q_n[:, :, 32:96]), reads=[Bq_n], writes=[Bq_r])
                apply_rope(q_r[:, 4:8], k_n[:, :, :], cs_t[:, 0, i, :], cs_t[:, 1, i, :], 4, 16, rt_, [Bk_n, Bcs_t], [Bq_r], Brt_)
                K.op("act", lambda e: e.copy(q_r[:, 4:8, 32:96], k_n[:, :, 32:96]), reads=[Bk_n], writes=[Bq_r])
                K.op("act", lambda e: e.copy(va[:, i, :, 0:64], pkv3[:, :, 64:128]), reads=[Bpkv], writes=[Bva[i]])
                for grp in range(2):
                    pt2, Bpt2 = P.psg.get()
                    pt2b = pt2[:, :].bitcast(BF16)
                    for h in range(4):
                        K.op("pe", lambda e: e.transpose(pt2b[0:96, h * 128:(h + 1) * 128], q_r[:, grp * 4 + h, :], identb[:]),
                             reads=[Bq_r, Bidentb], writes=[Bpt2])
                    dstT = qT if grp == 0 else kT
                    BdT = BqT if grp == 0 else BkT
                    K.op("act" if grp == 0 else "dve",
                         (lambda e: e.copy(dstT[0:96, :, ts_], pt2b[0:96, 0:512].rearrange("p (h t) -> p h t", h=4))) if grp == 0 else
                         (lambda e: e.tensor_copy(dstT[0:96, :, ts_], pt2b[0:96, 0:512].rearrange("p (h t) -> p h t", h=4))),
                         reads=[Bpt2], writes=[BdT[i]])
            IL.run(NT, 3, _mk_mp, _body_mp)
            K.barrier_all()
            e1.close()
            scale = 96 ** -0.5
            def _mk_ma(w):
                per = 8 // 4
                nb = per - 1
                return NS(psg=allps.sub(range(w * per, w * per + nb)), psa=allps.sub(range(w * per + nb, (w + 1) * per)), pexp=Pool(es, nc, "mpe", [128, 512], BF16, 3), yo=Pool(es, nc, "myo", [128, 256], F32, 2), yst=Pool(es, nc, "myst", [128, 8], F32, 2), yb=Pool(es, nc, "myb", [128, 256], BF16, 2))
            def _body_ma(qt, P):
                qs = slice(qt * 128, (qt + 1) * 128)
                y_o, By_o = P.yo.get()
                yst_, Byst = P.yst.get()
                for h in range(4):
                    acc, Bacc = P.psa.get()
                    nk = qt + 1
                    for g0 in range(0, nk, 4):
                        kts = list(range(g0, min(g0 + 4, nk)))
                        sp_, Bsp = P.psg.get()
                        for a, kt in enumerate(kts):
                            K.op("pe", lambda e: e.matmul(sp_[:, a * 128:(a + 1) * 128], kT[:, h, kt * 128:(kt + 1) * 128], qT[:, h, qs], start=True, stop=True),
                                 reads=[BkT[kt], BqT[qt]], writes=[Bsp])
                        pe_, Bpe = P.pexp.get()
                        w = len(kts) * 128
                        K.op("act", lambda e: e.activation(pe_[:, 0:w], sp_[:, 0:w], AF.Exp, scale=scale), reads=[Bsp], writes=[Bpe])
                        if kts[-1] == qt:
                            a = len(kts) - 1
                            K.op("dve", lambda e: e.tensor_tensor(pe_[:, a * 128:(a + 1) * 128], pe_[:, a * 128:(a + 1) * 128], tri4[:, 0, :], ALU.mult),
                                 reads=[Bpe, Btri], writes=[Bpe])
                        for a, kt in enumerate(kts):
                            K.op("pe", lambda e: e.matmul(acc[:, 0:65], pe_[:, a * 128:(a + 1) * 128], va[:, kt, h, :], start=(kt == 0), stop=(kt == qt)),
                                 reads=[Bpe, Bva[kt]], writes=[Bacc])
                    K.op("dve", lambda e: e.reciprocal(yst_[:, h:h + 1], acc[:, 64:65]), reads=[Bacc], writes=[Byst])
                    K.op("dve", lambda e: e.tensor_scalar(y_o[:, h * 64:(h + 1) * 64], acc[:, 0:64], yst_[:, h:h + 1], None, ALU.mult),
                         reads=[Bacc, Byst], writes=[By_o])
                tm_groupnorm(P.psg, y_o, By_o, yst_, Byst, P.yb, 0, l, qt)
            IL.run(NT, 4, _mk_ma, _body_ma)
            dbg_dump(0)
            wout_partial(es, l, 0)
            phase_end(es)

        def tm_groupnorm(psgp, y_o, By_o, yst_, Byst, ybpool, m, l, qt):
            y_b, By_b = ybpool.get()
            K.op("act", lambda e: e.activation(y_b[:], y_o[:], AF.Square, accum_out=yst_[:, 4:5]), reads=[By_o], writes=[By_b, Byst])
            K.op("act", lambda e: e.activation(yst_[:, 5:6], yst_[:, 4:5], AF.Ln, bias=epsb[:, 0:1], scale=1.0 / 256), reads=[Byst, Beps], writes=[Byst])
            K.op("act", lambda e: e.activation(yst_[:, 5:6], yst_[:, 5:6], AF.Exp, scale=-0.5), reads=[Byst], writes=[Byst])
            K.op("dve", lambda e: e.scalar_tensor_tensor(y_b[:], y_o[:], yst_[:, 5:6], gon[:, m, :], ALU.mult, ALU.mult),
                 reads=[By_o, Byst, Bgon, By_b], writes=[By_b])
            pt, Bpt = psgp.get()
            ptb = pt[:, :].bitcast(BF16)
            for c in range(2):
                K.op("pe", lambda e: e.transpose(ptb[:, c * 128:(c + 1) * 128], y_b[:, c * 128:(c + 1) * 128], identb[:]), reads=[By_b, Bidentb], writes=[Bpt])
            K.op("act", lambda e: e.copy(ymT_box["t"][:, :, qt * 128:(qt + 1) * 128], ptb[:, 0:256].rearrange("p (c t) -> p c t", c=2)),
                 reads=[Bpt], writes=[ymT_box["b"][qt // 4]])

        def nsa_phase(s, l):
            es = ExitStack()
            ymT, BymT = alloc_ymT(es, 3)
            gv = sb(es, "ngv", [128, 4, 64], F32); Bgv = Buf("ngv")
            K.dma("sp", gv[:, 0, :], dr["nsa_g_q"][l:l + 1, :].to_broadcast([128, 64]), writes=[Bgv])
            for b3 in range(3):
                K.dma("sp", gv[:, 1 + b3, :], dr["nsa_g_k"][l, b3:b3 + 1, :].to_broadcast([128, 64]), writes=[Bgv])
            qT = sb(es, "nqT", [128, NT, 4, 128], BF16); BqT = [Buf(f"nqT{i}") for i in range(NT)]
            kTs = sb(es, "nkTs", [128, T], BF16); BkTs = [Buf(f"nkTs{i}") for i in range(NT)]
            kTw = sb(es, "nkTw", [128, T], BF16); BkTw = [Buf(f"nkTw{i}") for i in range(NT)]
            K.op("pool", lambda e: e.memset(qT[64:128], 0.0), writes=BqT)
            K.op("pool", lambda e: e.memset(kTs[64:128], 0.0), writes=BkTs)
            K.op("pool", lambda e: e.memset(kTw[64:128], 0.0), writes=BkTw)
            vs = sb(es, "nvs", [128, NT, 65], BF16); Bvs = [Buf(f"nvs{i}") for i in range(NT)]
            vw = sb(es, "nvw", [128, NT, 65], BF16); Bvw = [Buf(f"nvw{i}") for i in range(NT)]
            gts = sb(es, "ngts", [128, NT, 12], F32); Bgts = [Buf(f"ngts{i}") for i in range(NT)]
            kcT = sb(es, "nkcT", [128, 128], BF16); BkcT = Buf("nkcT")
            K.op("pool", lambda e: e.memset(kcT[64:128], 0.0), writes=[BkcT])
            vc = sb(es, "nvc", [128, 97], BF16); Bvc = Buf("nvc")
            K.op("pool", lambda e: e.memset(vs[:, :, 64:65], 1.0), writes=Bvs)
            K.op("pool", lambda e: e.memset(vw[:, :, 64:65], 1.0), writes=Bvw)
            K.op("pool", lambda e: e.memset(vc[:, 64:65], 1.0), writes=[Bvc])
            K.dma("pool", vc[:, 65:97], dr["c_ov"][:], writes=[Bvc])
            e1 = ExitStack()
            wn = sb(e1, "wn", [128, DC, 652], BF16); Bwn = Buf("wn")
            K.dma("pool", wn[:], dr["w_in"][l, :, 1120:1772].rearrange("(c p) n -> p c n", p=128), writes=[Bwn])
            cs_t, Bcs_t = rope_tables(e1, "nr", "c_inv_nsa", 8, posf[:, :], Bposf)
            pci = sb(e1, "npci", [128, 1], I32); Bpci = Buf("npci")
            pcf = sb(e1, "npcf", [128, 1], F32); Bpcf = Buf("npcf")
            K.op("dve", lambda e: e.memset(pcf[:], 0.0), writes=[Bpcf])
            pos_src = dr["pos"][s:s + 1, 31::16].rearrange("o n -> n o")
            K.dma("sp", pci[0:127, :], pos_src, writes=[Bpci], allow_slow_non_contiguous=True)
            K.op("dve", lambda e: e.tensor_copy(pcf[0:127, :], pci[0:127, :]), reads=[Bpci, Bpcf], writes=[Bpcf])
            csc, Bcsc = rope_tables(e1, "nrc", "c_inv_nsa", 8, pcf[:, :], Bpcf)
            cmpT = sb(e1, "ncmpT", [128, T], BF16); BcmpT = Buf("ncmpT")
            for ch in range(4):
                cs = slice(ch * 512, (ch + 1) * 512)
                p1, Bp1 = psg.get()
                for k in range(DC):
                    K.op("pe", lambda e: e.matmul(p1[:, :], wn[:, k, 256:384], xnT[:, k, cs], start=(k == 0), stop=(k == DC - 1)),
                         reads=[Bwn] + xnT_bufs(ch), writes=[Bp1])
                K.op("act", lambda e: e.copy(cmpT[:, cs], p1[:, :]), reads=[Bp1], writes=[BcmpT])
            def _mk_np(w):
                per = 8 // 4
                nb = per - 0
                return NS(psg=allps.sub(range(w * per, w * per + nb)), psa=allps.sub(range(w * per + nb, (w + 1) * per)), st=Pool(e1, nc, "nst", [128, 16], F32, 2), jk=Pool(e1, nc, "njk", [128, 64], F32, 1), qn=Pool(e1, nc, "nqn", [128, 6, 64], F32, 1), qr=Pool(e1, nc, "nqr", [128, 6, 64], BF16, 1), rtmp=Pool(e1, nc, "nrt", [128, 6, 16], F32, 1))
            def _body_np(i, P):
                ts_ = slice(i * 128, (i + 1) * 128)
                pa, Bpa = P.psg.get()
                pb, Bpb = P.psg.get()
                for c in range(DC):
                    K.op("pe", lambda e: e.matmul(pa[:, 0:256], xnT[:, c, ts_], wn[:, c, 0:256], start=(c == 0), stop=(c == DC - 1)),
                         reads=[Bwn, BxnT[i][0], BxnT[i][1]], writes=[Bpa])
                for c in range(DC):
                    K.op("pe", lambda e: e.matmul(pb[:, 0:268], xnT[:, c, ts_], wn[:, c, 384:652], start=(c == 0), stop=(c == DC - 1)),
                         reads=[Bwn, BxnT[i][0], BxnT[i][1]], writes=[Bpb])
                pa3 = pa[:, 0:256].rearrange("p (h d) -> p h d", h=4)
                sq, Bsq = P.st.get()
                j_, Bj_ = P.jk.get()
                for h in range(4):
                    K.op("act", lambda e: e.activation(j_[:], pa3[:, h, :], AF.Square, accum_out=sq[:, h:h + 1]), reads=[Bpa], writes=[Bj_, Bsq])
                K.op("act", lambda e: e.activation(j_[:], pb[:, 0:64], AF.Square, accum_out=sq[:, 4:5]), reads=[Bpb], writes=[Bj_, Bsq])
                K.op("act", lambda e: e.activation(j_[:], pb[:, 128:192], AF.Square, accum_out=sq[:, 5:6]), reads=[Bpb], writes=[Bj_, Bsq])
                rstd_from(sq[:, 8:14], sq[:, 0:6], 64, [Bsq], [Bsq])
                q_n, Bq_n = P.qn.get()
                for h in range(4):
                    K.op("dve", lambda e: e.scalar_tensor_tensor(q_n[:, h, :], pa3[:, h, :], sq[:, 8 + h:9 + h], gv[:, 0, :], ALU.mult, ALU.mult),
                         reads=[Bpa, Bsq, Bgv], writes=[Bq_n])
                K.op("dve", lambda e: e.scalar_tensor_tensor(q_n[:, 4, :], pb[:, 0:64], sq[:, 12:13], gv[:, 2, :], ALU.mult, ALU.mult),
                     reads=[Bpb, Bsq, Bgv], writes=[Bq_n])
                K.op("dve", lambda e: e.scalar_tensor_tensor(q_n[:, 5, :], pb[:, 128:192], sq[:, 13:14], gv[:, 3, :], ALU.mult, ALU.mult),
                     reads=[Bpb, Bsq, Bgv], writes=[Bq_n])
                q_r, Bq_r = P.qr.get()
                rt_, Brt_ = P.rtmp.get()
                apply_rope(q_r[:, :, :], q_n[:, :, :], cs_t[:, 0, i, :], cs_t[:, 1, i, :], 6, 8, rt_, [Bq_n, Bcs_t], [Bq_r], Brt_)
                K.op("act", lambda e: e.copy(q_r[:, :, 16:64], q_n[:, :, 16:64]), reads=[Bq_n], writes=[Bq_r])
                K.op("act", lambda e: e.copy(vs[:, i, 0:64], pb[:, 64:128]), reads=[Bpb], writes=[Bvs[i]])
                K.op("act", lambda e: e.copy(vw[:, i, 0:64], pb[:, 192:256]), reads=[Bpb], writes=[Bvw[i]])
                K.op("act", lambda e: e.copy(gts[:, i, :], pb[:, 256:268]), reads=[Bpb], writes=[Bgts[i]])
                pt, Bpt = P.psg.get()
                ptb = pt[:, :].bitcast(BF16)
                for h in range(6):
                    K.op("pe", lambda e: e.transpose(ptb[0:64, h * 128:(h + 1) * 128], q_r[:, h, :], identb[:]), reads=[Bq_r, Bidentb], writes=[Bpt])
                K.op("act", lambda e: e.copy(qT[0:64, i, :, :], ptb[0:64, 0:512].rearrange("p (h t) -> p h t", h=4)), reads=[Bpt], writes=[BqT[i]])
                K.op("dve", lambda e: e.tensor_copy(kTs[0:64, ts_], ptb[0:64, 512:640]), reads=[Bpt], writes=[BkTs[i]])
                K.op("dve", lambda e: e.tensor_copy(kTw[0:64, ts_], ptb[0:64, 640:768]), reads=[Bpt], writes=[BkTw[i]])
            IL.run(NT, 4, _mk_np, _body_np)
            st = Pool(e1, nc, "nst2", [128, 16], F32, 1)
            jk = Pool(e1, nc, "njk2", [128, 64], F32, 1)
            qn = Pool(e1, nc, "nqn2", [128, 6, 64], F32, 1)
            qr = Pool(e1, nc, "nqr2", [128, 6, 64], BF16, 1)
            rtmp = Pool(e1, nc, "nrt2", [128, 6, 16], F32, 1)
            w1 = sb(e1, "nw1", [128, 32, 128], BF16); Bw1 = Buf("nw1")
            pe16 = sb(e1, "npe16", [128, 32], BF16); Bpe16 = Buf("npe16")
            w2 = sb(e1, "nw2", [128, 2, 64], BF16); Bw2 = Buf("nw2")
            for kv in range(2):
                K.dma("pool", w1[kv * 64:kv * 64 + 64], dr["nsa_w1"][l, kv], writes=[Bw1])
                K.dma("pool", pe16[kv * 64:kv * 64 + 64, :], dr["nsa_pe"][l, kv], writes=[Bpe16])
                K.dma("pool", w2[:, kv, :], dr["nsa_w2"][l, kv], writes=[Bw2])
            hb = sb(e1, "nhb", [128, 2], F32); Bhb = Buf("nhb")
            hid = sb(e1, "nhid", [128, 2, 128], BF16); Bhid = Buf("nhid")
            for kv in range(2):
                rows = slice(kv * 64, kv * 64 + 64)
                ph, Bph = psg.get()
                pbias, Bpbias = psg.get()
                for j in range(32):
                    lw = w1[rows, j, :]
                    rhs = cmpT[rows, j:j + 16 * 126 + 1:16]
                    K.op("pe", lambda e: e.matmul(ph[:, 0:127], lw, rhs, start=(j == 0), stop=(j == 31)), reads=[Bw1, BcmpT], writes=[Bph])
                for j in range(32):
                    lw = w1[rows, j, :]
                    K.op("pe", lambda e: e.matmul(pbias[:, 0:1], lw, pe16[rows, j:j + 1], start=(j == 0), stop=(j == 31)), reads=[Bw1, Bpe16], writes=[Bpbias])
                K.op("act", lambda e: e.copy(hb[:, kv:kv + 1], pbias[:, 0:1]), reads=[Bpbias], writes=[Bhb])
                K.op("act", lambda e: e.activation(hid[:, kv, 0:127], ph[:, 0:127], AF.Gelu_apprx_tanh, bias=hb[:, kv:kv + 1]), reads=[Bph, Bhb], writes=[Bhid])
                po, Bpo = psg.get()
                K.op("pe", lambda e: e.matmul(po[0:127, 0:64], hid[:, kv, 0:127], w2[:, kv, :], start=True, stop=True), reads=[Bhid, Bw2], writes=[Bpo])
                if kv == 1:
                    K.op("act", lambda e: e.copy(vc[0:127, 0:64], po[0:127, 0:64]), reads=[Bpo], writes=[Bvc])
                else:
                    sq, Bsq = st.get()
                    j_, Bj_ = jk.get()
                    q_n, Bq_n = qn.get()
                    q_r, Bq_r = qr.get()
                    rt_, Brt_ = rtmp.get()
                    K.op("dve", lambda e: e.memset(q_n[:, 0, :], 0.0), writes=[Bq_n])
                    K.op("act", lambda e: e.activation(j_[0:127, :], po[0:127, 0:64], AF.Square, accum_out=sq[0:127, 0:1]), reads=[Bpo], writes=[Bj_, Bsq])
                    rstd_from(sq[0:127, 1:2], sq[0:127, 0:1], 64, [Bsq], [Bsq])
                    K.op("dve", lambda e: e.scalar_tensor_tensor(q_n[0:127, 0, :], po[0:127, 0:64], sq[0:127, 1:2], gv[0:127, 1, :], ALU.mult, ALU.mult),
                         reads=[Bpo, Bsq, Bgv, Bq_n], writes=[Bq_n])
                    apply_rope(q_r[:, 0:1, :], q_n[:, 0:1, :], csc[:, 0, 0, :], csc[:, 1, 0, :], 1, 8, rt_, [Bq_n, Bcsc], [Bq_r], Brt_)
                    K.op("act", lambda e: e.copy(q_r[:, 0:1, 16:64], q_n[:, 0:1, 16:64]), reads=[Bq_n], writes=[Bq_r])
                    pt, Bpt = psg.get()
                    ptb = pt[:, :].bitcast(BF16)
                    K.op("pe", lambda e: e.transpose(ptb[0:64, 0:128], q_r[:, 0, :], identb[:]), reads=[Bq_r, Bidentb], writes=[Bpt])
                    K.op("act", lambda e: e.copy(kcT[0:64, :], ptb[0:64, 0:128]), reads=[Bpt], writes=[BkcT])
            K.barrier_all()
            e1.close()
            cmask = sb(es, "ncmask", [128, T], BF16); Bcmask = Buf("ncmask")
            K.dma("pool", cmask[:], dr["c_cmpmask"][:], writes=[Bcmask])
            keep = sb(es, "nkeep", [128, NT, 32], F32); Bkeep = Buf("nkeep")
            base = sb(es, "nbase", [128, NT, 32], F32); Bbase = Buf("nbase")
            K.dma("sp", keep[:], dr["c_keep"][:], writes=[Bkeep])
            K.dma("sp", base[:], dr["c_base"][:], writes=[Bbase])
            Em = sb(es, "nEm", [128, NT, 128], BF16); BEm = Buf("nEm")
            K.op("pool", lambda e: e.memset(Em[:], 0.0), writes=[BEm])
            K.dma("pool", Em[0:32], dr["c_E"][:], writes=[BEm])
            scale = 64 ** -0.5
            def _mk_na(w):
                per = 8 // 4
                nb = per - 1
                return NS(psg=allps.sub(range(w * per, w * per + nb)), psa=allps.sub(range(w * per + nb, (w + 1) * per)), nsp=_zeroed(Pool(es, nc, "nselT", [128, 4, 128], BF16, 1)), pexp=Pool(es, nc, "npx", [128, 512], BF16, 3), sst=Pool(es, nc, "nsst", [128, 80], F32, 1), impp=Pool(es, nc, "nimp", [128, 32], F32, 1), yo=Pool(es, nc, "nyo", [128, 256], F32, 1), yst=Pool(es, nc, "nyst", [128, 8], F32, 1), yb=Pool(es, nc, "nyb", [128, 256], BF16, 1))
            def _body_na(qt, P):
                qs = slice(qt * 128, (qt + 1) * 128)
                qrhs = qT[:, qt].rearrange("p h t -> p (h t)")
                y_o, By_o = P.yo.get()
                yst_, Byst = P.yst.get()
                st_, Bst_ = P.sst.get()
                imp, Bimp = P.impp.get()
                K.op("act", lambda e: e.activation(gts[:, qt, :], gts[:, qt, :], AF.Exp, scale=-1.0), reads=[Bgts[qt]], writes=[Bgts[qt]])
                K.op("dve", lambda e: e.tensor_scalar(gts[:, qt, :], gts[:, qt, :], 1.0, None, ALU.add), reads=[Bgts[qt]], writes=[Bgts[qt]])
                K.op("dve", lambda e: e.reciprocal(gts[:, qt, :], gts[:, qt, :]), reads=[Bgts[qt]], writes=[Bgts[qt]])
                sp_, Bsp = P.psg.get()
                K.op("pe", lambda e: e.matmul(sp_[0:127, :], kcT[:, 0:127], qrhs, start=True, stop=True), reads=[BkcT, BqT[qt]], writes=[Bsp])
                pe_, Bpe = P.pexp.get()
                K.op("act", lambda e: e.activation(pe_[0:127, :], sp_[0:127, :], AF.Exp, scale=scale), reads=[Bsp], writes=[Bpe])
                K.op("dve", lambda e: e.tensor_tensor(pe_[0:127, :].rearrange("p (h t) -> p h t", h=4), pe_[0:127, :].rearrange("p (h t) -> p h t", h=4),
                                                       cmask[0:127, qs].unsqueeze(1).to_broadcast([127, 4, 128]), ALU.mult),
                     reads=[Bpe, Bcmask], writes=[Bpe])
                for h in range(4):
                    acc, Bacc = P.psa.get()
                    K.op("pe", lambda e: e.matmul(acc[:, 0:97], pe_[0:127, h * 128:(h + 1) * 128], vc[0:127, :], start=True, stop=True),
                         reads=[Bpe, Bvc], writes=[Bacc])
                    K.op("dve", lambda e: e.tensor_scalar(st_[:, h:h + 1], acc[:, 64:65], 1e-30, None, ALU.add), reads=[Bacc], writes=[Bst_])
                    K.op("dve", lambda e: e.reciprocal(st_[:, h:h + 1], st_[:, h:h + 1]), reads=[Bst_], writes=[Bst_])
                    if h == 0:
                        K.op("dve", lambda e: e.tensor_scalar(imp[:], acc[:, 65:97], st_[:, h:h + 1], None, ALU.mult), reads=[Bacc, Bst_], writes=[Bimp])
                    else:
                        K.op("dve", lambda e: e.scalar_tensor_tensor(imp[:], acc[:, 65:97], st_[:, h:h + 1], imp[:], ALU.mult, ALU.add),
                             reads=[Bacc, Bst_, Bimp], writes=[Bimp])
                    K.op("dve", lambda e: e.tensor_tensor(st_[:, 4 + h:5 + h], st_[:, h:h + 1], gts[:, qt, 3 * h:3 * h + 1], ALU.mult), reads=[Bst_, Bgts[qt]], writes=[Bst_])
                    K.op("dve", lambda e: e.tensor_scalar(y_o[:, h * 64:(h + 1) * 64], acc[:, 0:64], st_[:, 4 + h:5 + h], None, ALU.mult),
                         reads=[Bacc, Bst_], writes=[By_o])
                K.op("dve", lambda e: e.tensor_tensor(imp[:], imp[:], keep[:, qt, :], ALU.mult), reads=[Bimp, Bkeep], writes=[Bimp])
                K.op("dve", lambda e: e.tensor_tensor(imp[:], imp[:], base[:, qt, :], ALU.add), reads=[Bimp, Bbase], writes=[Bimp])
                K.op("dve", lambda e: e.max(st_[:, 8:16], imp[:]), reads=[Bimp], writes=[Bst_])
                K.op("dve", lambda e: e.tensor_scalar(st_[:, 16:48], imp[:], st_[:, 12:13], -1.0, ALU.is_ge, ALU.add), reads=[Bimp, Bst_], writes=[Bst_])
                pt, Bpt = P.psg.get()
                K.op("pe", lambda e: e.transpose(pt[0:32, 0:128], st_[:, 16:48], ident[:]), reads=[Bst_, Bident], writes=[Bpt])
                nselT, BnselT = P.nsp.get()
                K.op("act", lambda e: e.copy(nselT[0:32, :, :], pt[0:32, 0:128].unsqueeze(1).to_broadcast([32, 4, 128])), reads=[Bpt], writes=[BnselT])
                for br_ in range(2):
                    kTb = kTs if br_ == 0 else kTw
                    BkTb = BkTs if br_ == 0 else BkTw
                    vb = vs if br_ == 0 else vw
                    Bvb = Bvs if br_ == 0 else Bvw
                    kts = list(range(0, qt + 1)) if br_ == 0 else list(range(max(0, qt - 4), qt + 1))
                    acc, Bacc = P.psa.get()
                    K.op("dve", lambda e: e.memset(acc[:, 0:260], 0.0), writes=[Bacc])
                    for kt in kts:
                        sp_, Bsp = P.psg.get()
                        K.op("pe", lambda e: e.matmul(sp_[:, :], kTb[:, kt * 128:(kt + 1) * 128], qrhs, start=True, stop=(br_ == 1)),
                             reads=[BkTb[kt], BqT[qt]], writes=[Bsp])
                        if br_ == 0:
                            K.op("pe", lambda e: e.matmul(sp_[:, :], Em[:, kt, :], nselT[:, :, :].rearrange("p h t -> p (h t)"), start=False, stop=True),
                                 reads=[BEm, BnselT], writes=[Bsp])
                        pe_, Bpe = P.pexp.get()
                        K.op("act", lambda e: e.activation(pe_[:, :], sp_[:, :], AF.Exp, scale=scale), reads=[Bsp], writes=[Bpe])
                        if kt == qt:
                            K.op("dve", lambda e: e.tensor_tensor(pe_[:, :], pe_[:, :], tri4[:].rearrange("p h t -> p (h t)"), ALU.mult),
                                 reads=[Bpe, Btri], writes=[Bpe])
                        elif br_ == 1 and kt == qt - 4:
                            K.op("dve", lambda e: e.tensor_tensor(pe_[:, :], pe_[:, :], anti4[:].rearrange("p h t -> p (h t)"), ALU.mult),
                                 reads=[Bpe, Banti], writes=[Bpe])
                        for h in range(4):
                            co = h * 65
                            K.op("pe", lambda e: e.matmul(acc[:, co:co + 65], pe_[:, h * 128:(h + 1) * 128], vb[:, kt, :], start=False, stop=(kt == kts[-1]),
                                                          skip_group_check=True),
                                 reads=[Bpe, Bvb[kt]], writes=[Bacc])
                    for h in range(4):
                        co = h * 65
                        cc_ = 50 + 4 * br_ + h
                        K.op("dve", lambda e: e.reciprocal(st_[:, cc_:cc_ + 1], acc[:, co + 64:co + 65]), reads=[Bacc], writes=[Bst_])
                        K.op("dve", lambda e: e.tensor_tensor(st_[:, cc_:cc_ + 1], st_[:, cc_:cc_ + 1], gts[:, qt, 3 * h + 1 + br_:3 * h + 2 + br_], ALU.mult),
                             reads=[Bst_, Bgts[qt]], writes=[Bst_])
                        K.op("dve", lambda e: e.scalar_tensor_tensor(y_o[:, h * 64:(h + 1) * 64], acc[:, co:co + 64], st_[:, cc_:cc_ + 1], y_o[:, h * 64:(h + 1) * 64],
                                                                     ALU.mult, ALU.add),
                             reads=[Bacc, Bst_, By_o], writes=[By_o])
                tm_groupnorm(P.psg, y_o, By_o, yst_, Byst, P.yb, 3, l, qt)
            IL.run(NT, 4, _mk_na, _body_na)
            dbg_dump(3)
            wout_partial(es, l, 3)
            phase_end(es)

        combT = sb(top, "combT", [128, T], BF16)
        BcombT = [Buf(f"combT{c}") for c in range(4)]
        K.op("pool", lambda e: e.memset(combT[:], 0.0), writes=BcombT)
        gon = sb(top, "gon", [128, 4, 256], F32); Bgon = Buf("gon")
        gcolt = sb(top, "gcolt", [128, 8], F32); Bgcol = Buf("gcolt")

        io_box = {"final": False, "stored": set(), "loaded": set()}
        for s in range(n_seq):
            for i in range(NT):
                if (s, i) not in io_box["loaded"]:
                    K.dma("sp", x_sb[:, i, :], dr["x"][s, i * 128:(i + 1) * 128, :], writes=Bx[i])
            K.dma("sp", posi[:], dr["pos"][s].rearrange("(n p) -> p n", p=128), writes=[Bposi], allow_slow_non_contiguous=True)
            K.op("dve", lambda e: e.tensor_copy(posf[:], posi[:]), reads=[Bposi], writes=[Bposf])
            for l in range(depth):
                for m in range(4):
                    K.dma("sp", gon[:, m, :], dr["out_norm"][l, m:m + 1, :].to_broadcast([128, 256]), writes=[Bgon])
                K.dma("sp", gcolt[:], dr["out_norm_t"][l], writes=[Bgcol])
                l_box[0] = l
                io_box["final"] = (l == depth - 1) and ("moe" in phases)
                norm_phase(s, l, "mix_norm", False)
                if "mla" in phases:
                    mla_phase(s, l)
                if "lru" in phases:
                    lru_phase(s, l)
                if "s5" in phases:
                    s5_phase(s, l)
                if "nsa" in phases:
                    nsa_phase(s, l)
                if "moe" in phases:
                    moe_phase(s, l)
            if dbg:
                K.dma("pool", dbg_d["xnT"][:], xnT[:], reads=[b for t_ in BxnT for b in t_], writes=[Bout])
                for i in range(NT):
                    K.dma("sp", dbg_d["x"][i * 128:(i + 1) * 128, :], x_sb[:, i, :], reads=Bx[i], writes=[Bout])
            for i in range(NT):
                if (s, i) not in io_box["stored"]:
                    K.dma("sp", out_d[s, i * 128:(i + 1) * 128, :], x_sb[:, i, :], reads=Bx[i], writes=[Bout])
            K.barrier_all()
        K.barrier_all()
        K.close()
    return nc, K


def prep_weights(inp):
    f = np.float32
    L = DEPTH
    w = {}
    for k in ("mix_norm", "ffn_norm", "w_in", "w_out", "mla_g_cq", "mla_g_ckv", "mla_w_uq", "mla_w_ukv", "mla_g_q", "mla_g_k",
              "s5_w_glu", "nsa_g_q", "nsa_g_k", "out_norm", "moe_w_gate", "moe_w_up", "moe_w_down"):
        w[k] = np.ascontiguousarray(inp[k], dtype=f)
    def pc(v):
        return np.ascontiguousarray(np.asarray(v, f).reshape(L, 2, 128).transpose(0, 2, 1))
    w["lru_cw"] = np.ascontiguousarray(np.asarray(inp["lru_conv_w"], f).reshape(L, 4, 2, 128).transpose(0, 3, 2, 1))
    w["lru_vec"] = np.ascontiguousarray(np.stack([pc(inp["lru_conv_b"]), pc(np.asarray(inp["lru_b_a"]).reshape(L, 256)),
                                                  pc(np.asarray(inp["lru_b_i"]).reshape(L, 256)), pc(inp["lru_lambda"]),
                                                  np.zeros((L, 128, 2), f)], axis=2))
    for nm, src in (("lru_wa", "lru_w_a"), ("lru_wi", "lru_w_i")):
        a = np.zeros((L, 2, 128, 128), f)
        W = np.asarray(inp[src], f)
        for c in range(2):
            for hh in range(2):
                a[:, c, hh * 64:(hh + 1) * 64, hh * 64:(hh + 1) * 64] = W[:, 2 * c + hh]
        w[nm] = a
    def st(v):
        return np.asarray(v, f).reshape(L, 8, 128).transpose(0, 2, 1)
    ldt = np.repeat(np.asarray(inp["s5_log_dt"], f)[:, :, None], 64, axis=2)
    w["s5_par"] = np.ascontiguousarray(np.stack([st(inp["s5_a_re"]), st(inp["s5_a_im"]), st(ldt)], axis=2))
    def stb(v):
        return np.asarray(v, f).reshape(L, 8, 128, 16).transpose(0, 2, 1, 3)
    w["s5_b"] = np.ascontiguousarray(np.stack([stb(inp["s5_b_re"]), stb(inp["s5_b_im"])], axis=2))
    cpad = np.zeros((L, 2, 8, 128, 128), f)
    for ri, nm in enumerate(("s5_c_re", "s5_c_im")):
        Cm = np.asarray(inp[nm], f)
        for g in range(16):
            j = g // 2
            rows = slice((g % 2) * 64, (g % 2) * 64 + 64)
            cols = slice((16 * g) % 128, (16 * g) % 128 + 16)
            cpad[:, ri, j, rows, cols] = Cm[:, g].transpose(0, 2, 1)
    w["s5_c"] = cpad
    w["s5_vec"] = np.ascontiguousarray(np.stack([pc(inp["s5_d"]), pc(inp["s5_b_glu"]), np.zeros((L, 128, 2), f)], axis=2))
    w["nsa_pe"] = np.ascontiguousarray(np.stack([np.asarray(inp["nsa_pe_k"], f).transpose(0, 2, 1),
                                                 np.asarray(inp["nsa_pe_v"], f).transpose(0, 2, 1)], axis=1))
    w["nsa_w1"] = np.ascontiguousarray(np.stack([np.asarray(inp["nsa_w1_k"], f).reshape(L, 32, 64, 128).transpose(0, 2, 1, 3),
                                                 np.asarray(inp["nsa_w1_v"], f).reshape(L, 32, 64, 128).transpose(0, 2, 1, 3)], axis=1))
    w["nsa_w2"] = np.ascontiguousarray(np.stack([np.asarray(inp["nsa_w2_k"], f), np.asarray(inp["nsa_w2_v"], f)], axis=1))
    w["out_norm_t"] = np.ascontiguousarray(np.asarray(inp["out_norm"], f).reshape(L, 8, 128).transpose(0, 2, 1))
    w["moe_wr"] = np.ascontiguousarray(np.concatenate([np.asarray(inp["moe_w_rg"], f), np.asarray(inp["moe_w_re"], f)], axis=2))
    w["moe_br"] = np.ascontiguousarray(np.concatenate([np.asarray(inp["moe_b_rg"], f), np.asarray(inp["moe_b_re"], f)], axis=1))
    w["c_ident"] = np.eye(128, dtype=f)
    kk = np.arange(128)[:, None]; qq = np.arange(128)[None, :]
    w["c_tri"] = (qq >= kk).astype(f)
    w["c_anti"] = (kk > qq).astype(f)
    cc = np.arange(128)[:, None]; tq = np.arange(T)[None, :]
    w["c_cmpmask"] = ((16 * cc + 31 <= tq) & (cc < 127)).astype(f)
    csn = np.arange(127) * 16; ssn = np.arange(32) * 64
    ov = np.clip(np.minimum(csn[:, None] + 32, ssn[None, :] + 64) - np.maximum(csn[:, None], ssn[None, :]), 0, None) / 16.0
    ovp = np.zeros((128, 32), f); ovp[:127] = ov
    w["c_ov"] = ovp
    tpos = np.arange(T); cur = tpos // 64; sbk = np.arange(32)
    forced = (sbk[None, :] == 0) | (sbk[None, :] == cur[:, None]) | (sbk[None, :] == cur[:, None] - 1)
    future = sbk[None, :] > cur[:, None]
    keep = (~forced & ~future).astype(f)
    base = np.where(future, -1e30, np.where(forced, 1e30, 0.0)).astype(f)
    w["c_keep"] = np.ascontiguousarray(keep.reshape(NT, 128, 32).transpose(1, 0, 2))
    w["c_base"] = np.ascontiguousarray(base.reshape(NT, 128, 32).transpose(1, 0, 2))
    E = np.zeros((32, NT, 128), f)
    for kt in range(NT):
        for m_ in range(128):
            E[2 * kt + m_ // 64, kt, m_] = BIGNEG
    w["c_E"] = E
    w["c_inv_mla"] = np.tile((500000.0 ** (-np.arange(16, dtype=np.float64) * 2.0 / 32)).astype(f)[None, :], (128, 1))
    w["c_inv_nsa"] = np.tile((500000.0 ** (-np.arange(8, dtype=np.float64) * 2.0 / 16)).astype(f)[None, :], (128, 1))
    sE = np.zeros((32, 16, 128), f)
    for e_ in range(16):
        sE[e_, e_, :] = 1.0
        sE[16 + e_, e_, :] = 1.0
    w["c_selE"] = sE
    for k, shp in W_SPECS.items():
        assert list(w[k].shape) == shp, (k, w[k].shape, shp)
    return w


_CACHE = {}


def kernel(**inputs):
    n_cores = 8
    x = np.ascontiguousarray(inputs["x"], dtype=np.float32)
    pos = np.ascontiguousarray(inputs["positions"], dtype=np.int32)
    w = prep_weights(inputs)
    if "nc" not in _CACHE:
        _CACHE["nc"] = build_program(n_seq=2)[0]
    nc = _CACHE["nc"]
    in_maps = []
    for c in range(n_cores):
        m = dict(w)
        m["x"] = x[2 * c:2 * c + 2]
        m["pos"] = pos[2 * c:2 * c + 2]
        in_maps.append(m)
    res = run_bass_kernel_spmd(nc, in_maps, core_ids=list(range(n_cores)))
    return np.concatenate([r["out"] for r in res.results], axis=0)
```

```python
import numpy as np
from contextlib import ExitStack
import concourse.bass as bass
import concourse.mybir as mybir
from concourse.bass_utils import run_bass_kernel_spmd

F32 = mybir.dt.float32
BF16 = mybir.dt.bfloat16
I32 = mybir.dt.int32
AF = mybir.ActivationFunctionType
ALU = mybir.AluOpType
AX = mybir.AxisListType

T = 2048
NT = 16
D = 1024
DC = 8
DEPTH = 2
EPS = 1e-6
TWO_PI = 6.283185307179586
BIGNEG = 30000.0
EPOCH = 30000


class Buf:
    __slots__ = ("name", "last_w", "readers", "excl")

    def __init__(self, name, excl=False):
        self.name = name
        self.last_w = None
        self.readers = []
        self.excl = excl


class Prod:
    def __init__(self, K, key, step):
        self.K = K
        self.key = key
        self.step = step
        self.count = 0
        self.sems = []

    def sem_for(self, idx):
        ep = idx // EPOCH
        while len(self.sems) <= ep:
            self.sems.append(self.K.new_sem(f"{self.key}_{len(self.sems)}"))
        return self.sems[ep], ((idx % EPOCH) + 1) * self.step, ep


class _PEProxy:
    def __init__(self, real):
        self.real = real
        self.last_stop = True

    def matmul(self, *a, **k):
        self.last_stop = bool(k.get("stop", True))
        return self.real.matmul(*a, **k)

    def transpose(self, *a, **k):
        self.last_stop = True
        return self.real.transpose(*a, **k)


class Kern:
    def __init__(self, nc, n_dma_lanes=16):
        self.nc = nc
        self._sem_ctx = []
        self.prods = {}
        self.engs = {"pe": nc.tensor, "act": nc.scalar, "dve": nc.vector, "pool": nc.gpsimd, "sp": nc.sync}
        for k in self.engs:
            self.prods[k] = Prod(self, k, 1)
        self.lanes = {}
        self.lane_rr = {}
        for q in ("sp", "pool", "act"):
            self.lanes[q] = []
            self.lane_rr[q] = 0
            for i in range(n_dma_lanes // 2):
                p = Prod(self, f"dma_{q}{i}", 16)
                self.prods[p.key] = p
                self.lanes[q].append(p)
        self._pe_proxy = _PEProxy(nc.tensor)
        self.bar_scratch = None
        self._switch = None
        self.waited = {}
        self.n_inst = 0
        self.n_wait = 0

    def new_sem(self, name):
        ctx = self.nc.semaphore(name)
        s = ctx.__enter__()
        self._sem_ctx.append(ctx)
        return s

    def close(self):
        for c in reversed(self._sem_ctx):
            c.__exit__(None, None, None)
        self._sem_ctx = []

    def _deps(self, me_key, reads, writes):
        deps = set()
        for b in reads:
            if b.last_w is not None:
                deps.add(b.last_w)
            if b.excl:
                for r in b.readers:
                    if r[0] != me_key:
                        deps.add(r)
        for b in writes:
            if b.last_w is not None:
                deps.add(b.last_w)
            deps.update(b.readers)
        return deps

    def _emit_waits(self, engname, deps, self_key=None, attach=False):
        eng = self.engs[engname]
        need = {}
        for (pk, idx) in deps:
            if pk == self_key and pk == "pe":
                continue
            sem, val, ep = self.prods[pk].sem_for(idx)
            k = (pk, ep)
            if need.get(k, (None, 0))[1] < val:
                need[k] = (sem, val)
        pend = []
        for (pk, ep), (sem, val) in need.items():
            wk = (engname, pk, ep)
            if self.waited.get(wk, 0) >= val:
                continue
            pend.append((sem, val))
            self.waited[wk] = val
        last = pend.pop() if (attach and pend) else None
        for (sem, val) in pend:
            eng.wait_ge(sem, val)
            self.n_wait += 1
        return last

    def _record(self, me, reads, writes):
        for b in reads:
            b.readers.append(me)
            if len(b.readers) > 48:
                b.readers = b.readers[-48:]
        for b in writes:
            b.last_w = me
            b.readers = []

    def op(self, engname, fn, reads=(), writes=()):
        prod = self.prods[engname]
        deps = self._deps(engname, reads, writes)
        last = self._emit_waits(engname, deps, self_key=engname, attach=True)
        if engname == "pe":
            self._pe_proxy.last_stop = True
            ins = fn(self._pe_proxy)
            inc = self._pe_proxy.last_stop
        else:
            ins = fn(self.engs[engname])
            inc = True
        if last is not None:
            ins._wait_ge(last[0], last[1])
        idx = prod.count
        self.n_inst += 1
        if inc:
            sem, val, ep = prod.sem_for(idx)
            ins.then_inc(sem, 1)
            prod.count += 1
        self._record((engname, idx), reads, writes)
        if self._switch is not None:
            self._switch()
        return ins

    def dma(self, qname, out, in_, reads=(), writes=(), **kw):
        lane = self.lanes[qname][self.lane_rr[qname]]
        self.lane_rr[qname] = (self.lane_rr[qname] + 1) % len(self.lanes[qname])
        deps = self._deps(lane.key, reads, writes)
        if lane.count > 0:
            deps.add((lane.key, lane.count - 1))
        last = self._emit_waits(qname, deps, attach=True)
        idx = lane.count
        sem, val, ep = lane.sem_for(idx)
        ins = self.engs[qname].dma_start(out=out, in_=in_, **kw)
        if last is not None:
            ins._wait_ge(last[0], last[1])
        ins.then_inc(sem, 16)
        lane.count += 1
        self.n_inst += 1
        self._record((lane.key, idx), reads, writes)
        if self._switch is not None:
            self._switch()
        return ins

    def barrier_all(self):
        deps = set()
        for pk, p in self.prods.items():
            if p.count > 0:
                deps.add((pk, p.count - 1))
        if self.bar_scratch is None:
            for e in self.engs:
                self._emit_waits(e, deps)
            return
        self._emit_waits("pool", deps)
        snap = {k: v for k, v in self.waited.items() if k[0] == "pool"}
        sw = self._switch
        self._switch = None
        scr = self.bar_scratch
        self.op("pool", lambda e: e.memset(scr, 0.0))
        self._switch = sw
        idx = self.prods["pool"].count - 1
        for e in self.engs:
            if e == "pool":
                continue
            self._emit_waits(e, {("pool", idx)})
            for (_, pk, ep), val in snap.items():
                wk = (e, pk, ep)
                if self.waited.get(wk, 0) < val:
                    self.waited[wk] = val


class NS:
    def __init__(self, **kw):
        self.__dict__.update(kw)


class Interleaver:
    def __init__(self, K):
        self.K = K

    def run(self, n, W, mk, body):
        import threading
        K = self.K
        W = max(1, min(W, n))
        ctxs = [mk(w) for w in range(W)]
        if W == 1:
            for i in range(n):
                body(i, ctxs[0])
            return
        sems = [threading.Semaphore(0) for _ in range(W)]
        alive = [True] * W
        done = threading.Event()
        err = []
        state = {"cur": 0}

        def next_live(w):
            for d in range(1, W + 1):
                v = (w + d) % W
                if alive[v]:
                    return v
            return None

        def switch():
            w = state["cur"]
            v = next_live(w)
            if v is None or v == w:
                return
            state["cur"] = v
            sems[v].release()
            sems[w].acquire()

        def worker(w):
            sems[w].acquire()
            try:
                if not err:
                    for i in range(w, n, W):
                        body(i, ctxs[w])
                        if err:
                            break
            except BaseException as e:
                err.append(e)
            alive[w] = False
            v = next_live(w)
            if v is None:
                done.set()
            else:
                state["cur"] = v
                sems[v].release()

        ths = [threading.Thread(target=worker, args=(w,)) for w in range(W)]
        for t in ths:
            t.start()
        K._switch = switch
        state["cur"] = 0
        sems[0].release()
        done.wait()
        K._switch = None
        for t in ths:
            t.join()
        if err:
            raise err[0]


class Pool:
    _uid = [0]

    def __init__(self, es, nc, name, shape, dtype, n, psum=False):
        self.items = []
        Pool._uid[0] += 1
        name = f"{name}_u{Pool._uid[0]}_"
        for i in range(n):
            if psum:
                t = es.enter_context(nc.psum_tensor(f"{name}{i}", shape, dtype))
            else:
                t = es.enter_context(nc.sbuf_tensor(f"{name}{i}", shape, dtype))
            self.items.append((t, Buf(f"{name}{i}", excl=psum)))
        self.i = 0

    def get(self):
        it = self.items[self.i]
        self.i = (self.i + 1) % len(self.items)
        return it

    def sub(self, idxs):
        p = Pool.__new__(Pool)
        p.items = [self.items[k] for k in idxs]
        p.i = 0
        return p


W_SPECS = {
    "mix_norm": [DEPTH, D], "ffn_norm": [DEPTH, D], "w_in": [DEPTH, D, 1772], "w_out": [DEPTH, D, D],
    "mla_g_cq": [DEPTH, 192], "mla_g_ckv": [DEPTH, 128], "mla_w_uq": [DEPTH, 192, 384],
    "mla_w_ukv": [DEPTH, 128, 512], "mla_g_q": [DEPTH, 96], "mla_g_k": [DEPTH, 96],
    "lru_cw": [DEPTH, 128, 2, 4], "lru_vec": [DEPTH, 128, 5, 2], "lru_wa": [DEPTH, 2, 128, 128],
    "lru_wi": [DEPTH, 2, 128, 128],
    "s5_par": [DEPTH, 128, 3, 8], "s5_b": [DEPTH, 128, 2, 8, 16], "s5_c": [DEPTH, 2, 8, 128, 128],
    "s5_vec": [DEPTH, 128, 3, 2], "s5_w_glu": [DEPTH, 256, 256],
    "nsa_g_q": [DEPTH, 64], "nsa_g_k": [DEPTH, 3, 64], "nsa_pe": [DEPTH, 2, 64, 32],
    "nsa_w1": [DEPTH, 2, 64, 32, 128], "nsa_w2": [DEPTH, 2, 128, 64],
    "out_norm": [DEPTH, 4, 256], "out_norm_t": [DEPTH, 128, 8],
    "moe_wr": [DEPTH, D, 20], "moe_br": [DEPTH, 20],
    "moe_w_gate": [DEPTH, 16, D, 256], "moe_w_up": [DEPTH, 16, D, 256], "moe_w_down": [DEPTH, 16, 256, D],
    "c_ident": [128, 128], "c_tri": [128, 128], "c_anti": [128, 128], "c_cmpmask": [128, T],
    "c_ov": [128, 32], "c_keep": [128, NT, 32], "c_base": [128, NT, 32], "c_E": [32, NT, 128],
    "c_inv_mla": [128, 16], "c_inv_nsa": [128, 8], "c_selE": [32, 16, 128],
}


def build_program(n_seq=2, depth=DEPTH, dbg=None, phases=("mla", "lru", "s5", "nsa", "moe")):
    nc = bass.Bass("TRN2", target_bir_lowering=False)
    dr = {}
    dr["x"] = nc.dram_tensor("x", [n_seq, T, D], F32, kind="ExternalInput").ap()
    dr["pos"] = nc.dram_tensor("pos", [n_seq, T], I32, kind="ExternalInput").ap()
    for k, shp in W_SPECS.items():
        dr[k] = nc.dram_tensor(k, shp, F32, kind="ExternalInput").ap()
    out_d = nc.dram_tensor("out", [n_seq, T, D], F32, kind="ExternalOutput").ap()
    dbg_d = {}
    if dbg:
        dbg_d["ymT"] = nc.dram_tensor("dbg_ymT", [4, 128, 2, T], F32, kind="ExternalOutput").ap()
        dbg_d["x"] = nc.dram_tensor("dbg_x", [T, D], F32, kind="ExternalOutput").ap()
        dbg_d["xnT"] = nc.dram_tensor("dbg_xnT", [128, DC, T], F32, kind="ExternalOutput").ap()

    K = Kern(nc)
    IL = Interleaver(K)
    Bout = Buf("out")
    with ExitStack() as top:
        def sb(es, name, shape, dt):
            Pool._uid[0] += 1
            return es.enter_context(nc.sbuf_tensor(f"{name}_u{Pool._uid[0]}", shape, dt))

        x_sb = sb(top, "x_sb", [128, NT, D], F32)
        Bx = [[Buf(f"x{i}_{h}") for h in range(2)] for i in range(NT)]
        xnT = sb(top, "xnT", [128, DC, T], BF16)
        BxnT = [[Buf(f"xnT{i}_{h}") for h in range(2)] for i in range(NT)]
        ymT_box = {}
        l_box = [0]

        def xnT_bufs(ch):
            return [BxnT[i][h] for i in range(ch * 4, ch * 4 + 4) for h in range(2)]

        ident = sb(top, "ident", [128, 128], F32); Bident = Buf("ident")
        identb = sb(top, "identb", [128, 128], BF16); Bidentb = Buf("identb")
        ones16 = sb(top, "ones16", [128, 128], BF16); Bones = Buf("ones16")
        tri4 = sb(top, "tri4", [128, 4, 128], BF16); Btri = Buf("tri4")
        anti4 = sb(top, "anti4", [128, 4, 128], BF16); Banti = Buf("anti4")
        posf = sb(top, "posf", [128, NT], F32); Bposf = Buf("posf")
        posi = sb(top, "posi", [128, NT], I32); Bposi = Buf("posi")
        gain = sb(top, "gain", [128, D], F32); Bgain = Buf("gain")
        ss = sb(top, "ss", [128, NT], F32); Bss = Buf("ss")
        rs = sb(top, "rs", [128, NT], F32); Brs = Buf("rs")
        epsb = sb(top, "epsb", [128, 1], F32); Beps = Buf("epsb")
        barscr = sb(top, "barscr", [128, 4], F32)
        K.bar_scratch = barscr[:, 0:1]

        psg = Pool(top, nc, "psg", [128, 512], F32, 6, psum=True)
        psa = Pool(top, nc, "psa", [128, 512], F32, 2, psum=True)
        allps = psg.sub(range(6))
        allps.items = psg.items + psa.items

        K.dma("sp", ident[:], dr["c_ident"][:], writes=[Bident])
        K.op("act", lambda e: e.copy(identb[:], ident[:]), reads=[Bident], writes=[Bidentb])
        K.op("dve", lambda e: e.memset(ones16[:], 1.0), writes=[Bones])
        K.op("dve", lambda e: e.memset(epsb[:], EPS), writes=[Beps])
        for h in range(4):
            K.dma("pool", tri4[:, h, :], dr["c_tri"][:], writes=[Btri])
            K.dma("pool", anti4[:, h, :], dr["c_anti"][:], writes=[Banti])

        def _zeroed(pool):
            for (t_, b_) in pool.items:
                K.op("pool", lambda e: e.memset(t_[:], 0.0), writes=[b_])
            return pool

        def phase_end(es):
            K.barrier_all()
            es.close()

        def rstd_from(out_ap, in_ap, n_feat, reads, writes, eng_tmp=None):
            K.op("act", lambda e: e.activation(out_ap, in_ap, AF.Sqrt, bias=epsb[0:out_ap.shape[0], 0:1], scale=1.0 / n_feat),
                 reads=list(reads) + [Beps], writes=writes)
            K.op("dve", lambda e: e.reciprocal(out_ap, out_ap), reads=writes, writes=writes)

        def norm_phase(s, l, which, router):
            es = ExitStack()
            tmpA = Pool(es, nc, "nrmA", [128, D], BF16, 2)
            K.dma("sp", gain[:], dr[which][l:l + 1, :].to_broadcast([128, D]), writes=[Bgain])
            if router:
                wr = sb(es, "wr", [128, DC, 20], F32); Bwr = Buf("wr")
                br = sb(es, "br", [128, 20], F32); Bbr = Buf("br")
                K.dma("sp", wr[:], dr["moe_wr"][l].rearrange("(c p) n -> p c n", p=128), writes=[Bwr])
                K.dma("sp", br[:], dr["moe_br"][l:l + 1, :].to_broadcast([128, 20]), writes=[Bbr])
            for i in range(NT):
                junk, Bj = tmpA.get()
                K.op("act", lambda e: e.activation(junk[:], x_sb[:, i, :], AF.Square, accum_out=ss[:, i:i + 1]),
                     reads=Bx[i], writes=[Bj, Bss])
            rstd_from(rs[:, :], ss[:, :], D, [Bss], [Brs])
            def _mk_nr(w):
                per = 8 // 2
                d_ = dict(psg=allps.sub(range(w * per, w * per + per)), tmpA=Pool(es, nc, "nrmX", [128, D], F32, 1))
                if router:
                    d_["xT32"] = Pool(es, nc, "xT32", [128, DC, 128], F32, 1)
                    d_["rt"] = Pool(es, nc, "rt", [128, 96], F32, 2)
                else:
                    d_["xb"] = Pool(es, nc, "nrmB", [128, D], BF16, 1)
                return NS(**d_)

            def _body_nr(i, P):
                if not router:
                    xb, Bxb = P.xb.get()
                    K.op("dve", lambda e: e.scalar_tensor_tensor(xb[:], x_sb[:, i, :], rs[:, i:i + 1], gain[:], ALU.mult, ALU.mult),
                         reads=Bx[i] + [Brs, Bgain], writes=[Bxb])
                    pb, Bpb = P.psg.get()
                    pbb = pb[:, :].bitcast(BF16)
                    for c in range(DC):
                        K.op("pe", lambda e: e.transpose(pbb[:, c * 128:(c + 1) * 128], xb[:, c * 128:(c + 1) * 128], identb[:]),
                             reads=[Bxb, Bidentb], writes=[Bpb])
                    K.op("act", lambda e: e.copy(xnT[:, :, i * 128:(i + 1) * 128], pbb[:, :].rearrange("p (c t) -> p c t", c=DC)),
                         reads=[Bpb], writes=[BxnT[i][0], BxnT[i][1]])
                    return
                xn, Bxn = P.tmpA.get()
                K.op("dve", lambda e: e.scalar_tensor_tensor(xn[:], x_sb[:, i, :], rs[:, i:i + 1], gain[:], ALU.mult, ALU.mult),
                     reads=Bx[i] + [Brs, Bgain], writes=[Bxn])
                if router:
                    xt, Bxt = P.xT32.get()
                for half in range(2):
                    pb, Bpb = P.psg.get()
                    for cc in range(4):
                        c = half * 4 + cc
                        K.op("pe", lambda e: e.transpose(pb[:, cc * 128:(cc + 1) * 128], xn[:, c * 128:(c + 1) * 128], ident[:]),
                             reads=[Bxn, Bident], writes=[Bpb])
                    src = pb[:, :].rearrange("p (c t) -> p c t", c=4)
                    K.op("act", lambda e: e.copy(xnT[:, half * 4:half * 4 + 4, i * 128:(i + 1) * 128], src),
                         reads=[Bpb], writes=[BxnT[i][half]])
                    if router:
                        K.op("dve", lambda e: e.tensor_copy(xt[:, half * 4:half * 4 + 4, :], src), reads=[Bpb], writes=[Bxt])
                if router:
                    lg, Blg = P.psg.get()
                    for c in range(DC):
                        K.op("pe", lambda e: e.matmul(lg[:, 0:20], xt[:, c, :], wr[:, c, :], start=(c == 0), stop=(c == DC - 1)),
                             reads=[Bxt, Bwr], writes=[Blg])
                    r, Br_ = P.rt.get()
                    R = [Br_]
                    Lg = r[:, 0:20]; m = r[:, 20:21]; nm = r[:, 21:22]; e4 = r[:, 22:26]; se = r[:, 26:27]
                    oh = r[:, 27:31]; pen = r[:, 31:35]; lem = r[:, 35:51]; top8 = r[:, 51:59]; sel = r[:, 59:75]
                    nv1 = r[:, 75:76]; den = r[:, 76:77]; fac = r[:, 77:78]
                    r2, Br2 = P.rt.get()
                    ew = r2[:, 0:16]; sw = r2[:, 16:32]; comb = r2[:, 32:48]
                    R2 = [Br2]
                    K.op("dve", lambda e: e.tensor_tensor(Lg, lg[:, 0:20], br[:], ALU.add), reads=[Blg, Bbr], writes=R)
                    K.op("dve", lambda e: e.tensor_reduce(m, r[:, 0:4], AX.X, ALU.max), reads=R, writes=R)
                    K.op("dve", lambda e: e.tensor_scalar(nm, m, -1.0, None, ALU.mult), reads=R, writes=R)
                    K.op("act", lambda e: e.activation(e4, r[:, 0:4], AF.Exp, bias=nm, accum_out=se), reads=R, writes=R)
                    K.op("dve", lambda e: e.tensor_scalar(oh, r[:, 0:4], m, None, ALU.is_ge), reads=R, writes=R)
                    K.op("dve", lambda e: e.tensor_scalar(pen, oh, 1.0, 1e30, ALU.subtract, ALU.mult), reads=R, writes=R)
                    K.op("dve", lambda e: e.tensor_tensor(lem.rearrange("p (g i) -> p g i", g=4),
                                                          r[:, 4:20].rearrange("p (g i) -> p g i", g=4),
                                                          pen.unsqueeze(2).to_broadcast([128, 4, 4]), ALU.add), reads=R, writes=R)
                    K.op("dve", lambda e: e.max(top8, lem), reads=R, writes=R)
                    K.op("dve", lambda e: e.tensor_scalar(sel, lem, r[:, 52:53], None, ALU.is_ge), reads=R, writes=R)
                    K.op("dve", lambda e: e.tensor_scalar(nv1, r[:, 51:52], -1.0, None, ALU.mult), reads=R, writes=R)
                    K.op("act", lambda e: e.activation(ew, lem, AF.Exp, bias=nv1), reads=R, writes=R2)
                    K.op("dve", lambda e: e.scalar_tensor_tensor(sw, sel, 1.0, ew, ALU.mult, ALU.mult, accum_out=den), reads=R + R2, writes=R + R2)
                    K.op("dve", lambda e: e.tensor_tensor(fac, den, se, ALU.mult), reads=R, writes=R)
                    K.op("dve", lambda e: e.reciprocal(fac, fac), reads=R, writes=R)
                    K.op("dve", lambda e: e.tensor_scalar(comb, sw, fac, None, ALU.mult), reads=R + R2, writes=R2)
                    chl = r2[:, 48:64].bitcast(BF16)
                    K.op("dve", lambda e: e.tensor_copy(chl[:, 0:16], comb), reads=R2, writes=R2)
                    K.op("dve", lambda e: e.tensor_copy(r2[:, 64:80], chl[:, 0:16]), reads=R2, writes=R2)
                    K.op("dve", lambda e: e.tensor_tensor(chl[:, 16:32], comb, r2[:, 64:80], ALU.subtract), reads=R2, writes=R2)
                    pt, Bpt = P.psg.get()
                    ptb = pt[:, :].bitcast(BF16)
                    K.op("pe", lambda e: e.transpose(ptb[0:32, 0:128], chl, identb[:]), reads=R2 + [Bidentb], writes=[Bpt])
                    K.op("act", lambda e: e.copy(combT[0:32, i * 128:(i + 1) * 128], ptb[0:32, 0:128]), reads=[Bpt], writes=[BcombT[i // 4]])

            IL.run(NT, 2, _mk_nr, _body_nr)
            phase_end(es)

        def moe_phase(s, l):
            es = ExitStack()
            selE = sb(es, "selE", [128, 16, 128], BF16); BselE = Buf("selE")
            K.op("pool", lambda e: e.memset(selE[:], 0.0), writes=[BselE])
            K.dma("pool", selE[0:32], dr["c_selE"][:], writes=[BselE])
            wgu = Pool(es, nc, "wgu", [128, DC, 512], BF16, 2)
            wdp = Pool(es, nc, "wdp", [128, 2, D], BF16, 2)
            cbp = Pool(es, nc, "cbp", [128, 512], F32, 2)
            sgp = Pool(es, nc, "sgp", [128, 512], F32, 3)
            hep = Pool(es, nc, "hep", [128, 2, 512], BF16, 2)
            def load_expert(ex):
                wg, Bwg = wgu.get()
                wd, Bwd = wdp.get()
                K.dma("pool", wg[:, :, 0:256], dr["moe_w_gate"][l, ex].rearrange("(c p) f -> p c f", p=128), writes=[Bwg])
                K.dma("pool", wg[:, :, 256:512], dr["moe_w_up"][l, ex].rearrange("(c p) f -> p c f", p=128), writes=[Bwg])
                K.dma("pool", wd[:], dr["moe_w_down"][l, ex].rearrange("(c p) f -> p c f", p=128), writes=[Bwd])
                return wg, Bwg, wd, Bwd
            W = {}
            W[0] = load_expert(0)
            norm_phase(s, l, "ffn_norm", True)
            steps = [(ex, ch) for ex in range(16) for ch in range(4)]
            hes = {}

            def stage_a(ex, ch):
                wg, Bwg, wd, Bwd = W[ex]
                cs = slice(ch * 512, (ch + 1) * 512)
                cbps, Bcbps = psg.get()
                K.op("pe", lambda e: e.matmul(cbps[:, :], selE[:, ex, :], combT[:, cs], start=True, stop=True),
                     reads=[BselE, BcombT[ch]], writes=[Bcbps])
                cb, Bcb = cbp.get()
                K.op("act", lambda e: e.copy(cb[:], cbps[:, :]), reads=[Bcbps], writes=[Bcb])
                he, Bhe = hep.get()
                for fc in range(2):
                    gps, Bgps = psg.get()
                    ups, Bups = psg.get()
                    for c in range(DC):
                        K.op("pe", lambda e: e.matmul(gps[:, :], wg[:, c, fc * 128:(fc + 1) * 128], xnT[:, c, cs],
                                                      start=(c == 0), stop=(c == DC - 1)),
                             reads=[Bwg] + xnT_bufs(ch), writes=[Bgps])
                    for c in range(DC):
                        K.op("pe", lambda e: e.matmul(ups[:, :], wg[:, c, 256 + fc * 128:256 + (fc + 1) * 128], xnT[:, c, cs],
                                                      start=(c == 0), stop=(c == DC - 1)),
                             reads=[Bwg] + xnT_bufs(ch), writes=[Bups])
                    sg, Bsg = sgp.get()
                    K.op("act", lambda e: e.activation(sg[:], gps[:, :], AF.Silu), reads=[Bgps], writes=[Bsg])
                    K.op("dve", lambda e: e.tensor_tensor(sg[:], sg[:], cb[:], ALU.mult), reads=[Bsg, Bcb], writes=[Bsg])
                    K.op("dve", lambda e: e.tensor_tensor(he[:, fc, :], sg[:], ups[:, :], ALU.mult), reads=[Bsg, Bups], writes=[Bhe])
                hes[(ex, ch)] = (he, Bhe)

            def stage_b(ex, ch):
                wg, Bwg, wd, Bwd = W[ex]
                he, Bhe = hes.pop((ex, ch))
                for ts in range(4):
                    i = ch * 4 + ts
                    for half in range(2):
                        ops_, Bops = psg.get()
                        for fc in range(2):
                            K.op("pe", lambda e: e.matmul(ops_[:, :], he[:, fc, ts * 128:(ts + 1) * 128],
                                                          wd[:, fc, half * 512:(half + 1) * 512], start=(fc == 0), stop=(fc == 1)),
                                 reads=[Bhe, Bwd], writes=[Bops])
                        xs = x_sb[:, i, half * 512:(half + 1) * 512]
                        K.op("dve", lambda e: e.tensor_tensor(xs, xs, ops_[:, :], ALU.add), reads=[Bops, Bx[i][half]], writes=[Bx[i][half]])
                    if ex == 15 and io_box["final"] and not dbg:
                        K.dma("sp", out_d[s, i * 128:(i + 1) * 128, :], x_sb[:, i, :], reads=Bx[i], writes=[Bout])
                        io_box["stored"].add((s, i))
                        if s + 1 < n_seq:
                            K.dma("sp", x_sb[:, i, :], dr["x"][s + 1, i * 128:(i + 1) * 128, :], writes=Bx[i])
                            io_box["loaded"].add((s + 1, i))

            for k, (ex, ch) in enumerate(steps):
                stage_a(ex, ch)
                if k > 0:
                    pex, pch = steps[k - 1]
                    stage_b(pex, pch)
                    if pch == 3 and ex + 1 < 16:
                        W.pop(pex)
                        W[ex + 1] = load_expert(ex + 1)
                elif ex + 1 < 16:
                    W[1] = load_expert(1)
            stage_b(*steps[-1])
            phase_end(es)

        def alloc_ymT(es, m):
            ymT = sb(es, f"ymT{m}", [128, 2, T], BF16)
            BymT = [Buf(f"ymT{m}_{ch}") for ch in range(4)]
            ymT_box["t"] = ymT; ymT_box["b"] = BymT
            wo = sb(es, f"wo{m}", [128, 2, D], BF16); Bwo = Buf("wo")
            for c in range(2):
                K.dma("pool", wo[:, c, :], dr["w_out"][l_box[0], 256 * m + c * 128:256 * m + (c + 1) * 128, :], writes=[Bwo])
            ymT_box["wo"] = wo; ymT_box["Bwo"] = Bwo
            return ymT, BymT

        def wout_partial(es, l, m):
            ymT = ymT_box["t"]; BymT = ymT_box["b"]
            wo = ymT_box["wo"]; Bwo = ymT_box["Bwo"]
            for i in range(NT):
                for half in range(2):
                    ops_, Bops = psg.get()
                    for c in range(2):
                        K.op("pe", lambda e: e.matmul(ops_[:, :], ymT[:, c, i * 128:(i + 1) * 128], wo[:, c, half * 512:(half + 1) * 512],
                                                      start=(c == 0), stop=(c == 1)),
                             reads=[Bwo, BymT[i // 4]], writes=[Bops])
                    xs = x_sb[:, i, half * 512:(half + 1) * 512]
                    K.op("dve", lambda e: e.tensor_tensor(xs, xs, ops_[:, :], ALU.add), reads=[Bops, Bx[i][half]], writes=[Bx[i][half]])

        def fm_groupnorm(es_phase, es_name, yv, Byv, m, l, gcol):
            es = es_phase
            sqp = Pool(es, nc, es_name + "sq", [128, 2, 512], BF16, 2)
            rsp = Pool(es, nc, es_name + "rs", [128, 512], F32, 2)
            for ch in range(4):
                cs = slice(ch * 512, (ch + 1) * 512)
                sq, Bsq = sqp.get()
                K.op("act", lambda e: e.activation(sq[:, :, :], yv[:, :, cs], AF.Square), reads=[Byv], writes=[Bsq])
                sps, Bsps = psg.get()
                for c in range(2):
                    K.op("pe", lambda e: e.matmul(sps[:, :], ones16[:], sq[:, c, :], start=(c == 0), stop=(c == 1)),
                         reads=[Bones, Bsq], writes=[Bsps])
                rr, Brr = rsp.get()
                rstd_from(rr[:], sps[:, :], 256, [Bsps], [Brr])
                for c in range(2):
                    K.op("dve", lambda e: e.scalar_tensor_tensor(ymT_box["t"][:, c, cs], yv[:, c, cs], gcol[:, 2 * m + c:2 * m + c + 1],
                                                                 rr[:], ALU.mult, ALU.mult),
                         reads=[Byv, Brr, Bgcol], writes=[ymT_box["b"][ch]])

        def dbg_dump(m):
            if dbg:
                K.dma("pool", dbg_d["ymT"][m], ymT_box["t"][:], reads=ymT_box["b"], writes=[Bout])

        def lru_phase(s, l):
            es = ExitStack()
            ymT, BymT = alloc_ymT(es, 1)
            cw = sb(es, "lcw", [128, 2, 4], F32); Bcw = Buf("lcw")
            vec = sb(es, "lvec", [128, 5, 2], F32); Bvec = Buf("lvec")
            K.dma("sp", cw[:], dr["lru_cw"][l], writes=[Bcw])
            K.dma("sp", vec[:], dr["lru_vec"][l], writes=[Bvec])
            wa = sb(es, "lwa", [128, 2, 128], BF16); Bwa = Buf("lwa")
            wi = sb(es, "lwi", [128, 2, 128], BF16); Bwi = Buf("lwi")
            for c in range(2):
                K.dma("pool", wa[:, c, :], dr["lru_wa"][l, c], writes=[Bwa])
                K.dma("pool", wi[:, c, :], dr["lru_wi"][l, c], writes=[Bwi])
            K.op("act", lambda e: e.activation(vec[:, 4, :], vec[:, 3, :], AF.Exp, scale=-1.0), reads=[Bvec], writes=[Bvec])
            K.op("act", lambda e: e.activation(vec[:, 4, :], vec[:, 4, :], AF.Ln, bias=1.0), reads=[Bvec], writes=[Bvec])
            K.op("dve", lambda e: e.tensor_scalar(vec[:, 4, :], vec[:, 4, :], -8.0, None, ALU.mult), reads=[Bvec], writes=[Bvec])
            yv = sb(es, "lyv", [128, 2, T], F32); Byv = Buf("lyv")
            e2 = ExitStack()

            def _mk_l(w):
                per = 8 // 2
                return NS(psg=allps.sub(range(w * per, w * per + per)),
                          xp=Pool(e2, nc, "lxp", [128, T + 3], F32, 1), u=Pool(e2, nc, "lu", [128, T], F32, 1),
                          wl=Pool(e2, nc, "lwl", [128, DC, 256], BF16, 1), tp=Pool(e2, nc, "ltp", [128, 512], F32, 5),
                          u16=Pool(e2, nc, "lu16", [128, 512], BF16, 2))

            def _body_l(c, P):
                xp, Bxp = P.xp.get()
                u, Bu = P.u.get()
                wl, Bwl = P.wl.get()
                K.dma("pool", wl[:, :, 0:128], dr["w_in"][l, :, 352 + c * 128:352 + (c + 1) * 128].rearrange("(k p) n -> p k n", p=128), writes=[Bwl])
                K.dma("pool", wl[:, :, 128:256], dr["w_in"][l, :, 608 + c * 128:608 + (c + 1) * 128].rearrange("(k p) n -> p k n", p=128), writes=[Bwl])
                K.op("dve", lambda e: e.memset(xp[:, 0:3], 0.0), writes=[Bxp])
                for ch in range(4):
                    cs = slice(ch * 512, (ch + 1) * 512)
                    p1, Bp1 = P.psg.get()
                    for k in range(DC):
                        K.op("pe", lambda e: e.matmul(p1[:, :], wl[:, k, 0:128], xnT[:, k, cs], start=(k == 0), stop=(k == DC - 1)),
                             reads=[Bwl] + xnT_bufs(ch), writes=[Bp1])
                    K.op("act", lambda e: e.copy(xp[:, 3 + ch * 512:3 + (ch + 1) * 512], p1[:, :]), reads=[Bp1], writes=[Bxp])
                    p2, Bp2 = P.psg.get()
                    for k in range(DC):
                        K.op("pe", lambda e: e.matmul(p2[:, :], wl[:, k, 128:256], xnT[:, k, cs], start=(k == 0), stop=(k == DC - 1)),
                             reads=[Bwl] + xnT_bufs(ch), writes=[Bp2])
                    K.op("act", lambda e: e.activation(yv[:, c, cs], p2[:, :], AF.Gelu_apprx_tanh), reads=[Bp2], writes=[Byv])
                K.op("dve", lambda e: e.tensor_scalar(u[:], xp[:, 0:T], cw[:, c, 0:1], vec[:, 0, c:c + 1], ALU.mult, ALU.add),
                     reads=[Bxp, Bcw, Bvec], writes=[Bu])
                for j in range(1, 4):
                    K.op("dve", lambda e: e.scalar_tensor_tensor(u[:], xp[:, j:j + T], cw[:, c, j:j + 1], u[:], ALU.mult, ALU.add),
                         reads=[Bxp, Bcw, Bu], writes=[Bu])
                for ch in range(4):
                    cs = slice(ch * 512, (ch + 1) * 512)
                    u16, Bu16 = P.u16.get()
                    K.op("act", lambda e: e.copy(u16[:], u[:, cs]), reads=[Bu], writes=[Bu16])
                    pa, Bpa = P.psg.get()
                    K.op("pe", lambda e: e.matmul(pa[:, :], wa[:, c, :], u16[:], start=True, stop=True), reads=[Bwa, Bu16], writes=[Bpa])
                    pi, Bpi = P.psg.get()
                    K.op("pe", lambda e: e.matmul(pi[:, :], wi[:, c, :], u16[:], start=True, stop=True), reads=[Bwi, Bu16], writes=[Bpi])
                    r_, Br_ = P.tp.get()
                    gi, Bgi = P.tp.get()
                    mu, Bmu = P.tp.get()
                    aa, Baa = P.tp.get()
                    K.op("act", lambda e: e.activation(r_[:], pa[:, :], AF.Sigmoid, bias=vec[:, 1, c:c + 1]), reads=[Bpa, Bvec], writes=[Br_])
                    K.op("act", lambda e: e.activation(gi[:], pi[:, :], AF.Sigmoid, bias=vec[:, 2, c:c + 1]), reads=[Bpi, Bvec], writes=[Bgi])
                    K.op("act", lambda e: e.activation(aa[:], r_[:], AF.Exp, scale=vec[:, 4, c:c + 1]), reads=[Br_, Bvec], writes=[Baa])
                    K.op("act", lambda e: e.activation(mu[:], aa[:], AF.Square), reads=[Baa], writes=[Bmu])
                    K.op("act", lambda e: e.activation(mu[:], mu[:], AF.Sqrt, scale=-1.0, bias=1.0), reads=[Bmu], writes=[Bmu])
                    if ch == 0:
                        K.op("dve", lambda e: e.memset(mu[:, 0:1], 1.0), reads=[Bmu], writes=[Bmu])
                    K.op("dve", lambda e: e.tensor_tensor(mu[:], mu[:], gi[:], ALU.mult), reads=[Bmu, Bgi], writes=[Bmu])
                    K.op("dve", lambda e: e.tensor_tensor(xp[:, cs], mu[:], u[:, cs], ALU.mult), reads=[Bmu, Bu, Bxp], writes=[Bxp])
                    init = 0.0 if ch == 0 else u[:, ch * 512 - 1:ch * 512]
                    K.op("dve", lambda e: e.tensor_tensor_scan(u[:, cs], aa[:], xp[:, cs], init, ALU.mult, ALU.add),
                         reads=[Baa, Bxp, Bu], writes=[Bu])
                    K.op("dve", lambda e: e.tensor_tensor(yv[:, c, cs], yv[:, c, cs], u[:, cs], ALU.mult), reads=[Byv, Bu], writes=[Byv])
            IL.run(2, 2, _mk_l, _body_l)
            K.barrier_all()
            e2.close()
            fm_groupnorm(es, "lg", yv, Byv, 1, l, gcolt)
            dbg_dump(1)
            wout_partial(es, l, 1)
            phase_end(es)

        def s5_phase(s, l):
            es = ExitStack()
            ymT, BymT = alloc_ymT(es, 2)
            par = sb(es, "s5par", [128, 3, 8], F32); Bpar = Buf("s5par")
            bb = sb(es, "s5b", [128, 2, 8, 16], F32); Bbb = Buf("s5b")
            vec = sb(es, "s5vec", [128, 3, 2], F32); Bvec = Buf("s5vec")
            K.dma("sp", par[:], dr["s5_par"][l], writes=[Bpar])
            K.dma("sp", bb[:], dr["s5_b"][l], writes=[Bbb])
            K.dma("sp", vec[:], dr["s5_vec"][l], writes=[Bvec])
            wglu = sb(es, "s5glu", [128, 2, 256], BF16); Bwglu = Buf("s5glu")
            K.dma("pool", wglu[:], dr["s5_w_glu"][l].rearrange("(c p) n -> p c n", p=128), writes=[Bwglu])
            ypre = sb(es, "s5ypre", [128, 2, T], F32); Bypre = Buf("s5ypre")
            u16 = sb(es, "s5u16", [128, 2, T], BF16); Bu16 = Buf("s5u16")
            es2 = ExitStack()
            ws = sb(es2, "ws5", [128, DC, 256], BF16); Bws = Buf("ws5")
            K.dma("pool", ws[:], dr["w_in"][l, :, 864:1120].rearrange("(c p) n -> p c n", p=128), writes=[Bws])
            for c in range(2):
                for ch in range(4):
                    cs = slice(ch * 512, (ch + 1) * 512)
                    p1, Bp1 = psg.get()
                    for k in range(DC):
                        K.op("pe", lambda e: e.matmul(p1[:, :], ws[:, k, c * 128:(c + 1) * 128], xnT[:, k, cs], start=(k == 0), stop=(k == DC - 1)),
                             reads=[Bws] + xnT_bufs(ch), writes=[Bp1])
                    K.op("act", lambda e: e.activation(ypre[:, c, cs], p1[:, :], AF.Identity, scale=vec[:, 0, c:c + 1]), reads=[Bp1, Bvec], writes=[Bypre])
                    K.op("dve", lambda e: e.tensor_copy(u16[:, c, cs], p1[:, :]), reads=[Bp1], writes=[Bu16])
            es.enter_context(es2)
            sc = sb(es, "s5sc", [128, 16, 8], F32); Bsc = Buf("s5sc")
            SC = [Bsc]
            are = par[:, 0, :]; aim = par[:, 1, :]; ldt = par[:, 2, :]
            dt = sc[:, 0, :]; mag = sc[:, 1, :]; th = sc[:, 2, :]; cth = sc[:, 3, :]; sth = sc[:, 4, :]
            abr = sc[:, 5, :]; abi = sc[:, 6, :]; den = sc[:, 7, :]; gre = sc[:, 8, :]; gim = sc[:, 9, :]
            t0 = sc[:, 10, :]; t1 = sc[:, 11, :]; nre = sc[:, 12, :]
            ki = sb(es, "s5ki", [128, 8], I32); Bki = Buf("s5ki")
            RP = [Bpar, Bsc]
            K.op("act", lambda e: e.activation(dt, ldt, AF.Exp), reads=RP, writes=SC)
            K.op("dve", lambda e: e.tensor_tensor(t0, dt, are, ALU.mult), reads=RP, writes=SC)
            K.op("act", lambda e: e.activation(mag, t0, AF.Exp), reads=RP, writes=SC)
            K.op("dve", lambda e: e.tensor_tensor(th, dt, aim, ALU.mult), reads=RP, writes=SC)
            for (o, shift) in ((sth, 0.0), (cth, 0.5 * np.pi)):
                K.op("dve", lambda e: e.tensor_scalar(ki[:, :], th, shift, 1.0 / TWO_PI, ALU.add, ALU.mult), reads=RP, writes=[Bki])
                K.op("dve", lambda e: e.tensor_copy(t1, ki[:, :]), reads=[Bki], writes=SC)
                K.op("dve", lambda e: e.scalar_tensor_tensor(t1, t1, -TWO_PI, th, ALU.mult, ALU.add), reads=RP, writes=SC)
                K.op("dve", lambda e: e.tensor_scalar(t1, t1, shift, None, ALU.add), reads=RP, writes=SC)
                K.op("dve", lambda e: e.tensor_scalar(t1, t1, 3.14159, -3.14159, ALU.min, ALU.max), reads=RP, writes=SC)
                K.op("act", lambda e: e.activation(o, t1, AF.Sin), reads=RP, writes=SC)
            K.op("dve", lambda e: e.tensor_tensor(abr, mag, cth, ALU.mult), reads=RP, writes=SC)
            K.op("dve", lambda e: e.tensor_tensor(abi, mag, sth, ALU.mult), reads=RP, writes=SC)
            K.op("dve", lambda e: e.tensor_tensor(den, are, are, ALU.mult), reads=RP, writes=SC)
            K.op("dve", lambda e: e.tensor_tensor(t0, aim, aim, ALU.mult), reads=RP, writes=SC)
            K.op("dve", lambda e: e.tensor_tensor(den, den, t0, ALU.add), reads=RP, writes=SC)
            K.op("dve", lambda e: e.reciprocal(den, den), reads=RP, writes=SC)
            K.op("dve", lambda e: e.tensor_scalar(nre, abr, -1.0, None, ALU.add), reads=RP, writes=SC)
            K.op("dve", lambda e: e.tensor_tensor(t0, nre, are, ALU.mult), reads=RP, writes=SC)
            K.op("dve", lambda e: e.tensor_tensor(t1, abi, aim, ALU.mult), reads=RP, writes=SC)
            K.op("dve", lambda e: e.tensor_tensor(gre, t0, t1, ALU.add), reads=RP, writes=SC)
            K.op("dve", lambda e: e.tensor_tensor(gre, gre, den, ALU.mult), reads=RP, writes=SC)
            K.op("dve", lambda e: e.tensor_tensor(t0, abi, are, ALU.mult), reads=RP, writes=SC)
            K.op("dve", lambda e: e.tensor_tensor(t1, nre, aim, ALU.mult), reads=RP, writes=SC)
            K.op("dve", lambda e: e.tensor_tensor(gim, t0, t1, ALU.subtract), reads=RP, writes=SC)
            K.op("dve", lambda e: e.tensor_tensor(gim, gim, den, ALU.mult), reads=RP, writes=SC)
            bbar = sb(es, "s5bbar", [128, 2, 8, 16], F32); Bbbar = Buf("s5bbar")
            btmp = sb(es, "s5btmp", [128, 8, 16], F32); Bbtmp = Buf("s5btmp")
            gre_b = gre.unsqueeze(2).to_broadcast([128, 8, 16]); gim_b = gim.unsqueeze(2).to_broadcast([128, 8, 16])
            K.op("dve", lambda e: e.tensor_tensor(bbar[:, 0], bb[:, 0], gre_b, ALU.mult), reads=[Bbb, Bsc], writes=[Bbbar])
            K.op("dve", lambda e: e.tensor_tensor(btmp[:], bb[:, 1], gim_b, ALU.mult), reads=[Bbb, Bsc], writes=[Bbtmp])
            K.op("dve", lambda e: e.tensor_tensor(bbar[:, 0], bbar[:, 0], btmp[:], ALU.subtract), reads=[Bbbar, Bbtmp], writes=[Bbbar])
            K.op("dve", lambda e: e.tensor_tensor(bbar[:, 1], bb[:, 1], gre_b, ALU.mult), reads=[Bbb, Bsc], writes=[Bbbar])
            K.op("dve", lambda e: e.tensor_tensor(btmp[:], bb[:, 0], gim_b, ALU.mult), reads=[Bbb, Bsc, Bbbar], writes=[Bbtmp])
            K.op("dve", lambda e: e.tensor_tensor(bbar[:, 1], bbar[:, 1], btmp[:], ALU.add), reads=[Bbbar, Bbtmp], writes=[Bbbar])
            mmA = sb(es, "s5mmA", [128, 10, 2, 8], F32); BmmA = Buf("s5mmA")
            mtA = sb(es, "s5mtA", [128, 3, 8], F32); BmtA = Buf("s5mtA")
            K.op("act", lambda e: e.copy(mmA[:, 0, 0, :], sc[:, 3, :]), reads=[Bsc], writes=[BmmA])
            K.op("act", lambda e: e.copy(mmA[:, 0, 1, :], sc[:, 4, :]), reads=[Bsc], writes=[BmmA])
            for k in range(1, 10):
                pr_ = mmA[:, k - 1, 0, :]; pi_ = mmA[:, k - 1, 1, :]
                K.op("dve", lambda e: e.tensor_tensor(mtA[:, 0, :], pr_, pr_, ALU.mult), reads=[BmmA], writes=[BmtA])
                K.op("dve", lambda e: e.tensor_tensor(mtA[:, 1, :], pi_, pi_, ALU.mult), reads=[BmmA], writes=[BmtA])
                K.op("dve", lambda e: e.tensor_tensor(mmA[:, k, 0, :], mtA[:, 0, :], mtA[:, 1, :], ALU.subtract), reads=[BmtA, BmmA], writes=[BmmA])
                K.op("dve", lambda e: e.tensor_tensor(mtA[:, 2, :], pr_, pi_, ALU.mult), reads=[BmmA], writes=[BmtA])
                K.op("dve", lambda e: e.tensor_scalar(mmA[:, k, 1, :], mtA[:, 2, :], 2.0, None, ALU.mult), reads=[BmtA, BmmA], writes=[BmmA])
            e3 = ExitStack()
            def _mk_s5(w):
                per = 8 // 2
                nb = per - 1
                return NS(psg=allps.sub(range(w * per, w * per + nb)), psa=allps.sub(range(w * per + nb, (w + 1) * per)), bexpP=Pool(e3, nc, "s5bexp", [128, 2, 128], F32, 1), blp=Pool(e3, nc, "s5bl", [128, 2, 128], BF16, 1), cwp=Pool(e3, nc, "s5cw", [128, 4, 128], BF16, 1), ctmp=Pool(e3, nc, "s5ct", [128, 2, 128], F32, 1), cosP=Pool(e3, nc, "s5cos", [128, 512], F32, 1), sinP=Pool(e3, nc, "s5sin", [128, 512], F32, 1), mmP=Pool(e3, nc, "s5mm", [128, 12, 2], F32, 1), mtP=Pool(e3, nc, "s5mt", [128, 8], F32, 1), carP=Pool(e3, nc, "s5car", [128, 4], F32, 1), big=Pool(e3, nc, "s5big", [128, 512], F32, 6), pr16=Pool(e3, nc, "s5pr", [128, 4, 512], BF16, 1))
            def _body_s5(j, P):
                bexp, Bbexp = P.bexpP.get()
                cosT, Bcos = P.cosP.get()
                sinT, Bsin = P.sinP.get()
                mm_, Bmm_unused = P.mmP.get()
                mt, Bmt = P.mtP.get()
                car, Bcar = P.carP.get()
                ct_ = (32 * j) // 128
                off = (32 * j) % 128
                K.op("pool", lambda e: e.memset(bexp[:], 0.0), reads=[], writes=[Bbexp])
                for ri in range(2):
                    K.op("pool", lambda e: e.tensor_copy(bexp[0:64, ri, off:off + 16], bbar[0:64, ri, j, :]), reads=[Bbbar], writes=[Bbexp])
                    K.op("pool", lambda e: e.tensor_copy(bexp[64:128, ri, off + 16:off + 32], bbar[64:128, ri, j, :]), reads=[Bbbar], writes=[Bbexp])
                pt, Bpt = P.psg.get()
                for ri in range(2):
                    K.op("pe", lambda e: e.transpose(pt[:, ri * 128:(ri + 1) * 128], bexp[:, ri, :], ident[:]), reads=[Bbexp, Bident], writes=[Bpt])
                bl, Bbl = P.blp.get()
                K.op("act", lambda e: e.copy(bl[:, :, :], pt[:, 0:256].rearrange("p (r n) -> p r n", r=2)), reads=[Bpt], writes=[Bbl])
                ct, Bct = P.ctmp.get()
                for ri in range(2):
                    K.dma("sp", ct[:, ri, :], dr["s5_c"][l, ri, j], writes=[Bct])
                cw4, Bcw4 = P.cwp.get()
                K.op("act", lambda e: e.copy(cw4[:, 0, :], ct[:, 0, :]), reads=[Bct], writes=[Bcw4])
                K.op("act", lambda e: e.mul(cw4[:, 1, :], ct[:, 0, :], -1.0), reads=[Bct], writes=[Bcw4])
                K.op("act", lambda e: e.mul(cw4[:, 2, :], ct[:, 1, :], -1.0), reads=[Bct], writes=[Bcw4])
                K.op("act", lambda e: e.mul(cw4[:, 3, :], ct[:, 1, :], -1.0), reads=[Bct], writes=[Bcw4])
                K.op("dve", lambda e: e.memset(cosT[:, 0:1], 1.0), writes=[Bcos])
                K.op("dve", lambda e: e.memset(sinT[:, 0:1], 0.0), writes=[Bsin])
                tb, Btb = P.big.get()
                for k in range(9):
                    n = 1 << k
                    mr = mmA[:, k, 0, j:j + 1]; mi = mmA[:, k, 1, j:j + 1]
                    K.op("dve", lambda e: e.tensor_scalar(tb[:, 0:n], sinT[:, 0:n], mi, None, ALU.mult), reads=[Bsin, BmmA], writes=[Btb])
                    K.op("dve", lambda e: e.scalar_tensor_tensor(cosT[:, n:2 * n], cosT[:, 0:n], mr, tb[:, 0:n], ALU.mult, ALU.subtract),
                         reads=[Bcos, BmmA, Btb], writes=[Bcos])
                    K.op("dve", lambda e: e.tensor_scalar(tb[:, 0:n], sinT[:, 0:n], mr, None, ALU.mult), reads=[Bsin, BmmA, Bcos], writes=[Btb])
                    K.op("dve", lambda e: e.scalar_tensor_tensor(sinT[:, n:2 * n], cosT[:, 0:n], mi, tb[:, 0:n], ALU.mult, ALU.add),
                         reads=[Bcos, BmmA, Btb], writes=[Bsin])
                magb = sc[:, 1, j:j + 1].to_broadcast([128, 512])
                m9r = mmA[:, 9, 0, j:j + 1]; m9i = mmA[:, 9, 1, j:j + 1]
                for ch in range(4):
                    cs = slice(ch * 512, (ch + 1) * 512)
                    pre, Bpre = P.psg.get()
                    K.op("pe", lambda e: e.matmul(pre[:, :], bl[:, 0, :], u16[:, ct_, cs], start=True, stop=True), reads=[Bbl, Bu16], writes=[Bpre])
                    pim, Bpim = P.psg.get()
                    K.op("pe", lambda e: e.matmul(pim[:, :], bl[:, 1, :], u16[:, ct_, cs], start=True, stop=True), reads=[Bbl, Bu16], writes=[Bpim])
                    brr, Bbrr = P.big.get()
                    bri, Bbri = P.big.get()
                    ta, Bta = P.big.get()
                    tb2, Btb2 = P.big.get()
                    K.op("dve", lambda e: e.tensor_tensor(brr[:], cosT[:], pre[:, :], ALU.mult), reads=[Bcos, Bpre], writes=[Bbrr])
                    K.op("dve", lambda e: e.tensor_tensor(ta[:], sinT[:], pim[:, :], ALU.mult), reads=[Bsin, Bpim], writes=[Bta])
                    K.op("pool", lambda e: e.tensor_tensor(brr[:], brr[:], ta[:], ALU.add), reads=[Bbrr, Bta], writes=[Bbrr])
                    K.op("dve", lambda e: e.tensor_tensor(bri[:], cosT[:], pim[:, :], ALU.mult), reads=[Bcos, Bpim], writes=[Bbri])
                    K.op("dve", lambda e: e.tensor_tensor(tb2[:], sinT[:], pre[:, :], ALU.mult), reads=[Bsin, Bpre], writes=[Btb2])
                    K.op("pool", lambda e: e.tensor_tensor(bri[:], bri[:], tb2[:], ALU.subtract), reads=[Bbri, Btb2], writes=[Bbri])
                    if ch == 0:
                        ir, ii = 0.0, 0.0
                    else:
                        K.op("dve", lambda e: e.tensor_tensor(mt[:, 4:5], car[:, 0:1], m9r, ALU.mult), reads=[Bcar, BmmA], writes=[Bmt])
                        K.op("dve", lambda e: e.tensor_tensor(mt[:, 5:6], car[:, 1:2], m9i, ALU.mult), reads=[Bcar, BmmA], writes=[Bmt])
                        K.op("dve", lambda e: e.tensor_tensor(mt[:, 6:7], car[:, 1:2], m9r, ALU.mult), reads=[Bcar, BmmA], writes=[Bmt])
                        K.op("dve", lambda e: e.tensor_tensor(mt[:, 7:8], car[:, 0:1], m9i, ALU.mult), reads=[Bcar, BmmA], writes=[Bmt])
                        K.op("dve", lambda e: e.tensor_tensor(car[:, 2:3], mt[:, 4:5], mt[:, 5:6], ALU.subtract), reads=[Bmt, Bcar], writes=[Bcar])
                        K.op("dve", lambda e: e.tensor_tensor(car[:, 3:4], mt[:, 6:7], mt[:, 7:8], ALU.add), reads=[Bmt, Bcar], writes=[Bcar])
                        ir, ii = car[:, 2:3], car[:, 3:4]
                    K.op("dve", lambda e: e.tensor_tensor_scan(ta[:], magb, brr[:], ir, ALU.mult, ALU.add), reads=[Bsc, Bbrr, Bta, Bcar], writes=[Bta])
                    K.op("dve", lambda e: e.tensor_tensor_scan(tb2[:], magb, bri[:], ii, ALU.mult, ALU.add), reads=[Bsc, Bbri, Btb2, Bcar], writes=[Btb2])
                    K.op("act", lambda e: e.copy(car[:, 0:1], ta[:, 511:512]), reads=[Bta, Bcar], writes=[Bcar])
                    K.op("act", lambda e: e.copy(car[:, 1:2], tb2[:, 511:512]), reads=[Btb2, Bcar], writes=[Bcar])
                    pp, Bpp = P.pr16.get()
                    K.op("dve", lambda e: e.tensor_tensor(pp[:, 0, :], cosT[:], ta[:], ALU.mult), reads=[Bcos, Bta], writes=[Bpp])
                    K.op("pool", lambda e: e.tensor_tensor(pp[:, 1, :], sinT[:], tb2[:], ALU.mult), reads=[Bsin, Btb2], writes=[Bpp])
                    K.op("dve", lambda e: e.tensor_tensor(pp[:, 2, :], sinT[:], ta[:], ALU.mult), reads=[Bsin, Bta], writes=[Bpp])
                    K.op("pool", lambda e: e.tensor_tensor(pp[:, 3, :], cosT[:], tb2[:], ALU.mult), reads=[Bcos, Btb2], writes=[Bpp])
                    yp, Byp = P.psg.get()
                    for v in range(4):
                        K.op("pe", lambda e: e.matmul(yp[:, :], cw4[:, v, :], pp[:, v, :], start=(v == 0), stop=(v == 3)), reads=[Bcw4, Bpp], writes=[Byp])
                    K.op("dve", lambda e: e.tensor_tensor(ypre[:, ct_, cs], ypre[:, ct_, cs], yp[:, :], ALU.add), reads=[Byp, Bypre], writes=[Bypre])
            IL.run(8, 2, _mk_s5, _body_s5)
            K.barrier_all()
            e3.close()
            yg16 = sb(es, "s5yg16", [128, 2, T], BF16); Byg16 = Buf("s5yg16")
            for c in range(2):
                K.op("act", lambda e: e.activation(ypre[:, c, :], ypre[:, c, :], AF.Gelu_apprx_tanh), reads=[Bypre], writes=[Bypre])
                K.op("act", lambda e: e.copy(yg16[:, c, :], ypre[:, c, :]), reads=[Bypre], writes=[Byg16])
            sgp = Pool(es, nc, "s5sg", [128, 512], F32, 2)
            for c in range(2):
                for ch in range(4):
                    cs = slice(ch * 512, (ch + 1) * 512)
                    zp, Bzp = psg.get()
                    for k in range(2):
                        K.op("pe", lambda e: e.matmul(zp[:, :], wglu[:, k, c * 128:(c + 1) * 128], yg16[:, k, cs], start=(k == 0), stop=(k == 1)),
                             reads=[Bwglu, Byg16], writes=[Bzp])
                    sg, Bsg = sgp.get()
                    K.op("act", lambda e: e.activation(sg[:], zp[:, :], AF.Sigmoid, bias=vec[:, 1, c:c + 1]), reads=[Bzp, Bvec], writes=[Bsg])
                    K.op("dve", lambda e: e.tensor_tensor(ypre[:, c, cs], ypre[:, c, cs], sg[:], ALU.mult), reads=[Bypre, Bsg], writes=[Bypre])
            fm_groupnorm(es, "sg", ypre, Bypre, 2, l, gcolt)
            dbg_dump(2)
            wout_partial(es, l, 2)
            phase_end(es)

        def rope_tables(es, name, inv_name, half, pos_cols, Bpos_in, npart=128):
            ncol = pos_cols.shape[1]
            inv = sb(es, name + "inv", [128, half], F32); Binv = Buf(name + "inv")
            K.dma("sp", inv[:], dr[inv_name][:], writes=[Binv])
            ang = sb(es, name + "ang", [128, ncol, half], F32); Bang = Buf(name + "ang")
            tmp = sb(es, name + "tmp", [128, ncol, half], F32); Btmp = Buf(name + "tmp")
            kk = sb(es, name + "kk", [128, ncol, half], I32); Bkk = Buf(name + "kk")
            cs_ = sb(es, name + "cs", [128, 2, ncol, half], F32); Bcs = Buf(name + "cs")
            P = npart
            K.op("dve", lambda e: e.tensor_tensor(ang[0:P], inv[0:P].unsqueeze(1).to_broadcast([P, ncol, half]),
                                                  pos_cols.unsqueeze(2).to_broadcast([P, ncol, half]), ALU.mult),
                 reads=[Binv, Bpos_in], writes=[Bang])
            for idx, shift in ((0, 0.5 * np.pi), (1, 0.0)):
                K.op("dve", lambda e: e.tensor_scalar(kk[0:P], ang[0:P], shift, 1.0 / TWO_PI, ALU.add, ALU.mult), reads=[Bang], writes=[Bkk])
                K.op("dve", lambda e: e.tensor_copy(tmp[0:P], kk[0:P]), reads=[Bkk], writes=[Btmp])
                K.op("dve", lambda e: e.scalar_tensor_tensor(tmp[0:P], tmp[0:P], -TWO_PI, ang[0:P], ALU.mult, ALU.add), reads=[Btmp, Bang], writes=[Btmp])
                K.op("dve", lambda e: e.tensor_scalar(tmp[0:P], tmp[0:P], shift, None, ALU.add), reads=[Btmp], writes=[Btmp])
                K.op("dve", lambda e: e.tensor_scalar(tmp[0:P], tmp[0:P], 3.14159, -3.14159, ALU.min, ALU.max), reads=[Btmp], writes=[Btmp])
                K.op("act", lambda e: e.activation(cs_[0:P, idx], tmp[0:P], AF.Sin), reads=[Btmp], writes=[Bcs])
            return cs_, Bcs

        def apply_rope(dst, src, cos_t, sin_t, nh, half, tmp, reads, writes, Btmp):
            x1 = src[:, :, 0:half]; x2 = src[:, :, half:2 * half]
            cb = cos_t.unsqueeze(1).to_broadcast([128, nh, half]); sb_ = sin_t.unsqueeze(1).to_broadcast([128, nh, half])
            ta = tmp[:, 0:nh, 0:half]; tb_ = tmp[:, 0:nh, half:2 * half]
            K.op("dve", lambda e: e.tensor_tensor(ta, x1, cb, ALU.mult), reads=reads, writes=[Btmp])
            K.op("dve", lambda e: e.tensor_tensor(tb_, x2, sb_, ALU.mult), reads=reads, writes=[Btmp])
            K.op("dve", lambda e: e.tensor_tensor(dst[:, :, 0:half], ta, tb_, ALU.subtract), reads=[Btmp], writes=writes)
            K.op("dve", lambda e: e.tensor_tensor(ta, x1, sb_, ALU.mult), reads=reads + [Btmp], writes=[Btmp])
            K.op("dve", lambda e: e.tensor_tensor(tb_, x2, cb, ALU.mult), reads=reads + [Btmp], writes=[Btmp])
            K.op("dve", lambda e: e.tensor_tensor(dst[:, :, half:2 * half], ta, tb_, ALU.add), reads=[Btmp], writes=writes)

        def attn_block_group(s_items, acc, Bacc, first_group, last_group):
            pass

        def mla_phase(s, l):
            es = ExitStack()
            ymT, BymT = alloc_ymT(es, 0)
            qT = sb(es, "mqT", [128, 4, T], BF16); BqT = [Buf(f"mqT{i}") for i in range(NT)]
            kT = sb(es, "mkT", [128, 4, T], BF16); BkT = [Buf(f"mkT{i}") for i in range(NT)]
            K.op("pool", lambda e: e.memset(qT[64:128], 0.0), writes=BqT)
            K.op("pool", lambda e: e.memset(kT[64:128], 0.0), writes=BkT)
            va = sb(es, "mva", [128, NT, 4, 65], BF16); Bva = [Buf(f"mva{i}") for i in range(NT)]
            K.op("pool", lambda e: e.memset(va[:, :, :, 64:65], 1.0), writes=Bva)
            e1 = ExitStack()
            wm = sb(e1, "wm", [128, DC, 352], BF16); Bwm = Buf("wm")
            K.dma("pool", wm[:], dr["w_in"][l, :, 0:352].rearrange("(c p) n -> p c n", p=128), writes=[Bwm])
            wuq = sb(e1, "wuq", [128, 2, 384], BF16); Bwuq = Buf("wuq")
            K.dma("pool", wuq[:, 0, :], dr["mla_w_uq"][l, 0:128, :], writes=[Bwuq])
            K.dma("pool", wuq[0:64, 1, :], dr["mla_w_uq"][l, 128:192, :], writes=[Bwuq])
            wukv = sb(e1, "wukv", [128, 512], BF16); Bwukv = Buf("wukv")
            K.dma("pool", wukv[:], dr["mla_w_ukv"][l], writes=[Bwukv])
            gv = sb(e1, "mgv", [128, 192 + 128 + 96 + 96], F32); Bgv = Buf("mgv")
            K.dma("sp", gv[:, 0:192], dr["mla_g_cq"][l:l + 1, :].to_broadcast([128, 192]), writes=[Bgv])
            K.dma("sp", gv[:, 192:320], dr["mla_g_ckv"][l:l + 1, :].to_broadcast([128, 128]), writes=[Bgv])
            K.dma("sp", gv[:, 320:416], dr["mla_g_q"][l:l + 1, :].to_broadcast([128, 96]), writes=[Bgv])
            K.dma("sp", gv[:, 416:512], dr["mla_g_k"][l:l + 1, :].to_broadcast([128, 96]), writes=[Bgv])
            g_cq = gv[:, 0:192]; g_ckv = gv[:, 192:320]; g_q = gv[:, 320:416]; g_k = gv[:, 416:512]
            cs_t, Bcs_t = rope_tables(e1, "mr", "c_inv_mla", 16, posf[:, :], Bposf)
            def _mk_mp(w):
                per = 8 // 4
                nb = per - 0
                return NS(psg=allps.sub(range(w * per, w * per + nb)), psa=allps.sub(range(w * per + nb, (w + 1) * per)), st=Pool(e1, nc, "mst", [128, 16], F32, 2), cn=Pool(e1, nc, "mcn", [128, 320], BF16, 1), cT=Pool(e1, nc, "mcT", [128, 3, 128], BF16, 1), qn=Pool(e1, nc, "mqn", [128, 4, 96], F32, 1), kn=Pool(e1, nc, "mkn", [128, 4, 96], F32, 1), qr=Pool(e1, nc, "mqr", [128, 8, 96], BF16, 1), jk=Pool(e1, nc, "mjk", [128, 192], BF16, 1), rtmp=Pool(e1, nc, "mrt", [128, 4, 32], F32, 1), kpe=Pool(e1, nc, "mkpe", [128, 32], F32, 1))
            def _body_mp(i, P):
                ts_ = slice(i * 128, (i + 1) * 128)
                pp, Bpp = P.psg.get()
                for c in range(DC):
                    K.op("pe", lambda e: e.matmul(pp[:, 0:352], xnT[:, c, ts_], wm[:, c, :], start=(c == 0), stop=(c == DC - 1)),
                         reads=[Bwm, BxnT[i][0], BxnT[i][1]], writes=[Bpp])
                sq, Bsq = P.st.get()
                j_, Bj_ = P.jk.get()
                K.op("act", lambda e: e.activation(j_[:, 0:192], pp[:, 0:192], AF.Square, accum_out=sq[:, 0:1]), reads=[Bpp], writes=[Bj_, Bsq])
                K.op("act", lambda e: e.activation(j_[:, 0:128], pp[:, 192:320], AF.Square, accum_out=sq[:, 1:2]), reads=[Bpp], writes=[Bj_, Bsq])
                K.op("act", lambda e: e.activation(j_[:, 0:32], pp[:, 320:352], AF.Square, accum_out=sq[:, 2:3]), reads=[Bpp], writes=[Bj_, Bsq])
                K.op("dve", lambda e: e.tensor_scalar(sq[:, 0:1], sq[:, 0:1], 128.0 / 192.0, None, ALU.mult), reads=[Bsq], writes=[Bsq])
                rstd_from(sq[:, 4:6], sq[:, 0:2], 128, [Bsq], [Bsq])
                c_n, Bc_n = P.cn.get()
                K.op("dve", lambda e: e.scalar_tensor_tensor(c_n[:, 0:192], pp[:, 0:192], sq[:, 4:5], g_cq, ALU.mult, ALU.mult),
                     reads=[Bpp, Bsq, Bgv], writes=[Bc_n])
                K.op("dve", lambda e: e.scalar_tensor_tensor(c_n[:, 192:320], pp[:, 192:320], sq[:, 5:6], g_ckv, ALU.mult, ALU.mult),
                     reads=[Bpp, Bsq, Bgv], writes=[Bc_n])
                kp, Bkp = P.kpe.get()
                K.op("act", lambda e: e.copy(kp[:], pp[:, 320:352]), reads=[Bpp], writes=[Bkp])
                pt, Bpt = P.psg.get()
                ptb = pt[:, :].bitcast(BF16)
                K.op("pe", lambda e: e.transpose(ptb[:, 0:128], c_n[:, 0:128], identb[:]), reads=[Bc_n, Bidentb], writes=[Bpt])
                K.op("pe", lambda e: e.transpose(ptb[0:64, 128:256], c_n[:, 128:192], identb[:]), reads=[Bc_n, Bidentb], writes=[Bpt])
                K.op("pe", lambda e: e.transpose(ptb[:, 256:384], c_n[:, 192:320], identb[:]), reads=[Bc_n, Bidentb], writes=[Bpt])
                ct, Bct = P.cT.get()
                K.op("act", lambda e: e.copy(ct[:, 0, :], ptb[:, 0:128]), reads=[Bpt], writes=[Bct])
                K.op("act", lambda e: e.copy(ct[0:64, 1, :], ptb[0:64, 128:256]), reads=[Bpt], writes=[Bct])
                K.op("act", lambda e: e.copy(ct[:, 2, :], ptb[:, 256:384]), reads=[Bpt], writes=[Bct])
                pq, Bpq = P.psg.get()
                K.op("pe", lambda e: e.matmul(pq[:, 0:384], ct[:, 0, :], wuq[:, 0, :], start=True, stop=False), reads=[Bct, Bwuq], writes=[Bpq])
                K.op("pe", lambda e: e.matmul(pq[:, 0:384], ct[0:64, 1, :], wuq[0:64, 1, :], start=False, stop=True), reads=[Bct, Bwuq], writes=[Bpq])
                pkv, Bpkv = P.psg.get()
                K.op("pe", lambda e: e.matmul(pkv[:, :], ct[:, 2, :], wukv[:], start=True, stop=True), reads=[Bct, Bwukv], writes=[Bpkv])
                pq3 = pq[:, 0:384].rearrange("p (h d) -> p h d", h=4)
                pkv3 = pkv[:, :].rearrange("p (h d) -> p h d", h=4)
                sq2, Bsq2 = P.st.get()
                for h in range(4):
                    K.op("act", lambda e: e.activation(j_[:, 0:96], pq3[:, h, :], AF.Square, accum_out=sq2[:, h:h + 1]), reads=[Bpq], writes=[Bj_, Bsq2])
                    K.op("act", lambda e: e.activation(j_[:, 0:64], pkv3[:, h, 0:64], AF.Square, accum_out=sq2[:, 4 + h:5 + h]), reads=[Bpkv], writes=[Bj_, Bsq2])
                K.op("dve", lambda e: e.tensor_scalar(sq2[:, 4:8], sq2[:, 4:8], sq[:, 2:3], None, ALU.add), reads=[Bsq2, Bsq], writes=[Bsq2])
                rstd_from(sq2[:, 8:16], sq2[:, 0:8], 96, [Bsq2], [Bsq2])
                q_n, Bq_n = P.qn.get()
                k_n, Bk_n = P.kn.get()
                for h in range(4):
                    K.op("dve", lambda e: e.scalar_tensor_tensor(q_n[:, h, :], pq3[:, h, :], sq2[:, 8 + h:9 + h], g_q, ALU.mult, ALU.mult),
                         reads=[Bpq, Bsq2, Bgv], writes=[Bq_n])
                    K.op("dve", lambda e: e.scalar_tensor_tensor(k_n[:, h, 32:96], pkv3[:, h, 0:64], sq2[:, 12 + h:13 + h], g_k[:, 32:96], ALU.mult, ALU.mult),
                         reads=[Bpkv, Bsq2, Bgv], writes=[Bk_n])
                    K.op("dve", lambda e: e.scalar_tensor_tensor(k_n[:, h, 0:32], kp[:], sq2[:, 12 + h:13 + h], g_k[:, 0:32], ALU.mult, ALU.mult),
                         reads=[Bkp, Bsq2, Bgv], writes=[Bk_n])
                q_r, Bq_r = P.qr.get()
                rt_, Brt_ = P.rtmp.get()
                apply_rope(q_r[:, 0:4], q_n[:, :, :], cs_t[:, 0, i, :], cs_t[:, 1, i, :], 4, 16, rt_, [Bq_n, Bcs_t], [Bq_r], Brt_)
                K.op("act", lambda e: e.copy(q_r[:, 0:4, 32:96], q_n[:, :, 32:96]), reads=[Bq_n], writes=[Bq_r])
                apply_rope(q_r[:, 4:8], k_n[:, :, :], cs_t[:, 0, i, :], cs_t[:, 1, i, :], 4, 16, rt_, [Bk_n, Bcs_t], [Bq_r], Brt_)
                K.op("act", lambda e: e.copy(q_r[:, 4:8, 32:96], k_n[:, :, 32:96]), reads=[Bk_n], writes=[Bq_r])
                K.op("act", lambda e: e.copy(va[:, i, :, 0:64], pkv3[:, :, 64:128]), reads=[Bpkv], writes=[Bva[i]])
                for grp in range(2):
                    pt2, Bpt2 = P.psg.get()
                    pt2b = pt2[:, :].bitcast(BF16)
                    for h in range(4):
                        K.op("pe", lambda e: e.transpose(pt2b[0:96, h * 128:(h + 1) * 128], q_r[:, grp * 4 + h, :], identb[:]),
                             reads=[Bq_r, Bidentb], writes=[Bpt2])
                    dstT = qT if grp == 0 else kT
                    BdT = BqT if grp == 0 else BkT
                    K.op("act" if grp == 0 else "dve",
                         (lambda e: e.copy(dstT[0:96, :, ts_], pt2b[0:96, 0:512].rearrange("p (h t) -> p h t", h=4))) if grp == 0 else
                         (lambda e: e.tensor_copy(dstT[0:96, :, ts_], pt2b[0:96, 0:512].rearrange("p (h t) -> p h t", h=4))),
                         reads=[Bpt2], writes=[BdT[i]])
            IL.run(NT, 4, _mk_mp, _body_mp)
            K.barrier_all()
            e1.close()
            scale = 96 ** -0.5
            def _mk_ma(w):
                per = 8 // 4
                nb = per - 1
                return NS(psg=allps.sub(range(w * per, w * per + nb)), psa=allps.sub(range(w * per + nb, (w + 1) * per)), pexp=Pool(es, nc, "mpe", [128, 512], BF16, 3), yo=Pool(es, nc, "myo", [128, 256], F32, 2), yst=Pool(es, nc, "myst", [128, 8], F32, 2), yb=Pool(es, nc, "myb", [128, 256], BF16, 2))
            def _body_ma(qt, P):
                qs = slice(qt * 128, (qt + 1) * 128)
                y_o, By_o = P.yo.get()
                yst_, Byst = P.yst.get()
                for h in range(4):
                    acc, Bacc = P.psa.get()
                    nk = qt + 1
                    for g0 in range(0, nk, 4):
                        kts = list(range(g0, min(g0 + 4, nk)))
                        sp_, Bsp = P.psg.get()
                        for a, kt in enumerate(kts):
                            K.op("pe", lambda e: e.matmul(sp_[:, a * 128:(a + 1) * 128], kT[:, h, kt * 128:(kt + 1) * 128], qT[:, h, qs], start=True, stop=True),
                                 reads=[BkT[kt], BqT[qt]], writes=[Bsp])
                        pe_, Bpe = P.pexp.get()
                        w = len(kts) * 128
                        K.op("act", lambda e: e.activation(pe_[:, 0:w], sp_[:, 0:w], AF.Exp, scale=scale), reads=[Bsp], writes=[Bpe])
                        if kts[-1] == qt:
                            a = len(kts) - 1
                            K.op("dve", lambda e: e.tensor_tensor(pe_[:, a * 128:(a + 1) * 128], pe_[:, a * 128:(a + 1) * 128], tri4[:, 0, :], ALU.mult),
                                 reads=[Bpe, Btri], writes=[Bpe])
                        for a, kt in enumerate(kts):
                            K.op("pe", lambda e: e.matmul(acc[:, 0:65], pe_[:, a * 128:(a + 1) * 128], va[:, kt, h, :], start=(kt == 0), stop=(kt == qt)),
                                 reads=[Bpe, Bva[kt]], writes=[Bacc])
                    K.op("dve", lambda e: e.reciprocal(yst_[:, h:h + 1], acc[:, 64:65]), reads=[Bacc], writes=[Byst])
                    K.op("dve", lambda e: e.tensor_scalar(y_o[:, h * 64:(h + 1) * 64], acc[:, 0:64], yst_[:, h:h + 1], None, ALU.mult),
                         reads=[Bacc, Byst], writes=[By_o])
                tm_groupnorm(P.psg, y_o, By_o, yst_, Byst, P.yb, 0, l, qt)
            IL.run(NT, 4, _mk_ma, _body_ma)
            dbg_dump(0)
            wout_partial(es, l, 0)
            phase_end(es)

        def tm_groupnorm(psgp, y_o, By_o, yst_, Byst, ybpool, m, l, qt):
            y_b, By_b = ybpool.get()
            K.op("act", lambda e: e.activation(y_b[:], y_o[:], AF.Square, accum_out=yst_[:, 4:5]), reads=[By_o], writes=[By_b, Byst])
            K.op("act", lambda e: e.activation(yst_[:, 5:6], yst_[:, 4:5], AF.Ln, bias=epsb[:, 0:1], scale=1.0 / 256), reads=[Byst, Beps], writes=[Byst])
            K.op("act", lambda e: e.activation(yst_[:, 5:6], yst_[:, 5:6], AF.Exp, scale=-0.5), reads=[Byst], writes=[Byst])
            K.op("dve", lambda e: e.scalar_tensor_tensor(y_b[:], y_o[:], yst_[:, 5:6], gon[:, m, :], ALU.mult, ALU.mult),
                 reads=[By_o, Byst, Bgon, By_b], writes=[By_b])
            pt, Bpt = psgp.get()
            ptb = pt[:, :].bitcast(BF16)
            for c in range(2):
                K.op("pe", lambda e: e.transpose(ptb[:, c * 128:(c + 1) * 128], y_b[:, c * 128:(c + 1) * 128], identb[:]), reads=[By_b, Bidentb], writes=[Bpt])
            K.op("act", lambda e: e.copy(ymT_box["t"][:, :, qt * 128:(qt + 1) * 128], ptb[:, 0:256].rearrange("p (c t) -> p c t", c=2)),
                 reads=[Bpt], writes=[ymT_box["b"][qt // 4]])

        def nsa_phase(s, l):
            es = ExitStack()
            ymT, BymT = alloc_ymT(es, 3)
            gv = sb(es, "ngv", [128, 4, 64], F32); Bgv = Buf("ngv")
            K.dma("sp", gv[:, 0, :], dr["nsa_g_q"][l:l + 1, :].to_broadcast([128, 64]), writes=[Bgv])
            for b3 in range(3):
                K.dma("sp", gv[:, 1 + b3, :], dr["nsa_g_k"][l, b3:b3 + 1, :].to_broadcast([128, 64]), writes=[Bgv])
            qT = sb(es, "nqT", [128, NT, 4, 128], BF16); BqT = [Buf(f"nqT{i}") for i in range(NT)]
            kTs = sb(es, "nkTs", [128, T], BF16); BkTs = [Buf(f"nkTs{i}") for i in range(NT)]
            kTw = sb(es, "nkTw", [128, T], BF16); BkTw = [Buf(f"nkTw{i}") for i in range(NT)]
            K.op("pool", lambda e: e.memset(qT[64:128], 0.0), writes=BqT)
            K.op("pool", lambda e: e.memset(kTs[64:128], 0.0), writes=BkTs)
            K.op("pool", lambda e: e.memset(kTw[64:128], 0.0), writes=BkTw)
            vs = sb(es, "nvs", [128, NT, 65], BF16); Bvs = [Buf(f"nvs{i}") for i in range(NT)]
            vw = sb(es, "nvw", [128, NT, 65], BF16); Bvw = [Buf(f"nvw{i}") for i in range(NT)]
            gts = sb(es, "ngts", [128, NT, 12], F32); Bgts = [Buf(f"ngts{i}") for i in range(NT)]
            kcT = sb(es, "nkcT", [128, 128], BF16); BkcT = Buf("nkcT")
            K.op("pool", lambda e: e.memset(kcT[64:128], 0.0), writes=[BkcT])
            vc = sb(es, "nvc", [128, 97], BF16); Bvc = Buf("nvc")
            K.op("pool", lambda e: e.memset(vs[:, :, 64:65], 1.0), writes=Bvs)
            K.op("pool", lambda e: e.memset(vw[:, :, 64:65], 1.0), writes=Bvw)
            K.op("pool", lambda e: e.memset(vc[:, 64:65], 1.0), writes=[Bvc])
            K.dma("pool", vc[:, 65:97], dr["c_ov"][:], writes=[Bvc])
            e1 = ExitStack()
            wn = sb(e1, "wn", [128, DC, 652], BF16); Bwn = Buf("wn")
            K.dma("pool", wn[:], dr["w_in"][l, :, 1120:1772].rearrange("(c p) n -> p c n", p=128), writes=[Bwn])
            cs_t, Bcs_t = rope_tables(e1, "nr", "c_inv_nsa", 8, posf[:, :], Bposf)
            pci = sb(e1, "npci", [128, 1], I32); Bpci = Buf("npci")
            pcf = sb(e1, "npcf", [128, 1], F32); Bpcf = Buf("npcf")
            K.op("dve", lambda e: e.memset(pcf[:], 0.0), writes=[Bpcf])
            pos_src = dr["pos"][s:s + 1, 31::16].rearrange("o n -> n o")
            K.dma("sp", pci[0:127, :], pos_src, writes=[Bpci], allow_slow_non_contiguous=True)
            K.op("dve", lambda e: e.tensor_copy(pcf[0:127, :], pci[0:127, :]), reads=[Bpci, Bpcf], writes=[Bpcf])
            csc, Bcsc = rope_tables(e1, "nrc", "c_inv_nsa", 8, pcf[:, :], Bpcf)
            cmpT = sb(e1, "ncmpT", [128, T], BF16); BcmpT = Buf("ncmpT")
            for ch in range(4):
                cs = slice(ch * 512, (ch + 1) * 512)
                p1, Bp1 = psg.get()
                for k in range(DC):
                    K.op("pe", lambda e: e.matmul(p1[:, :], wn[:, k, 256:384], xnT[:, k, cs], start=(k == 0), stop=(k == DC - 1)),
                         reads=[Bwn] + xnT_bufs(ch), writes=[Bp1])
                K.op("act", lambda e: e.copy(cmpT[:, cs], p1[:, :]), reads=[Bp1], writes=[BcmpT])
            def _mk_np(w):
                per = 8 // 4
                nb = per - 0
                return NS(psg=allps.sub(range(w * per, w * per + nb)), psa=allps.sub(range(w * per + nb, (w + 1) * per)), st=Pool(e1, nc, "nst", [128, 16], F32, 2), jk=Pool(e1, nc, "njk", [128, 64], F32, 1), qn=Pool(e1, nc, "nqn", [128, 6, 64], F32, 1), qr=Pool(e1, nc, "nqr", [128, 6, 64], BF16, 1), rtmp=Pool(e1, nc, "nrt", [128, 6, 16], F32, 1))
            def _body_np(i, P):
                ts_ = slice(i * 128, (i + 1) * 128)
                pa, Bpa = P.psg.get()
                pb, Bpb = P.psg.get()
                for c in range(DC):
                    K.op("pe", lambda e: e.matmul(pa[:, 0:256], xnT[:, c, ts_], wn[:, c, 0:256], start=(c == 0), stop=(c == DC - 1)),
                         reads=[Bwn, BxnT[i][0], BxnT[i][1]], writes=[Bpa])
                for c in range(DC):
                    K.op("pe", lambda e: e.matmul(pb[:, 0:268], xnT[:, c, ts_], wn[:, c, 384:652], start=(c == 0), stop=(c == DC - 1)),
                         reads=[Bwn, BxnT[i][0], BxnT[i][1]], writes=[Bpb])
                pa3 = pa[:, 0:256].rearrange("p (h d) -> p h d", h=4)
                sq, Bsq = P.st.get()
                j_, Bj_ = P.jk.get()
                for h in range(4):
                    K.op("act", lambda e: e.activation(j_[:], pa3[:, h, :], AF.Square, accum_out=sq[:, h:h + 1]), reads=[Bpa], writes=[Bj_, Bsq])
                K.op("act", lambda e: e.activation(j_[:], pb[:, 0:64], AF.Square, accum_out=sq[:, 4:5]), reads=[Bpb], writes=[Bj_, Bsq])
                K.op("act", lambda e: e.activation(j_[:], pb[:, 128:192], AF.Square, accum_out=sq[:, 5:6]), reads=[Bpb], writes=[Bj_, Bsq])
                rstd_from(sq[:, 8:14], sq[:, 0:6], 64, [Bsq], [Bsq])
                q_n, Bq_n = P.qn.get()
                for h in range(4):
                    K.op("dve", lambda e: e.scalar_tensor_tensor(q_n[:, h, :], pa3[:, h, :], sq[:, 8 + h:9 + h], gv[:, 0, :], ALU.mult, ALU.mult),
                         reads=[Bpa, Bsq, Bgv], writes=[Bq_n])
                K.op("dve", lambda e: e.scalar_tensor_tensor(q_n[:, 4, :], pb[:, 0:64], sq[:, 12:13], gv[:, 2, :], ALU.mult, ALU.mult),
                     reads=[Bpb, Bsq, Bgv], writes=[Bq_n])
                K.op("dve", lambda e: e.scalar_tensor_tensor(q_n[:, 5, :], pb[:, 128:192], sq[:, 13:14], gv[:, 3, :], ALU.mult, ALU.mult),
                     reads=[Bpb, Bsq, Bgv], writes=[Bq_n])
                q_r, Bq_r = P.qr.get()
                rt_, Brt_ = P.rtmp.get()
                apply_rope(q_r[:, :, :], q_n[:, :, :], cs_t[:, 0, i, :], cs_t[:, 1, i, :], 6, 8, rt_, [Bq_n, Bcs_t], [Bq_r], Brt_)
                K.op("act", lambda e: e.copy(q_r[:, :, 16:64], q_n[:, :, 16:64]), reads=[Bq_n], writes=[Bq_r])
                K.op("act", lambda e: e.copy(vs[:, i, 0:64], pb[:, 64:128]), reads=[Bpb], writes=[Bvs[i]])
                K.op("act", lambda e: e.copy(vw[:, i, 0:64], pb[:, 192:256]), reads=[Bpb], writes=[Bvw[i]])
                K.op("act", lambda e: e.copy(gts[:, i, :], pb[:, 256:268]), reads=[Bpb], writes=[Bgts[i]])
                pt, Bpt = P.psg.get()
                ptb = pt[:, :].bitcast(BF16)
                for h in range(6):
                    K.op("pe", lambda e: e.transpose(ptb[0:64, h * 128:(h + 1) * 128], q_r[:, h, :], identb[:]), reads=[Bq_r, Bidentb], writes=[Bpt])
                K.op("act", lambda e: e.copy(qT[0:64, i, :, :], ptb[0:64, 0:512].rearrange("p (h t) -> p h t", h=4)), reads=[Bpt], writes=[BqT[i]])
                K.op("dve", lambda e: e.tensor_copy(kTs[0:64, ts_], ptb[0:64, 512:640]), reads=[Bpt], writes=[BkTs[i]])
                K.op("dve", lambda e: e.tensor_copy(kTw[0:64, ts_], ptb[0:64, 640:768]), reads=[Bpt], writes=[BkTw[i]])
            IL.run(NT, 4, _mk_np, _body_np)
            st = Pool(e1, nc, "nst2", [128, 16], F32, 1)
            jk = Pool(e1, nc, "njk2", [128, 64], F32, 1)
            qn = Pool(e1, nc, "nqn2", [128, 6, 64], F32, 1)
            qr = Pool(e1, nc, "nqr2", [128, 6, 64], BF16, 1)
            rtmp = Pool(e1, nc, "nrt2", [128, 6, 16], F32, 1)
            w1 = sb(e1, "nw1", [128, 32, 128], BF16); Bw1 = Buf("nw1")
            pe16 = sb(e1, "npe16", [128, 32], BF16); Bpe16 = Buf("npe16")
            w2 = sb(e1, "nw2", [128, 2, 64], BF16); Bw2 = Buf("nw2")
            for kv in range(2):
                K.dma("pool", w1[kv * 64:kv * 64 + 64], dr["nsa_w1"][l, kv], writes=[Bw1])
                K.dma("pool", pe16[kv * 64:kv * 64 + 64, :], dr["nsa_pe"][l, kv], writes=[Bpe16])
                K.dma("pool", w2[:, kv, :], dr["nsa_w2"][l, kv], writes=[Bw2])
            hb = sb(e1, "nhb", [128, 2], F32); Bhb = Buf("nhb")
            hid = sb(e1, "nhid", [128, 2, 128], BF16); Bhid = Buf("nhid")
            for kv in range(2):
                rows = slice(kv * 64, kv * 64 + 64)
                ph, Bph = psg.get()
                pbias, Bpbias = psg.get()
                for j in range(32):
                    lw = w1[rows, j, :]
                    rhs = cmpT[rows, j:j + 16 * 126 + 1:16]
                    K.op("pe", lambda e: e.matmul(ph[:, 0:127], lw, rhs, start=(j == 0), stop=(j == 31)), reads=[Bw1, BcmpT], writes=[Bph])
                for j in range(32):
                    lw = w1[rows, j, :]
                    K.op("pe", lambda e: e.matmul(pbias[:, 0:1], lw, pe16[rows, j:j + 1], start=(j == 0), stop=(j == 31)), reads=[Bw1, Bpe16], writes=[Bpbias])
                K.op("act", lambda e: e.copy(hb[:, kv:kv + 1], pbias[:, 0:1]), reads=[Bpbias], writes=[Bhb])
                K.op("act", lambda e: e.activation(hid[:, kv, 0:127], ph[:, 0:127], AF.Gelu_apprx_tanh, bias=hb[:, kv:kv + 1]), reads=[Bph, Bhb], writes=[Bhid])
                po, Bpo = psg.get()
                K.op("pe", lambda e: e.matmul(po[0:127, 0:64], hid[:, kv, 0:127], w2[:, kv, :], start=True, stop=True), reads=[Bhid, Bw2], writes=[Bpo])
                if kv == 1:
                    K.op("act", lambda e: e.copy(vc[0:127, 0:64], po[0:127, 0:64]), reads=[Bpo], writes=[Bvc])
                else:
                    sq, Bsq = st.get()
                    j_, Bj_ = jk.get()
                    q_n, Bq_n = qn.get()
                    q_r, Bq_r = qr.get()
                    rt_, Brt_ = rtmp.get()
                    K.op("dve", lambda e: e.memset(q_n[:, 0, :], 0.0), writes=[Bq_n])
                    K.op("act", lambda e: e.activation(j_[0:127, :], po[0:127, 0:64], AF.Square, accum_out=sq[0:127, 0:1]), reads=[Bpo], writes=[Bj_, Bsq])
                    rstd_from(sq[0:127, 1:2], sq[0:127, 0:1], 64, [Bsq], [Bsq])
                    K.op("dve", lambda e: e.scalar_tensor_tensor(q_n[0:127, 0, :], po[0:127, 0:64], sq[0:127, 1:2], gv[0:127, 1, :], ALU.mult, ALU.mult),
                         reads=[Bpo, Bsq, Bgv, Bq_n], writes=[Bq_n])
                    apply_rope(q_r[:, 0:1, :], q_n[:, 0:1, :], csc[:, 0, 0, :], csc[:, 1, 0, :], 1, 8, rt_, [Bq_n, Bcsc], [Bq_r], Brt_)
                    K.op("act", lambda e: e.copy(q_r[:, 0:1, 16:64], q_n[:, 0:1, 16:64]), reads=[Bq_n], writes=[Bq_r])
                    pt, Bpt = psg.get()
                    ptb = pt[:, :].bitcast(BF16)
                    K.op("pe", lambda e: e.transpose(ptb[0:64, 0:128], q_r[:, 0, :], identb[:]), reads=[Bq_r, Bidentb], writes=[Bpt])
                    K.op("act", lambda e: e.copy(kcT[0:64, :], ptb[0:64, 0:128]), reads=[Bpt], writes=[BkcT])
            K.barrier_all()
            e1.close()
            cmask = sb(es, "ncmask", [128, T], BF16); Bcmask = Buf("ncmask")
            K.dma("pool", cmask[:], dr["c_cmpmask"][:], writes=[Bcmask])
            keep = sb(es, "nkeep", [128, NT, 32], F32); Bkeep = Buf("nkeep")
            base = sb(es, "nbase", [128, NT, 32], F32); Bbase = Buf("nbase")
            K.dma("sp", keep[:], dr["c_keep"][:], writes=[Bkeep])
            K.dma("sp", base[:], dr["c_base"][:], writes=[Bbase])
            Em = sb(es, "nEm", [128, NT, 128], BF16); BEm = Buf("nEm")
            K.op("pool", lambda e: e.memset(Em[:], 0.0), writes=[BEm])
            K.dma("pool", Em[0:32], dr["c_E"][:], writes=[BEm])
            scale = 64 ** -0.5
            def _mk_na(w):
                per = 8 // 4
                nb = per - 1
                return NS(psg=allps.sub(range(w * per, w * per + nb)), psa=allps.sub(range(w * per + nb, (w + 1) * per)), nsp=_zeroed(Pool(es, nc, "nselT", [128, 4, 128], BF16, 1)), pexp=Pool(es, nc, "npx", [128, 512], BF16, 3), sst=Pool(es, nc, "nsst", [128, 80], F32, 1), impp=Pool(es, nc, "nimp", [128, 32], F32, 1), yo=Pool(es, nc, "nyo", [128, 256], F32, 1), yst=Pool(es, nc, "nyst", [128, 8], F32, 1), yb=Pool(es, nc, "nyb", [128, 256], BF16, 1))
            def _body_na(qt, P):
                qs = slice(qt * 128, (qt + 1) * 128)
                qrhs = qT[:, qt].rearrange("p h t -> p (h t)")
                y_o, By_o = P.yo.get()
                yst_, Byst = P.yst.get()
                st_, Bst_ = P.sst.get()
                imp, Bimp = P.impp.get()
                K.op("act", lambda e: e.activation(gts[:, qt, :], gts[:, qt, :], AF.Exp, scale=-1.0), reads=[Bgts[qt]], writes=[Bgts[qt]])
                K.op("dve", lambda e: e.tensor_scalar(gts[:, qt, :], gts[:, qt, :], 1.0, None, ALU.add), reads=[Bgts[qt]], writes=[Bgts[qt]])
                K.op("dve", lambda e: e.reciprocal(gts[:, qt, :], gts[:, qt, :]), reads=[Bgts[qt]], writes=[Bgts[qt]])
                sp_, Bsp = P.psg.get()
                K.op("pe", lambda e: e.matmul(sp_[0:127, :], kcT[:, 0:127], qrhs, start=True, stop=True), reads=[BkcT, BqT[qt]], writes=[Bsp])
                pe_, Bpe = P.pexp.get()
                K.op("act", lambda e: e.activation(pe_[0:127, :], sp_[0:127, :], AF.Exp, scale=scale), reads=[Bsp], writes=[Bpe])
                K.op("dve", lambda e: e.tensor_tensor(pe_[0:127, :].rearrange("p (h t) -> p h t", h=4), pe_[0:127, :].rearrange("p (h t) -> p h t", h=4),
                                                       cmask[0:127, qs].unsqueeze(1).to_broadcast([127, 4, 128]), ALU.mult),
                     reads=[Bpe, Bcmask], writes=[Bpe])
                for h in range(4):
                    acc, Bacc = P.psa.get()
                    K.op("pe", lambda e: e.matmul(acc[:, 0:97], pe_[0:127, h * 128:(h + 1) * 128], vc[0:127, :], start=True, stop=True),
                         reads=[Bpe, Bvc], writes=[Bacc])
                    K.op("dve", lambda e: e.tensor_scalar(st_[:, h:h + 1], acc[:, 64:65], 1e-30, None, ALU.add), reads=[Bacc], writes=[Bst_])
                    K.op("dve", lambda e: e.reciprocal(st_[:, h:h + 1], st_[:, h:h + 1]), reads=[Bst_], writes=[Bst_])
                    if h == 0:
                        K.op("dve", lambda e: e.tensor_scalar(imp[:], acc[:, 65:97], st_[:, h:h + 1], None, ALU.mult), reads=[Bacc, Bst_], writes=[Bimp])
                    else:
                        K.op("dve", lambda e: e.scalar_tensor_tensor(imp[:], acc[:, 65:97], st_[:, h:h + 1], imp[:], ALU.mult, ALU.add),
                             reads=[Bacc, Bst_, Bimp], writes=[Bimp])
                    K.op("dve", lambda e: e.tensor_tensor(st_[:, 4 + h:5 + h], st_[:, h:h + 1], gts[:, qt, 3 * h:3 * h + 1], ALU.mult), reads=[Bst_, Bgts[qt]], writes=[Bst_])
                    K.op("dve", lambda e: e.tensor_scalar(y_o[:, h * 64:(h + 1) * 64], acc[:, 0:64], st_[:, 4 + h:5 + h], None, ALU.mult),
                         reads=[Bacc, Bst_], writes=[By_o])
                K.op("dve", lambda e: e.tensor_tensor(imp[:], imp[:], keep[:, qt, :], ALU.mult), reads=[Bimp, Bkeep], writes=[Bimp])
                K.op("dve", lambda e: e.tensor_tensor(imp[:], imp[:], base[:, qt, :], ALU.add), reads=[Bimp, Bbase], writes=[Bimp])
                K.op("dve", lambda e: e.max(st_[:, 8:16], imp[:]), reads=[Bimp], writes=[Bst_])
                K.op("dve", lambda e: e.tensor_scalar(st_[:, 16:48], imp[:], st_[:, 12:13], -1.0, ALU.is_ge, ALU.add), reads=[Bimp, Bst_], writes=[Bst_])
                pt, Bpt = P.psg.get()
                K.op("pe", lambda e: e.transpose(pt[0:32, 0:128], st_[:, 16:48], ident[:]), reads=[Bst_, Bident], writes=[Bpt])
                nselT, BnselT = P.nsp.get()
                K.op("act", lambda e: e.copy(nselT[0:32, :, :], pt[0:32, 0:128].unsqueeze(1).to_broadcast([32, 4, 128])), reads=[Bpt], writes=[BnselT])
                for br_ in range(2):
                    kTb = kTs if br_ == 0 else kTw
                    BkTb = BkTs if br_ == 0 else BkTw
                    vb = vs if br_ == 0 else vw
                    Bvb = Bvs if br_ == 0 else Bvw
                    kts = list(range(0, qt + 1)) if br_ == 0 else list(range(max(0, qt - 4), qt + 1))
                    acc, Bacc = P.psa.get()
                    K.op("dve", lambda e: e.memset(acc[:, 0:260], 0.0), writes=[Bacc])
                    for kt in kts:
                        sp_, Bsp = P.psg.get()
                        K.op("pe", lambda e: e.matmul(sp_[:, :], kTb[:, kt * 128:(kt + 1) * 128], qrhs, start=True, stop=(br_ == 1)),
                             reads=[BkTb[kt], BqT[qt]], writes=[Bsp])
                        if br_ == 0:
                            K.op("pe", lambda e: e.matmul(sp_[:, :], Em[:, kt, :], nselT[:, :, :].rearrange("p h t -> p (h t)"), start=False, stop=True),
                                 reads=[BEm, BnselT], writes=[Bsp])
                        pe_, Bpe = P.pexp.get()
                        K.op("act", lambda e: e.activation(pe_[:, :], sp_[:, :], AF.Exp, scale=scale), reads=[Bsp], writes=[Bpe])
                        if kt == qt:
                            K.op("dve", lambda e: e.tensor_tensor(pe_[:, :], pe_[:, :], tri4[:].rearrange("p h t -> p (h t)"), ALU.mult),
                                 reads=[Bpe, Btri], writes=[Bpe])
                        elif br_ == 1 and kt == qt - 4:
                            K.op("dve", lambda e: e.tensor_tensor(pe_[:, :], pe_[:, :], anti4[:].rearrange("p h t -> p (h t)"), ALU.mult),
                                 reads=[Bpe, Banti], writes=[Bpe])
                        for h in range(4):
                            co = h * 65
                            K.op("pe", lambda e: e.matmul(acc[:, co:co + 65], pe_[:, h * 128:(h + 1) * 128], vb[:, kt, :], start=False, stop=(kt == kts[-1]),
                                                          skip_group_check=True),
                                 reads=[Bpe, Bvb[kt]], writes=[Bacc])
                    for h in range(4):
                        co = h * 65
                        cc_ = 50 + 4 * br_ + h
                        K.op("dve", lambda e: e.reciprocal(st_[:, cc_:cc_ + 1], acc[:, co + 64:co + 65]), reads=[Bacc], writes=[Bst_])
                        K.op("dve", lambda e: e.tensor_tensor(st_[:, cc_:cc_ + 1], st_[:, cc_:cc_ + 1], gts[:, qt, 3 * h + 1 + br_:3 * h + 2 + br_], ALU.mult),
                             reads=[Bst_, Bgts[qt]], writes=[Bst_])
                        K.op("dve", lambda e: e.scalar_tensor_tensor(y_o[:, h * 64:(h + 1) * 64], acc[:, co:co + 64], st_[:, cc_:cc_ + 1], y_o[:, h * 64:(h + 1) * 64],
                                                                     ALU.mult, ALU.add),
                             reads=[Bacc, Bst_, By_o], writes=[By_o])
                tm_groupnorm(P.psg, y_o, By_o, yst_, Byst, P.yb, 3, l, qt)
            IL.run(NT, 4, _mk_na, _body_na)
            dbg_dump(3)
            wout_partial(es, l, 3)
            phase_end(es)

        combT = sb(top, "combT", [128, T], BF16)
        BcombT = [Buf(f"combT{c}") for c in range(4)]
        K.op("pool", lambda e: e.memset(combT[:], 0.0), writes=BcombT)
        gon = sb(top, "gon", [128, 4, 256], F32); Bgon = Buf("gon")
        gcolt = sb(top, "gcolt", [128, 8], F32); Bgcol = Buf("gcolt")

        io_box = {"final": False, "stored": set(), "loaded": set()}
        for s in range(n_seq):
            for i in range(NT):
                if (s, i) not in io_box["loaded"]:
                    K.dma("sp", x_sb[:, i, :], dr["x"][s, i * 128:(i + 1) * 128, :], writes=Bx[i])
            K.dma("sp", posi[:], dr["pos"][s].rearrange("(n p) -> p n", p=128), writes=[Bposi], allow_slow_non_contiguous=True)
            K.op("dve", lambda e: e.tensor_copy(posf[:], posi[:]), reads=[Bposi], writes=[Bposf])
            for l in range(depth):
                for m in range(4):
                    K.dma("sp", gon[:, m, :], dr["out_norm"][l, m:m + 1, :].to_broadcast([128, 256]), writes=[Bgon])
                K.dma("sp", gcolt[:], dr["out_norm_t"][l], writes=[Bgcol])
                l_box[0] = l
                io_box["final"] = (l == depth - 1) and ("moe" in phases)
                norm_phase(s, l, "mix_norm", False)
                if "mla" in phases:
                    mla_phase(s, l)
                if "lru" in phases:
                    lru_phase(s, l)
                if "s5" in phases:
                    s5_phase(s, l)
                if "nsa" in phases:
                    nsa_phase(s, l)
                if "moe" in phases:
                    moe_phase(s, l)
            if dbg:
                K.dma("pool", dbg_d["xnT"][:], xnT[:], reads=[b for t_ in BxnT for b in t_], writes=[Bout])
                for i in range(NT):
                    K.dma("sp", dbg_d["x"][i * 128:(i + 1) * 128, :], x_sb[:, i, :], reads=Bx[i], writes=[Bout])
            for i in range(NT):
                if (s, i) not in io_box["stored"]:
                    K.dma("sp", out_d[s, i * 128:(i + 1) * 128, :], x_sb[:, i, :], reads=Bx[i], writes=[Bout])
            K.barrier_all()
        K.barrier_all()
        K.close()
    return nc, K


def prep_weights(inp):
    f = np.float32
    L = DEPTH
    w = {}
    for k in ("mix_norm", "ffn_norm", "w_in", "w_out", "mla_g_cq", "mla_g_ckv", "mla_w_uq", "mla_w_ukv", "mla_g_q", "mla_g_k",
              "s5_w_glu", "nsa_g_q", "nsa_g_k", "out_norm", "moe_w_gate", "moe_w_up", "moe_w_down"):
        w[k] = np.ascontiguousarray(inp[k], dtype=f)
    def pc(v):
        return np.ascontiguousarray(np.asarray(v, f).reshape(L, 2, 128).transpose(0, 2, 1))
    w["lru_cw"] = np.ascontiguousarray(np.asarray(inp["lru_conv_w"], f).reshape(L, 4, 2, 128).transpose(0, 3, 2, 1))
    w["lru_vec"] = np.ascontiguousarray(np.stack([pc(inp["lru_conv_b"]), pc(np.asarray(inp["lru_b_a"]).reshape(L, 256)),
                                                  pc(np.asarray(inp["lru_b_i"]).reshape(L, 256)), pc(inp["lru_lambda"]),
                                                  np.zeros((L, 128, 2), f)], axis=2))
    for nm, src in (("lru_wa", "lru_w_a"), ("lru_wi", "lru_w_i")):
        a = np.zeros((L, 2, 128, 128), f)
        W = np.asarray(inp[src], f)
        for c in range(2):
            for hh in range(2):
                a[:, c, hh * 64:(hh + 1) * 64, hh * 64:(hh + 1) * 64] = W[:, 2 * c + hh]
        w[nm] = a
    def st(v):
        return np.asarray(v, f).reshape(L, 8, 128).transpose(0, 2, 1)
    ldt = np.repeat(np.asarray(inp["s5_log_dt"], f)[:, :, None], 64, axis=2)
    w["s5_par"] = np.ascontiguousarray(np.stack([st(inp["s5_a_re"]), st(inp["s5_a_im"]), st(ldt)], axis=2))
    def stb(v):
        return np.asarray(v, f).reshape(L, 8, 128, 16).transpose(0, 2, 1, 3)
    w["s5_b"] = np.ascontiguousarray(np.stack([stb(inp["s5_b_re"]), stb(inp["s5_b_im"])], axis=2))
    cpad = np.zeros((L, 2, 8, 128, 128), f)
    for ri, nm in enumerate(("s5_c_re", "s5_c_im")):
        Cm = np.asarray(inp[nm], f)
        for g in range(16):
            j = g // 2
            rows = slice((g % 2) * 64, (g % 2) * 64 + 64)
            cols = slice((16 * g) % 128, (16 * g) % 128 + 16)
            cpad[:, ri, j, rows, cols] = Cm[:, g].transpose(0, 2, 1)
    w["s5_c"] = cpad
    w["s5_vec"] = np.ascontiguousarray(np.stack([pc(inp["s5_d"]), pc(inp["s5_b_glu"]), np.zeros((L, 128, 2), f)], axis=2))
    w["nsa_pe"] = np.ascontiguousarray(np.stack([np.asarray(inp["nsa_pe_k"], f).transpose(0, 2, 1),
                                                 np.asarray(inp["nsa_pe_v"], f).transpose(0, 2, 1)], axis=1))
    w["nsa_w1"] = np.ascontiguousarray(np.stack([np.asarray(inp["nsa_w1_k"], f).reshape(L, 32, 64, 128).transpose(0, 2, 1, 3),
                                                 np.asarray(inp["nsa_w1_v"], f).reshape(L, 32, 64, 128).transpose(0, 2, 1, 3)], axis=1))
    w["nsa_w2"] = np.ascontiguousarray(np.stack([np.asarray(inp["nsa_w2_k"], f), np.asarray(inp["nsa_w2_v"], f)], axis=1))
    w["out_norm_t"] = np.ascontiguousarray(np.asarray(inp["out_norm"], f).reshape(L, 8, 128).transpose(0, 2, 1))
    w["moe_wr"] = np.ascontiguousarray(np.concatenate([np.asarray(inp["moe_w_rg"], f), np.asarray(inp["moe_w_re"], f)], axis=2))
    w["moe_br"] = np.ascontiguousarray(np.concatenate([np.asarray(inp["moe_b_rg"], f), np.asarray(inp["moe_b_re"], f)], axis=1))
    w["c_ident"] = np.eye(128, dtype=f)
    kk = np.arange(128)[:, None]; qq = np.arange(128)[None, :]
    w["c_tri"] = (qq >= kk).astype(f)
    w["c_anti"] = (kk > qq).astype(f)
    cc = np.arange(128)[:, None]; tq = np.arange(T)[None, :]
    w["c_cmpmask"] = ((16 * cc + 31 <= tq) & (cc < 127)).astype(f)
    csn = np.arange(127) * 16; ssn = np.arange(32) * 64
    ov = np.clip(np.minimum(csn[:, None] + 32, ssn[None, :] + 64) - np.maximum(csn[:, None], ssn[None, :]), 0, None) / 16.0
    ovp = np.zeros((128, 32), f); ovp[:127] = ov
    w["c_ov"] = ovp
    tpos = np.arange(T); cur = tpos // 64; sbk = np.arange(32)
    forced = (sbk[None, :] == 0) | (sbk[None, :] == cur[:, None]) | (sbk[None, :] == cur[:, None] - 1)
    future = sbk[None, :] > cur[:, None]
    keep = (~forced & ~future).astype(f)
    base = np.where(future, -1e30, np.where(forced, 1e30, 0.0)).astype(f)
    w["c_keep"] = np.ascontiguousarray(keep.reshape(NT, 128, 32).transpose(1, 0, 2))
    w["c_base"] = np.ascontiguousarray(base.reshape(NT, 128, 32).transpose(1, 0, 2))
    E = np.zeros((32, NT, 128), f)
    for kt in range(NT):
        for m_ in range(128):
            E[2 * kt + m_ // 64, kt, m_] = BIGNEG
    w["c_E"] = E
    w["c_inv_mla"] = np.tile((500000.0 ** (-np.arange(16, dtype=np.float64) * 2.0 / 32)).astype(f)[None, :], (128, 1))
    w["c_inv_nsa"] = np.tile((500000.0 ** (-np.arange(8, dtype=np.float64) * 2.0 / 16)).astype(f)[None, :], (128, 1))
    sE = np.zeros((32, 16, 128), f)
    for e_ in range(16):
        sE[e_, e_, :] = 1.0
        sE[16 + e_, e_, :] = 1.0
    w["c_selE"] = sE
    for k, shp in W_SPECS.items():
        assert list(w[k].shape) == shp, (k, w[k].shape, shp)
    return w


_CACHE = {}


def kernel(**inputs):
    n_cores = 8
    x = np.ascontiguousarray(inputs["x"], dtype=np.float32)
    pos = np.ascontiguousarray(inputs["positions"], dtype=np.int32)
    w = prep_weights(inputs)
    if "nc" not in _CACHE:
        _CACHE["nc"] = build_program(n_seq=2)[0]
    nc = _CACHE["nc"]
    in_maps = []
    for c in range(n_cores):
        m = dict(w)
        m["x"] = x[2 * c:2 * c + 2]
        m["pos"] = pos[2 * c:2 * c + 2]
        in_maps.append(m)
    res = run_bass_kernel_spmd(nc, in_maps, core_ids=list(range(n_cores)))
    return np.concatenate([r["out"] for r in res.results], axis=0)
```

```python
import numpy as np
from contextlib import ExitStack
import concourse.bass as bass
import concourse.mybir as mybir
from concourse.bass_utils import run_bass_kernel_spmd

F32 = mybir.dt.float32
BF16 = mybir.dt.bfloat16
I32 = mybir.dt.int32
AF = mybir.ActivationFunctionType
ALU = mybir.AluOpType
AX = mybir.AxisListType

T = 2048
NT = 16
D = 1024
DC = 8
DEPTH = 2
EPS = 1e-6
TWO_PI = 6.283185307179586
BIGNEG = 30000.0
EPOCH = 30000


class Buf:
    __slots__ = ("name", "last_w", "readers", "excl")

    def __init__(self, name, excl=False):
        self.name = name
        self.last_w = None
        self.readers = []
        self.excl = excl


class Prod:
    def __init__(self, K, key, step):
        self.K = K
        self.key = key
        self.step = step
        self.count = 0
        self.sems = []

    def sem_for(self, idx):
        ep = idx // EPOCH
        while len(self.sems) <= ep:
            self.sems.append(self.K.new_sem(f"{self.key}_{len(self.sems)}"))
        return self.sems[ep], ((idx % EPOCH) + 1) * self.step, ep


class _PEProxy:
    def __init__(self, real):
        self.real = real
        self.last_stop = True

    def matmul(self, *a, **k):
        self.last_stop = bool(k.get("stop", True))
        return self.real.matmul(*a, **k)

    def transpose(self, *a, **k):
        self.last_stop = True
        return self.real.transpose(*a, **k)


class Kern:
    def __init__(self, nc, n_dma_lanes=16):
        self.nc = nc
        self._sem_ctx = []
        self.prods = {}
        self.engs = {"pe": nc.tensor, "act": nc.scalar, "dve": nc.vector, "pool": nc.gpsimd, "sp": nc.sync}
        for k in self.engs:
            self.prods[k] = Prod(self, k, 1)
        self.lanes = {}
        self.lane_rr = {}
        for q in ("sp", "pool", "act"):
            self.lanes[q] = []
            self.lane_rr[q] = 0
            for i in range(n_dma_lanes // 2):
                p = Prod(self, f"dma_{q}{i}", 16)
                self.prods[p.key] = p
                self.lanes[q].append(p)
        self._pe_proxy = _PEProxy(nc.tensor)
        self.bar_scratch = None
        self._switch = None
        self.waited = {}
        self.n_inst = 0
        self.n_wait = 0

    def new_sem(self, name):
        ctx = self.nc.semaphore(name)
        s = ctx.__enter__()
        self._sem_ctx.append(ctx)
        return s

    def close(self):
        for c in reversed(self._sem_ctx):
            c.__exit__(None, None, None)
        self._sem_ctx = []

    def _deps(self, me_key, reads, writes):
        deps = set()
        for b in reads:
            if b.last_w is not None:
                deps.add(b.last_w)
            if b.excl:
                for r in b.readers:
                    if r[0] != me_key:
                        deps.add(r)
        for b in writes:
            if b.last_w is not None:
                deps.add(b.last_w)
            deps.update(b.readers)
        return deps

    def _emit_waits(self, engname, deps, self_key=None, attach=False):
        eng = self.engs[engname]
        need = {}
        for (pk, idx) in deps:
            if pk == self_key and pk == "pe":
                continue
            sem, val, ep = self.prods[pk].sem_for(idx)
            k = (pk, ep)
            if need.get(k, (None, 0))[1] < val:
                need[k] = (sem, val)
        pend = []
        for (pk, ep), (sem, val) in need.items():
            wk = (engname, pk, ep)
            if self.waited.get(wk, 0) >= val:
                continue
            pend.append((sem, val))
            self.waited[wk] = val
        last = pend.pop() if (attach and pend) else None
        for (sem, val) in pend:
            eng.wait_ge(sem, val)
            self.n_wait += 1
        return last

    def _record(self, me, reads, writes):
        for b in reads:
            b.readers.append(me)
            if len(b.readers) > 48:
                b.readers = b.readers[-48:]
        for b in writes:
            b.last_w = me
            b.readers = []

    def op(self, engname, fn, reads=(), writes=()):
        prod = self.prods[engname]
        deps = self._deps(engname, reads, writes)
        last = self._emit_waits(engname, deps, self_key=engname, attach=True)
        if engname == "pe":
            self._pe_proxy.last_stop = True
            ins = fn(self._pe_proxy)
            inc = self._pe_proxy.last_stop
        else:
            ins = fn(self.engs[engname])
            inc = True
        if last is not None:
            ins._wait_ge(last[0], last[1])
        idx = prod.count
        self.n_inst += 1
        if inc:
            sem, val, ep = prod.sem_for(idx)
            ins.then_inc(sem, 1)
            prod.count += 1
        self._record((engname, idx), reads, writes)
        if self._switch is not None:
            self._switch()
        return ins

    def dma(self, qname, out, in_, reads=(), writes=(), **kw):
        lane = self.lanes[qname][self.lane_rr[qname]]
        self.lane_rr[qname] = (self.lane_rr[qname] + 1) % len(self.lanes[qname])
        deps = self._deps(lane.key, reads, writes)
        if lane.count > 0:
            deps.add((lane.key, lane.count - 1))
        last = self._emit_waits(qname, deps, attach=True)
        idx = lane.count
        sem, val, ep = lane.sem_for(idx)
        ins = self.engs[qname].dma_start(out=out, in_=in_, **kw)
        if last is not None:
            ins._wait_ge(last[0], last[1])
        ins.then_inc(sem, 16)
        lane.count += 1
        self.n_inst += 1
        self._record((lane.key, idx), reads, writes)
        if self._switch is not None:
            self._switch()
        return ins

    def barrier_all(self):
        deps = set()
        for pk, p in self.prods.items():
            if p.count > 0:
                deps.add((pk, p.count - 1))
        if self.bar_scratch is None:
            for e in self.engs:
                self._emit_waits(e, deps)
            return
        self._emit_waits("pool", deps)
        snap = {k: v for k, v in self.waited.items() if k[0] == "pool"}
        sw = self._switch
        self._switch = None
        scr = self.bar_scratch
        self.op("pool", lambda e: e.memset(scr, 0.0))
        self._switch = sw
        idx = self.prods["pool"].count - 1
        for e in self.engs:
            if e == "pool":
                continue
            self._emit_waits(e, {("pool", idx)})
            for (_, pk, ep), val in snap.items():
                wk = (e, pk, ep)
                if self.waited.get(wk, 0) < val:
                    self.waited[wk] = val


class NS:
    def __init__(self, **kw):
        self.__dict__.update(kw)


class Interleaver:
    def __init__(self, K):
        self.K = K

    def run(self, n, W, mk, body):
        import threading
        K = self.K
        W = max(1, min(W, n))
        ctxs = [mk(w) for w in range(W)]
        if W == 1:
            for i in range(n):
                body(i, ctxs[0])
            return
        sems = [threading.Semaphore(0) for _ in range(W)]
        alive = [True] * W
        done = threading.Event()
        err = []
        state = {"cur": 0}

        def next_live(w):
            for d in range(1, W + 1):
                v = (w + d) % W
                if alive[v]:
                    return v
            return None

        def switch():
            w = state["cur"]
            v = next_live(w)
            if v is None or v == w:
                return
            state["cur"] = v
            sems[v].release()
            sems[w].acquire()

        def worker(w):
            sems[w].acquire()
            try:
                if not err:
                    for i in range(w, n, W):
                        body(i, ctxs[w])
                        if err:
                            break
            except BaseException as e:
                err.append(e)
            alive[w] = False
            v = next_live(w)
            if v is None:
                done.set()
            else:
                state["cur"] = v
                sems[v].release()

        ths = [threading.Thread(target=worker, args=(w,)) for w in range(W)]
        for t in ths:
            t.start()
        K._switch = switch
        state["cur"] = 0
        sems[0].release()
        done.wait()
        K._switch = None
        for t in ths:
            t.join()
        if err:
            raise err[0]


class Pool:
    _uid = [0]

    def __init__(self, es, nc, name, shape, dtype, n, psum=False):
        self.items = []
        Pool._uid[0] += 1
        name = f"{name}_u{Pool._uid[0]}_"
        for i in range(n):
            if psum:
                t = es.enter_context(nc.psum_tensor(f"{name}{i}", shape, dtype))
            else:
                t = es.enter_context(nc.sbuf_tensor(f"{name}{i}", shape, dtype))
            self.items.append((t, Buf(f"{name}{i}", excl=psum)))
        self.i = 0

    def get(self):
        it = self.items[self.i]
        self.i = (self.i + 1) % len(self.items)
        return it

    def sub(self, idxs):
        p = Pool.__new__(Pool)
        p.items = [self.items[k] for k in idxs]
        p.i = 0
        return p


W_SPECS = {
    "mix_norm": [DEPTH, D], "ffn_norm": [DEPTH, D], "w_in": [DEPTH, D, 1772], "w_out": [DEPTH, D, D],
    "mla_g_cq": [DEPTH, 192], "mla_g_ckv": [DEPTH, 128], "mla_w_uq": [DEPTH, 192, 384],
    "mla_w_ukv": [DEPTH, 128, 512], "mla_g_q": [DEPTH, 96], "mla_g_k": [DEPTH, 96],
    "lru_cw": [DEPTH, 128, 2, 4], "lru_vec": [DEPTH, 128, 5, 2], "lru_wa": [DEPTH, 2, 128, 128],
    "lru_wi": [DEPTH, 2, 128, 128],
    "s5_par": [DEPTH, 128, 3, 8], "s5_b": [DEPTH, 128, 2, 8, 16], "s5_c": [DEPTH, 2, 8, 128, 128],
    "s5_vec": [DEPTH, 128, 3, 2], "s5_w_glu": [DEPTH, 256, 256],
    "nsa_g_q": [DEPTH, 64], "nsa_g_k": [DEPTH, 3, 64], "nsa_pe": [DEPTH, 2, 64, 32],
    "nsa_w1": [DEPTH, 2, 64, 32, 128], "nsa_w2": [DEPTH, 2, 128, 64],
    "out_norm": [DEPTH, 4, 256], "out_norm_t": [DEPTH, 128, 8],
    "moe_wr": [DEPTH, D, 20], "moe_br": [DEPTH, 20],
    "moe_w_gate": [DEPTH, 16, D, 256], "moe_w_up": [DEPTH, 16, D, 256], "moe_w_down": [DEPTH, 16, 256, D],
    "c_ident": [128, 128], "c_tri": [128, 128], "c_anti": [128, 128], "c_cmpmask": [128, T],
    "c_ov": [128, 32], "c_keep": [128, NT, 32], "c_base": [128, NT, 32], "c_E": [32, NT, 128],
    "c_inv_mla": [128, 16], "c_inv_nsa": [128, 8], "c_selE": [32, 16, 128],
}


def build_program(n_seq=2, depth=DEPTH, dbg=None, phases=("mla", "lru", "s5", "nsa", "moe")):
    nc = bass.Bass("TRN2", target_bir_lowering=False)
    dr = {}
    dr["x"] = nc.dram_tensor("x", [n_seq, T, D], F32, kind="ExternalInput").ap()
    dr["pos"] = nc.dram_tensor("pos", [n_seq, T], I32, kind="ExternalInput").ap()
    for k, shp in W_SPECS.items():
        dr[k] = nc.dram_tensor(k, shp, F32, kind="ExternalInput").ap()
    out_d = nc.dram_tensor("out", [n_seq, T, D], F32, kind="ExternalOutput").ap()
    dbg_d = {}
    if dbg:
        dbg_d["ymT"] = nc.dram_tensor("dbg_ymT", [4, 128, 2, T], F32, kind="ExternalOutput").ap()
        dbg_d["x"] = nc.dram_tensor("dbg_x", [T, D], F32, kind="ExternalOutput").ap()
        dbg_d["xnT"] = nc.dram_tensor("dbg_xnT", [128, DC, T], F32, kind="ExternalOutput").ap()

    K = Kern(nc)
    IL = Interleaver(K)
    Bout = Buf("out")
    with ExitStack() as top:
        def sb(es, name, shape, dt):
            Pool._uid[0] += 1
            return es.enter_context(nc.sbuf_tensor(f"{name}_u{Pool._uid[0]}", shape, dt))

        x_sb = sb(top, "x_sb", [128, NT, D], F32)
        Bx = [[Buf(f"x{i}_{h}") for h in range(2)] for i in range(NT)]
        xnT = sb(top, "xnT", [128, DC, T], BF16)
        BxnT = [[Buf(f"xnT{i}_{h}") for h in range(2)] for i in range(NT)]
        ymT_box = {}
        l_box = [0]

        def xnT_bufs(ch):
            return [BxnT[i][h] for i in range(ch * 4, ch * 4 + 4) for h in range(2)]

        ident = sb(top, "ident", [128, 128], F32); Bident = Buf("ident")
        identb = sb(top, "identb", [128, 128], BF16); Bidentb = Buf("identb")
        ones16 = sb(top, "ones16", [128, 128], BF16); Bones = Buf("ones16")
        tri4 = sb(top, "tri4", [128, 4, 128], BF16); Btri = Buf("tri4")
        anti4 = sb(top, "anti4", [128, 4, 128], BF16); Banti = Buf("anti4")
        posf = sb(top, "posf", [128, NT], F32); Bposf = Buf("posf")
        posi = sb(top, "posi", [128, NT], I32); Bposi = Buf("posi")
        gain = sb(top, "gain", [128, D], F32); Bgain = Buf("gain")
        ss = sb(top, "ss", [128, NT], F32); Bss = Buf("ss")
        rs = sb(top, "rs", [128, NT], F32); Brs = Buf("rs")
        epsb = sb(top, "epsb", [128, 1], F32); Beps = Buf("epsb")
        barscr = sb(top, "barscr", [128, 4], F32)
        K.bar_scratch = barscr[:, 0:1]

        psg = Pool(top, nc, "psg", [128, 512], F32, 6, psum=True)
        psa = Pool(top, nc, "psa", [128, 512], F32, 2, psum=True)
        allps = psg.sub(range(6))
        allps.items = psg.items + psa.items

        K.dma("sp", ident[:], dr["c_ident"][:], writes=[Bident])
        K.op("act", lambda e: e.copy(identb[:], ident[:]), reads=[Bident], writes=[Bidentb])
        K.op("dve", lambda e: e.memset(ones16[:], 1.0), writes=[Bones])
        K.op("dve", lambda e: e.memset(epsb[:], EPS), writes=[Beps])
        for h in range(4):
            K.dma("pool", tri4[:, h, :], dr["c_tri"][:], writes=[Btri])
            K.dma("pool", anti4[:, h, :], dr["c_anti"][:], writes=[Banti])

        def _zeroed(pool):
            for (t_, b_) in pool.items:
                K.op("pool", lambda e: e.memset(t_[:], 0.0), writes=[b_])
            return pool

        def phase_end(es):
            K.barrier_all()
            es.close()

        def rstd_from(out_ap, in_ap, n_feat, reads, writes, eng_tmp=None):
            K.op("act", lambda e: e.activation(out_ap, in_ap, AF.Sqrt, bias=epsb[0:out_ap.shape[0], 0:1], scale=1.0 / n_feat),
                 reads=list(reads) + [Beps], writes=writes)
            K.op("dve", lambda e: e.reciprocal(out_ap, out_ap), reads=writes, writes=writes)

        def norm_phase(s, l, which, router):
            es = ExitStack()
            tmpA = Pool(es, nc, "nrmA", [128, D], BF16, 2)
            K.dma("sp", gain[:], dr[which][l:l + 1, :].to_broadcast([128, D]), writes=[Bgain])
            if router:
                wr = sb(es, "wr", [128, DC, 20], F32); Bwr = Buf("wr")
                br = sb(es, "br", [128, 20], F32); Bbr = Buf("br")
                K.dma("sp", wr[:], dr["moe_wr"][l].rearrange("(c p) n -> p c n", p=128), writes=[Bwr])
                K.dma("sp", br[:], dr["moe_br"][l:l + 1, :].to_broadcast([128, 20]), writes=[Bbr])
            for i in range(NT):
                junk, Bj = tmpA.get()
                K.op("act", lambda e: e.activation(junk[:], x_sb[:, i, :], AF.Square, accum_out=ss[:, i:i + 1]),
                     reads=Bx[i], writes=[Bj, Bss])
            rstd_from(rs[:, :], ss[:, :], D, [Bss], [Brs])
            def _mk_nr(w):
                per = 8 // 4
                d_ = dict(psg=allps.sub(range(w * per, w * per + per)), tmpA=Pool(es, nc, "nrmX", [128, D], F32, 1))
                if router:
                    d_["xT32"] = Pool(es, nc, "xT32", [128, DC, 128], F32, 1)
                    d_["rt"] = Pool(es, nc, "rt", [128, 96], F32, 2)
                else:
                    d_["xb"] = Pool(es, nc, "nrmB", [128, D], BF16, 1)
                return NS(**d_)

            def _body_nr(i, P):
                if not router:
                    xb, Bxb = P.xb.get()
                    K.op("dve", lambda e: e.scalar_tensor_tensor(xb[:], x_sb[:, i, :], rs[:, i:i + 1], gain[:], ALU.mult, ALU.mult),
                         reads=Bx[i] + [Brs, Bgain], writes=[Bxb])
                    pb, Bpb = P.psg.get()
                    pbb = pb[:, :].bitcast(BF16)
                    for c in range(DC):
                        K.op("pe", lambda e: e.transpose(pbb[:, c * 128:(c + 1) * 128], xb[:, c * 128:(c + 1) * 128], identb[:]),
                             reads=[Bxb, Bidentb], writes=[Bpb])
                    K.op("act", lambda e: e.copy(xnT[:, :, i * 128:(i + 1) * 128], pbb[:, :].rearrange("p (c t) -> p c t", c=DC)),
                         reads=[Bpb], writes=[BxnT[i][0], BxnT[i][1]])
                    return
                xn, Bxn = P.tmpA.get()
                K.op("dve", lambda e: e.scalar_tensor_tensor(xn[:], x_sb[:, i, :], rs[:, i:i + 1], gain[:], ALU.mult, ALU.mult),
                     reads=Bx[i] + [Brs, Bgain], writes=[Bxn])
                if router:
                    xt, Bxt = P.xT32.get()
                for half in range(2):
                    pb, Bpb = P.psg.get()
                    for cc in range(4):
                        c = half * 4 + cc
                        K.op("pe", lambda e: e.transpose(pb[:, cc * 128:(cc + 1) * 128], xn[:, c * 128:(c + 1) * 128], ident[:]),
                             reads=[Bxn, Bident], writes=[Bpb])
                    src = pb[:, :].rearrange("p (c t) -> p c t", c=4)
                    K.op("act", lambda e: e.copy(xnT[:, half * 4:half * 4 + 4, i * 128:(i + 1) * 128], src),
                         reads=[Bpb], writes=[BxnT[i][half]])
                    if router:
                        K.op("dve", lambda e: e.tensor_copy(xt[:, half * 4:half * 4 + 4, :], src), reads=[Bpb], writes=[Bxt])
                if router:
                    lg, Blg = P.psg.get()
                    for c in range(DC):
                        K.op("pe", lambda e: e.matmul(lg[:, 0:20], xt[:, c, :], wr[:, c, :], start=(c == 0), stop=(c == DC - 1)),
                             reads=[Bxt, Bwr], writes=[Blg])
                    r, Br_ = P.rt.get()
                    R = [Br_]
                    Lg = r[:, 0:20]; m = r[:, 20:21]; nm = r[:, 21:22]; e4 = r[:, 22:26]; se = r[:, 26:27]
                    oh = r[:, 27:31]; pen = r[:, 31:35]; lem = r[:, 35:51]; top8 = r[:, 51:59]; sel = r[:, 59:75]
                    nv1 = r[:, 75:76]; den = r[:, 76:77]; fac = r[:, 77:78]
                    r2, Br2 = P.rt.get()
                    ew = r2[:, 0:16]; sw = r2[:, 16:32]; comb = r2[:, 32:48]
                    R2 = [Br2]
                    K.op("dve", lambda e: e.tensor_tensor(Lg, lg[:, 0:20], br[:], ALU.add), reads=[Blg, Bbr], writes=R)
                    K.op("dve", lambda e: e.tensor_reduce(m, r[:, 0:4], AX.X, ALU.max), reads=R, writes=R)
                    K.op("dve", lambda e: e.tensor_scalar(nm, m, -1.0, None, ALU.mult), reads=R, writes=R)
                    K.op("act", lambda e: e.activation(e4, r[:, 0:4], AF.Exp, bias=nm, accum_out=se), reads=R, writes=R)
                    K.op("dve", lambda e: e.tensor_scalar(oh, r[:, 0:4], m, None, ALU.is_ge), reads=R, writes=R)
                    K.op("dve", lambda e: e.tensor_scalar(pen, oh, 1.0, 1e30, ALU.subtract, ALU.mult), reads=R, writes=R)
                    K.op("dve", lambda e: e.tensor_tensor(lem.rearrange("p (g i) -> p g i", g=4),
                                                          r[:, 4:20].rearrange("p (g i) -> p g i", g=4),
                                                          pen.unsqueeze(2).to_broadcast([128, 4, 4]), ALU.add), reads=R, writes=R)
                    K.op("dve", lambda e: e.max(top8, lem), reads=R, writes=R)
                    K.op("dve", lambda e: e.tensor_scalar(sel, lem, r[:, 52:53], None, ALU.is_ge), reads=R, writes=R)
                    K.op("dve", lambda e: e.tensor_scalar(nv1, r[:, 51:52], -1.0, None, ALU.mult), reads=R, writes=R)
                    K.op("act", lambda e: e.activation(ew, lem, AF.Exp, bias=nv1), reads=R, writes=R2)
                    K.op("dve", lambda e: e.scalar_tensor_tensor(sw, sel, 1.0, ew, ALU.mult, ALU.mult, accum_out=den), reads=R + R2, writes=R + R2)
                    K.op("dve", lambda e: e.tensor_tensor(fac, den, se, ALU.mult), reads=R, writes=R)
                    K.op("dve", lambda e: e.reciprocal(fac, fac), reads=R, writes=R)
                    K.op("dve", lambda e: e.tensor_scalar(comb, sw, fac, None, ALU.mult), reads=R + R2, writes=R2)
                    chl = r2[:, 48:64].bitcast(BF16)
                    K.op("dve", lambda e: e.tensor_copy(chl[:, 0:16], comb), reads=R2, writes=R2)
                    K.op("dve", lambda e: e.tensor_copy(r2[:, 64:80], chl[:, 0:16]), reads=R2, writes=R2)
                    K.op("dve", lambda e: e.tensor_tensor(chl[:, 16:32], comb, r2[:, 64:80], ALU.subtract), reads=R2, writes=R2)
                    pt, Bpt = P.psg.get()
                    ptb = pt[:, :].bitcast(BF16)
                    K.op("pe", lambda e: e.transpose(ptb[0:32, 0:128], chl, identb[:]), reads=R2 + [Bidentb], writes=[Bpt])
                    K.op("act", lambda e: e.copy(combT[0:32, i * 128:(i + 1) * 128], ptb[0:32, 0:128]), reads=[Bpt], writes=[BcombT[i // 4]])

            IL.run(NT, 4, _mk_nr, _body_nr)
            phase_end(es)

        def moe_phase(s, l):
            es = ExitStack()
            selE = sb(es, "selE", [128, 16, 128], BF16); BselE = Buf("selE")
            K.op("pool", lambda e: e.memset(selE[:], 0.0), writes=[BselE])
            K.dma("pool", selE[0:32], dr["c_selE"][:], writes=[BselE])
            wgu = Pool(es, nc, "wgu", [128, DC, 512], BF16, 2)
            wdp = Pool(es, nc, "wdp", [128, 2, D], BF16, 2)
            cbp = Pool(es, nc, "cbp", [128, 512], F32, 2)
            sgp = Pool(es, nc, "sgp", [128, 512], F32, 3)
            hep = Pool(es, nc, "hep", [128, 2, 512], BF16, 2)
            def load_expert(ex):
                wg, Bwg = wgu.get()
                wd, Bwd = wdp.get()
                K.dma("pool", wg[:, :, 0:256], dr["moe_w_gate"][l, ex].rearrange("(c p) f -> p c f", p=128), writes=[Bwg])
                K.dma("pool", wg[:, :, 256:512], dr["moe_w_up"][l, ex].rearrange("(c p) f -> p c f", p=128), writes=[Bwg])
                K.dma("pool", wd[:], dr["moe_w_down"][l, ex].rearrange("(c p) f -> p c f", p=128), writes=[Bwd])
                return wg, Bwg, wd, Bwd
            W = {}
            W[0] = load_expert(0)
            norm_phase(s, l, "ffn_norm", True)
            steps = [(ex, ch) for ex in range(16) for ch in range(4)]
            hes = {}

            def stage_a(ex, ch):
                wg, Bwg, wd, Bwd = W[ex]
                cs = slice(ch * 512, (ch + 1) * 512)
                cbps, Bcbps = psg.get()
                K.op("pe", lambda e: e.matmul(cbps[:, :], selE[:, ex, :], combT[:, cs], start=True, stop=True),
                     reads=[BselE, BcombT[ch]], writes=[Bcbps])
                he, Bhe = hep.get()
                for fc in range(2):
                    gps, Bgps = psg.get()
                    ups, Bups = psg.get()
                    for c in range(DC):
                        K.op("pe", lambda e: e.matmul(gps[:, :], wg[:, c, fc * 128:(fc + 1) * 128], xnT[:, c, cs],
                                                      start=(c == 0), stop=(c == DC - 1)),
                             reads=[Bwg] + xnT_bufs(ch), writes=[Bgps])
                    for c in range(DC):
                        K.op("pe", lambda e: e.matmul(ups[:, :], wg[:, c, 256 + fc * 128:256 + (fc + 1) * 128], xnT[:, c, cs],
                                                      start=(c == 0), stop=(c == DC - 1)),
                             reads=[Bwg] + xnT_bufs(ch), writes=[Bups])
                    sg, Bsg = sgp.get()
                    K.op("act", lambda e: e.activation(sg[:], gps[:, :], AF.Silu), reads=[Bgps], writes=[Bsg])
                    K.op("dve", lambda e: e.tensor_tensor(sg[:], sg[:], cbps[:, :], ALU.mult), reads=[Bsg, Bcbps], writes=[Bsg])
                    K.op("dve", lambda e: e.tensor_tensor(he[:, fc, :], sg[:], ups[:, :], ALU.mult), reads=[Bsg, Bups], writes=[Bhe])
                hes[(ex, ch)] = (he, Bhe)

            def stage_b(ex, ch):
                wg, Bwg, wd, Bwd = W[ex]
                he, Bhe = hes.pop((ex, ch))
                for ts in range(4):
                    i = ch * 4 + ts
                    for half in range(2):
                        ops_, Bops = psg.get()
                        for fc in range(2):
                            K.op("pe", lambda e: e.matmul(ops_[:, :], he[:, fc, ts * 128:(ts + 1) * 128],
                                                          wd[:, fc, half * 512:(half + 1) * 512], start=(fc == 0), stop=(fc == 1)),
                                 reads=[Bhe, Bwd], writes=[Bops])
                        xs = x_sb[:, i, half * 512:(half + 1) * 512]
                        K.op("dve", lambda e: e.tensor_tensor(xs, xs, ops_[:, :], ALU.add), reads=[Bops, Bx[i][half]], writes=[Bx[i][half]])
                    if ex == 15 and io_box["final"] and not dbg:
                        K.dma("sp", out_d[s, i * 128:(i + 1) * 128, :], x_sb[:, i, :], reads=Bx[i], writes=[Bout])
                        io_box["stored"].add((s, i))
                        if s + 1 < n_seq:
                            K.dma("sp", x_sb[:, i, :], dr["x"][s + 1, i * 128:(i + 1) * 128, :], writes=Bx[i])
                            io_box["loaded"].add((s + 1, i))

            for k, (ex, ch) in enumerate(steps):
                stage_a(ex, ch)
                if k > 0:
                    pex, pch = steps[k - 1]
                    stage_b(pex, pch)
                    if pch == 3 and ex + 1 < 16:
                        W.pop(pex)
                        W[ex + 1] = load_expert(ex + 1)
                elif ex + 1 < 16:
                    W[1] = load_expert(1)
            stage_b(*steps[-1])
            phase_end(es)

        def alloc_ymT(es, m):
            ymT = sb(es, f"ymT{m}", [128, 2, T], BF16)
            BymT = [Buf(f"ymT{m}_{ch}") for ch in range(4)]
            ymT_box["t"] = ymT; ymT_box["b"] = BymT
            wo = sb(es, f"wo{m}", [128, 2, D], BF16); Bwo = Buf("wo")
            for c in range(2):
                K.dma("pool", wo[:, c, :], dr["w_out"][l_box[0], 256 * m + c * 128:256 * m + (c + 1) * 128, :], writes=[Bwo])
            ymT_box["wo"] = wo; ymT_box["Bwo"] = Bwo
            return ymT, BymT

        def wout_partial(es, l, m):
            ymT = ymT_box["t"]; BymT = ymT_box["b"]
            wo = ymT_box["wo"]; Bwo = ymT_box["Bwo"]
            for i in range(NT):
                for half in range(2):
                    ops_, Bops = psg.get()
                    for c in range(2):
                        K.op("pe", lambda e: e.matmul(ops_[:, :], ymT[:, c, i * 128:(i + 1) * 128], wo[:, c, half * 512:(half + 1) * 512],
                                                      start=(c == 0), stop=(c == 1)),
                             reads=[Bwo, BymT[i // 4]], writes=[Bops])
                    xs = x_sb[:, i, half * 512:(half + 1) * 512]
                    K.op("dve", lambda e: e.tensor_tensor(xs, xs, ops_[:, :], ALU.add), reads=[Bops, Bx[i][half]], writes=[Bx[i][half]])

        def fm_groupnorm(es_phase, es_name, yv, Byv, m, l, gcol):
            es = es_phase
            sqp = Pool(es, nc, es_name + "sq", [128, 2, 512], BF16, 2)
            rsp = Pool(es, nc, es_name + "rs", [128, 512], F32, 2)
            for ch in range(4):
                cs = slice(ch * 512, (ch + 1) * 512)
                sq, Bsq = sqp.get()
                K.op("act", lambda e: e.activation(sq[:, :, :], yv[:, :, cs], AF.Square), reads=[Byv], writes=[Bsq])
                sps, Bsps = psg.get()
                for c in range(2):
                    K.op("pe", lambda e: e.matmul(sps[:, :], ones16[:], sq[:, c, :], start=(c == 0), stop=(c == 1)),
                         reads=[Bones, Bsq], writes=[Bsps])
                rr, Brr = rsp.get()
                rstd_from(rr[:], sps[:, :], 256, [Bsps], [Brr])
                for c in range(2):
                    K.op("dve", lambda e: e.scalar_tensor_tensor(ymT_box["t"][:, c, cs], yv[:, c, cs], gcol[:, 2 * m + c:2 * m + c + 1],
                                                                 rr[:], ALU.mult, ALU.mult),
                         reads=[Byv, Brr, Bgcol], writes=[ymT_box["b"][ch]])

        def dbg_dump(m):
            if dbg:
                K.dma("pool", dbg_d["ymT"][m], ymT_box["t"][:], reads=ymT_box["b"], writes=[Bout])

        def lru_phase(s, l):
            es = ExitStack()
            ymT, BymT = alloc_ymT(es, 1)
            cw = sb(es, "lcw", [128, 2, 4], F32); Bcw = Buf("lcw")
            vec = sb(es, "lvec", [128, 5, 2], F32); Bvec = Buf("lvec")
            K.dma("sp", cw[:], dr["lru_cw"][l], writes=[Bcw])
            K.dma("sp", vec[:], dr["lru_vec"][l], writes=[Bvec])
            wa = sb(es, "lwa", [128, 2, 128], BF16); Bwa = Buf("lwa")
            wi = sb(es, "lwi", [128, 2, 128], BF16); Bwi = Buf("lwi")
            for c in range(2):
                K.dma("pool", wa[:, c, :], dr["lru_wa"][l, c], writes=[Bwa])
                K.dma("pool", wi[:, c, :], dr["lru_wi"][l, c], writes=[Bwi])
            K.op("act", lambda e: e.activation(vec[:, 4, :], vec[:, 3, :], AF.Exp, scale=-1.0), reads=[Bvec], writes=[Bvec])
            K.op("act", lambda e: e.activation(vec[:, 4, :], vec[:, 4, :], AF.Ln, bias=1.0), reads=[Bvec], writes=[Bvec])
            K.op("dve", lambda e: e.tensor_scalar(vec[:, 4, :], vec[:, 4, :], -8.0, None, ALU.mult), reads=[Bvec], writes=[Bvec])
            vec2 = sb(es, "lvec2", [128, 2], F32); Bvec2 = Buf("lvec2")
            K.op("dve", lambda e: e.tensor_scalar(vec2[:], vec[:, 4, :], 2.0, None, ALU.mult), reads=[Bvec], writes=[Bvec2])
            yv = sb(es, "lyv", [128, 2, T], F32); Byv = Buf("lyv")
            e2 = ExitStack()

            def _mk_l(w):
                per = 8 // 2
                return NS(psg=allps.sub(range(w * per, w * per + per)),
                          xp=Pool(e2, nc, "lxp", [128, T + 3], F32, 1), u=Pool(e2, nc, "lu", [128, T], F32, 1),
                          wl=Pool(e2, nc, "lwl", [128, DC, 256], BF16, 1), tp=Pool(e2, nc, "ltp", [128, 512], F32, 5),
                          u16=Pool(e2, nc, "lu16", [128, 512], BF16, 2))

            def _body_l(c, P):
                xp, Bxp = P.xp.get()
                u, Bu = P.u.get()
                wl, Bwl = P.wl.get()
                K.dma("pool", wl[:, :, 0:128], dr["w_in"][l, :, 352 + c * 128:352 + (c + 1) * 128].rearrange("(k p) n -> p k n", p=128), writes=[Bwl])
                K.dma("pool", wl[:, :, 128:256], dr["w_in"][l, :, 608 + c * 128:608 + (c + 1) * 128].rearrange("(k p) n -> p k n", p=128), writes=[Bwl])
                K.op("dve", lambda e: e.memset(xp[:, 0:3], 0.0), writes=[Bxp])
                for ch in range(4):
                    cs = slice(ch * 512, (ch + 1) * 512)
                    p1, Bp1 = P.psg.get()
                    for k in range(DC):
                        K.op("pe", lambda e: e.matmul(p1[:, :], wl[:, k, 0:128], xnT[:, k, cs], start=(k == 0), stop=(k == DC - 1)),
                             reads=[Bwl] + xnT_bufs(ch), writes=[Bp1])
                    K.op("act", lambda e: e.copy(xp[:, 3 + ch * 512:3 + (ch + 1) * 512], p1[:, :]), reads=[Bp1], writes=[Bxp])
                    p2, Bp2 = P.psg.get()
                    for k in range(DC):
                        K.op("pe", lambda e: e.matmul(p2[:, :], wl[:, k, 128:256], xnT[:, k, cs], start=(k == 0), stop=(k == DC - 1)),
                             reads=[Bwl] + xnT_bufs(ch), writes=[Bp2])
                    K.op("act", lambda e: e.activation(yv[:, c, cs], p2[:, :], AF.Gelu_apprx_tanh), reads=[Bp2], writes=[Byv])
                K.op("dve", lambda e: e.tensor_scalar(u[:], xp[:, 0:T], cw[:, c, 0:1], vec[:, 0, c:c + 1], ALU.mult, ALU.add),
                     reads=[Bxp, Bcw, Bvec], writes=[Bu])
                for j in range(1, 4):
                    K.op("dve", lambda e: e.scalar_tensor_tensor(u[:], xp[:, j:j + T], cw[:, c, j:j + 1], u[:], ALU.mult, ALU.add),
                         reads=[Bxp, Bcw, Bu], writes=[Bu])
                for ch in range(4):
                    cs = slice(ch * 512, (ch + 1) * 512)
                    u16, Bu16 = P.u16.get()
                    K.op("act", lambda e: e.copy(u16[:], u[:, cs]), reads=[Bu], writes=[Bu16])
                    pa, Bpa = P.psg.get()
                    K.op("pe", lambda e: e.matmul(pa[:, :], wa[:, c, :], u16[:], start=True, stop=True), reads=[Bwa, Bu16], writes=[Bpa])
                    pi, Bpi = P.psg.get()
                    K.op("pe", lambda e: e.matmul(pi[:, :], wi[:, c, :], u16[:], start=True, stop=True), reads=[Bwi, Bu16], writes=[Bpi])
                    r_, Br_ = P.tp.get()
                    gi, Bgi = P.tp.get()
                    mu, Bmu = P.tp.get()
                    aa, Baa = P.tp.get()
                    K.op("act", lambda e: e.activation(r_[:], pa[:, :], AF.Sigmoid, bias=vec[:, 1, c:c + 1]), reads=[Bpa, Bvec], writes=[Br_])
                    K.op("act", lambda e: e.activation(gi[:], pi[:, :], AF.Sigmoid, bias=vec[:, 2, c:c + 1]), reads=[Bpi, Bvec], writes=[Bgi])
                    K.op("act", lambda e: e.activation(aa[:], r_[:], AF.Exp, scale=vec[:, 4, c:c + 1]), reads=[Br_, Bvec], writes=[Baa])
                    K.op("act", lambda e: e.activation(mu[:], r_[:], AF.Exp, scale=vec2[:, c:c + 1]), reads=[Br_, Bvec2], writes=[Bmu])
                    K.op("act", lambda e: e.activation(mu[:], mu[:], AF.Sqrt, scale=-1.0, bias=1.0), reads=[Bmu], writes=[Bmu])
                    if ch == 0:
                        K.op("dve", lambda e: e.memset(mu[:, 0:1], 1.0), reads=[Bmu], writes=[Bmu])
                    K.op("dve", lambda e: e.tensor_tensor(mu[:], mu[:], gi[:], ALU.mult), reads=[Bmu, Bgi], writes=[Bmu])
                    K.op("dve", lambda e: e.tensor_tensor(xp[:, cs], mu[:], u[:, cs], ALU.mult), reads=[Bmu, Bu, Bxp], writes=[Bxp])
                    init = 0.0 if ch == 0 else u[:, ch * 512 - 1:ch * 512]
                    K.op("dve", lambda e: e.tensor_tensor_scan(u[:, cs], aa[:], xp[:, cs], init, ALU.mult, ALU.add),
                         reads=[Baa, Bxp, Bu], writes=[Bu])
                    K.op("dve", lambda e: e.tensor_tensor(yv[:, c, cs], yv[:, c, cs], u[:, cs], ALU.mult), reads=[Byv, Bu], writes=[Byv])
            IL.run(2, 2, _mk_l, _body_l)
            K.barrier_all()
            e2.close()
            fm_groupnorm(es, "lg", yv, Byv, 1, l, gcolt)
            dbg_dump(1)
            wout_partial(es, l, 1)
            phase_end(es)

        def s5_phase(s, l):
            es = ExitStack()
            ymT, BymT = alloc_ymT(es, 2)
            par = sb(es, "s5par", [128, 3, 8], F32); Bpar = Buf("s5par")
            bb = sb(es, "s5b", [128, 2, 8, 16], F32); Bbb = Buf("s5b")
            vec = sb(es, "s5vec", [128, 3, 2], F32); Bvec = Buf("s5vec")
            K.dma("sp", par[:], dr["s5_par"][l], writes=[Bpar])
            K.dma("sp", bb[:], dr["s5_b"][l], writes=[Bbb])
            K.dma("sp", vec[:], dr["s5_vec"][l], writes=[Bvec])
            wglu = sb(es, "s5glu", [128, 2, 256], BF16); Bwglu = Buf("s5glu")
            K.dma("pool", wglu[:], dr["s5_w_glu"][l].rearrange("(c p) n -> p c n", p=128), writes=[Bwglu])
            ypre = sb(es, "s5ypre", [128, 2, T], F32); Bypre = Buf("s5ypre")
            u16 = sb(es, "s5u16", [128, 2, T], BF16); Bu16 = Buf("s5u16")
            es2 = ExitStack()
            ws = sb(es2, "ws5", [128, DC, 256], BF16); Bws = Buf("ws5")
            K.dma("pool", ws[:], dr["w_in"][l, :, 864:1120].rearrange("(c p) n -> p c n", p=128), writes=[Bws])
            for c in range(2):
                for ch in range(4):
                    cs = slice(ch * 512, (ch + 1) * 512)
                    p1, Bp1 = psg.get()
                    for k in range(DC):
                        K.op("pe", lambda e: e.matmul(p1[:, :], ws[:, k, c * 128:(c + 1) * 128], xnT[:, k, cs], start=(k == 0), stop=(k == DC - 1)),
                             reads=[Bws] + xnT_bufs(ch), writes=[Bp1])
                    K.op("act", lambda e: e.activation(ypre[:, c, cs], p1[:, :], AF.Identity, scale=vec[:, 0, c:c + 1]), reads=[Bp1, Bvec], writes=[Bypre])
                    K.op("dve", lambda e: e.tensor_copy(u16[:, c, cs], p1[:, :]), reads=[Bp1], writes=[Bu16])
            es.enter_context(es2)
            sc = sb(es, "s5sc", [128, 16, 8], F32); Bsc = Buf("s5sc")
            SC = [Bsc]
            are = par[:, 0, :]; aim = par[:, 1, :]; ldt = par[:, 2, :]
            dt = sc[:, 0, :]; mag = sc[:, 1, :]; th = sc[:, 2, :]; cth = sc[:, 3, :]; sth = sc[:, 4, :]
            abr = sc[:, 5, :]; abi = sc[:, 6, :]; den = sc[:, 7, :]; gre = sc[:, 8, :]; gim = sc[:, 9, :]
            t0 = sc[:, 10, :]; t1 = sc[:, 11, :]; nre = sc[:, 12, :]
            ki = sb(es, "s5ki", [128, 8], I32); Bki = Buf("s5ki")
            RP = [Bpar, Bsc]
            K.op("act", lambda e: e.activation(dt, ldt, AF.Exp), reads=RP, writes=SC)
            K.op("dve", lambda e: e.tensor_tensor(t0, dt, are, ALU.mult), reads=RP, writes=SC)
            K.op("act", lambda e: e.activation(mag, t0, AF.Exp), reads=RP, writes=SC)
            K.op("dve", lambda e: e.tensor_tensor(th, dt, aim, ALU.mult), reads=RP, writes=SC)
            for (o, shift) in ((sth, 0.0), (cth, 0.5 * np.pi)):
                K.op("dve", lambda e: e.tensor_scalar(ki[:, :], th, shift, 1.0 / TWO_PI, ALU.add, ALU.mult), reads=RP, writes=[Bki])
                K.op("dve", lambda e: e.tensor_copy(t1, ki[:, :]), reads=[Bki], writes=SC)
                K.op("dve", lambda e: e.scalar_tensor_tensor(t1, t1, -TWO_PI, th, ALU.mult, ALU.add), reads=RP, writes=SC)
                K.op("dve", lambda e: e.tensor_scalar(t1, t1, shift, None, ALU.add), reads=RP, writes=SC)
                K.op("dve", lambda e: e.tensor_scalar(t1, t1, 3.14159, -3.14159, ALU.min, ALU.max), reads=RP, writes=SC)
                K.op("act", lambda e: e.activation(o, t1, AF.Sin), reads=RP, writes=SC)
            K.op("dve", lambda e: e.tensor_tensor(abr, mag, cth, ALU.mult), reads=RP, writes=SC)
            K.op("dve", lambda e: e.tensor_tensor(abi, mag, sth, ALU.mult), reads=RP, writes=SC)
            K.op("dve", lambda e: e.tensor_tensor(den, are, are, ALU.mult), reads=RP, writes=SC)
            K.op("dve", lambda e: e.tensor_tensor(t0, aim, aim, ALU.mult), reads=RP, writes=SC)
            K.op("dve", lambda e: e.tensor_tensor(den, den, t0, ALU.add), reads=RP, writes=SC)
            K.op("dve", lambda e: e.reciprocal(den, den), reads=RP, writes=SC)
            K.op("dve", lambda e: e.tensor_scalar(nre, abr, -1.0, None, ALU.add), reads=RP, writes=SC)
            K.op("dve", lambda e: e.tensor_tensor(t0, nre, are, ALU.mult), reads=RP, writes=SC)
            K.op("dve", lambda e: e.tensor_tensor(t1, abi, aim, ALU.mult), reads=RP, writes=SC)
            K.op("dve", lambda e: e.tensor_tensor(gre, t0, t1, ALU.add), reads=RP, writes=SC)
            K.op("dve", lambda e: e.tensor_tensor(gre, gre, den, ALU.mult), reads=RP, writes=SC)
            K.op("dve", lambda e: e.tensor_tensor(t0, abi, are, ALU.mult), reads=RP, writes=SC)
            K.op("dve", lambda e: e.tensor_tensor(t1, nre, aim, ALU.mult), reads=RP, writes=SC)
            K.op("dve", lambda e: e.tensor_tensor(gim, t0, t1, ALU.subtract), reads=RP, writes=SC)
            K.op("dve", lambda e: e.tensor_tensor(gim, gim, den, ALU.mult), reads=RP, writes=SC)
            bbar = sb(es, "s5bbar", [128, 2, 8, 16], F32); Bbbar = Buf("s5bbar")
            btmp = sb(es, "s5btmp", [128, 8, 16], F32); Bbtmp = Buf("s5btmp")
            gre_b = gre.unsqueeze(2).to_broadcast([128, 8, 16]); gim_b = gim.unsqueeze(2).to_broadcast([128, 8, 16])
            K.op("dve", lambda e: e.tensor_tensor(bbar[:, 0], bb[:, 0], gre_b, ALU.mult), reads=[Bbb, Bsc], writes=[Bbbar])
            K.op("dve", lambda e: e.tensor_tensor(btmp[:], bb[:, 1], gim_b, ALU.mult), reads=[Bbb, Bsc], writes=[Bbtmp])
            K.op("dve", lambda e: e.tensor_tensor(bbar[:, 0], bbar[:, 0], btmp[:], ALU.subtract), reads=[Bbbar, Bbtmp], writes=[Bbbar])
            K.op("dve", lambda e: e.tensor_tensor(bbar[:, 1], bb[:, 1], gre_b, ALU.mult), reads=[Bbb, Bsc], writes=[Bbbar])
            K.op("dve", lambda e: e.tensor_tensor(btmp[:], bb[:, 0], gim_b, ALU.mult), reads=[Bbb, Bsc, Bbbar], writes=[Bbtmp])
            K.op("dve", lambda e: e.tensor_tensor(bbar[:, 1], bbar[:, 1], btmp[:], ALU.add), reads=[Bbbar, Bbtmp], writes=[Bbbar])
            mmA = sb(es, "s5mmA", [128, 10, 2, 8], F32); BmmA = Buf("s5mmA")
            mtA = sb(es, "s5mtA", [128, 3, 8], F32); BmtA = Buf("s5mtA")
            K.op("act", lambda e: e.copy(mmA[:, 0, 0, :], sc[:, 3, :]), reads=[Bsc], writes=[BmmA])
            K.op("act", lambda e: e.copy(mmA[:, 0, 1, :], sc[:, 4, :]), reads=[Bsc], writes=[BmmA])
            for k in range(1, 10):
                pr_ = mmA[:, k - 1, 0, :]; pi_ = mmA[:, k - 1, 1, :]
                K.op("dve", lambda e: e.tensor_tensor(mtA[:, 0, :], pr_, pr_, ALU.mult), reads=[BmmA], writes=[BmtA])
                K.op("dve", lambda e: e.tensor_tensor(mtA[:, 1, :], pi_, pi_, ALU.mult), reads=[BmmA], writes=[BmtA])
                K.op("dve", lambda e: e.tensor_tensor(mmA[:, k, 0, :], mtA[:, 0, :], mtA[:, 1, :], ALU.subtract), reads=[BmtA, BmmA], writes=[BmmA])
                K.op("dve", lambda e: e.tensor_tensor(mtA[:, 2, :], pr_, pi_, ALU.mult), reads=[BmmA], writes=[BmtA])
                K.op("dve", lambda e: e.tensor_scalar(mmA[:, k, 1, :], mtA[:, 2, :], 2.0, None, ALU.mult), reads=[BmtA, BmmA], writes=[BmmA])
            e3 = ExitStack()
            def _mk_s5(w):
                per = 8 // 2
                nb = per - 1
                return NS(psg=allps.sub(range(w * per, w * per + nb)), psa=allps.sub(range(w * per + nb, (w + 1) * per)), bexpP=Pool(e3, nc, "s5bexp", [128, 2, 128], F32, 1), blp=Pool(e3, nc, "s5bl", [128, 2, 128], BF16, 1), cwp=Pool(e3, nc, "s5cw", [128, 4, 128], BF16, 1), ctmp=Pool(e3, nc, "s5ct", [128, 2, 128], F32, 1), cosP=Pool(e3, nc, "s5cos", [128, 512], F32, 1), sinP=Pool(e3, nc, "s5sin", [128, 512], F32, 1), mmP=Pool(e3, nc, "s5mm", [128, 12, 2], F32, 1), mtP=Pool(e3, nc, "s5mt", [128, 8], F32, 1), carP=Pool(e3, nc, "s5car", [128, 4], F32, 1), big=Pool(e3, nc, "s5big", [128, 512], F32, 6), pr16=Pool(e3, nc, "s5pr", [128, 4, 512], BF16, 1))
            def _body_s5(j, P):
                bexp, Bbexp = P.bexpP.get()
                cosT, Bcos = P.cosP.get()
                sinT, Bsin = P.sinP.get()
                mm_, Bmm_unused = P.mmP.get()
                mt, Bmt = P.mtP.get()
                car, Bcar = P.carP.get()
                ct_ = (32 * j) // 128
                off = (32 * j) % 128
                K.op("pool", lambda e: e.memset(bexp[:], 0.0), reads=[], writes=[Bbexp])
                for ri in range(2):
                    K.op("pool", lambda e: e.tensor_copy(bexp[0:64, ri, off:off + 16], bbar[0:64, ri, j, :]), reads=[Bbbar], writes=[Bbexp])
                    K.op("pool", lambda e: e.tensor_copy(bexp[64:128, ri, off + 16:off + 32], bbar[64:128, ri, j, :]), reads=[Bbbar], writes=[Bbexp])
                pt, Bpt = P.psg.get()
                for ri in range(2):
                    K.op("pe", lambda e: e.transpose(pt[:, ri * 128:(ri + 1) * 128], bexp[:, ri, :], ident[:]), reads=[Bbexp, Bident], writes=[Bpt])
                bl, Bbl = P.blp.get()
                K.op("act", lambda e: e.copy(bl[:, :, :], pt[:, 0:256].rearrange("p (r n) -> p r n", r=2)), reads=[Bpt], writes=[Bbl])
                ct, Bct = P.ctmp.get()
                for ri in range(2):
                    K.dma("sp", ct[:, ri, :], dr["s5_c"][l, ri, j], writes=[Bct])
                cw4, Bcw4 = P.cwp.get()
                K.op("act", lambda e: e.copy(cw4[:, 0, :], ct[:, 0, :]), reads=[Bct], writes=[Bcw4])
                K.op("act", lambda e: e.mul(cw4[:, 1, :], ct[:, 0, :], -1.0), reads=[Bct], writes=[Bcw4])
                K.op("act", lambda e: e.mul(cw4[:, 2, :], ct[:, 1, :], -1.0), reads=[Bct], writes=[Bcw4])
                K.op("act", lambda e: e.mul(cw4[:, 3, :], ct[:, 1, :], -1.0), reads=[Bct], writes=[Bcw4])
                K.op("dve", lambda e: e.memset(cosT[:, 0:1], 1.0), writes=[Bcos])
                K.op("dve", lambda e: e.memset(sinT[:, 0:1], 0.0), writes=[Bsin])
                tb, Btb = P.big.get()
                for k in range(9):
                    n = 1 << k
                    mr = mmA[:, k, 0, j:j + 1]; mi = mmA[:, k, 1, j:j + 1]
                    K.op("dve", lambda e: e.tensor_scalar(tb[:, 0:n], sinT[:, 0:n], mi, None, ALU.mult), reads=[Bsin, BmmA], writes=[Btb])
                    K.op("dve", lambda e: e.scalar_tensor_tensor(cosT[:, n:2 * n], cosT[:, 0:n], mr, tb[:, 0:n], ALU.mult, ALU.subtract),
                         reads=[Bcos, BmmA, Btb], writes=[Bcos])
                    K.op("dve", lambda e: e.tensor_scalar(tb[:, 0:n], sinT[:, 0:n], mr, None, ALU.mult), reads=[Bsin, BmmA, Bcos], writes=[Btb])
                    K.op("dve", lambda e: e.scalar_tensor_tensor(sinT[:, n:2 * n], cosT[:, 0:n], mi, tb[:, 0:n], ALU.mult, ALU.add),
                         reads=[Bcos, BmmA, Btb], writes=[Bsin])
                magb = sc[:, 1, j:j + 1].to_broadcast([128, 512])
                m9r = mmA[:, 9, 0, j:j + 1]; m9i = mmA[:, 9, 1, j:j + 1]
                for ch in range(4):
                    cs = slice(ch * 512, (ch + 1) * 512)
                    pre, Bpre = P.psg.get()
                    K.op("pe", lambda e: e.matmul(pre[:, :], bl[:, 0, :], u16[:, ct_, cs], start=True, stop=True), reads=[Bbl, Bu16], writes=[Bpre])
                    pim, Bpim = P.psg.get()
                    K.op("pe", lambda e: e.matmul(pim[:, :], bl[:, 1, :], u16[:, ct_, cs], start=True, stop=True), reads=[Bbl, Bu16], writes=[Bpim])
                    brr, Bbrr = P.big.get()
                    bri, Bbri = P.big.get()
                    ta, Bta = P.big.get()
                    tb2, Btb2 = P.big.get()
                    K.op("dve", lambda e: e.tensor_tensor(brr[:], cosT[:], pre[:, :], ALU.mult), reads=[Bcos, Bpre], writes=[Bbrr])
                    K.op("dve", lambda e: e.tensor_tensor(ta[:], sinT[:], pim[:, :], ALU.mult), reads=[Bsin, Bpim], writes=[Bta])
                    K.op("pool", lambda e: e.tensor_tensor(brr[:], brr[:], ta[:], ALU.add), reads=[Bbrr, Bta], writes=[Bbrr])
                    K.op("dve", lambda e: e.tensor_tensor(bri[:], cosT[:], pim[:, :], ALU.mult), reads=[Bcos, Bpim], writes=[Bbri])
                    K.op("dve", lambda e: e.tensor_tensor(tb2[:], sinT[:], pre[:, :], ALU.mult), reads=[Bsin, Bpre], writes=[Btb2])
                    K.op("pool", lambda e: e.tensor_tensor(bri[:], bri[:], tb2[:], ALU.subtract), reads=[Bbri, Btb2], writes=[Bbri])
                    if ch == 0:
                        ir, ii = 0.0, 0.0
                    else:
                        K.op("dve", lambda e: e.tensor_tensor(mt[:, 4:5], car[:, 0:1], m9r, ALU.mult), reads=[Bcar, BmmA], writes=[Bmt])
                        K.op("dve", lambda e: e.tensor_tensor(mt[:, 5:6], car[:, 1:2], m9i, ALU.mult), reads=[Bcar, BmmA], writes=[Bmt])
                        K.op("dve", lambda e: e.tensor_tensor(mt[:, 6:7], car[:, 1:2], m9r, ALU.mult), reads=[Bcar, BmmA], writes=[Bmt])
                        K.op("dve", lambda e: e.tensor_tensor(mt[:, 7:8], car[:, 0:1], m9i, ALU.mult), reads=[Bcar, BmmA], writes=[Bmt])
                        K.op("dve", lambda e: e.tensor_tensor(car[:, 2:3], mt[:, 4:5], mt[:, 5:6], ALU.subtract), reads=[Bmt, Bcar], writes=[Bcar])
                        K.op("dve", lambda e: e.tensor_tensor(car[:, 3:4], mt[:, 6:7], mt[:, 7:8], ALU.add), reads=[Bmt, Bcar], writes=[Bcar])
                        ir, ii = car[:, 2:3], car[:, 3:4]
                    K.op("dve", lambda e: e.tensor_tensor_scan(ta[:], magb, brr[:], ir, ALU.mult, ALU.add), reads=[Bsc, Bbrr, Bta, Bcar], writes=[Bta])
                    K.op("dve", lambda e: e.tensor_tensor_scan(tb2[:], magb, bri[:], ii, ALU.mult, ALU.add), reads=[Bsc, Bbri, Btb2, Bcar], writes=[Btb2])
                    K.op("act", lambda e: e.copy(car[:, 0:1], ta[:, 511:512]), reads=[Bta, Bcar], writes=[Bcar])
                    K.op("act", lambda e: e.copy(car[:, 1:2], tb2[:, 511:512]), reads=[Btb2, Bcar], writes=[Bcar])
                    pp, Bpp = P.pr16.get()
                    K.op("dve", lambda e: e.tensor_tensor(pp[:, 0, :], cosT[:], ta[:], ALU.mult), reads=[Bcos, Bta], writes=[Bpp])
                    K.op("pool", lambda e: e.tensor_tensor(pp[:, 1, :], sinT[:], tb2[:], ALU.mult), reads=[Bsin, Btb2], writes=[Bpp])
                    K.op("dve", lambda e: e.tensor_tensor(pp[:, 2, :], sinT[:], ta[:], ALU.mult), reads=[Bsin, Bta], writes=[Bpp])
                    K.op("pool", lambda e: e.tensor_tensor(pp[:, 3, :], cosT[:], tb2[:], ALU.mult), reads=[Bcos, Btb2], writes=[Bpp])
                    yp, Byp = P.psg.get()
                    for v in range(4):
                        K.op("pe", lambda e: e.matmul(yp[:, :], cw4[:, v, :], pp[:, v, :], start=(v == 0), stop=(v == 3)), reads=[Bcw4, Bpp], writes=[Byp])
                    K.op("dve", lambda e: e.tensor_tensor(ypre[:, ct_, cs], ypre[:, ct_, cs], yp[:, :], ALU.add), reads=[Byp, Bypre], writes=[Bypre])
            IL.run(8, 2, _mk_s5, _body_s5)
            K.barrier_all()
            e3.close()
            yg16 = sb(es, "s5yg16", [128, 2, T], BF16); Byg16 = Buf("s5yg16")
            for c in range(2):
                K.op("act", lambda e: e.activation(ypre[:, c, :], ypre[:, c, :], AF.Gelu_apprx_tanh), reads=[Bypre], writes=[Bypre])
                K.op("act", lambda e: e.copy(yg16[:, c, :], ypre[:, c, :]), reads=[Bypre], writes=[Byg16])
            sgp = Pool(es, nc, "s5sg", [128, 512], F32, 2)
            for c in range(2):
                for ch in range(4):
                    cs = slice(ch * 512, (ch + 1) * 512)
                    zp, Bzp = psg.get()
                    for k in range(2):
                        K.op("pe", lambda e: e.matmul(zp[:, :], wglu[:, k, c * 128:(c + 1) * 128], yg16[:, k, cs], start=(k == 0), stop=(k == 1)),
                             reads=[Bwglu, Byg16], writes=[Bzp])
                    sg, Bsg = sgp.get()
                    K.op("act", lambda e: e.activation(sg[:], zp[:, :], AF.Sigmoid, bias=vec[:, 1, c:c + 1]), reads=[Bzp, Bvec], writes=[Bsg])
                    K.op("dve", lambda e: e.tensor_tensor(ypre[:, c, cs], ypre[:, c, cs], sg[:], ALU.mult), reads=[Bypre, Bsg], writes=[Bypre])
            fm_groupnorm(es, "sg", ypre, Bypre, 2, l, gcolt)
            dbg_dump(2)
            wout_partial(es, l, 2)
            phase_end(es)

        def rope_tables(es, name, inv_name, half, pos_cols, Bpos_in, npart=128):
            ncol = pos_cols.shape[1]
            inv = sb(es, name + "inv", [128, half], F32); Binv = Buf(name + "inv")
            K.dma("sp", inv[:], dr[inv_name][:], writes=[Binv])
            ang = sb(es, name + "ang", [128, ncol, half], F32); Bang = Buf(name + "ang")
            tmp = sb(es, name + "tmp", [128, ncol, half], F32); Btmp = Buf(name + "tmp")
            kk = sb(es, name + "kk", [128, ncol, half], I32); Bkk = Buf(name + "kk")
            cs_ = sb(es, name + "cs", [128, 2, ncol, half], F32); Bcs = Buf(name + "cs")
            P = npart
            K.op("dve", lambda e: e.tensor_tensor(ang[0:P], inv[0:P].unsqueeze(1).to_broadcast([P, ncol, half]),
                                                  pos_cols.unsqueeze(2).to_broadcast([P, ncol, half]), ALU.mult),
                 reads=[Binv, Bpos_in], writes=[Bang])
            for idx, shift in ((0, 0.5 * np.pi), (1, 0.0)):
                K.op("dve", lambda e: e.tensor_scalar(kk[0:P], ang[0:P], shift, 1.0 / TWO_PI, ALU.add, ALU.mult), reads=[Bang], writes=[Bkk])
                K.op("dve", lambda e: e.tensor_copy(tmp[0:P], kk[0:P]), reads=[Bkk], writes=[Btmp])
                K.op("dve", lambda e: e.scalar_tensor_tensor(tmp[0:P], tmp[0:P], -TWO_PI, ang[0:P], ALU.mult, ALU.add), reads=[Btmp, Bang], writes=[Btmp])
                K.op("dve", lambda e: e.tensor_scalar(tmp[0:P], tmp[0:P], shift, None, ALU.add), reads=[Btmp], writes=[Btmp])
                K.op("dve", lambda e: e.tensor_scalar(tmp[0:P], tmp[0:P], 3.14159, -3.14159, ALU.min, ALU.max), reads=[Btmp], writes=[Btmp])
                K.op("act", lambda e: e.activation(cs_[0:P, idx], tmp[0:P], AF.Sin), reads=[Btmp], writes=[Bcs])
            return cs_, Bcs

        def apply_rope(dst, src, cos_t, sin_t, nh, half, tmp, reads, writes, Btmp):
            x1 = src[:, :, 0:half]; x2 = src[:, :, half:2 * half]
            cb = cos_t.unsqueeze(1).to_broadcast([128, nh, half]); sb_ = sin_t.unsqueeze(1).to_broadcast([128, nh, half])
            ta = tmp[:, 0:nh, 0:half]; tb_ = tmp[:, 0:nh, half:2 * half]
            K.op("dve", lambda e: e.tensor_tensor(ta, x1, cb, ALU.mult), reads=reads, writes=[Btmp])
            K.op("dve", lambda e: e.tensor_tensor(tb_, x2, sb_, ALU.mult), reads=reads, writes=[Btmp])
            K.op("dve", lambda e: e.tensor_tensor(dst[:, :, 0:half], ta, tb_, ALU.subtract), reads=[Btmp], writes=writes)
            K.op("dve", lambda e: e.tensor_tensor(ta, x1, sb_, ALU.mult), reads=reads + [Btmp], writes=[Btmp])
            K.op("dve", lambda e: e.tensor_tensor(tb_, x2, cb, ALU.mult), reads=reads + [Btmp], writes=[Btmp])
            K.op("dve", lambda e: e.tensor_tensor(dst[:, :, half:2 * half], ta, tb_, ALU.add), reads=[Btmp], writes=writes)

        def attn_block_group(s_items, acc, Bacc, first_group, last_group):
            pass

        def mla_phase(s, l):
            es = ExitStack()
            ymT, BymT = alloc_ymT(es, 0)
            qT = sb(es, "mqT", [128, 4, T], BF16); BqT = [Buf(f"mqT{i}") for i in range(NT)]
            kT = sb(es, "mkT", [128, 4, T], BF16); BkT = [Buf(f"mkT{i}") for i in range(NT)]
            K.op("pool", lambda e: e.memset(qT[64:128], 0.0), writes=BqT)
            K.op("pool", lambda e: e.memset(kT[64:128], 0.0), writes=BkT)
            va = sb(es, "mva", [128, NT, 4, 65], BF16); Bva = [Buf(f"mva{i}") for i in range(NT)]
            K.op("pool", lambda e: e.memset(va[:, :, :, 64:65], 1.0), writes=Bva)
            e1 = ExitStack()
            wm = sb(e1, "wm", [128, DC, 352], BF16); Bwm = Buf("wm")
            K.dma("pool", wm[:], dr["w_in"][l, :, 0:352].rearrange("(c p) n -> p c n", p=128), writes=[Bwm])
            wuq = sb(e1, "wuq", [128, 2, 384], BF16); Bwuq = Buf("wuq")
            K.dma("pool", wuq[:, 0, :], dr["mla_w_uq"][l, 0:128, :], writes=[Bwuq])
            K.dma("pool", wuq[0:64, 1, :], dr["mla_w_uq"][l, 128:192, :], writes=[Bwuq])
            wukv = sb(e1, "wukv", [128, 512], BF16); Bwukv = Buf("wukv")
            K.dma("pool", wukv[:], dr["mla_w_ukv"][l], writes=[Bwukv])
            gv = sb(e1, "mgv", [128, 192 + 128 + 96 + 96], F32); Bgv = Buf("mgv")
            K.dma("sp", gv[:, 0:192], dr["mla_g_cq"][l:l + 1, :].to_broadcast([128, 192]), writes=[Bgv])
            K.dma("sp", gv[:, 192:320], dr["mla_g_ckv"][l:l + 1, :].to_broadcast([128, 128]), writes=[Bgv])
            K.dma("sp", gv[:, 320:416], dr["mla_g_q"][l:l + 1, :].to_broadcast([128, 96]), writes=[Bgv])
            K.dma("sp", gv[:, 416:512], dr["mla_g_k"][l:l + 1, :].to_broadcast([128, 96]), writes=[Bgv])
            g_cq = gv[:, 0:192]; g_ckv = gv[:, 192:320]; g_q = gv[:, 320:416]; g_k = gv[:, 416:512]
            cs_t, Bcs_t = rope_tables(e1, "mr", "c_inv_mla", 16, posf[:, :], Bposf)
            def _mk_mp(w):
                per = 8 // 4
                nb = per - 0
                return NS(psg=allps.sub(range(w * per, w * per + nb)), psa=allps.sub(range(w * per + nb, (w + 1) * per)), st=Pool(e1, nc, "mst", [128, 16], F32, 2), cn=Pool(e1, nc, "mcn", [128, 320], BF16, 1), cT=Pool(e1, nc, "mcT", [128, 3, 128], BF16, 1), qn=Pool(e1, nc, "mqn", [128, 4, 96], F32, 1), kn=Pool(e1, nc, "mkn", [128, 4, 96], F32, 1), qr=Pool(e1, nc, "mqr", [128, 8, 96], BF16, 1), jk=Pool(e1, nc, "mjk", [128, 192], BF16, 1), rtmp=Pool(e1, nc, "mrt", [128, 4, 32], F32, 1), kpe=Pool(e1, nc, "mkpe", [128, 32], F32, 1))
            def _body_mp(i, P):
                ts_ = slice(i * 128, (i + 1) * 128)
                pp, Bpp = P.psg.get()
                for c in range(DC):
                    K.op("pe", lambda e: e.matmul(pp[:, 0:352], xnT[:, c, ts_], wm[:, c, :], start=(c == 0), stop=(c == DC - 1)),
                         reads=[Bwm, BxnT[i][0], BxnT[i][1]], writes=[Bpp])
                sq, Bsq = P.st.get()
                j_, Bj_ = P.jk.get()
                K.op("act", lambda e: e.activation(j_[:, 0:192], pp[:, 0:192], AF.Square, accum_out=sq[:, 0:1]), reads=[Bpp], writes=[Bj_, Bsq])
                K.op("act", lambda e: e.activation(j_[:, 0:128], pp[:, 192:320], AF.Square, accum_out=sq[:, 1:2]), reads=[Bpp], writes=[Bj_, Bsq])
                K.op("act", lambda e: e.activation(j_[:, 0:32], pp[:, 320:352], AF.Square, accum_out=sq[:, 2:3]), reads=[Bpp], writes=[Bj_, Bsq])
                K.op("dve", lambda e: e.tensor_scalar(sq[:, 0:1], sq[:, 0:1], 128.0 / 192.0, None, ALU.mult), reads=[Bsq], writes=[Bsq])
                rstd_from(sq[:, 4:6], sq[:, 0:2], 128, [Bsq], [Bsq])
                c_n, Bc_n = P.cn.get()
                K.op("dve", lambda e: e.scalar_tensor_tensor(c_n[:, 0:192], pp[:, 0:192], sq[:, 4:5], g_cq, ALU.mult, ALU.mult),
                     reads=[Bpp, Bsq, Bgv], writes=[Bc_n])
                K.op("dve", lambda e: e.scalar_tensor_tensor(c_n[:, 192:320], pp[:, 192:320], sq[:, 5:6], g_ckv, ALU.mult, ALU.mult),
                     reads=[Bpp, Bsq, Bgv], writes=[Bc_n])
                kp, Bkp = P.kpe.get()
                K.op("act", lambda e: e.copy(kp[:], pp[:, 320:352]), reads=[Bpp], writes=[Bkp])
                pt, Bpt = P.psg.get()
                ptb = pt[:, :].bitcast(BF16)
                K.op("pe", lambda e: e.transpose(ptb[:, 0:128], c_n[:, 0:128], identb[:]), reads=[Bc_n, Bidentb], writes=[Bpt])
                K.op("pe", lambda e: e.transpose(ptb[0:64, 128:256], c_n[:, 128:192], identb[:]), reads=[Bc_n, Bidentb], writes=[Bpt])
                K.op("pe", lambda e: e.transpose(ptb[:, 256:384], c_n[:, 192:320], identb[:]), reads=[Bc_n, Bidentb], writes=[Bpt])
                ct, Bct = P.cT.get()
                K.op("act", lambda e: e.copy(ct[:, 0, :], ptb[:, 0:128]), reads=[Bpt], writes=[Bct])
                K.op("act", lambda e: e.copy(ct[0:64, 1, :], ptb[0:64, 128:256]), reads=[Bpt], writes=[Bct])
                K.op("act", lambda e: e.copy(ct[:, 2, :], ptb[:, 256:384]), reads=[Bpt], writes=[Bct])
                pq, Bpq = P.psg.get()
                K.op("pe", lambda e: e.matmul(pq[:, 0:384], ct[:, 0, :], wuq[:, 0, :], start=True, stop=False), reads=[Bct, Bwuq], writes=[Bpq])
                K.op("pe", lambda e: e.matmul(pq[:, 0:384], ct[0:64, 1, :], wuq[0:64, 1, :], start=False, stop=True), reads=[Bct, Bwuq], writes=[Bpq])
                pkv, Bpkv = P.psg.get()
                K.op("pe", lambda e: e.matmul(pkv[:, :], ct[:, 2, :], wukv[:], start=True, stop=True), reads=[Bct, Bwukv], writes=[Bpkv])
                pq3 = pq[:, 0:384].rearrange("p (h d) -> p h d", h=4)
                pkv3 = pkv[:, :].rearrange("p (h d) -> p h d", h=4)
                sq2, Bsq2 = P.st.get()
                for h in range(4):
                    K.op("act", lambda e: e.activation(j_[:, 0:96], pq3[:, h, :], AF.Square, accum_out=sq2[:, h:h + 1]), reads=[Bpq], writes=[Bj_, Bsq2])
                    K.op("act", lambda e: e.activation(j_[:, 0:64], pkv3[:, h, 0:64], AF.Square, accum_out=sq2[:, 4 + h:5 + h]), reads=[Bpkv], writes=[Bj_, Bsq2])
                K.op("dve", lambda e: e.tensor_scalar(sq2[:, 4:8], sq2[:, 4:8], sq[:, 2:3], None, ALU.add), reads=[Bsq2, Bsq], writes=[Bsq2])
                rstd_from(sq2[:, 8:16], sq2[:, 0:8], 96, [Bsq2], [Bsq2])
                q_n, Bq_n = P.qn.get()
                k_n, Bk_n = P.kn.get()
                for h in range(4):
                    K.op("dve", lambda e: e.scalar_tensor_tensor(q_n[:, h, :], pq3[:, h, :], sq2[:, 8 + h:9 + h], g_q, ALU.mult, ALU.mult),
                         reads=[Bpq, Bsq2, Bgv], writes=[Bq_n])
                    K.op("dve", lambda e: e.scalar_tensor_tensor(k_n[:, h, 32:96], pkv3[:, h, 0:64], sq2[:, 12 + h:13 + h], g_k[:, 32:96], ALU.mult, ALU.mult),
                         reads=[Bpkv, Bsq2, Bgv], writes=[Bk_n])
                    K.op("dve", lambda e: e.scalar_tensor_tensor(k_n[:, h, 0:32], kp[:], sq2[:, 12 + h:13 + h], g_k[:, 0:32], ALU.mult, ALU.mult),
                         reads=[Bkp, Bsq2, Bgv], writes=[Bk_n])
                q_r, Bq_r = P.qr.get()
                rt_, Brt_ = P.rtmp.get()
                apply_rope(q_r[:, 0:4], q_n[:, :, :], cs_t[:, 0, i, :], cs_t[:, 1, i, :], 4, 16, rt_, [Bq_n, Bcs_t], [Bq_r], Brt_)
                K.op("act", lambda e: e.copy(q_r[:, 0:4, 32:96], q_n[:, :, 32:96]), reads=[Bq_n], writes=[Bq_r])
                apply_rope(q_r[:, 4:8], k_n[:, :, :], cs_t[:, 0, i, :], cs_t[:, 1, i, :], 4, 16, rt_, [Bk_n, Bcs_t], [Bq_r], Brt_)
                K.op("act", lambda e: e.copy(q_r[:, 4:8, 32:96], k_n[:, :, 32:96]), reads=[Bk_n], writes=[Bq_r])
                K.op("act", lambda e: e.copy(va[:, i, :, 0:64], pkv3[:, :, 64:128]), reads=[Bpkv], writes=[Bva[i]])
                for grp in range(2):
                    pt2, Bpt2 = P.psg.get()
                    pt2b = pt2[:, :].bitcast(BF16)
                    for h in range(4):
                        K.op("pe", lambda e: e.transpose(pt2b[0:96, h * 128:(h + 1) * 128], q_r[:, grp * 4 + h, :], identb[:]),
                             reads=[Bq_r, Bidentb], writes=[Bpt2])
                    dstT = qT if grp == 0 else kT
                    BdT = BqT if grp == 0 else BkT
                    K.op("act" if grp == 0 else "dve",
                         (lambda e: e.copy(dstT[0:96, :, ts_], pt2b[0:96, 0:512].rearrange("p (h t) -> p h t", h=4))) if grp == 0 else
                         (lambda e: e.tensor_copy(dstT[0:96, :, ts_], pt2b[0:96, 0:512].rearrange("p (h t) -> p h t", h=4))),
                         reads=[Bpt2], writes=[BdT[i]])
            IL.run(NT, 4, _mk_mp, _body_mp)
            K.barrier_all()
            e1.close()
            scale = 96 ** -0.5
            def _mk_ma(w):
                per = 8 // 4
                nb = per - 1
                return NS(psg=allps.sub(range(w * per, w * per + nb)), psa=allps.sub(range(w * per + nb, (w + 1) * per)), pexp=Pool(es, nc, "mpe", [128, 512], BF16, 3), yo=Pool(es, nc, "myo", [128, 256], F32, 2), yst=Pool(es, nc, "myst", [128, 8], F32, 2), yb=Pool(es, nc, "myb", [128, 256], BF16, 2))
            def _body_ma(qt, P):
                qs = slice(qt * 128, (qt + 1) * 128)
                y_o, By_o = P.yo.get()
                yst_, Byst = P.yst.get()
                for h in range(4):
                    acc, Bacc = P.psa.get()
                    nk = qt + 1
                    for g0 in range(0, nk, 4):
                        kts = list(range(g0, min(g0 + 4, nk)))
                        sp_, Bsp = P.psg.get()
                        for a, kt in enumerate(kts):
                            K.op("pe", lambda e: e.matmul(sp_[:, a * 128:(a + 1) * 128], kT[:, h, kt * 128:(kt + 1) * 128], qT[:, h, qs], start=True, stop=True),
                                 reads=[BkT[kt], BqT[qt]], writes=[Bsp])
                        pe_, Bpe = P.pexp.get()
                        w = len(kts) * 128
                        K.op("act", lambda e: e.activation(pe_[:, 0:w], sp_[:, 0:w], AF.Exp, scale=scale), reads=[Bsp], writes=[Bpe])
                        if kts[-1] == qt:
                            a = len(kts) - 1
                            K.op("dve", lambda e: e.tensor_tensor(pe_[:, a * 128:(a + 1) * 128], pe_[:, a * 128:(a + 1) * 128], tri4[:, 0, :], ALU.mult),
                                 reads=[Bpe, Btri], writes=[Bpe])
                        for a, kt in enumerate(kts):
                            K.op("pe", lambda e: e.matmul(acc[:, 0:65], pe_[:, a * 128:(a + 1) * 128], va[:, kt, h, :], start=(kt == 0), stop=(kt == qt)),
                                 reads=[Bpe, Bva[kt]], writes=[Bacc])
                    K.op("dve", lambda e: e.reciprocal(yst_[:, h:h + 1], acc[:, 64:65]), reads=[Bacc], writes=[Byst])
                    K.op("dve", lambda e: e.tensor_scalar(y_o[:, h * 64:(h + 1) * 64], acc[:, 0:64], yst_[:, h:h + 1], None, ALU.mult),
                         reads=[Bacc, Byst], writes=[By_o])
                tm_groupnorm(P.psg, y_o, By_o, yst_, Byst, P.yb, 0, l, qt)
            IL.run(NT, 4, _mk_ma, _body_ma)
            dbg_dump(0)
            wout_partial(es, l, 0)
            phase_end(es)

        def tm_groupnorm(psgp, y_o, By_o, yst_, Byst, ybpool, m, l, qt):
            y_b, By_b = ybpool.get()
            K.op("act", lambda e: e.activation(y_b[:], y_o[:], AF.Square, accum_out=yst_[:, 4:5]), reads=[By_o], writes=[By_b, Byst])
            K.op("act", lambda e: e.activation(yst_[:, 5:6], yst_[:, 4:5], AF.Ln, bias=epsb[:, 0:1], scale=1.0 / 256), reads=[Byst, Beps], writes=[Byst])
            K.op("act", lambda e: e.activation(yst_[:, 5:6], yst_[:, 5:6], AF.Exp, scale=-0.5), reads=[Byst], writes=[Byst])
            K.op("dve", lambda e: e.scalar_tensor_tensor(y_b[:], y_o[:], yst_[:, 5:6], gon[:, m, :], ALU.mult, ALU.mult),
                 reads=[By_o, Byst, Bgon, By_b], writes=[By_b])
            pt, Bpt = psgp.get()
            ptb = pt[:, :].bitcast(BF16)
            for c in range(2):
                K.op("pe", lambda e: e.transpose(ptb[:, c * 128:(c + 1) * 128], y_b[:, c * 128:(c + 1) * 128], identb[:]), reads=[By_b, Bidentb], writes=[Bpt])
            K.op("act", lambda e: e.copy(ymT_box["t"][:, :, qt * 128:(qt + 1) * 128], ptb[:, 0:256].rearrange("p (c t) -> p c t", c=2)),
                 reads=[Bpt], writes=[ymT_box["b"][qt // 4]])

        def nsa_phase(s, l):
            es = ExitStack()
            ymT, BymT = alloc_ymT(es, 3)
            gv = sb(es, "ngv", [128, 4, 64], F32); Bgv = Buf("ngv")
            K.dma("sp", gv[:, 0, :], dr["nsa_g_q"][l:l + 1, :].to_broadcast([128, 64]), writes=[Bgv])
            for b3 in range(3):
                K.dma("sp", gv[:, 1 + b3, :], dr["nsa_g_k"][l, b3:b3 + 1, :].to_broadcast([128, 64]), writes=[Bgv])
            qT = sb(es, "nqT", [128, NT, 4, 128], BF16); BqT = [Buf(f"nqT{i}") for i in range(NT)]
            kTs = sb(es, "nkTs", [128, T], BF16); BkTs = [Buf(f"nkTs{i}") for i in range(NT)]
            kTw = sb(es, "nkTw", [128, T], BF16); BkTw = [Buf(f"nkTw{i}") for i in range(NT)]
            K.op("pool", lambda e: e.memset(qT[64:128], 0.0), writes=BqT)
            K.op("pool", lambda e: e.memset(kTs[64:128], 0.0), writes=BkTs)
            K.op("pool", lambda e: e.memset(kTw[64:128], 0.0), writes=BkTw)
            vs = sb(es, "nvs", [128, NT, 65], BF16); Bvs = [Buf(f"nvs{i}") for i in range(NT)]
            vw = sb(es, "nvw", [128, NT, 65], BF16); Bvw = [Buf(f"nvw{i}") for i in range(NT)]
            gts = sb(es, "ngts", [128, NT, 12], F32); Bgts = [Buf(f"ngts{i}") for i in range(NT)]
            kcT = sb(es, "nkcT", [128, 128], BF16); BkcT = Buf("nkcT")
            K.op("pool", lambda e: e.memset(kcT[64:128], 0.0), writes=[BkcT])
            vc = sb(es, "nvc", [128, 97], BF16); Bvc = Buf("nvc")
            K.op("pool", lambda e: e.memset(vs[:, :, 64:65], 1.0), writes=Bvs)
            K.op("pool", lambda e: e.memset(vw[:, :, 64:65], 1.0), writes=Bvw)
            K.op("pool", lambda e: e.memset(vc[:, 64:65], 1.0), writes=[Bvc])
            K.dma("pool", vc[:, 65:97], dr["c_ov"][:], writes=[Bvc])
            e1 = ExitStack()
            wn = sb(e1, "wn", [128, DC, 652], BF16); Bwn = Buf("wn")
            K.dma("pool", wn[:], dr["w_in"][l, :, 1120:1772].rearrange("(c p) n -> p c n", p=128), writes=[Bwn])
            cs_t, Bcs_t = rope_tables(e1, "nr", "c_inv_nsa", 8, posf[:, :], Bposf)
            pci = sb(e1, "npci", [128, 1], I32); Bpci = Buf("npci")
            pcf = sb(e1, "npcf", [128, 1], F32); Bpcf = Buf("npcf")
            K.op("dve", lambda e: e.memset(pcf[:], 0.0), writes=[Bpcf])
            pos_src = dr["pos"][s:s + 1, 31::16].rearrange("o n -> n o")
            K.dma("sp", pci[0:127, :], pos_src, writes=[Bpci], allow_slow_non_contiguous=True)
            K.op("dve", lambda e: e.tensor_copy(pcf[0:127, :], pci[0:127, :]), reads=[Bpci, Bpcf], writes=[Bpcf])
            csc, Bcsc = rope_tables(e1, "nrc", "c_inv_nsa", 8, pcf[:, :], Bpcf)
            cmpT = sb(e1, "ncmpT", [128, T], BF16); BcmpT = Buf("ncmpT")
            for ch in range(4):
                cs = slice(ch * 512, (ch + 1) * 512)
                p1, Bp1 = psg.get()
                for k in range(DC):
                    K.op("pe", lambda e: e.matmul(p1[:, :], wn[:, k, 256:384], xnT[:, k, cs], start=(k == 0), stop=(k == DC - 1)),
                         reads=[Bwn] + xnT_bufs(ch), writes=[Bp1])
                K.op("act", lambda e: e.copy(cmpT[:, cs], p1[:, :]), reads=[Bp1], writes=[BcmpT])
            def _mk_np(w):
                per = 8 // 4
                nb = per - 0
                return NS(psg=allps.sub(range(w * per, w * per + nb)), psa=allps.sub(range(w * per + nb, (w + 1) * per)), st=Pool(e1, nc, "nst", [128, 16], F32, 2), jk=Pool(e1, nc, "njk", [128, 64], F32, 1), qn=Pool(e1, nc, "nqn", [128, 6, 64], F32, 1), qr=Pool(e1, nc, "nqr", [128, 6, 64], BF16, 1), rtmp=Pool(e1, nc, "nrt", [128, 6, 16], F32, 1))
            def _body_np(i, P):
                ts_ = slice(i * 128, (i + 1) * 128)
                pa, Bpa = P.psg.get()
                pb, Bpb = P.psg.get()
                for c in range(DC):
                    K.op("pe", lambda e: e.matmul(pa[:, 0:256], xnT[:, c, ts_], wn[:, c, 0:256], start=(c == 0), stop=(c == DC - 1)),
                         reads=[Bwn, BxnT[i][0], BxnT[i][1]], writes=[Bpa])
                for c in range(DC):
                    K.op("pe", lambda e: e.matmul(pb[:, 0:268], xnT[:, c, ts_], wn[:, c, 384:652], start=(c == 0), stop=(c == DC - 1)),
                         reads=[Bwn, BxnT[i][0], BxnT[i][1]], writes=[Bpb])
                pa3 = pa[:, 0:256].rearrange("p (h d) -> p h d", h=4)
                sq, Bsq = P.st.get()
                j_, Bj_ = P.jk.get()
                for h in range(4):
                    K.op("act", lambda e: e.activation(j_[:], pa3[:, h, :], AF.Square, accum_out=sq[:, h:h + 1]), reads=[Bpa], writes=[Bj_, Bsq])
                K.op("act", lambda e: e.activation(j_[:], pb[:, 0:64], AF.Square, accum_out=sq[:, 4:5]), reads=[Bpb], writes=[Bj_, Bsq])
                K.op("act", lambda e: e.activation(j_[:], pb[:, 128:192], AF.Square, accum_out=sq[:, 5:6]), reads=[Bpb], writes=[Bj_, Bsq])
                rstd_from(sq[:, 8:14], sq[:, 0:6], 64, [Bsq], [Bsq])
                q_n, Bq_n = P.qn.get()
                for h in range(4):
                    K.op("dve", lambda e: e.scalar_tensor_tensor(q_n[:, h, :], pa3[:, h, :], sq[:, 8 + h:9 + h], gv[:, 0, :], ALU.mult, ALU.mult),
                         reads=[Bpa, Bsq, Bgv], writes=[Bq_n])
                K.op("dve", lambda e: e.scalar_tensor_tensor(q_n[:, 4, :], pb[:, 0:64], sq[:, 12:13], gv[:, 2, :], ALU.mult, ALU.mult),
                     reads=[Bpb, Bsq, Bgv], writes=[Bq_n])
                K.op("dve", lambda e: e.scalar_tensor_tensor(q_n[:, 5, :], pb[:, 128:192], sq[:, 13:14], gv[:, 3, :], ALU.mult, ALU.mult),
                     reads=[Bpb, Bsq, Bgv], writes=[Bq_n])
                q_r, Bq_r = P.qr.get()
                rt_, Brt_ = P.rtmp.get()
                apply_rope(q_r[:, :, :], q_n[:, :, :], cs_t[:, 0, i, :], cs_t[:, 1, i, :], 6, 8, rt_, [Bq_n, Bcs_t], [Bq_r], Brt_)
                K.op("act", lambda e: e.copy(q_r[:, :, 16:64], q_n[:, :, 16:64]), reads=[Bq_n], writes=[Bq_r])
                K.op("act", lambda e: e.copy(vs[:, i, 0:64], pb[:, 64:128]), reads=[Bpb], writes=[Bvs[i]])
                K.op("act", lambda e: e.copy(vw[:, i, 0:64], pb[:, 192:256]), reads=[Bpb], writes=[Bvw[i]])
                K.op("act", lambda e: e.copy(gts[:, i, :], pb[:, 256:268]), reads=[Bpb], writes=[Bgts[i]])
                pt, Bpt = P.psg.get()
                ptb = pt[:, :].bitcast(BF16)
                for h in range(6):
                    K.op("pe", lambda e: e.transpose(ptb[0:64, h * 128:(h + 1) * 128], q_r[:, h, :], identb[:]), reads=[Bq_r, Bidentb], writes=[Bpt])
                K.op("act", lambda e: e.copy(qT[0:64, i, :, :], ptb[0:64, 0:512].rearrange("p (h t) -> p h t", h=4)), reads=[Bpt], writes=[BqT[i]])
                K.op("dve", lambda e: e.tensor_copy(kTs[0:64, ts_], ptb[0:64, 512:640]), reads=[Bpt], writes=[BkTs[i]])
                K.op("dve", lambda e: e.tensor_copy(kTw[0:64, ts_], ptb[0:64, 640:768]), reads=[Bpt], writes=[BkTw[i]])
            IL.run(NT, 4, _mk_np, _body_np)
            st = Pool(e1, nc, "nst2", [128, 16], F32, 1)
            jk = Pool(e1, nc, "njk2", [128, 64], F32, 1)
            qn = Pool(e1, nc, "nqn2", [128, 6, 64], F32, 1)
            qr = Pool(e1, nc, "nqr2", [128, 6, 64], BF16, 1)
            rtmp = Pool(e1, nc, "nrt2", [128, 6, 16], F32, 1)
            w1 = sb(e1, "nw1", [128, 32, 128], BF16); Bw1 = Buf("nw1")
            pe16 = sb(e1, "npe16", [128, 32], BF16); Bpe16 = Buf("npe16")
            w2 = sb(e1, "nw2", [128, 2, 64], BF16); Bw2 = Buf("nw2")
            for kv in range(2):
                K.dma("pool", w1[kv * 64:kv * 64 + 64], dr["nsa_w1"][l, kv], writes=[Bw1])
                K.dma("pool", pe16[kv * 64:kv * 64 + 64, :], dr["nsa_pe"][l, kv], writes=[Bpe16])
                K.dma("pool", w2[:, kv, :], dr["nsa_w2"][l, kv], writes=[Bw2])
            hb = sb(e1, "nhb", [128, 2], F32); Bhb = Buf("nhb")
            hid = sb(e1, "nhid", [128, 2, 128], BF16); Bhid = Buf("nhid")
            for kv in range(2):
                rows = slice(kv * 64, kv * 64 + 64)
                ph, Bph = psg.get()
                pbias, Bpbias = psg.get()
                for j in range(32):
                    lw = w1[rows, j, :]
                    rhs = cmpT[rows, j:j + 16 * 126 + 1:16]
                    K.op("pe", lambda e: e.matmul(ph[:, 0:127], lw, rhs, start=(j == 0), stop=(j == 31)), reads=[Bw1, BcmpT], writes=[Bph])
                for j in range(32):
                    lw = w1[rows, j, :]
                    K.op("pe", lambda e: e.matmul(pbias[:, 0:1], lw, pe16[rows, j:j + 1], start=(j == 0), stop=(j == 31)), reads=[Bw1, Bpe16], writes=[Bpbias])
                K.op("act", lambda e: e.copy(hb[:, kv:kv + 1], pbias[:, 0:1]), reads=[Bpbias], writes=[Bhb])
                K.op("act", lambda e: e.activation(hid[:, kv, 0:127], ph[:, 0:127], AF.Gelu_apprx_tanh, bias=hb[:, kv:kv + 1]), reads=[Bph, Bhb], writes=[Bhid])
                po, Bpo = psg.get()
                K.op("pe", lambda e: e.matmul(po[0:127, 0:64], hid[:, kv, 0:127], w2[:, kv, :], start=True, stop=True), reads=[Bhid, Bw2], writes=[Bpo])
                if kv == 1:
                    K.op("act", lambda e: e.copy(vc[0:127, 0:64], po[0:127, 0:64]), reads=[Bpo], writes=[Bvc])
                else:
                    sq, Bsq = st.get()
                    j_, Bj_ = jk.get()
                    q_n, Bq_n = qn.get()
                    q_r, Bq_r = qr.get()
                    rt_, Brt_ = rtmp.get()
                    K.op("dve", lambda e: e.memset(q_n[:, 0, :], 0.0), writes=[Bq_n])
                    K.op("act", lambda e: e.activation(j_[0:127, :], po[0:127, 0:64], AF.Square, accum_out=sq[0:127, 0:1]), reads=[Bpo], writes=[Bj_, Bsq])
                    rstd_from(sq[0:127, 1:2], sq[0:127, 0:1], 64, [Bsq], [Bsq])
                    K.op("dve", lambda e: e.scalar_tensor_tensor(q_n[0:127, 0, :], po[0:127, 0:64], sq[0:127, 1:2], gv[0:127, 1, :], ALU.mult, ALU.mult),
                         reads=[Bpo, Bsq, Bgv, Bq_n], writes=[Bq_n])
                    apply_rope(q_r[:, 0:1, :], q_n[:, 0:1, :], csc[:, 0, 0, :], csc[:, 1, 0, :], 1, 8, rt_, [Bq_n, Bcsc], [Bq_r], Brt_)
                    K.op("act", lambda e: e.copy(q_r[:, 0:1, 16:64], q_n[:, 0:1, 16:64]), reads=[Bq_n], writes=[Bq_r])
                    pt, Bpt = psg.get()
                    ptb = pt[:, :].bitcast(BF16)
                    K.op("pe", lambda e: e.transpose(ptb[0:64, 0:128], q_r[:, 0, :], identb[:]), reads=[Bq_r, Bidentb], writes=[Bpt])
                    K.op("act", lambda e: e.copy(kcT[0:64, :], ptb[0:64, 0:128]), reads=[Bpt], writes=[BkcT])
            K.barrier_all()
            e1.close()
            cmask = sb(es, "ncmask", [128, T], BF16); Bcmask = Buf("ncmask")
            K.dma("pool", cmask[:], dr["c_cmpmask"][:], writes=[Bcmask])
            keep = sb(es, "nkeep", [128, NT, 32], F32); Bkeep = Buf("nkeep")
            base = sb(es, "nbase", [128, NT, 32], F32); Bbase = Buf("nbase")
            K.dma("sp", keep[:], dr["c_keep"][:], writes=[Bkeep])
            K.dma("sp", base[:], dr["c_base"][:], writes=[Bbase])
            Em = sb(es, "nEm", [128, NT, 128], BF16); BEm = Buf("nEm")
            K.op("pool", lambda e: e.memset(Em[:], 0.0), writes=[BEm])
            K.dma("pool", Em[0:32], dr["c_E"][:], writes=[BEm])
            scale = 64 ** -0.5
            def _mk_na(w):
                per = 8 // 4
                nb = per - 1
                return NS(psg=allps.sub(range(w * per, w * per + nb)), psa=allps.sub(range(w * per + nb, (w + 1) * per)), nsp=_zeroed(Pool(es, nc, "nselT", [128, 4, 128], BF16, 1)), pexp=Pool(es, nc, "npx", [128, 512], BF16, 3), sst=Pool(es, nc, "nsst", [128, 80], F32, 1), impp=Pool(es, nc, "nimp", [128, 32], F32, 1), yo=Pool(es, nc, "nyo", [128, 256], F32, 1), yst=Pool(es, nc, "nyst", [128, 8], F32, 1), yb=Pool(es, nc, "nyb", [128, 256], BF16, 1))
            def _body_na(qt, P):
                qs = slice(qt * 128, (qt + 1) * 128)
                qrhs = qT[:, qt].rearrange("p h t -> p (h t)")
                y_o, By_o = P.yo.get()
                yst_, Byst = P.yst.get()
                st_, Bst_ = P.sst.get()
                imp, Bimp = P.impp.get()
                K.op("act", lambda e: e.activation(gts[:, qt, :], gts[:, qt, :], AF.Exp, scale=-1.0), reads=[Bgts[qt]], writes=[Bgts[qt]])
                K.op("dve", lambda e: e.tensor_scalar(gts[:, qt, :], gts[:, qt, :], 1.0, None, ALU.add), reads=[Bgts[qt]], writes=[Bgts[qt]])
                K.op("dve", lambda e: e.reciprocal(gts[:, qt, :], gts[:, qt, :]), reads=[Bgts[qt]], writes=[Bgts[qt]])
                sp_, Bsp = P.psg.get()
                K.op("pe", lambda e: e.matmul(sp_[0:127, :], kcT[:, 0:127], qrhs, start=True, stop=True), reads=[BkcT, BqT[qt]], writes=[Bsp])
                pe_, Bpe = P.pexp.get()
                K.op("act", lambda e: e.activation(pe_[0:127, :], sp_[0:127, :], AF.Exp, scale=scale), reads=[Bsp], writes=[Bpe])
                K.op("dve", lambda e: e.tensor_tensor(pe_[0:127, :].rearrange("p (h t) -> p h t", h=4), pe_[0:127, :].rearrange("p (h t) -> p h t", h=4),
                                                       cmask[0:127, qs].unsqueeze(1).to_broadcast([127, 4, 128]), ALU.mult),
                     reads=[Bpe, Bcmask], writes=[Bpe])
                for h in range(4):
                    acc, Bacc = P.psa.get()
                    K.op("pe", lambda e: e.matmul(acc[:, 0:97], pe_[0:127, h * 128:(h + 1) * 128], vc[0:127, :], start=True, stop=True),
                         reads=[Bpe, Bvc], writes=[Bacc])
                    K.op("dve", lambda e: e.tensor_scalar(st_[:, h:h + 1], acc[:, 64:65], 1e-30, None, ALU.add), reads=[Bacc], writes=[Bst_])
                    K.op("dve", lambda e: e.reciprocal(st_[:, h:h + 1], st_[:, h:h + 1]), reads=[Bst_], writes=[Bst_])
                    if h == 0:
                        K.op("dve", lambda e: e.tensor_scalar(imp[:], acc[:, 65:97], st_[:, h:h + 1], None, ALU.mult), reads=[Bacc, Bst_], writes=[Bimp])
                    else:
                        K.op("dve", lambda e: e.scalar_tensor_tensor(imp[:], acc[:, 65:97], st_[:, h:h + 1], imp[:], ALU.mult, ALU.add),
                             reads=[Bacc, Bst_, Bimp], writes=[Bimp])
                    K.op("dve", lambda e: e.tensor_tensor(st_[:, 4 + h:5 + h], st_[:, h:h + 1], gts[:, qt, 3 * h:3 * h + 1], ALU.mult), reads=[Bst_, Bgts[qt]], writes=[Bst_])
                    K.op("dve", lambda e: e.tensor_scalar(y_o[:, h * 64:(h + 1) * 64], acc[:, 0:64], st_[:, 4 + h:5 + h], None, ALU.mult),
                         reads=[Bacc, Bst_], writes=[By_o])
                K.op("dve", lambda e: e.tensor_tensor(imp[:], imp[:], keep[:, qt, :], ALU.mult), reads=[Bimp, Bkeep], writes=[Bimp])
                K.op("dve", lambda e: e.tensor_tensor(imp[:], imp[:], base[:, qt, :], ALU.add), reads=[Bimp, Bbase], writes=[Bimp])
                K.op("dve", lambda e: e.max(st_[:, 8:16], imp[:]), reads=[Bimp], writes=[Bst_])
                K.op("dve", lambda e: e.tensor_scalar(st_[:, 16:48], imp[:], st_[:, 12:13], -1.0, ALU.is_ge, ALU.add), reads=[Bimp, Bst_], writes=[Bst_])
                pt, Bpt = P.psg.get()
                K.op("pe", lambda e: e.transpose(pt[0:32, 0:128], st_[:, 16:48], ident[:]), reads=[Bst_, Bident], writes=[Bpt])
                nselT, BnselT = P.nsp.get()
                K.op("act", lambda e: e.copy(nselT[0:32, :, :], pt[0:32, 0:128].unsqueeze(1).to_broadcast([32, 4, 128])), reads=[Bpt], writes=[BnselT])
                for br_ in range(2):
                    kTb = kTs if br_ == 0 else kTw
                    BkTb = BkTs if br_ == 0 else BkTw
                    vb = vs if br_ == 0 else vw
                    Bvb = Bvs if br_ == 0 else Bvw
                    kts = list(range(0, qt + 1)) if br_ == 0 else list(range(max(0, qt - 4), qt + 1))
                    acc, Bacc = P.psa.get()
                    K.op("dve", lambda e: e.memset(acc[:, 0:260], 0.0), writes=[Bacc])
                    for kt in kts:
                        sp_, Bsp = P.psg.get()
                        K.op("pe", lambda e: e.matmul(sp_[:, :], kTb[:, kt * 128:(kt + 1) * 128], qrhs, start=True, stop=(br_ == 1)),
                             reads=[BkTb[kt], BqT[qt]], writes=[Bsp])
                        if br_ == 0:
                            K.op("pe", lambda e: e.matmul(sp_[:, :], Em[:, kt, :], nselT[:, :, :].rearrange("p h t -> p (h t)"), start=False, stop=True),
                                 reads=[BEm, BnselT], writes=[Bsp])
                        pe_, Bpe = P.pexp.get()
                        K.op("act", lambda e: e.activation(pe_[:, :], sp_[:, :], AF.Exp, scale=scale), reads=[Bsp], writes=[Bpe])
                        if kt == qt:
                            K.op("dve", lambda e: e.tensor_tensor(pe_[:, :], pe_[:, :], tri4[:].rearrange("p h t -> p (h t)"), ALU.mult),
                                 reads=[Bpe, Btri], writes=[Bpe])
                        elif br_ == 1 and kt == qt - 4:
                            K.op("dve", lambda e: e.tensor_tensor(pe_[:, :], pe_[:, :], anti4[:].rearrange("p h t -> p (h t)"), ALU.mult),
                                 reads=[Bpe, Banti], writes=[Bpe])
                        for h in range(4):
                            co = h * 65
                            K.op("pe", lambda e: e.matmul(acc[:, co:co + 65], pe_[:, h * 128:(h + 1) * 128], vb[:, kt, :], start=False, stop=(kt == kts[-1]),
                                                          skip_group_check=True),
                                 reads=[Bpe, Bvb[kt]], writes=[Bacc])
                    for h in range(4):
                        co = h * 65
                        cc_ = 50 + 4 * br_ + h
                        K.op("dve", lambda e: e.reciprocal(st_[:, cc_:cc_ + 1], acc[:, co + 64:co + 65]), reads=[Bacc], writes=[Bst_])
                        K.op("dve", lambda e: e.tensor_tensor(st_[:, cc_:cc_ + 1], st_[:, cc_:cc_ + 1], gts[:, qt, 3 * h + 1 + br_:3 * h + 2 + br_], ALU.mult),
                             reads=[Bst_, Bgts[qt]], writes=[Bst_])
                        K.op("dve", lambda e: e.scalar_tensor_tensor(y_o[:, h * 64:(h + 1) * 64], acc[:, co:co + 64], st_[:, cc_:cc_ + 1], y_o[:, h * 64:(h + 1) * 64],
                                                                     ALU.mult, ALU.add),
                             reads=[Bacc, Bst_, By_o], writes=[By_o])
                tm_groupnorm(P.psg, y_o, By_o, yst_, Byst, P.yb, 3, l, qt)
            IL.run(NT, 4, _mk_na, _body_na)
            dbg_dump(3)
            wout_partial(es, l, 3)
            phase_end(es)

        combT = sb(top, "combT", [128, T], BF16)
        BcombT = [Buf(f"combT{c}") for c in range(4)]
        K.op("pool", lambda e: e.memset(combT[:], 0.0), writes=BcombT)
        gon = sb(top, "gon", [128, 4, 256], F32); Bgon = Buf("gon")
        gcolt = sb(top, "gcolt", [128, 8], F32); Bgcol = Buf("gcolt")

        io_box = {"final": False, "stored": set(), "loaded": set()}
        for s in range(n_seq):
            for i in range(NT):
                if (s, i) not in io_box["loaded"]:
                    K.dma("sp", x_sb[:, i, :], dr["x"][s, i * 128:(i + 1) * 128, :], writes=Bx[i])
            K.dma("sp", posi[:], dr["pos"][s].rearrange("(n p) -> p n", p=128), writes=[Bposi], allow_slow_non_contiguous=True)
            K.op("dve", lambda e: e.tensor_copy(posf[:], posi[:]), reads=[Bposi], writes=[Bposf])
            for l in range(depth):
                for m in range(4):
                    K.dma("sp", gon[:, m, :], dr["out_norm"][l, m:m + 1, :].to_broadcast([128, 256]), writes=[Bgon])
                K.dma("sp", gcolt[:], dr["out_norm_t"][l], writes=[Bgcol])
                l_box[0] = l
                io_box["final"] = (l == depth - 1) and ("moe" in phases)
                norm_phase(s, l, "mix_norm", False)
                if "mla" in phases:
                    mla_phase(s, l)
                if "lru" in phases:
                    lru_phase(s, l)
                if "s5" in phases:
                    s5_phase(s, l)
                if "nsa" in phases:
                    nsa_phase(s, l)
                if "moe" in phases:
                    moe_phase(s, l)
            if dbg:
                K.dma("pool", dbg_d["xnT"][:], xnT[:], reads=[b for t_ in BxnT for b in t_], writes=[Bout])
                for i in range(NT):
                    K.dma("sp", dbg_d["x"][i * 128:(i + 1) * 128, :], x_sb[:, i, :], reads=Bx[i], writes=[Bout])
            for i in range(NT):
                if (s, i) not in io_box["stored"]:
                    K.dma("sp", out_d[s, i * 128:(i + 1) * 128, :], x_sb[:, i, :], reads=Bx[i], writes=[Bout])
            K.barrier_all()
        K.barrier_all()
        K.close()
    return nc, K


def prep_weights(inp):
    f = np.float32
    L = DEPTH
    w = {}
    for k in ("mix_norm", "ffn_norm", "w_in", "w_out", "mla_g_cq", "mla_g_ckv", "mla_w_uq", "mla_w_ukv", "mla_g_q", "mla_g_k",
              "s5_w_glu", "nsa_g_q", "nsa_g_k", "out_norm", "moe_w_gate", "moe_w_up", "moe_w_down"):
        w[k] = np.ascontiguousarray(inp[k], dtype=f)
    def pc(v):
        return np.ascontiguousarray(np.asarray(v, f).reshape(L, 2, 128).transpose(0, 2, 1))
    w["lru_cw"] = np.ascontiguousarray(np.asarray(inp["lru_conv_w"], f).reshape(L, 4, 2, 128).transpose(0, 3, 2, 1))
    w["lru_vec"] = np.ascontiguousarray(np.stack([pc(inp["lru_conv_b"]), pc(np.asarray(inp["lru_b_a"]).reshape(L, 256)),
                                                  pc(np.asarray(inp["lru_b_i"]).reshape(L, 256)), pc(inp["lru_lambda"]),
                                                  np.zeros((L, 128, 2), f)], axis=2))
    for nm, src in (("lru_wa", "lru_w_a"), ("lru_wi", "lru_w_i")):
        a = np.zeros((L, 2, 128, 128), f)
        W = np.asarray(inp[src], f)
        for c in range(2):
            for hh in range(2):
                a[:, c, hh * 64:(hh + 1) * 64, hh * 64:(hh + 1) * 64] = W[:, 2 * c + hh]
        w[nm] = a
    def st(v):
        return np.asarray(v, f).reshape(L, 8, 128).transpose(0, 2, 1)
    ldt = np.repeat(np.asarray(inp["s5_log_dt"], f)[:, :, None], 64, axis=2)
    w["s5_par"] = np.ascontiguousarray(np.stack([st(inp["s5_a_re"]), st(inp["s5_a_im"]), st(ldt)], axis=2))
    def stb(v):
        return np.asarray(v, f).reshape(L, 8, 128, 16).transpose(0, 2, 1, 3)
    w["s5_b"] = np.ascontiguousarray(np.stack([stb(inp["s5_b_re"]), stb(inp["s5_b_im"])], axis=2))
    cpad = np.zeros((L, 2, 8, 128, 128), f)
    for ri, nm in enumerate(("s5_c_re", "s5_c_im")):
        Cm = np.asarray(inp[nm], f)
        for g in range(16):
            j = g // 2
            rows = slice((g % 2) * 64, (g % 2) * 64 + 64)
            cols = slice((16 * g) % 128, (16 * g) % 128 + 16)
            cpad[:, ri, j, rows, cols] = Cm[:, g].transpose(0, 2, 1)
    w["s5_c"] = cpad
    w["s5_vec"] = np.ascontiguousarray(np.stack([pc(inp["s5_d"]), pc(inp["s5_b_glu"]), np.zeros((L, 128, 2), f)], axis=2))
    w["nsa_pe"] = np.ascontiguousarray(np.stack([np.asarray(inp["nsa_pe_k"], f).transpose(0, 2, 1),
                                                 np.asarray(inp["nsa_pe_v"], f).transpose(0, 2, 1)], axis=1))
    w["nsa_w1"] = np.ascontiguousarray(np.stack([np.asarray(inp["nsa_w1_k"], f).reshape(L, 32, 64, 128).transpose(0, 2, 1, 3),
                                                 np.asarray(inp["nsa_w1_v"], f).reshape(L, 32, 64, 128).transpose(0, 2, 1, 3)], axis=1))
    w["nsa_w2"] = np.ascontiguousarray(np.stack([np.asarray(inp["nsa_w2_k"], f), np.asarray(inp["nsa_w2_v"], f)], axis=1))
    w["out_norm_t"] = np.ascontiguousarray(np.asarray(inp["out_norm"], f).reshape(L, 8, 128).transpose(0, 2, 1))
    w["moe_wr"] = np.ascontiguousarray(np.concatenate([np.asarray(inp["moe_w_rg"], f), np.asarray(inp["moe_w_re"], f)], axis=2))
    w["moe_br"] = np.ascontiguousarray(np.concatenate([np.asarray(inp["moe_b_rg"], f), np.asarray(inp["moe_b_re"], f)], axis=1))
    w["c_ident"] = np.eye(128, dtype=f)
    kk = np.arange(128)[:, None]; qq = np.arange(128)[None, :]
    w["c_tri"] = (qq >= kk).astype(f)
    w["c_anti"] = (kk > qq).astype(f)
    cc = np.arange(128)[:, None]; tq = np.arange(T)[None, :]
    w["c_cmpmask"] = ((16 * cc + 31 <= tq) & (cc < 127)).astype(f)
    csn = np.arange(127) * 16; ssn = np.arange(32) * 64
    ov = np.clip(np.minimum(csn[:, None] + 32, ssn[None, :] + 64) - np.maximum(csn[:, None], ssn[None, :]), 0, None) / 16.0
    ovp = np.zeros((128, 32), f); ovp[:127] = ov
    w["c_ov"] = ovp
    tpos = np.arange(T); cur = tpos // 64; sbk = np.arange(32)
    forced = (sbk[None, :] == 0) | (sbk[None, :] == cur[:, None]) | (sbk[None, :] == cur[:, None] - 1)
    future = sbk[None, :] > cur[:, None]
    keep = (~forced & ~future).astype(f)
    base = np.where(future, -1e30, np.where(forced, 1e30, 0.0)).astype(f)
    w["c_keep"] = np.ascontiguousarray(keep.reshape(NT, 128, 32).transpose(1, 0, 2))
    w["c_base"] = np.ascontiguousarray(base.reshape(NT, 128, 32).transpose(1, 0, 2))
    E = np.zeros((32, NT, 128), f)
    for kt in range(NT):
        for m_ in range(128):
            E[2 * kt + m_ // 64, kt, m_] = BIGNEG
    w["c_E"] = E
    w["c_inv_mla"] = np.tile((500000.0 ** (-np.arange(16, dtype=np.float64) * 2.0 / 32)).astype(f)[None, :], (128, 1))
    w["c_inv_nsa"] = np.tile((500000.0 ** (-np.arange(8, dtype=np.float64) * 2.0 / 16)).astype(f)[None, :], (128, 1))
    sE = np.zeros((32, 16, 128), f)
    for e_ in range(16):
        sE[e_, e_, :] = 1.0
        sE[16 + e_, e_, :] = 1.0
    w["c_selE"] = sE
    for k, shp in W_SPECS.items():
        assert list(w[k].shape) == shp, (k, w[k].shape, shp)
    return w


_CACHE = {}


def kernel(**inputs):
    n_cores = 8
    x = np.ascontiguousarray(inputs["x"], dtype=np.float32)
    pos = np.ascontiguousarray(inputs["positions"], dtype=np.int32)
    w = prep_weights(inputs)
    if "nc" not in _CACHE:
        _CACHE["nc"] = build_program(n_seq=2)[0]
    nc = _CACHE["nc"]
    in_maps = []
    for c in range(n_cores):
        m = dict(w)
        m["x"] = x[2 * c:2 * c + 2]
        m["pos"] = pos[2 * c:2 * c + 2]
        in_maps.append(m)
    res = run_bass_kernel_spmd(nc, in_maps, core_ids=list(range(n_cores)))
    return np.concatenate([r["out"] for r in res.results], axis=0)
```
